# Optimizing a Trainium2 kernel written in Bass

```python
import math
import jax, jax.numpy as jnp
from jax import lax
import numpy as np

D_MODEL = 2048
BATCH = 1
SEQ = 16384
DEPTH = 1

HEAD_DIM = 64
NSA_HEADS = 8
NSA_KV_HEADS = 2
NSA_HPG = NSA_HEADS // NSA_KV_HEADS
FOX_HEADS = 8
MIX_W = NSA_HEADS * HEAD_DIM
CMP_LEN = 32
CMP_STRIDE = 16
CMP_HID = 256
SLC_LEN = 64
SLC_TOPK = 16
WINDOW = 512
Q_BLOCK = 128
T5_BUCKETS = 32
T5_MAX_EXACT = 16
T5_MAX_DIST = 128
N_GROUPS = 8
EXPERTS_PER_GROUP = 8
N_EXPERTS = N_GROUPS * EXPERTS_PER_GROUP
TOP_K_EXPERTS = 2
D_EXPERT = 512
ROW_BLOCK = 128
LN_EPS = 1e-5
NEG_INF = -1e30
FORCE_SCORE = 1e4
DEEPNORM_ALPHA = (2 * DEPTH) ** 0.25
DEEPNORM_BETA = (8 * DEPTH) ** -0.25

C_NSA_Q = NSA_HEADS * HEAD_DIM
C_NSA_KV = 6 * NSA_KV_HEADS * HEAD_DIM
C_NSA_GATE = 3 * NSA_HEADS
C_FOX_QKV = 3 * FOX_HEADS * HEAD_DIM
C_FOX_F = FOX_HEADS
C_MERGE = 2 * D_MODEL
OFF_KV = C_NSA_Q
OFF_GATE = OFF_KV + C_NSA_KV
OFF_FOX = OFF_GATE + C_NSA_GATE
OFF_FGT = OFF_FOX + C_FOX_QKV
OFF_MERGE = OFF_FGT + C_FOX_F
IN_COLS = OFF_MERGE + C_MERGE

kernel_name = "hybrid_nsa_fox_hmoe_block"


def layer_norm(x, g=None, b=None):
    xf = x.astype(jnp.float32)
    mu = xf.mean(-1, keepdims=True)
    var = jnp.square(xf - mu).mean(-1, keepdims=True)
    y = (xf - mu) * lax.rsqrt(var + LN_EPS)
    if g is not None:
        y = y * g.astype(jnp.float32) + b.astype(jnp.float32)
    return y.astype(x.dtype)


def masked_softmax(s, mask):
    p = jax.nn.softmax(jnp.where(mask, s, NEG_INF), axis=-1)
    return p * mask.any(-1, keepdims=True)


def t5_bucket(dist):
    n = jnp.maximum(dist, 0)
    ratio = jnp.log(jnp.maximum(n, T5_MAX_EXACT).astype(jnp.float32) / T5_MAX_EXACT)
    big = T5_MAX_EXACT + (ratio / math.log(T5_MAX_DIST / T5_MAX_EXACT)
                          * (T5_BUCKETS - T5_MAX_EXACT)).astype(jnp.int32)
    return jnp.where(n < T5_MAX_EXACT, n, jnp.minimum(big, T5_BUCKETS - 1))


def split_heads(a, n_heads):
    B, S, _ = a.shape
    return a.reshape(B, S, n_heads, HEAD_DIM).transpose(0, 2, 1, 3)


def compress_kv(kv, pe, w1, b1, w2):
    S = kv.shape[3]
    n_cmp = (S - CMP_LEN) // CMP_STRIDE + 1
    idx = jnp.arange(n_cmp)[:, None] * CMP_STRIDE + jnp.arange(CMP_LEN)[None, :]
    blocks = kv[:, :, :, idx, :] + pe[:, None, None, None]
    flat = blocks.reshape(*blocks.shape[:4], CMP_LEN * HEAD_DIM)
    hid = jax.nn.gelu(jnp.einsum('zbgnf,zfh->zbgnh', flat, w1) + b1[:, None, None, None])
    return jnp.einsum('zbgnh,zhd->zbgnd', hid, w2)


def gather_blocks(blocks, sel):
    return jax.vmap(jax.vmap(lambda bl, ix: bl[ix]))(blocks, sel)


def nsa_attention(q, k_cmp, v_cmp, k_slc, v_slc, k_win, v_win, gates, t5_table):
    B, G, HPG, S, HD = q.shape
    n_cmp = k_cmp.shape[2]
    n_slc = S // SLC_LEN
    n_sel = min(SLC_TOPK, n_slc)
    ratio = SLC_LEN // CMP_STRIDE
    scale = HD ** -0.5
    tb = t5_table.T.reshape(G, HPG, T5_BUCKETS).astype(jnp.float32)
    g_ix = jnp.arange(G)[None, :, None, None, None]
    h_ix = jnp.arange(HPG)[None, None, :, None, None]
    cmp_end = jnp.arange(n_cmp) * CMP_STRIDE + CMP_LEN - 1
    w_main = jnp.array([0.5] + [1.0] * (ratio - 1), jnp.float32)
    blk = jnp.arange(n_slc)
    win_off = jnp.arange(WINDOW + Q_BLOCK)

    def block(qb):
        q0 = qb * Q_BLOCK
        t = q0 + jnp.arange(Q_BLOCK)
        qblk = lax.dynamic_slice_in_dim(q, q0, Q_BLOCK, axis=3)
        gblk = lax.dynamic_slice_in_dim(gates, q0, Q_BLOCK, axis=3)
        s_c = (jnp.einsum('bghtd,bgnd->bghtn', qblk, k_cmp).astype(jnp.float32) * scale
               + tb[:, :, t5_bucket(t[:, None] - cmp_end[None, :])])
        p_c = masked_softmax(s_c, cmp_end[None, :] <= t[:, None])
        o_c = jnp.einsum('bghtn,bgnd->bghtd', p_c.astype(v_cmp.dtype), v_cmp)
        imp = jnp.pad(p_c.sum(2), ((0, 0), (0, 0), (0, 0), (1, ratio * n_slc - n_cmp)))
        p_slc = (imp[..., :ratio * n_slc].reshape(B, G, Q_BLOCK, n_slc, ratio) @ w_main
                 + 0.5 * imp[..., ratio::ratio])
        cur = (t // SLC_LEN)[:, None]
        forced = (blk == 0) | (blk == cur) | (blk == cur - 1)
        score = jnp.where(forced, FORCE_SCORE, jnp.where(blk <= cur, p_slc, -1.0))
        _, sel = lax.top_k(score, n_sel)
        k_sel = gather_blocks(k_slc, sel).reshape(B, G, Q_BLOCK, n_sel * SLC_LEN, HD)
        v_sel = gather_blocks(v_slc, sel).reshape(B, G, Q_BLOCK, n_sel * SLC_LEN, HD)
        kpos = (sel[..., None] * SLC_LEN + jnp.arange(SLC_LEN)).reshape(B, G, Q_BLOCK, n_sel * SLC_LEN)
        bucket_s = t5_bucket(t[:, None] - kpos)[:, :, None]
        s_s = (jnp.einsum('bghtd,bgtkd->bghtk', qblk, k_sel).astype(jnp.float32) * scale
               + tb[g_ix, h_ix, bucket_s])
        p_s = masked_softmax(s_s, (kpos <= t[:, None])[:, :, None])
        o_s = jnp.einsum('bghtk,bgtkd->bghtd', p_s.astype(v_sel.dtype), v_sel)
        kw = lax.dynamic_slice_in_dim(k_win, q0, WINDOW + Q_BLOCK, axis=2)
        vw = lax.dynamic_slice_in_dim(v_win, q0, WINDOW + Q_BLOCK, axis=2)
        wpos = q0 - WINDOW + win_off
        dist = t[:, None] - wpos[None, :]
        s_w = (jnp.einsum('bghtd,bgkd->bghtk', qblk, kw).astype(jnp.float32) * scale
               + tb[:, :, t5_bucket(dist)])
        p_w = masked_softmax(s_w, (dist >= 0) & (dist < WINDOW) & (wpos[None, :] >= 0))
        o_w = jnp.einsum('bghtk,bgkd->bghtd', p_w.astype(vw.dtype), vw)
        return gblk[..., 0:1] * o_c + gblk[..., 1:2] * o_s + gblk[..., 2:3] * o_w

    out = lax.map(block, jnp.arange(S // Q_BLOCK))
    return out.transpose(1, 0, 4, 2, 3, 5).reshape(B, S, G * HPG * HD)


def forgetting_attention(q, k, v, log_f):
    B, H, S, HD = q.shape
    scale = HD ** -0.5
    F = lax.cumsum(log_f, axis=2)
    kpos = jnp.arange(S)

    def block(qb):
        q0 = qb * Q_BLOCK
        t = q0 + jnp.arange(Q_BLOCK)
        qblk = lax.dynamic_slice_in_dim(q, q0, Q_BLOCK, axis=2)
        Fq = lax.dynamic_slice_in_dim(F, q0, Q_BLOCK, axis=2)
        s = (jnp.einsum('bhtd,bhsd->bhts', qblk, k).astype(jnp.float32) * scale
             + (Fq[..., None] - F[:, :, None, :]))
        p = masked_softmax(s, kpos[None, :] <= t[:, None])
        return jnp.einsum('bhts,bhsd->bhtd', p.astype(v.dtype), v)

    out = lax.map(block, jnp.arange(S // Q_BLOCK))
    return out.transpose(1, 0, 3, 2, 4).reshape(B, S, H * HD)


def token_mixing(u, w_in, b_fgt, t5_table, cmp_pe, cmp_w1, cmp_b1, cmp_w2, w_br_nsa, w_br_fox, w_o):
    B, S, _ = u.shape
    proj = u @ w_in
    nsa_q, nsa_kv, nsa_g, fox_qkv, fox_f, merge = jnp.split(
        proj, [OFF_KV, OFF_GATE, OFF_FOX, OFF_FGT, OFF_MERGE], axis=-1)
    q = split_heads(nsa_q, NSA_HEADS).reshape(B, NSA_KV_HEADS, NSA_HPG, S, HEAD_DIM)
    kv = nsa_kv.reshape(B, S, 6, NSA_KV_HEADS, HEAD_DIM).transpose(2, 0, 3, 1, 4)
    kv_cmp = compress_kv(kv[0:2], cmp_pe, cmp_w1, cmp_b1, cmp_w2)
    kv_slc = kv[2:4].reshape(2, B, NSA_KV_HEADS, S // SLC_LEN, SLC_LEN, HEAD_DIM)
    kv_win = jnp.pad(kv[4:6], ((0, 0), (0, 0), (0, 0), (WINDOW, 0), (0, 0)))
    gates = jax.nn.sigmoid(nsa_g.reshape(B, S, NSA_KV_HEADS, NSA_HPG, 3).transpose(0, 2, 3, 1, 4))
    o_nsa = nsa_attention(q, kv_cmp[0], kv_cmp[1], kv_slc[0], kv_slc[1],
                          kv_win[0], kv_win[1], gates, t5_table)
    fq, fk, fv = jnp.split(fox_qkv, 3, axis=-1)
    log_f = jax.nn.log_sigmoid((fox_f + b_fgt).astype(jnp.float32)).transpose(0, 2, 1)
    o_fox = forgetting_attention(split_heads(fq, FOX_HEADS), split_heads(fk, FOX_HEADS),
                                 split_heads(fv, FOX_HEADS), log_f)
    g_nsa, g_fox = jnp.split(jax.nn.sigmoid(merge), 2, axis=-1)
    merged = g_nsa * (o_nsa @ w_br_nsa) + g_fox * (o_fox @ w_br_fox)
    return merged @ w_o


def hierarchical_moe(h, w_rg, b_rg, w_re, b_re, w_gate, w_up, w_down):
    B, S, D = h.shape
    T = B * S
    x = h.reshape(T, D)
    p_grp = jax.nn.softmax((x @ w_rg + b_rg).astype(jnp.float32), axis=-1)
    grp = jnp.argmax(p_grp, axis=-1)
    p_grp_sel = jnp.max(p_grp, axis=-1)
    e_logits = (jnp.einsum('td,dge->tge', x, w_re) + b_re)[jnp.arange(T), grp]
    p_top, e_top = lax.top_k(jax.nn.softmax(e_logits.astype(jnp.float32), axis=-1), TOP_K_EXPERTS)
    w_tok = p_grp_sel[:, None] * p_top / p_top.sum(-1, keepdims=True)
    eid = (grp[:, None] * EXPERTS_PER_GROUP + e_top).reshape(-1)
    tok = jnp.repeat(jnp.arange(T), TOP_K_EXPERTS)
    wts = w_tok.reshape(-1)
    n_rows = T * TOP_K_EXPERTS + N_EXPERTS * ROW_BLOCK
    n_blocks = n_rows // ROW_BLOCK
    order = jnp.argsort(eid)
    e_sorted = eid[order]
    counts = jnp.bincount(eid, length=N_EXPERTS)
    padded = (counts + ROW_BLOCK - 1) // ROW_BLOCK * ROW_BLOCK
    pad_end = jnp.cumsum(padded)
    pad_start = pad_end - padded
    start = jnp.cumsum(counts) - counts
    dest = pad_start[e_sorted] + jnp.arange(eid.shape[0]) - start[e_sorted]
    row_tok = jnp.zeros((n_rows,), jnp.int32).at[dest].set(tok[order])
    row_w = jnp.zeros((n_rows,), x.dtype).at[dest].set(wts[order].astype(x.dtype))
    blk_exp = jnp.minimum(jnp.searchsorted(pad_end, jnp.arange(n_blocks) * ROW_BLOCK, side='right'),
                          N_EXPERTS - 1)
    xs = x[row_tok].reshape(n_blocks, ROW_BLOCK, D)

    def expert_block(args):
        xb, e = args
        return (jax.nn.silu(xb @ w_gate[e]) * (xb @ w_up[e])) @ w_down[e]

    ys = lax.map(expert_block, (xs, blk_exp)).reshape(n_rows, D)
    y = jax.ops.segment_sum(ys * row_w[:, None], row_tok, num_segments=T)
    return y.reshape(B, S, D)


def setup_inputs(seed: int = 0) -> dict:
    key = jax.random.key(seed)
    ks = jax.random.split(key, 26)
    L = DEPTH

    def nrm(k, shape, s):
        return jax.random.normal(k, shape, jnp.float32) * s

    return {
        "x": nrm(ks[0], (BATCH, SEQ, D_MODEL), 1.0),
        "c": nrm(ks[1], (BATCH, D_MODEL), 1.0),
        "w_ada": nrm(ks[2], (L, D_MODEL, 6 * D_MODEL), 0.1 * D_MODEL ** -0.5),
        "b_ada": nrm(ks[3], (L, 6 * D_MODEL), 0.01),
        "w_in": nrm(ks[4], (L, D_MODEL, IN_COLS), D_MODEL ** -0.5),
        "b_fgt": jnp.linspace(1.0, 5.0, FOX_HEADS, dtype=jnp.float32)[None, :] + nrm(ks[5], (L, FOX_HEADS), 0.1),
        "t5_table": nrm(ks[6], (T5_BUCKETS, NSA_HEADS), 0.5),
        "cmp_pe": nrm(ks[7], (L, 2, CMP_LEN, HEAD_DIM), 0.02),
        "cmp_w1": nrm(ks[8], (L, 2, CMP_LEN * HEAD_DIM, CMP_HID), (CMP_LEN * HEAD_DIM) ** -0.5),
        "cmp_b1": nrm(ks[9], (L, 2, CMP_HID), 0.01),
        "cmp_w2": nrm(ks[10], (L, 2, CMP_HID, HEAD_DIM), CMP_HID ** -0.5),
        "w_br_nsa": nrm(ks[11], (L, MIX_W, D_MODEL), MIX_W ** -0.5),
        "w_br_fox": nrm(ks[12], (L, MIX_W, D_MODEL), MIX_W ** -0.5),
        "w_o": nrm(ks[13], (L, D_MODEL, D_MODEL), DEEPNORM_BETA * D_MODEL ** -0.5),
        "ln1_g": 1.0 + nrm(ks[14], (L, D_MODEL), 0.02),
        "ln1_b": nrm(ks[15], (L, D_MODEL), 0.01),
        "w_rg": nrm(ks[16], (L, D_MODEL, N_GROUPS), D_MODEL ** -0.5),
        "b_rg": nrm(ks[17], (L, N_GROUPS), 0.01),
        "w_re": nrm(ks[18], (L, D_MODEL, N_GROUPS, EXPERTS_PER_GROUP), D_MODEL ** -0.5),
        "b_re": nrm(ks[19], (L, N_GROUPS, EXPERTS_PER_GROUP), 0.01),
        "w_gate": nrm(ks[20], (L, N_EXPERTS, D_MODEL, D_EXPERT), D_MODEL ** -0.5),
        "w_up": nrm(ks[21], (L, N_EXPERTS, D_MODEL, D_EXPERT), D_MODEL ** -0.5),
        "w_down": nrm(ks[22], (L, N_EXPERTS, D_EXPERT, D_MODEL), DEEPNORM_BETA * D_EXPERT ** -0.5),
        "ln2_g": 1.0 + nrm(ks[23], (L, D_MODEL), 0.02),
        "ln2_b": nrm(ks[24], (L, D_MODEL), 0.01),
    }


def reference(x, c, w_ada, b_ada, w_in, b_fgt, t5_table, cmp_pe, cmp_w1, cmp_b1, cmp_w2,
              w_br_nsa, w_br_fox, w_o, ln1_g, ln1_b, w_rg, b_rg, w_re, b_re,
              w_gate, w_up, w_down, ln2_g, ln2_b):
    c_act = jax.nn.silu(c)
    for l in range(DEPTH):
        mod = (c_act @ w_ada[l] + b_ada[l])[:, None, :]
        sh1, sc1, g1, sh2, sc2, g2 = jnp.split(mod, 6, axis=-1)
        u = layer_norm(x) * (1.0 + sc1) + sh1
        y = token_mixing(u, w_in[l], b_fgt[l], t5_table, cmp_pe[l], cmp_w1[l], cmp_b1[l], cmp_w2[l],
                         w_br_nsa[l], w_br_fox[l], w_o[l])
        x = layer_norm(DEEPNORM_ALPHA * x + (1.0 + g1) * y, ln1_g[l], ln1_b[l])
        u = layer_norm(x) * (1.0 + sc2) + sh2
        y = hierarchical_moe(u, w_rg[l], b_rg[l], w_re[l], b_re[l], w_gate[l], w_up[l], w_down[l])
        x = layer_norm(DEEPNORM_ALPHA * x + (1.0 + g2) * y, ln2_g[l], ln2_b[l])
    return x
```

```python
import numpy as np
import concourse.bass as bass
import concourse.mybir as mybir
from concourse.bass_utils import run_bass_kernel_spmd

F32 = mybir.dt.float32
BF16 = mybir.dt.bfloat16
AF = mybir.ActivationFunctionType
ALU = mybir.AluOpType
AX = mybir.AxisListType

NCORES = 8
D = 2048
S = 16384
TPC = S // NCORES
IN_COLS = 6944
LN_EPS = 1e-5


class Trk:
    def __init__(self, nc, n_dma_sems=14):
        self.nc = nc
        self.eng = {"pe": nc.tensor, "act": nc.scalar, "dve": nc.vector,
                    "pool": nc.gpsimd, "sp": nc.sync}
        self.sem = {}
        self.cnt = {}
        self.waited = {k: {} for k in self.eng}
        self._ctx = []
        for k in ["pe", "act", "dve", "pool"]:
            g = nc.semaphore("s_" + k)
            self.sem[k] = g.__enter__()
            self._ctx.append(g)
            self.cnt[k] = 0
        self.dsem = []
        self.dcnt = []
        self.dq = {}
        for q in ["sp", "act", "pool"]:
            self.dq[q] = [len(self.dsem) + j for j in range(n_dma_sems)]
            for j in range(n_dma_sems):
                g = nc.semaphore("s_dma_%s%d" % (q, j))
                self.dsem.append(g.__enter__())
                self._ctx.append(g)
                self.dcnt.append(0)
        self.dqn = {"sp": 0, "act": 0, "pool": 0}
        self.dnext = 0
        self.st = {}
        self.n_ins = 0
        self.n_wait = 0

    def _semobj(self, sk):
        return self.sem[sk] if isinstance(sk, str) else self.dsem[sk]

    def _wait(self, e, deps):
        best = {}
        for d in deps:
            if d is None:
                continue
            sk, v = d
            if v > best.get(sk, 0):
                best[sk] = v
        for sk, v in best.items():
            if e == "pe" and sk == "pe":
                continue
            if self.waited[e].get(sk, 0) >= v:
                continue
            self.eng[e].wait_ge(self._semobj(sk), v)
            self.waited[e][sk] = v
            self.n_wait += 1

    def op(self, e, fn, reads=(), writes=(), dma=False):
        deps = []
        for k in reads:
            s = self.st.get(k)
            if s is not None:
                deps.append(s[0])
        for k in writes:
            s = self.st.get(k)
            if s is not None:
                deps.append(s[0])
                deps.extend(s[1])
        self._wait(e, deps)
        ins = fn(self.eng[e])
        if dma:
            i = self.dq[e][self.dqn[e] % len(self.dq[e])]
            self.dqn[e] += 1
            self.dcnt[i] += 16
            ins.then_inc(self.dsem[i], 16)
            tag = (i, self.dcnt[i])
        else:
            self.cnt[e] += 1
            ins.then_inc(self.sem[e], 1)
            tag = (e, self.cnt[e])
        for k in reads:
            s = self.st.setdefault(k, [None, []])
            s[1].append(tag)
            if len(s[1]) > 48:
                best = {}
                for sk, v in s[1]:
                    if v > best.get(sk, 0):
                        best[sk] = v
                s[1] = list(best.items())
        for k in writes:
            self.st[k] = [tag, []]
        self.n_ins += 1
        return tag

    def finish(self, e="sp"):
        deps = []
        for k, s in self.st.items():
            deps.append(s[0])
            deps.extend(s[1])
        self._wait(e, deps)


def make_ident(nc, T, dtype, name="ident"):
    ident = nc.alloc_sbuf_tensor(name, [128, 128], dtype)
    T.op("pool", lambda e: e.memset(ident[:], 1.0), writes=[name])
    T.op("pool", lambda e: e.affine_select(out=ident[:], in_=ident[:], pattern=[[-1, 128]],
                                           compare_op=ALU.is_equal, fill=0.0, base=0,
                                           channel_multiplier=1), reads=[name], writes=[name])
    return ident


def build_l1(stop=None, TPC=TPC):
    nc = bass.Bass("TRN2", target_bir_lowering=False)
    x = nc.dram_tensor("x", [TPC, D], F32, kind="ExternalInput").ap()
    cT = nc.dram_tensor("cT", [128, 16], F32, kind="ExternalInput").ap()
    wada = nc.dram_tensor("wada", [D, 8], F32, kind="ExternalInput").ap()
    badaT = nc.dram_tensor("badaT", [128, 32], F32, kind="ExternalInput").ap()
    w_in = nc.dram_tensor("w_in", [D, IN_COLS], F32, kind="ExternalInput").ap()
    proj = nc.dram_tensor("proj", [TPC, IN_COLS], F32, kind="ExternalOutput").ap()
    T = Trk(nc)
    NT = TPC // 128

    wst = [nc.alloc_sbuf_tensor("wst%d" % i, [128, 8, 512], F32) for i in range(2)]
    wbf = [nc.alloc_sbuf_tensor("wbf%d" % i, [128, 16, 512], BF16) for i in range(2)]
    uT = nc.alloc_sbuf_tensor("uT", [128, 16, TPC], BF16)
    xt = [nc.alloc_sbuf_tensor("xt%d" % i, [128, D], F32) for i in range(2)]
    xn = nc.alloc_sbuf_tensor("xn", [128, D], BF16)
    junk = nc.alloc_sbuf_tensor("junk", [128, D], BF16)
    ost = [nc.alloc_sbuf_tensor("ost%d" % i, [128, 512], F32) for i in range(4)]
    cs = nc.alloc_sbuf_tensor("cs", [128, 16], F32)
    ca = nc.alloc_sbuf_tensor("ca", [128, 16], F32)
    bad = nc.alloc_sbuf_tensor("bad", [128, 32], F32)
    modT = nc.alloc_sbuf_tensor("modT", [128, 32], F32)
    st = nc.alloc_sbuf_tensor("st", [128, 16], F32)
    tp = [nc.alloc_psum_tensor("tp%d" % i, [128, 8, 128], BF16) for i in range(2)]
    acc = [nc.alloc_psum_tensor("acc%d" % i, [128, 512], F32) for i in range(4)]
    modps = nc.alloc_psum_tensor("modps", [128, 512], F32)
    ident = make_ident(nc, T, BF16)

    T.op("sp", lambda e: e.dma_start(out=modT[:], in_=badaT[:, :]), writes=["modT"], dma=True)
    T.op("dve", lambda e: e.tensor_scalar(out=modT[:, 16:32], in0=modT[:, 16:32], scalar1=1.0, scalar2=None, op0=ALU.add),
         reads=["modT"], writes=["modT"])
    ld = 0

    if stop == "A":
        T.op("sp", lambda e: e.dma_start(out=proj[0:128, 0:32], in_=modT[:]), reads=["modT"], writes=["proj"], dma=True)
        T.finish("sp")
        return nc
    for i in range(NT):
        b = i % 2
        T.op("sp", lambda e: e.dma_start(out=xt[b][:], in_=x[i * 128:(i + 1) * 128, :]), writes=["xt%d" % b], dma=True)
        T.op("dve", lambda e: e.reduce_sum(out=st[:, 0:1], in_=xt[b][:], axis=AX.X), reads=["xt%d" % b], writes=["st0"])
        T.op("act", lambda e: e.activation(out=junk[:], in_=xt[b][:], func=AF.Square, accum_out=st[:, 1:2]),
             reads=["xt%d" % b], writes=["st1", "junk"])
        T.op("dve", lambda e: e.tensor_scalar(out=st[:, 2:3], in0=st[:, 0:1], scalar1=1.0 / D, scalar2=None, op0=ALU.mult),
             reads=["st0"], writes=["st2"])
        T.op("dve", lambda e: e.tensor_tensor(out=st[:, 3:4], in0=st[:, 2:3], in1=st[:, 2:3], op=ALU.mult),
             reads=["st2"], writes=["st3"])
        T.op("dve", lambda e: e.scalar_tensor_tensor(out=st[:, 4:5], in0=st[:, 1:2], scalar=1.0 / D, in1=st[:, 3:4],
                                                     op0=ALU.mult, op1=ALU.subtract), reads=["st1", "st3"], writes=["st4"])
        T.op("dve", lambda e: e.tensor_scalar(out=st[:, 6:7], in0=st[:, 4:5], scalar1=LN_EPS, scalar2=None, op0=ALU.add),
             reads=["st4"], writes=["st6"])
        T.op("act", lambda e: e.activation(out=st[:, 7:8], in_=st[:, 6:7], func=AF.Sqrt), reads=["st6"], writes=["st7"])
        T.op("dve", lambda e: e.reciprocal(out=st[:, 5:6], in_=st[:, 7:8]), reads=["st7"], writes=["st5"])
        T.op("dve", lambda e: e.tensor_scalar(out=xn[:], in0=xt[b][:], scalar1=st[:, 2:3], scalar2=st[:, 5:6],
                                              op0=ALU.subtract, op1=ALU.mult), reads=["xt%d" % b, "st2", "st5"], writes=["xn"])
        for kq in range(2):
            pb = kq % 2
            for j in range(8):
                k = kq * 8 + j
                T.op("pe", lambda e: e.transpose(out=tp[pb][:, j, :], in_=xn[:, k * 128:(k + 1) * 128], identity=ident[:]),
                     reads=["xn", "ident"], writes=["tp%d" % pb])
            for j in range(8):
                k = kq * 8 + j
                T.op("act", lambda e: e.activation(out=uT[:, k, i * 128:(i + 1) * 128], in_=tp[pb][:, j, :], func=AF.Identity,
                                                   scale=modT[:, 16 + k:17 + k], bias=modT[:, k:k + 1]),
                     reads=["modT"], writes=["uT_%d" % i, "tp%d" % pb])

    if stop in ("B", "B1", "B2"):
        T.op("sp", lambda e: e.dma_start(out=proj[0:128, 0:32], in_=modT[:]), reads=["modT"], writes=["proj"], dma=True)
        T.finish("sp")
        return nc
    w_v = w_in.rearrange("(k p) n -> p k n", p=128)
    ngrp = (IN_COLS + 511) // 512
    ev = 0
    for g in range(ngrp):
        c0 = g * 512
        cw = min(512, IN_COLS - c0)
        wb = g % 2
        for kh in range(2):
            b = ld % 2
            ld += 1
            T.op("sp", lambda e: e.dma_start(out=wst[b][:, :, 0:cw], in_=w_v[:, kh * 8:(kh + 1) * 8, c0:c0 + cw]),
                 writes=["wst%d" % b], dma=True)
            T.op("pool", lambda e: e.tensor_copy(out=wbf[wb][:, kh * 8:(kh + 1) * 8, 0:cw], in_=wst[b][:, :, 0:cw]),
                 reads=["wst%d" % b], writes=["wbf%d_%d" % (wb, kh)])
        for i in range(NT):
            a = ev % 4
            for k in range(16):
                T.op("pe", lambda e: e.matmul(acc[a][:, 0:cw], lhsT=uT[:, k, i * 128:(i + 1) * 128], rhs=wbf[wb][:, k, 0:cw],
                                              start=(k == 0), stop=(k == 15)),
                     reads=["uT_%d" % i, "wbf%d_%d" % (wb, k // 8)], writes=["acc%d" % a])
            if ev % 2 == 0:
                T.op("act", lambda e: e.copy(out=ost[a][:, 0:cw], in_=acc[a][:, 0:cw]), writes=["ost%d" % a, "acc%d" % a])
            else:
                T.op("dve", lambda e: e.tensor_copy(out=ost[a][:, 0:cw], in_=acc[a][:, 0:cw]), writes=["ost%d" % a, "acc%d" % a])
            T.op("pool", lambda e: e.dma_start(out=proj[i * 128:(i + 1) * 128, c0:c0 + cw], in_=ost[a][:, 0:cw]),
                 reads=["ost%d" % a], writes=["proj"], dma=True)
            ev += 1
    T.finish("sp")
    T.finish("pool")
    print("L1 n_ins", T.n_ins, "n_wait", T.n_wait)
    return nc


def run_l1(x, modrow, w_in, stop=None, TPC=TPC, NCORES=NCORES):
    nc = build_l1(stop, TPC)
    x2 = np.ascontiguousarray(x.reshape(S, D))
    mod1 = np.ascontiguousarray(np.concatenate([modrow[0:D].reshape(16, 128).T, modrow[D:2 * D].reshape(16, 128).T], axis=1))
    w = np.ascontiguousarray(w_in[0])
    dummy = np.zeros((128, 16), np.float32)
    in_maps = [{"x": x2[i * TPC:(i + 1) * TPC], "cT": dummy, "wada": np.zeros((D, 8), np.float32), "badaT": mod1, "w_in": w} for i in range(NCORES)]
    res = run_bass_kernel_spmd(nc, in_maps, core_ids=list(range(NCORES)))
    return np.concatenate([r["proj"] for r in res.results], axis=0)


def build_masks(nc, T, name="cmask"):
    mk = nc.alloc_sbuf_tensor(name, [128, 4, 512], BF16)
    T.op("pool", lambda e: e.memset(mk[:], 0.0), writes=[name])
    for j in range(4):
        T.op("pool", lambda e: e.affine_select(out=mk[:, j, :], in_=mk[:, j, :], pattern=[[1, 512]],
                                               compare_op=ALU.is_ge, fill=-30000.0, base=-128 * j,
                                               channel_multiplier=-1), reads=[name], writes=[name])
    return mk


def build_fox(Sx=S):
    nc = bass.Bass("TRN2", target_bir_lowering=False)
    NKB = Sx // 128
    NQC = Sx // 512
    qT = nc.dram_tensor("qT", [64, Sx], F32, kind="ExternalInput").ap()
    kT = nc.dram_tensor("kT", [64, Sx], F32, kind="ExternalInput").ap()
    v = nc.dram_tensor("v", [Sx, 64], F32, kind="ExternalInput").ap()
    f2 = nc.dram_tensor("f2", [128, NKB], F32, kind="ExternalInput").ap()
    bfg = nc.dram_tensor("bfg", [128, 1], F32, kind="ExternalInput").ap()
    o = nc.dram_tensor("o", [Sx, 64], F32, kind="ExternalOutput").ap()
    scr = nc.dram_tensor("scr", [2, Sx], BF16).ap()
    T = Trk(nc)

    KTa = nc.alloc_sbuf_tensor("KTa", [128, Sx], BF16)
    QTa = nc.alloc_sbuf_tensor("QTa", [128, Sx], BF16)
    Vp = nc.alloc_sbuf_tensor("Vp", [128, NKB, 65], BF16)
    PT = [nc.alloc_sbuf_tensor("PT%d" % i, [128, 512], BF16) for i in range(4)]
    ost = [nc.alloc_sbuf_tensor("ost%d" % i, [128, 4, 64], F32) for i in range(2)]
    rc = nc.alloc_sbuf_tensor("rc", [128, 4], F32)
    fz = nc.alloc_sbuf_tensor("fz", [128, NKB], F32)
    lf = nc.alloc_sbuf_tensor("lf", [128, NKB], F32)
    nb = nc.alloc_sbuf_tensor("nb", [128, 1], F32)
    Usb = nc.alloc_sbuf_tensor("Usb", [128, 128], F32)
    SUsb = nc.alloc_sbuf_tensor("SUsb", [128, 128], F32)
    ones = nc.alloc_sbuf_tensor("ones", [128, 128], F32)
    totT = nc.alloc_sbuf_tensor("totT", [128, 128], F32)
    Fsb = nc.alloc_sbuf_tensor("Fsb", [128, NKB], F32)
    Fofs = nc.alloc_sbuf_tensor("Fofs", [128, NKB], F32)
    Gq = nc.alloc_sbuf_tensor("Gq", [128, NKB], F32)
    Ghi = nc.alloc_sbuf_tensor("Ghi", [128, 128], BF16)
    Ghf = nc.alloc_sbuf_tensor("Ghf", [128, NKB], F32)
    Glo = nc.alloc_sbuf_tensor("Glo", [128, 128], BF16)
    GT = nc.alloc_sbuf_tensor("GT", [128, 2, 128], BF16)
    bm = [nc.alloc_sbuf_tensor("bm%d" % i, [128, NKB], F32) for i in range(2)]
    Sps = [nc.alloc_psum_tensor("Sps%d" % i, [128, 512], F32) for i in range(3)]
    Ops = [nc.alloc_psum_tensor("Ops%d" % i, [128, 4, 128], F32) for i in range(2)]
    Fps = nc.alloc_psum_tensor("Fps", [128, 512], F32)
    Tps = nc.alloc_psum_tensor("Tps", [128, 8, 128], BF16)
    identb = make_ident(nc, T, BF16, "identb")
    mk = build_masks(nc, T)

    T.op("sp", lambda e: e.dma_start(out=fz[:], in_=f2[:, :]), writes=["fz"], dma=True)
    T.op("sp", lambda e: e.dma_start(out=nb[:], in_=bfg[:, :]), writes=["nb"], dma=True)
    for h in range(0, Sx, 2048):
        w = min(2048, Sx - h)
        T.op("pool", lambda e: e.dma_start(out=KTa[0:64, h:h + w], in_=kT[:, h:h + w]), writes=["KTa"], dma=True)
        T.op("pool", lambda e: e.dma_start(out=QTa[0:64, h:h + w], in_=qT[:, h:h + w]), writes=["QTa"], dma=True)
    vv = v.rearrange("(kb p) d -> p kb d", p=128)
    for h in range(0, NKB, 16):
        w = min(16, NKB - h)
        T.op("pool", lambda e: e.dma_start(out=Vp[:, h:h + w, 0:64], in_=vv[:, h:h + w, :]), writes=["Vp"], dma=True)
    T.op("dve", lambda e: e.memset(Vp[:, :, 64:65], 1.0), writes=["Vp1"])
    T.op("dve", lambda e: e.memset(KTa[64:66, :], 8.0), writes=["KTa8"])

    T.op("pool", lambda e: e.memset(ones[:], 1.0), writes=["ones"])
    T.op("pool", lambda e: e.memset(Usb[:], 1.0), writes=["Usb"])
    T.op("pool", lambda e: e.affine_select(out=Usb[:], in_=Usb[:], pattern=[[1, 128]], compare_op=ALU.is_ge, fill=0.0,
                                           base=0, channel_multiplier=-1), reads=["Usb"], writes=["Usb"])
    T.op("pool", lambda e: e.memset(SUsb[:], 1.0), writes=["SUsb"])
    T.op("pool", lambda e: e.affine_select(out=SUsb[:], in_=SUsb[:], pattern=[[1, 128]], compare_op=ALU.is_ge, fill=0.0,
                                           base=-1, channel_multiplier=-1), reads=["SUsb"], writes=["SUsb"])

    T.op("dve", lambda e: e.tensor_scalar(out=nb[:], in0=nb[:], scalar1=-1.0, scalar2=None, op0=ALU.mult), reads=["nb"], writes=["nb"])
    T.op("act", lambda e: e.activation(out=lf[:], in_=fz[:], func=AF.Exp, scale=-1.0, bias=nb[:, 0:1]), reads=["fz", "nb"], writes=["lf"])
    T.op("act", lambda e: e.activation(out=lf[:], in_=lf[:], func=AF.Ln, bias=1.0), reads=["lf"], writes=["lf"])
    T.op("dve", lambda e: e.tensor_scalar(out=lf[:], in0=lf[:], scalar1=-1.0, scalar2=None, op0=ALU.mult), reads=["lf"], writes=["lf"])
    T.op("pe", lambda e: e.matmul(Fps[0:NKB, 0:128], lhsT=lf[:, 0:NKB], rhs=ones[:, :], start=True, stop=True),
         reads=["lf", "ones"], writes=["Fps"])
    T.op("dve", lambda e: e.memset(totT[:], 0.0), writes=["totT"])
    T.op("dve", lambda e: e.tensor_copy(out=totT[0:NKB, :], in_=Fps[0:NKB, 0:128]), writes=["totT", "Fps"])
    T.op("pe", lambda e: e.matmul(Fps[:, 128:128 + NKB], lhsT=totT[:, :], rhs=SUsb[:, 0:NKB], start=True, stop=True),
         reads=["totT", "SUsb"], writes=["Fps"])
    T.op("dve", lambda e: e.tensor_copy(out=Fofs[:], in_=Fps[:, 128:128 + NKB]), writes=["Fofs", "Fps"])
    T.op("pe", lambda e: e.matmul(Fps[:, 256:256 + NKB], lhsT=Usb[:, :], rhs=lf[:, 0:NKB], start=True, stop=True),
         reads=["Usb", "lf"], writes=["Fps"])
    T.op("dve", lambda e: e.tensor_tensor(out=Fsb[:], in0=Fps[:, 256:256 + NKB], in1=Fofs[:], op=ALU.add),
         reads=["Fofs"], writes=["Fsb", "Fps"])
    Gv = Gq[:].rearrange("j (c f) -> j c f", f=4)
    Fv = Fsb[:].rearrange("j (c f) -> j c f", f=4)
    Ov = Fofs[:].rearrange("j (c f) -> j c f", f=4)
    for f in range(4):
        T.op("dve", lambda e: e.tensor_tensor(out=Gv[:, :, f], in0=Fv[:, :, f], in1=Ov[:, :, 0], op=ALU.subtract),
             reads=["Fsb", "Fofs"], writes=["Gq"])
    T.op("dve", lambda e: e.memset(Ghi[:], 0.0), writes=["Ghi"])
    T.op("dve", lambda e: e.memset(Glo[:], 0.0), writes=["Glo"])
    T.op("dve", lambda e: e.tensor_copy(out=Ghi[:, 0:NKB], in_=Gq[:]), reads=["Gq"], writes=["Ghi"])
    T.op("dve", lambda e: e.tensor_copy(out=Ghf[:], in_=Ghi[:, 0:NKB]), reads=["Ghi"], writes=["Ghf"])
    T.op("dve", lambda e: e.tensor_tensor(out=Glo[:, 0:NKB], in0=Gq[:], in1=Ghf[:], op=ALU.subtract), reads=["Gq", "Ghf"], writes=["Glo"])
    T.op("pe", lambda e: e.transpose(out=Tps[:, 0, :], in_=Ghi[:], identity=identb[:]), reads=["Ghi", "identb"], writes=["Tps"])
    T.op("pe", lambda e: e.transpose(out=Tps[:, 1, :], in_=Glo[:], identity=identb[:]), reads=["Glo", "identb"], writes=["Tps"])
    T.op("dve", lambda e: e.tensor_copy(out=GT[:], in_=Tps[:, 0:2, :]), writes=["GT", "Tps"])
    for r in range(2):
        T.op("sp", lambda e: e.dma_start(out=scr[r:r + 1, :].rearrange("o (p j) -> (o p) j", j=128), in_=GT[0:NKB, r, :]),
             reads=["GT"], writes=["scr"], dma=True)
    T.op("sp", lambda e: e.dma_start(out=QTa[64:66, :], in_=scr[:, :]), reads=["scr"], writes=["QTaG"], dma=True)

    it = 0
    for qc in range(NQC):
        ob = qc % 2
        nk = 4 * qc + 4
        bmq = bm[qc % 2]
        T.op("dve", lambda e: e.tensor_scalar(out=bmq[:, 0:nk], in0=Fsb[:, 0:nk], scalar1=-1.0, scalar2=Fofs[:, 4 * qc:4 * qc + 1],
                                              op0=ALU.mult, op1=ALU.add), reads=["Fsb", "Fofs"], writes=["bm%d" % (qc % 2)])
        T.op("dve", lambda e: e.memset(Ops[ob][:], 0.0), writes=["Ops%d" % ob])

        def qk(kb, it):
            sb = it % 3
            j = kb - 4 * qc
            T.op("pe", lambda e: e.matmul(Sps[sb][:], lhsT=KTa[0:66, kb * 128:(kb + 1) * 128], rhs=QTa[0:66, qc * 512:(qc + 1) * 512],
                                          start=True, stop=(j < 0)),
                 reads=["KTa", "KTa8", "QTa", "QTaG"], writes=["Sps%d" % sb])
            if j >= 0:
                T.op("pe", lambda e: e.matmul(Sps[sb][:], lhsT=identb[:], rhs=mk[:, j, :], start=False, stop=True),
                     reads=["identb", "cmask"], writes=["Sps%d" % sb])

        def ex_pv(kb, it):
            sb = it % 3
            pb = it % 4
            j = kb - 4 * qc
            T.op("act", lambda e: e.activation(out=PT[pb][:], in_=Sps[sb][:], func=AF.Exp, scale=0.125, bias=bmq[:, kb:kb + 1]),
                 reads=["bm%d" % (qc % 2)], writes=["PT%d" % pb, "Sps%d" % sb])
            for jj in range(max(j, 0), 4):
                T.op("pe", lambda e: e.matmul(Ops[ob][:, jj, 0:65], lhsT=PT[pb][:, jj * 128:(jj + 1) * 128], rhs=Vp[:, kb, :],
                                              start=False, stop=False, skip_group_check=True),
                     reads=["PT%d" % pb, "Vp", "Vp1"], writes=["Ops%d" % ob])

        qk(0, it)
        for kb in range(nk):
            if kb + 1 < nk:
                qk(kb + 1, it + 1)
            ex_pv(kb, it)
            it += 1
        osb = ost[qc % 2]
        T.op("dve", lambda e: e.reciprocal(out=rc[:], in_=Ops[ob][:, :, 64]), writes=["rc", "Ops%d" % ob])
        for jj in range(4):
            T.op("dve", lambda e: e.tensor_scalar(out=osb[:, jj, :], in0=Ops[ob][:, jj, 0:64], scalar1=rc[:, jj:jj + 1], scalar2=None,
                                                  op0=ALU.mult), reads=["rc"], writes=["ost%d" % (qc % 2), "Ops%d" % ob])
        T.op("sp", lambda e: e.dma_start(out=o[qc * 512:(qc + 1) * 512, :].rearrange("(jj p) d -> p jj d", p=128), in_=osb[:]),
             reads=["ost%d" % (qc % 2)], writes=["o"], dma=True)
    T.finish("sp")
    print("FOX n_ins", T.n_ins, "n_wait", T.n_wait)
    return nc


def fox_inputs(proj, b_fgt, Sx=S):
    OFF_FOX = 512 + 768 + 24
    OFF_FGT = OFF_FOX + 1536
    maps = []
    for h in range(8):
        q = proj[:Sx, OFF_FOX + h * 64:OFF_FOX + (h + 1) * 64]
        k = proj[:Sx, OFF_FOX + 512 + h * 64:OFF_FOX + 512 + (h + 1) * 64]
        v = proj[:Sx, OFF_FOX + 1024 + h * 64:OFF_FOX + 1024 + (h + 1) * 64]
        f = proj[:Sx, OFF_FGT + h]
        maps.append({"qT": np.ascontiguousarray(q.T), "kT": np.ascontiguousarray(k.T), "v": np.ascontiguousarray(v),
                     "f2": np.ascontiguousarray(f.reshape(Sx // 128, 128).T),
                     "bfg": np.full((128, 1), b_fgt[0, h], np.float32)})
    return maps


def run_fox(proj, b_fgt, Sx=S, cores=NCORES):
    nc = build_fox(Sx)
    maps = fox_inputs(proj, b_fgt, Sx)[:cores]
    res = run_bass_kernel_spmd(nc, maps, core_ids=list(range(cores)))
    return np.concatenate([r["o"] for r in res.results], axis=1)


BIGNEG = -30000.0


def build_nsa(Sx=S):
    nc = bass.Bass("TRN2", target_bir_lowering=False)
    NKB = Sx // 128
    NB = Sx // 512
    NCT = max(1, Sx // 2048)
    NCMP = Sx // 16
    NJ = Sx // 64
    dI = lambda name, shape: nc.dram_tensor(name, shape, F32, kind="ExternalInput").ap()
    A = lambda name, shape, dt: nc.alloc_sbuf_tensor("sb_" + name, shape, dt)
    qT4 = dI("qT4", [64, NB, 512])
    kc_s = dI("kc_s", [64, Sx]); vc_s = dI("vc_s", [64, Sx])
    kslcT = dI("kslcT", [64, Sx]); kwinT = dI("kwinT", [64, Sx])
    vslc1 = dI("vslc1", [Sx, 65]); vwin1 = dI("vwin1", [Sx, 65])
    gates = dI("gates", [128, NB * 12])
    w1d = dI("w1", [64, 2 * 32 * 256]); b1T = dI("b1T", [128, 4]); w2d = dI("w2", [128, 4 * 64]); peT = dI("peT", [64, 64])
    D0d = dI("D0", [128, 512]); D1d = dI("D1", [128, 512]); CBd = dI("CB", [128, 18 * 512]); Cfd = dI("Cfull", [128, 512])
    crow = dI("crow", [1, 512])
    wseld = dI("wsel", [128, NCT * NJ]); cvald = dI("cvalid", [128, NCT]); f0d = dI("force0", [128, NJ])
    o = nc.dram_tensor("o", [NB * 128, 256], F32, kind="ExternalOutput").ap()
    T = Trk(nc)
    bufA = A("bufA", [128, Sx], BF16)
    bufB = A("bufB", [128, Sx], BF16)
    bufC = A("bufC", [128, max(2 * NKB * 65, 16384)], BF16)
    w1v = bufC[0:64, 0:16384].rearrange("d (z l h) -> d z l h", z=2, l=32)
    Vs = bufC[:, 0:NKB * 65].rearrange("p (k c) -> p k c", c=65)
    Vw = bufC[:, NKB * 65:2 * NKB * 65].rearrange("p (k c) -> p k c", c=65)
    E = A("E", [128, 64, 128], BF16)
    CB = A("CB", [128, 18, 512], BF16)
    D0 = A("D0", [128, 512], BF16); D1 = A("D1", [128, 512], BF16); W4 = A("W4", [128, 512], BF16)
    hidT = A("hidT", [128, 2, 2, NCMP], BF16)
    KcT = A("KcT", [128, NCMP], BF16)
    Vc = A("Vc", [128, NCT, 65], BF16)
    wsel = A("wsel", [128, NCT, NJ], BF16)
    cval = A("cval", [128, NCT], F32)
    f0 = A("f0", [128, NJ], F32)
    w2 = A("w2", [128, 2, 2, 64], BF16)
    b1 = A("b1", [128, 4], F32); cb = A("cb", [128, 4], F32)
    pe = A("pe", [64, 2, 32], BF16)
    gts = A("gts", [128, NB, 4, 3], F32)
    QT = [A("QT%d" % i, [128, 512], BF16) for i in range(2)]
    PT = [A("PT%d" % i, [128, 512], BF16) for i in range(4)]
    xs = A("xs", [128, 512], F32); x2 = A("x2", [128, 512], F32); sg = A("sg", [128, 512], F32)
    imp = A("imp", [128, NJ], F32); work = A("work", [128, NJ], F32); selm = A("selm", [128, NJ], BF16)
    mx8 = A("mx8", [128, 8], F32)
    selbT = A("selbT", [128, 2, 4, 128], BF16)
    rs = A("rs", [128, 3, 4], F32)
    oacc = [A("oacc%d" % i, [128, 4, 64], F32) for i in range(2)]
    vtmp = A("vtmp", [128, 64], F32)
    ident8 = A("ident8", [128, 128], BF16)
    P = nc.alloc_psum_tensor
    Sps = [P("Sps%d" % i, [128, 512], F32) for i in range(2)]
    Ops = [P("Ops%d" % i, [128, 4, 128], F32) for i in range(3)]
    Ips = [P("Ips%d" % i, [128, 2, 256], F32) for i in range(2)]
    Xps = P("Xps", [128, 8, 128], BF16)
    identb = make_ident(nc, T, BF16, "identb")
    T.op("pool", lambda e: e.tensor_scalar(out=ident8[:], in0=identb[:], scalar1=8.0, scalar2=None, op0=ALU.mult),
         reads=["identb"], writes=["ident8"])

    def cast_load(dst, src, key, eng="pool"):
        T.op(eng, lambda e: e.dma_start(out=dst, in_=src), writes=[key], dma=True)

    cast_load(CB[:].rearrange("p r c -> p (r c)"), CBd[:, :], "CB")
    cast_load(D0[:], D0d[:, :], "D0"); cast_load(D1[:], D1d[:, :], "D1"); cast_load(W4[:], Cfd[:, :], "W4")
    cast_load(wsel[:].rearrange("p a b -> p (a b)"), wseld[:, :], "wsel")
    cast_load(w2[:].rearrange("p z c d -> p (z c d)"), w2d[:, :], "w2")
    cast_load(pe[:].rearrange("d z l -> d (z l)"), peT[:, :], "pe")
    cast_load(w1v.rearrange("d z l h -> d (z l h)"), w1d[:, :], "bufC")
    T.op("sp", lambda e: e.dma_start(out=cval[:], in_=cvald[:, :]), writes=["cval"], dma=True)
    T.op("sp", lambda e: e.dma_start(out=f0[:], in_=f0d[:, :]), writes=["f0"], dma=True)
    T.op("sp", lambda e: e.dma_start(out=b1[:], in_=b1T[:, :]), writes=["b1"], dma=True)
    T.op("sp", lambda e: e.dma_start(out=gts[:].rearrange("p a h b -> p (a h b)"), in_=gates[:, :]), writes=["gts"], dma=True)
    T.op("act", lambda e: e.activation(out=gts[:].rearrange("p a h b -> p (a h b)"), in_=gts[:].rearrange("p a h b -> p (a h b)"), func=AF.Sigmoid),
         reads=["gts"], writes=["gts"])
    for i in range(2):
        cast_load(QT[i][64:65, :], crow[:, :], "QTc%d" % i)
    v4 = lambda t: t[:].rearrange("p (h t) -> p h t", h=4)
    T.op("pool", lambda e: e.affine_select(out=v4(D0), in_=v4(D0), pattern=[[0, 4], [1, 128]], compare_op=ALU.is_ge, fill=BIGNEG,
                                           base=0, channel_multiplier=-1), reads=["D0"], writes=["D0"])
    T.op("pool", lambda e: e.affine_select(out=v4(W4), in_=v4(W4), pattern=[[0, 4], [-1, 128]], compare_op=ALU.is_ge, fill=BIGNEG,
                                           base=-1, channel_multiplier=1), reads=["W4"], writes=["W4"])
    for R in range(18):
        cbv = CB[:, R, :].rearrange("p (h t) -> p h t", h=4)
        T.op("pool", lambda e: e.affine_select(out=cbv, in_=cbv, pattern=[[0, 4], [1, 128]], compare_op=ALU.is_ge, fill=BIGNEG,
                                               base=128 * R - 31, channel_multiplier=-16), reads=["CB"], writes=["CB"])
    T.op("pool", lambda e: e.memset(E[:], 1.0), writes=["E"])
    for hs in range(2):
        ev = E[:, :, hs * 64:(hs + 1) * 64]
        T.op("pool", lambda e: e.affine_select(out=ev, in_=ev, pattern=[[-2, 64], [0, 64]], compare_op=ALU.is_equal, fill=0.0,
                                               base=-hs, channel_multiplier=1), reads=["E"], writes=["E"])

    for h in range(0, Sx, 2048):
        w = min(2048, Sx - h)
        cast_load(bufA[0:64, h:h + w], kc_s[:, h:h + w], "bufA")
        cast_load(bufB[0:64, h:h + w], vc_s[:, h:h + w], "bufB")
    T.op("dve", lambda e: e.memset(hidT[:], 0.0), writes=["hidT"])
    T.op("dve", lambda e: e.memset(KcT[:], 0.0), writes=["KcT"])
    T.op("dve", lambda e: e.memset(KcT[64:65, :], 8.0), reads=[], writes=["KcT"])
    for z in range(2):
        for c2 in range(2):
            col = z * 2 + c2
            for l in range(32):
                T.op("pe", lambda e: e.matmul(Sps[0][:, col:col + 1], lhsT=w1v[:, z, l, c2 * 128:(c2 + 1) * 128], rhs=pe[:, z, l:l + 1],
                                              start=(l == 0), stop=(l == 31)), reads=["bufC", "pe"], writes=["Sps0"])
    T.op("dve", lambda e: e.tensor_tensor(out=cb[:], in0=Sps[0][:, 0:4], in1=b1[:], op=ALU.add), reads=["b1"], writes=["cb", "Sps0"])
    nvalid = NCMP - 1
    src = [bufA, bufB]
    it = 0
    for z in range(2):
        for c2 in range(2):
            for n0 in range(0, nvalid, 512):
                nw = min(512, nvalid - n0)
                sb = it % 2
                it += 1
                for l in range(32):
                    T.op("pe", lambda e: e.matmul(Sps[sb][:, 0:nw], lhsT=w1v[:, z, l, c2 * 128:(c2 + 1) * 128],
                                                  rhs=src[z][0:64, 16 * n0 + l:16 * n0 + l + 16 * (nw - 1) + 1:16],
                                                  start=(l == 0), stop=(l == 31)),
                         reads=["bufC", "bufA" if z == 0 else "bufB"], writes=["Sps%d" % sb])
                col = z * 2 + c2
                T.op("act", lambda e: e.activation(out=xs[:, 0:nw], in_=Sps[sb][:, 0:nw], func=AF.Identity, bias=cb[:, col:col + 1]),
                     reads=["cb"], writes=["xs", "Sps%d" % sb])
                T.op("dve", lambda e: e.tensor_tensor(out=x2[:, 0:nw], in0=xs[:, 0:nw], in1=xs[:, 0:nw], op=ALU.mult), reads=["xs"], writes=["x2"])
                T.op("dve", lambda e: e.tensor_scalar(out=x2[:, 0:nw], in0=x2[:, 0:nw], scalar1=0.044715, scalar2=1.0, op0=ALU.mult, op1=ALU.add),
                     reads=["x2"], writes=["x2"])
                T.op("dve", lambda e: e.tensor_tensor(out=x2[:, 0:nw], in0=x2[:, 0:nw], in1=xs[:, 0:nw], op=ALU.mult), reads=["x2", "xs"], writes=["x2"])
                T.op("act", lambda e: e.activation(out=sg[:, 0:nw], in_=x2[:, 0:nw], func=AF.Sigmoid, scale=1.5957691216057308),
                     reads=["x2"], writes=["sg"])
                T.op("dve", lambda e: e.tensor_tensor(out=hidT[:, z, c2, n0:n0 + nw], in0=xs[:, 0:nw], in1=sg[:, 0:nw], op=ALU.mult),
                     reads=["xs", "sg"], writes=["hidT"])
    for n0 in range(0, NCMP, 512):
        nw = min(512, NCMP - n0)
        for c2 in range(2):
            T.op("pe", lambda e: e.matmul(Sps[0][0:64, 0:nw], lhsT=w2[:, 0, c2, :], rhs=hidT[:, 0, c2, n0:n0 + nw], start=(c2 == 0), stop=(c2 == 1)),
                 reads=["w2", "hidT"], writes=["Sps0"])
        T.op("act", lambda e: e.copy(out=KcT[0:64, n0:n0 + nw], in_=Sps[0][0:64, 0:nw]), writes=["KcT", "Sps0"])
    for nt in range(NCT):
        for c2 in range(2):
            T.op("pe", lambda e: e.matmul(Sps[1][:, 0:64], lhsT=hidT[:, 1, c2, nt * 128:(nt + 1) * 128], rhs=w2[:, 1, c2, :], start=(c2 == 0), stop=(c2 == 1)),
                 reads=["w2", "hidT"], writes=["Sps1"])
        T.op("dve", lambda e: e.tensor_scalar(out=Vc[:, nt, 0:64], in0=Sps[1][:, 0:64], scalar1=cval[:, nt:nt + 1], scalar2=None, op0=ALU.mult),
             reads=["cval"], writes=["Vc", "Sps1"])
        T.op("dve", lambda e: e.tensor_copy(out=Vc[:, nt, 64:65], in_=cval[:, nt:nt + 1]), reads=["cval"], writes=["Vc"])

    for h in range(0, Sx, 2048):
        w = min(2048, Sx - h)
        cast_load(bufA[0:64, h:h + w], kslcT[:, h:h + w], "bufA")
        cast_load(bufB[0:64, h:h + w], kwinT[:, h:h + w], "bufB")
    T.op("dve", lambda e: e.memset(bufA[64:65, :], 8.0), writes=["bufA8"])
    T.op("dve", lambda e: e.memset(bufB[64:65, :], 8.0), writes=["bufB8"])
    vsv = vslc1.rearrange("(kb p) d -> p kb d", p=128)
    vwv = vwin1.rearrange("(kb p) d -> p kb d", p=128)
    for h in range(0, NKB, 16):
        w = min(16, NKB - h)
        cast_load(Vs[:, h:h + w, :], vsv[:, h:h + w, :], "bufC")
        cast_load(Vw[:, h:h + w, :], vwv[:, h:h + w, :], "bufC")

    sidx = [0]
    pidx = [0]

    def tile_step(Kbuf, kkey, col0, Vap, vkey, ob, qt, qkey, far, bias_tile, bias_key, sel_chunk=None, sel_m=None, imp_nt=None):
        sb = sidx[0] % 2
        sidx[0] += 1
        pb = pidx[0] % 4
        pidx[0] += 1
        kk = 65 if far else 64
        last_plain = (bias_tile is None and sel_chunk is None)
        T.op("pe", lambda e: e.matmul(Sps[sb][:], lhsT=Kbuf[0:kk, col0:col0 + 128], rhs=qt[0:kk, :], start=True, stop=last_plain),
             reads=[kkey, kkey + "8", qkey, qkey.replace("QT", "QTc")], writes=["Sps%d" % sb])
        if bias_tile is not None:
            T.op("pe", lambda e: e.matmul(Sps[sb][:], lhsT=ident8[:], rhs=bias_tile, start=False, stop=(sel_chunk is None)),
                 reads=["ident8", bias_key], writes=["Sps%d" % sb])
        if sel_chunk is not None:
            T.op("pe", lambda e: e.matmul(Sps[sb][:], lhsT=E[:, sel_m, :], rhs=selbT[:, sel_chunk, :, :].rearrange("p h t -> p (h t)"),
                                          start=False, stop=True), reads=["E", "selbT"], writes=["Sps%d" % sb])
        T.op("act", lambda e: e.activation(out=PT[pb][:], in_=Sps[sb][:], func=AF.Exp, scale=0.125), writes=["PT%d" % pb, "Sps%d" % sb])
        for h in range(4):
            T.op("pe", lambda e: e.matmul(Ops[ob][:, h, 0:65], lhsT=PT[pb][:, h * 128:(h + 1) * 128], rhs=Vap,
                                          start=False, stop=False, skip_group_check=True),
                 reads=["PT%d" % pb, vkey], writes=["Ops%d" % ob])
        if imp_nt is not None:
            for h in range(4):
                T.op("pe", lambda e: e.matmul(Ips[h // 2][:, h % 2, 0:NJ], lhsT=PT[pb][:, h * 128:(h + 1) * 128], rhs=wsel[:, imp_nt, :],
                                              start=False, stop=False, skip_group_check=True),
                     reads=["PT%d" % pb, "wsel"], writes=["Ips%d" % (h // 2)])

    for i in range(NB):
        md = 4 * i + 3
        qt = QT[i % 2]
        qkey = "QT%d" % (i % 2)
        cast_load(qt[0:64, :], qT4[:, i, :], qkey)
        for b in range(3):
            T.op("dve", lambda e: e.memset(Ops[b][:], 0.0), writes=["Ops%d" % b])
        for b in range(2):
            T.op("dve", lambda e: e.memset(Ips[b][:], 0.0), writes=["Ips%d" % b])
        for nt in range(NCT):
            R = md - 16 * nt
            if R < 0:
                continue
            far = R >= 18
            tile_step(KcT, "KcT", nt * 128, Vc[:, nt, :], "Vc", 0, qt, qkey, far,
                      None if far else CB[:, R, :], "CB", imp_nt=nt)
        T.op("dve", lambda e: e.tensor_scalar(out=rs[:, 0, :], in0=Ops[0][:, :, 64], scalar1=1e-30, scalar2=None, op0=ALU.max),
             writes=["rs0", "Ops0"])
        T.op("dve", lambda e: e.reciprocal(out=rs[:, 0, :], in_=rs[:, 0, :]), reads=["rs0"], writes=["rs0"])
        for h in range(4):
            if h == 0:
                T.op("dve", lambda e: e.tensor_scalar(out=imp[:], in0=Ips[0][:, 0, 0:NJ], scalar1=rs[:, 0, 0:1], scalar2=None, op0=ALU.mult),
                     reads=["rs0"], writes=["imp", "Ips0"])
            else:
                T.op("dve", lambda e: e.scalar_tensor_tensor(out=imp[:], in0=Ips[h // 2][:, h % 2, 0:NJ], scalar=rs[:, 0, h:h + 1], in1=imp[:],
                                                             op0=ALU.mult, op1=ALU.add), reads=["rs0", "imp"], writes=["imp", "Ips%d" % (h // 2)])
        c1 = 2 * md + 1
        if c1 + 1 < NJ:
            T.op("dve", lambda e: e.memset(imp[:, c1 + 1:NJ], -1.0), reads=["imp"], writes=["imp"])
        T.op("dve", lambda e: e.memset(imp[0:64, c1:c1 + 1], -1.0), reads=["imp"], writes=["imp"])
        T.op("dve", lambda e: e.memset(imp[64:128, c1:c1 + 1], 20000.0), reads=["imp"], writes=["imp"])
        T.op("dve", lambda e: e.memset(imp[0:64, c1 - 1:c1], 20000.0), reads=["imp"], writes=["imp"])
        T.op("dve", lambda e: e.memset(imp[64:128, c1 - 1:c1], 10000.0), reads=["imp"], writes=["imp"])
        T.op("dve", lambda e: e.memset(imp[0:64, c1 - 2:c1 - 1], 10000.0), reads=["imp"], writes=["imp"])
        T.op("dve", lambda e: e.tensor_tensor(out=imp[:], in0=imp[:], in1=f0[:], op=ALU.max), reads=["imp", "f0"], writes=["imp"])
        T.op("dve", lambda e: e.tensor_copy(out=work[:], in_=imp[:]), reads=["imp"], writes=["work"])
        for rnd in range(2):
            T.op("dve", lambda e: e.max(out=mx8[:], in_=work[:]), reads=["work"], writes=["mx8"])
            T.op("dve", lambda e: e.match_replace(out=work[:], in_to_replace=mx8[:], in_values=work[:], imm_value=-1e9),
                 reads=["mx8", "work"], writes=["work"])
        T.op("dve", lambda e: e.tensor_scalar(out=selm[:], in0=work[:], scalar1=-1e8, scalar2=None, op0=ALU.is_le), reads=["work"], writes=["selm"])
        nch = (NJ + 127) // 128
        for ch in range(nch):
            cwid = min(128, NJ - ch * 128)
            T.op("pe", lambda e: e.transpose(out=Xps[0:cwid, ch, :], in_=selm[:, ch * 128:ch * 128 + cwid], identity=identb[:]),
                 reads=["selm", "identb"], writes=["Xps"])
        for ch in range(nch):
            cwid = min(128, NJ - ch * 128)
            for h in range(4):
                T.op("dve", lambda e: e.tensor_scalar(out=selbT[0:cwid, ch, h, :], in0=Xps[0:cwid, ch, :], scalar1=-1.0, scalar2=-BIGNEG,
                                                      op0=ALU.add, op1=ALU.mult), writes=["selbT", "Xps"])
        for m in range(max(0, md - 4), md + 1):
            rel = md - m
            bt, bk = {0: (D0[:], "D0"), 1: (D1[:], "D1"), 4: (W4[:], "W4")}.get(rel, (None, None))
            tile_step(bufB, "bufB", m * 128, Vw[:, m, :], "bufC", 2, qt, qkey, bt is None, bt, bk)
        for m in range(0, md + 1):
            rel = md - m
            bt, bk = {0: (D0[:], "D0"), 1: (D1[:], "D1")}.get(rel, (None, None))
            tile_step(bufA, "bufA", m * 128, Vs[:, m, :], "bufC", 1, qt, qkey, bt is None, bt, bk, sel_chunk=m // 64, sel_m=m % 64)
        oa = oacc[i % 2]
        for b in range(3):
            if b > 0:
                T.op("dve", lambda e: e.tensor_scalar(out=rs[:, b, :], in0=Ops[b][:, :, 64], scalar1=1e-30, scalar2=None, op0=ALU.max),
                     writes=["rs%d" % b, "Ops%d" % b])
                T.op("dve", lambda e: e.reciprocal(out=rs[:, b, :], in_=rs[:, b, :]), reads=["rs%d" % b], writes=["rs%d" % b])
            T.op("dve", lambda e: e.tensor_tensor(out=rs[:, b, :], in0=rs[:, b, :], in1=gts[:, i, :, b], op=ALU.mult),
                 reads=["rs%d" % b, "gts"], writes=["rs%d" % b])
            for h in range(4):
                if b == 0:
                    T.op("dve", lambda e: e.tensor_scalar(out=oa[:, h, :], in0=Ops[b][:, h, 0:64], scalar1=rs[:, b, h:h + 1], scalar2=None, op0=ALU.mult),
                         reads=["rs%d" % b], writes=["oacc%d" % (i % 2), "Ops%d" % b])
                else:
                    T.op("dve", lambda e: e.scalar_tensor_tensor(out=oa[:, h, :], in0=Ops[b][:, h, 0:64], scalar=rs[:, b, h:h + 1], in1=oa[:, h, :],
                                                                 op0=ALU.mult, op1=ALU.add), reads=["rs%d" % b], writes=["oacc%d" % (i % 2), "Ops%d" % b])
        T.op("sp", lambda e: e.dma_start(out=o[i * 128:(i + 1) * 128, :], in_=oa[:].rearrange("p h d -> p (h d)")),
             reads=["oacc%d" % (i % 2)], writes=["o"], dma=True)
    T.finish("sp")
    print("NSA n_ins", T.n_ins, "n_wait", T.n_wait)
    return nc


def t5_bucket_np(dist):
    n = np.maximum(dist, 0)
    ratio = np.log(np.maximum(n, 16).astype(np.float32) / np.float32(16))
    big = 16 + (ratio / np.float32(np.log(128 / 16)) * np.float32(16)).astype(np.int32)
    return np.where(n < 16, n, np.minimum(big, 31))


def nsa_inputs(proj, t5_table, cmp_pe, cmp_w1, cmp_b1, cmp_w2, Sx=S):
    OFF_KV = 512
    OFF_GATE = 512 + 768
    NB = Sx // 512
    NCT = max(1, Sx // 2048)
    NJ = Sx // 64
    NCMP = Sx // 16
    maps = []
    si = np.arange(128)[:, None]
    ti = np.arange(128)[None, :]
    w1 = np.ascontiguousarray(cmp_w1[0].reshape(2, 32, 64, 256).transpose(2, 0, 1, 3).reshape(64, -1))
    b1T = np.ascontiguousarray(cmp_b1[0].reshape(2, 2, 128).transpose(2, 0, 1).reshape(128, 4))
    w2 = np.ascontiguousarray(cmp_w2[0].reshape(2, 2, 128, 64).transpose(2, 0, 1, 3).reshape(128, -1))
    peT = np.ascontiguousarray(cmp_pe[0].transpose(2, 0, 1).reshape(64, 64))
    for c in range(8):
        g, r = c // 4, c % 4
        sh = 3 - r
        p = proj[:Sx]
        tb = t5_table.T[4 * g:4 * g + 4]
        kvc = lambda z: p[:, OFF_KV + (z * 2 + g) * 64:OFF_KV + (z * 2 + g + 1) * 64]

        def shiftT(a):
            out = np.zeros((64, Sx), np.float32)
            if sh * 128 < Sx:
                out[:, sh * 128:] = a[:Sx - sh * 128].T
            return out

        def shiftV1(a):
            out = np.zeros((Sx, 65), np.float32)
            if sh * 128 < Sx:
                out[sh * 128:, 0:64] = a[:Sx - sh * 128]
                out[sh * 128:, 64] = 1.0
            return out
        q = p[:, g * 256:(g + 1) * 256].reshape(Sx // 128, 128, 4, 64)[r::4]
        qT4 = np.ascontiguousarray(q.transpose(3, 0, 2, 1).reshape(64, NB, 512))
        gt = p[:, OFF_GATE + g * 12:OFF_GATE + (g + 1) * 12].reshape(Sx // 128, 128, 12)[r::4]
        gates = np.ascontiguousarray(gt.transpose(1, 0, 2).reshape(128, NB * 12))
        D0 = np.stack([tb[h][t5_bucket_np(ti - si)] for h in range(4)], axis=1).reshape(128, 512)
        D1 = np.stack([tb[h][t5_bucket_np(128 + ti - si)] for h in range(4)], axis=1).reshape(128, 512)
        CB = np.stack([np.stack([tb[h][t5_bucket_np(128 * R + ti - 16 * si - 31)] for h in range(4)], axis=1).reshape(128, 512)
                       for R in range(18)], axis=1).reshape(128, 18 * 512)
        Cfull = np.ascontiguousarray(np.broadcast_to(np.repeat(tb[:, 31], 128)[None, :], (128, 512)))
        crow = np.ascontiguousarray(np.repeat(tb[:, 31], 128)[None, :])
        npad = 8 * sh
        nn = np.arange(NCT * 128)[:, None]
        jj = np.arange(NJ)[None, :]
        dlt = nn - 4 * jj
        wsel = np.where((dlt >= 0) & (dlt <= 2), 1.0, np.where((dlt == -1) | (dlt == 3), 0.5, 0.0)).astype(np.float32)
        wsel[:npad] = 0.0
        wsel[NCMP - 1 - 0:] = 0.0 if True else 0.0
        wsel = np.ascontiguousarray(wsel.reshape(NCT, 128, NJ).transpose(1, 0, 2).reshape(128, NCT * NJ))
        cvalid = (np.arange(NCT * 128) >= npad).astype(np.float32)
        cvalid[NCMP - 1:] = 0.0
        cvalid = np.ascontiguousarray(cvalid.reshape(NCT, 128).T)
        force0 = np.zeros((128, NJ), np.float32)
        force0[:, 2 * sh] = 30000.0
        maps.append({"qT4": qT4, "kc_s": shiftT(kvc(0)), "vc_s": shiftT(kvc(1)), "kslcT": shiftT(kvc(2)), "kwinT": shiftT(kvc(4)),
                     "vslc1": shiftV1(kvc(3)), "vwin1": shiftV1(kvc(5)), "gates": gates, "w1": w1, "b1T": b1T, "w2": w2, "peT": peT,
                     "D0": np.ascontiguousarray(D0), "D1": np.ascontiguousarray(D1), "CB": np.ascontiguousarray(CB), "Cfull": Cfull, "crow": crow,
                     "wsel": wsel, "cvalid": cvalid, "force0": force0})
    return maps


def run_nsa(proj, t5_table, cmp_pe, cmp_w1, cmp_b1, cmp_w2, Sx=S, cores=NCORES):
    nc = build_nsa(Sx)
    maps = nsa_inputs(proj, t5_table, cmp_pe, cmp_w1, cmp_b1, cmp_w2, Sx)[:cores]
    res = run_bass_kernel_spmd(nc, maps, core_ids=list(range(cores)))
    out = np.zeros((Sx, 512), np.float32)
    for c in range(cores):
        g, r = c // 4, c % 4
        oc = res.results[c]["o"].reshape(Sx // 512, 128, 256)
        out.reshape(Sx // 128, 128, 512)[r::4, :, g * 256:(g + 1) * 256] = oc
    return out


ALPHA = 2.0 ** 0.25


def build_l3a(TPC=TPC):
    nc = bass.Bass("TRN2", target_bir_lowering=False)
    NT = TPC // 128
    NG = TPC // 512
    dI = lambda name, shape: nc.dram_tensor(name, shape, F32, kind="ExternalInput").ap()
    A = lambda name, shape, dt: nc.alloc_sbuf_tensor("sb_" + name, shape, dt)
    onT = dI("onT", [512, TPC]); ofT = dI("ofT", [512, TPC]); mgT = dI("mgT", [4096, TPC]); x = dI("x", [TPC, D])
    wbn = dI("wbn", [512, D]); wbf = dI("wbf", [512, D]); wo = dI("wo", [D, D])
    g1b = dI("g1b", [128, D]); ln1g = dI("ln1g", [128, D]); ln1b = dI("ln1b", [128, D])
    mod2 = dI("mod2", [128, 32]); wr = dI("wr", [D, 72]); brb = dI("brb", [128, 72])
    x1o = nc.dram_tensor("x1", [TPC, D], F32, kind="ExternalOutput").ap()
    xn2o = nc.dram_tensor("xn2", [TPC, D], BF16, kind="ExternalOutput").ap()
    rwo = nc.dram_tensor("rw", [TPC, 64], F32, kind="ExternalOutput").ap()
    oho = nc.dram_tensor("oh", [TPC, 8], F32, kind="ExternalOutput").ap()
    mTd = nc.dram_tensor("mTd", [16, 128, TPC], BF16).ap()
    T = Trk(nc)
    big = A("big", [128, 32768], BF16)
    wbn_s = big[:, 0:8192].rearrange("p (k n) -> p k n", k=4)
    wbf_s = big[:, 8192:16384].rearrange("p (k n) -> p k n", k=4)
    onT_s = big[:, 16384:16384 + 4 * TPC].rearrange("p (k n) -> p k n", k=4)
    ofT_s = big[:, 24576:24576 + 4 * TPC].rearrange("p (k n) -> p k n", k=4)
    wo_s = big[:, :].rearrange("p (k n) -> p k n", k=16)
    mgs = [A("mgs%d" % i, [128, 2, 512], F32) for i in range(2)]
    t1 = A("t1", [128, 512], F32); t2 = A("t2", [128, 512], F32)
    mo = [A("mo%d" % i, [128, 512], BF16) for i in range(2)]
    P = nc.alloc_psum_tensor
    acc = [P("acc%d" % i, [128, 512], F32) for i in range(4)]
    tpf = [P("tpf%d" % i, [128, 4, 128], F32) for i in range(2)]
    rps = P("rps", [128, 512], F32)

    def cast_load(dst, src, key):
        T.op("pool", lambda e: e.dma_start(out=dst, in_=src), writes=[key], dma=True)
    cast_load(wbn_s, wbn.rearrange("(k p) n -> p k n", p=128), "big")
    cast_load(wbf_s, wbf.rearrange("(k p) n -> p k n", p=128), "big")
    cast_load(onT_s, onT.rearrange("(k p) n -> p k n", p=128), "big")
    cast_load(ofT_s, ofT.rearrange("(k p) n -> p k n", p=128), "big")
    it = 0
    for dc in range(16):
        for tg in range(NG):
            mb = mgs[it % 2]
            T.op("sp", lambda e: e.dma_start(out=mb[:, 0, :], in_=mgT[dc * 128:(dc + 1) * 128, tg * 512:(tg + 1) * 512]), writes=["mgs%d" % (it % 2)], dma=True)
            T.op("sp", lambda e: e.dma_start(out=mb[:, 1, :], in_=mgT[2048 + dc * 128:2048 + (dc + 1) * 128, tg * 512:(tg + 1) * 512]), writes=["mgs%d" % (it % 2)], dma=True)
            T.op("act", lambda e: e.activation(out=mb[:].rearrange("p a n -> p (a n)"), in_=mb[:].rearrange("p a n -> p (a n)"), func=AF.Sigmoid),
                 reads=["mgs%d" % (it % 2)], writes=["mgs%d" % (it % 2)])
            for br, (ws, os_) in enumerate([(wbn_s, onT_s), (wbf_s, ofT_s)]):
                a = (it % 2) * 2 + br
                for k in range(4):
                    T.op("pe", lambda e: e.matmul(acc[a][:], lhsT=ws[:, k, dc * 128:(dc + 1) * 128], rhs=os_[:, k, tg * 512:(tg + 1) * 512],
                                                  start=(k == 0), stop=(k == 3)), reads=["big"], writes=["acc%d" % a])
            a0 = (it % 2) * 2
            T.op("dve", lambda e: e.tensor_tensor(out=t1[:], in0=acc[a0][:], in1=mb[:, 0, :], op=ALU.mult), reads=["mgs%d" % (it % 2)], writes=["t1", "acc%d" % a0])
            T.op("dve", lambda e: e.tensor_tensor(out=t2[:], in0=acc[a0 + 1][:], in1=mb[:, 1, :], op=ALU.mult), reads=["mgs%d" % (it % 2)], writes=["t2", "acc%d" % (a0 + 1)])
            T.op("dve", lambda e: e.tensor_tensor(out=mo[it % 2][:], in0=t1[:], in1=t2[:], op=ALU.add), reads=["t1", "t2"], writes=["mo%d" % (it % 2)])
            T.op("sp", lambda e: e.dma_start(out=mTd[dc, :, tg * 512:(tg + 1) * 512], in_=mo[it % 2][:]), reads=["mo%d" % (it % 2)], writes=["mTd"], dma=True)
            it += 1
    for h in range(4):
        cast_load(wo_s[:, 4 * h:4 * h + 4, :], wo.rearrange("(k p) n -> p k n", p=128)[:, 4 * h:4 * h + 4, :], "big")
    g1p = A("g1p", [128, D], F32); lg_ = A("lng", [128, D], F32); lb_ = A("lnb", [128, D], F32)
    T.op("sp", lambda e: e.dma_start(out=g1p[:], in_=g1b[:, :]), writes=["g1p"], dma=True)
    T.op("sp", lambda e: e.dma_start(out=lg_[:], in_=ln1g[:, :]), writes=["lng"], dma=True)
    T.op("sp", lambda e: e.dma_start(out=lb_[:], in_=ln1b[:, :]), writes=["lnb"], dma=True)
    T.op("dve", lambda e: e.tensor_scalar(out=g1p[:], in0=g1p[:], scalar1=1.0, scalar2=None, op0=ALU.add), reads=["g1p"], writes=["g1p"])
    m2 = A("m2", [128, 32], F32); wrs = A("wrs", [128, 16, 72], F32); brs = A("brs", [128, 72], F32)
    T.op("sp", lambda e: e.dma_start(out=m2[:], in_=mod2[:, :]), writes=["m2"], dma=True)
    T.op("dve", lambda e: e.tensor_scalar(out=m2[:, 16:32], in0=m2[:, 16:32], scalar1=1.0, scalar2=None, op0=ALU.add), reads=["m2"], writes=["m2"])
    T.op("sp", lambda e: e.dma_start(out=wrs[:], in_=wr.rearrange("(k p) n -> p k n", p=128)), writes=["wrs"], dma=True)
    T.op("sp", lambda e: e.dma_start(out=brs[:], in_=brb[:, :]), writes=["brs"], dma=True)
    identf = make_ident(nc, T, F32, "identf")
    mt = [A("mt%d" % i, [128, 16, 128], BF16) for i in range(2)]
    xt = [A("xt%d" % i, [128, D], F32) for i in range(2)]
    v = A("v", [128, D], F32); xn = A("xn", [128, D], F32); junk = A("junk", [128, D], BF16)
    xnb = A("xnb", [128, D], BF16)
    u2T = A("u2T", [128, 16, 128], F32)
    st = A("st", [128, 16], F32)
    lgt = A("lgt", [128, 72], F32); ml = A("ml", [128, 64], F32); mx8 = A("mx8", [128, 8], F32)
    r1 = A("r1", [128, 16], F32); ra = A("ra", [128, 64], F32); rb = A("rb", [128, 64], F32); ohs = A("ohs", [128, 8], F32)
    ge = A("ge", [128, 8], F32)

    def ln_stats(src, key):
        T.op("dve", lambda e: e.reduce_sum(out=st[:, 0:1], in_=src, axis=AX.X), reads=[key], writes=["st0"])
        T.op("act", lambda e: e.activation(out=junk[:], in_=src, func=AF.Square, accum_out=st[:, 1:2]), reads=[key], writes=["st1", "junk"])
        T.op("dve", lambda e: e.tensor_scalar(out=st[:, 2:3], in0=st[:, 0:1], scalar1=1.0 / D, scalar2=None, op0=ALU.mult), reads=["st0"], writes=["st2"])
        T.op("dve", lambda e: e.tensor_tensor(out=st[:, 3:4], in0=st[:, 2:3], in1=st[:, 2:3], op=ALU.mult), reads=["st2"], writes=["st3"])
        T.op("dve", lambda e: e.scalar_tensor_tensor(out=st[:, 4:5], in0=st[:, 1:2], scalar=1.0 / D, in1=st[:, 3:4], op0=ALU.mult, op1=ALU.subtract),
             reads=["st1", "st3"], writes=["st4"])
        T.op("dve", lambda e: e.tensor_scalar(out=st[:, 6:7], in0=st[:, 4:5], scalar1=LN_EPS, scalar2=None, op0=ALU.add), reads=["st4"], writes=["st6"])
        T.op("act", lambda e: e.activation(out=st[:, 7:8], in_=st[:, 6:7], func=AF.Sqrt), reads=["st6"], writes=["st7"])
        T.op("dve", lambda e: e.reciprocal(out=st[:, 5:6], in_=st[:, 7:8]), reads=["st7"], writes=["st5"])

    for i in range(NT):
        b = i % 2
        T.op("sp", lambda e: e.dma_start(out=mt[b][:], in_=mTd[:, :, i * 128:(i + 1) * 128].rearrange("k p t -> p k t")), reads=["mTd"], writes=["mt%d" % b], dma=True)
        T.op("sp", lambda e: e.dma_start(out=xt[b][:], in_=x[i * 128:(i + 1) * 128, :]), writes=["xt%d" % b], dma=True)
        for cg in range(4):
            for k in range(16):
                T.op("pe", lambda e: e.matmul(acc[cg][:], lhsT=mt[b][:, k, :], rhs=wo_s[:, k, cg * 512:(cg + 1) * 512], start=(k == 0), stop=(k == 15)),
                     reads=["mt%d" % b, "big"], writes=["acc%d" % cg])
            T.op("dve", lambda e: e.tensor_tensor(out=v[:, cg * 512:(cg + 1) * 512], in0=acc[cg][:], in1=g1p[:, cg * 512:(cg + 1) * 512], op=ALU.mult),
                 reads=["g1p"], writes=["v", "acc%d" % cg])
        T.op("dve", lambda e: e.scalar_tensor_tensor(out=v[:], in0=xt[b][:], scalar=ALPHA, in1=v[:], op0=ALU.mult, op1=ALU.add),
             reads=["xt%d" % b, "v"], writes=["v"])
        ln_stats(v[:], "v")
        T.op("dve", lambda e: e.tensor_scalar(out=xn[:], in0=v[:], scalar1=st[:, 2:3], scalar2=st[:, 5:6], op0=ALU.subtract, op1=ALU.mult),
             reads=["v", "st2", "st5"], writes=["xn"])
        T.op("dve", lambda e: e.tensor_tensor(out=xn[:], in0=xn[:], in1=lg_[:], op=ALU.mult), reads=["xn", "lng"], writes=["xn"])
        T.op("dve", lambda e: e.tensor_tensor(out=xn[:], in0=xn[:], in1=lb_[:], op=ALU.add), reads=["xn", "lnb"], writes=["xn"])
        T.op("sp", lambda e: e.dma_start(out=x1o[i * 128:(i + 1) * 128, :], in_=xn[:]), reads=["xn"], writes=["x1o"], dma=True)
        ln_stats(xn[:], "xn")
        T.op("dve", lambda e: e.tensor_scalar(out=v[:], in0=xn[:], scalar1=st[:, 2:3], scalar2=st[:, 5:6], op0=ALU.subtract, op1=ALU.mult),
             reads=["xn", "st2", "st5"], writes=["v"])
        T.op("act", lambda e: e.copy(out=xnb[:], in_=v[:]), reads=["v"], writes=["xnb"])
        T.op("sp", lambda e: e.dma_start(out=xn2o[i * 128:(i + 1) * 128, :], in_=xnb[:]), reads=["xnb"], writes=["xn2o"], dma=True)
        for kq in range(4):
            pb = kq % 2
            for j in range(4):
                k = kq * 4 + j
                T.op("pe", lambda e: e.transpose(out=tpf[pb][:, j, :], in_=v[:, k * 128:(k + 1) * 128], identity=identf[:]),
                     reads=["v", "identf"], writes=["tpf%d" % pb])
            for j in range(4):
                k = kq * 4 + j
                T.op("act", lambda e: e.activation(out=u2T[:, k, :], in_=tpf[pb][:, j, :], func=AF.Identity, scale=m2[:, 16 + k:17 + k], bias=m2[:, k:k + 1]),
                     reads=["m2"], writes=["u2T", "tpf%d" % pb])
        for k in range(16):
            T.op("pe", lambda e: e.matmul(rps[:, 0:72], lhsT=u2T[:, k, :], rhs=wrs[:, k, :], start=(k == 0), stop=(k == 15)),
                 reads=["u2T", "wrs"], writes=["rps"])
        T.op("dve", lambda e: e.tensor_tensor(out=lgt[:], in0=rps[:, 0:72], in1=brs[:], op=ALU.add), reads=["brs"], writes=["lgt", "rps"])
        T.op("dve", lambda e: e.reduce_max(out=r1[:, 0:1], in_=lgt[:, 0:8], axis=AX.X), reads=["lgt"], writes=["r1a"])
        T.op("dve", lambda e: e.tensor_scalar(out=r1[:, 1:2], in0=r1[:, 0:1], scalar1=-1.0, scalar2=None, op0=ALU.mult), reads=["r1a"], writes=["r1b"])
        T.op("act", lambda e: e.activation(out=ge[:], in_=lgt[:, 0:8], func=AF.Exp, bias=r1[:, 1:2], accum_out=r1[:, 2:3]), reads=["lgt", "r1b"], writes=["ge", "r1c"])
        T.op("dve", lambda e: e.reciprocal(out=r1[:, 3:4], in_=r1[:, 2:3]), reads=["r1c"], writes=["r1d"])
        T.op("dve", lambda e: e.tensor_scalar(out=ohs[:], in0=lgt[:, 0:8], scalar1=r1[:, 0:1], scalar2=None, op0=ALU.is_equal), reads=["lgt", "r1a"], writes=["ohs"])
        T.op("sp", lambda e: e.dma_start(out=oho[i * 128:(i + 1) * 128, :], in_=ohs[:]), reads=["ohs"], writes=["oho"], dma=True)
        T.op("dve", lambda e: e.tensor_scalar(out=ge[:], in0=ohs[:], scalar1=-1.0, scalar2=1e9, op0=ALU.add, op1=ALU.mult), reads=["ohs", "ge"], writes=["ge"])
        for g in range(8):
            T.op("dve", lambda e: e.tensor_scalar(out=ml[:, g * 8:(g + 1) * 8], in0=lgt[:, 8 + g * 8:16 + g * 8], scalar1=ge[:, g:g + 1], scalar2=None, op0=ALU.add),
                 reads=["lgt", "ge"], writes=["ml"])
        T.op("dve", lambda e: e.max(out=mx8[:], in_=ml[:]), reads=["ml"], writes=["mx8"])
        T.op("dve", lambda e: e.tensor_tensor(out=r1[:, 4:5], in0=mx8[:, 1:2], in1=mx8[:, 0:1], op=ALU.subtract), reads=["mx8"], writes=["r1e"])
        T.op("act", lambda e: e.activation(out=r1[:, 5:6], in_=r1[:, 4:5], func=AF.Exp), reads=["r1e"], writes=["r1f"])
        T.op("dve", lambda e: e.tensor_scalar(out=r1[:, 6:7], in0=r1[:, 5:6], scalar1=1.0, scalar2=None, op0=ALU.add), reads=["r1f"], writes=["r1g"])
        T.op("dve", lambda e: e.reciprocal(out=r1[:, 7:8], in_=r1[:, 6:7]), reads=["r1g"], writes=["r1h"])
        T.op("dve", lambda e: e.tensor_tensor(out=r1[:, 8:9], in0=r1[:, 7:8], in1=r1[:, 3:4], op=ALU.mult), reads=["r1h", "r1d"], writes=["r1i"])
        T.op("dve", lambda e: e.tensor_tensor(out=r1[:, 9:10], in0=r1[:, 8:9], in1=r1[:, 5:6], op=ALU.mult), reads=["r1i", "r1f"], writes=["r1j"])
        T.op("dve", lambda e: e.tensor_scalar(out=ra[:], in0=ml[:], scalar1=mx8[:, 0:1], scalar2=r1[:, 8:9], op0=ALU.is_equal, op1=ALU.mult),
             reads=["ml", "mx8", "r1i"], writes=["ra"])
        T.op("dve", lambda e: e.tensor_scalar(out=rb[:], in0=ml[:], scalar1=mx8[:, 1:2], scalar2=r1[:, 9:10], op0=ALU.is_equal, op1=ALU.mult),
             reads=["ml", "mx8", "r1j"], writes=["rb"])
        T.op("dve", lambda e: e.tensor_tensor(out=ra[:], in0=ra[:], in1=rb[:], op=ALU.add), reads=["ra", "rb"], writes=["ra"])
        T.op("sp", lambda e: e.dma_start(out=rwo[i * 128:(i + 1) * 128, :], in_=ra[:]), reads=["ra"], writes=["rwo"], dma=True)
    T.finish("sp")
    print("L3a n_ins", T.n_ins, "n_wait", T.n_wait)
    return nc


def run_l3a(o_nsa, o_fox, proj, x, modrow, w_br_nsa, w_br_fox, w_o, ln1_g, ln1_b, w_rg, b_rg, w_re, b_re, TPC=TPC, cores=NCORES):
    nc = build_l3a(TPC)
    rep = lambda a: np.ascontiguousarray(np.broadcast_to(a.reshape(1, -1), (128, a.size))).astype(np.float32)
    wr = np.ascontiguousarray(np.concatenate([w_rg[0], w_re[0].reshape(D, 64)], axis=1))
    brb = rep(np.concatenate([b_rg[0], b_re[0].reshape(64)]))
    mod2 = np.ascontiguousarray(np.concatenate([modrow[3 * D:4 * D].reshape(16, 128).T, modrow[4 * D:5 * D].reshape(16, 128).T], axis=1))
    OFF_MERGE = 512 + 768 + 24 + 1536 + 8
    x2 = x.reshape(S, D)
    maps = []
    for c in range(cores):
        sl = slice(c * TPC, (c + 1) * TPC)
        maps.append({"onT": np.ascontiguousarray(o_nsa[sl].T), "ofT": np.ascontiguousarray(o_fox[sl].T),
                     "mgT": np.ascontiguousarray(proj[sl, OFF_MERGE:OFF_MERGE + 4096].T), "x": np.ascontiguousarray(x2[sl]),
                     "wbn": w_br_nsa[0], "wbf": w_br_fox[0], "wo": w_o[0], "g1b": rep(modrow[2 * D:3 * D]), "ln1g": rep(ln1_g[0]), "ln1b": rep(ln1_b[0]),
                     "mod2": mod2, "wr": wr, "brb": brb})
    res = run_bass_kernel_spmd(nc, maps, core_ids=list(range(cores)))
    cat = lambda k: np.concatenate([r[k] for r in res.results], axis=0)
    return cat("x1"), cat("xn2"), cat("rw"), cat("oh")


CAP = 512


def _rank_setup(nc, T, A, P_, oh, TPC):
    NT = TPC // 128
    ohs = A("ohs", [128, NT, 8], F32)
    ohb = A("ohb", [128, NT, 8], BF16)
    rank = A("rank", [128, NT, 8], F32)
    onesb = A("onesb", [128, 128], BF16)
    sub = A("sub", [128, 128], BF16)
    rkps = P_("rkps", [128, 512], F32)
    T.op("sp", lambda e: e.dma_start(out=ohs[:], in_=oh.rearrange("(c p) g -> p c g", p=128)), writes=["ohs"], dma=True)
    T.op("dve", lambda e: e.tensor_copy(out=ohb[:], in_=ohs[:]), reads=["ohs"], writes=["ohb"])
    T.op("pool", lambda e: e.memset(onesb[:], 1.0), writes=["onesb"])
    T.op("pool", lambda e: e.memset(sub[:], 1.0), writes=["sub"])
    T.op("pool", lambda e: e.affine_select(out=sub[:], in_=sub[:], pattern=[[1, 128]], compare_op=ALU.is_ge, fill=0.0, base=-1,
                                           channel_multiplier=-1), reads=["sub"], writes=["sub"])
    for c in range(NT):
        for c2 in range(c):
            T.op("pe", lambda e: e.matmul(rkps[:, c * 8:(c + 1) * 8], lhsT=onesb[:], rhs=ohb[:, c2, :], start=(c2 == 0), stop=False),
                 reads=["onesb", "ohb"], writes=["rkps"])
        T.op("pe", lambda e: e.matmul(rkps[:, c * 8:(c + 1) * 8], lhsT=sub[:], rhs=ohb[:, c, :], start=(c == 0), stop=True),
             reads=["sub", "ohb"], writes=["rkps"])
    T.op("dve", lambda e: e.tensor_copy(out=rank[:].rearrange("p c g -> p (c g)"), in_=rkps[:, 0:NT * 8]), writes=["rank", "rkps"])
    return ohs, rank


def build_dispatch(TPC=TPC):
    nc = bass.Bass("TRN2", target_bir_lowering=False)
    NT = TPC // 128
    A = lambda name, shape, dt: nc.alloc_sbuf_tensor("sb_" + name, shape, dt)
    P_ = nc.alloc_psum_tensor
    xn2 = nc.dram_tensor("xn2", [TPC, D], BF16, kind="ExternalInput").ap()
    oh = nc.dram_tensor("oh", [TPC, 8], F32, kind="ExternalInput").ap()
    rw = nc.dram_tensor("rw", [TPC, 64], F32, kind="ExternalInput").ap()
    mod2 = nc.dram_tensor("mod2", [128, 32], F32, kind="ExternalInput").ap()
    iotad = nc.dram_tensor("iota", [128, CAP], F32, kind="ExternalInput").ap()
    xso = nc.dram_tensor("xs", [8, D, CAP], BF16, kind="ExternalOutput").ap()
    rwso = nc.dram_tensor("rws", [8, CAP, 8], F32, kind="ExternalOutput").ap()
    T = Trk(nc)
    ohs, rank = _rank_setup(nc, T, A, P_, oh, TPC)
    xs_ = A("xn2s", [128, NT, D], BF16)
    for h in range(0, NT, 4):
        hw = min(4, NT - h)
        T.op("sp", lambda e: e.dma_start(out=xs_[:, h:h + hw, :], in_=xn2.rearrange("(c p) d -> p c d", p=128)[:, h:h + hw, :]), writes=["xn2s"], dma=True)
    rws_ = A("rwf", [128, NT, 64], F32)
    rwh = A("rwh", [128, NT, 64], BF16); rwhf = A("rwhf", [128, NT, 64], F32); rwl = A("rwl", [128, NT, 64], BF16)
    T.op("sp", lambda e: e.dma_start(out=rws_[:], in_=rw.rearrange("(c p) g -> p c g", p=128)), writes=["rwf"], dma=True)
    T.op("dve", lambda e: e.tensor_copy(out=rwh[:], in_=rws_[:]), reads=["rwf"], writes=["rwh"])
    T.op("dve", lambda e: e.tensor_copy(out=rwhf[:], in_=rwh[:]), reads=["rwh"], writes=["rwhf"])
    T.op("dve", lambda e: e.tensor_tensor(out=rwl[:], in0=rws_[:], in1=rwhf[:], op=ALU.subtract), reads=["rwf", "rwhf"], writes=["rwl"])
    m2 = A("m2", [128, 32], F32); iota = A("iota", [128, CAP], F32)
    T.op("sp", lambda e: e.dma_start(out=m2[:], in_=mod2[:, :]), writes=["m2"], dma=True)
    T.op("sp", lambda e: e.dma_start(out=iota[:], in_=iotad[:, :]), writes=["iota"], dma=True)
    T.op("dve", lambda e: e.tensor_scalar(out=m2[:, 16:32], in0=m2[:, 16:32], scalar1=1.0, scalar2=None, op0=ALU.add), reads=["m2"], writes=["m2"])
    Pm = [A("Pm%d" % i, [128, NT, CAP], BF16) for i in range(2)]
    xo = [A("xo%d" % i, [128, 4, CAP], BF16) for i in range(2)]
    ro = A("ro", [128, 4, 8], F32)
    acc = [P_("acc%d" % i, [128, 512], F32) for i in range(4)]
    rps = P_("rps", [128, 512], F32)
    ev = 0
    import os
    DBG = int(os.environ.get("DBG", "9"))
    for g in range(8 if DBG > 0 else 0):
        Pg = Pm[g % 2]
        pk = "Pm%d" % (g % 2)
        for c in range(NT):
            T.op("dve", lambda e: e.tensor_scalar(out=Pg[:, c, :], in0=iota[:], scalar1=rank[:, c, g:g + 1], scalar2=ohs[:, c, g:g + 1],
                                                  op0=ALU.is_equal, op1=ALU.mult), reads=["iota", "rank", "ohs"], writes=[pk])
        for dg in range(4 if DBG > 1 else 0):
            for c in range(NT):
                for dd in range(4):
                    k = dg * 4 + dd
                    T.op("pe", lambda e: e.matmul(acc[dd][:], lhsT=xs_[:, c, k * 128:(k + 1) * 128], rhs=Pg[:, c, :], start=(c == 0), stop=(c == NT - 1)),
                         reads=["xn2s", pk], writes=["acc%d" % dd])
            xb = xo[ev % 2]
            for dd in range(4):
                k = dg * 4 + dd
                T.op("act", lambda e: e.activation(out=xb[:, dd, :], in_=acc[dd][:], func=AF.Identity, scale=m2[:, 16 + k:17 + k], bias=m2[:, k:k + 1]),
                     reads=["m2"], writes=["xo%d" % (ev % 2), "acc%d" % dd])
            T.op("sp", lambda e: e.dma_start(out=xso[g, dg * 512:(dg + 1) * 512, :].rearrange("(dd p) s -> p dd s", p=128), in_=xb[:]),
                 reads=["xo%d" % (ev % 2)], writes=["xso"], dma=True)
            ev += 1
        for st in range(4 if DBG > 2 else 0):
            n = 0
            for c in range(NT):
                for hl in (rwh, rwl):
                    T.op("pe", lambda e: e.matmul(rps[:, st * 8:(st + 1) * 8], lhsT=Pg[:, c, st * 128:(st + 1) * 128], rhs=hl[:, c, g * 8:(g + 1) * 8],
                                                  start=(n == 0), stop=(n == 2 * NT - 1)), reads=[pk, "rwh", "rwl"], writes=["rps"])
                    n += 1
        if DBG > 3:
            T.op("dve", lambda e: e.tensor_copy(out=ro[:].rearrange("p a b -> p (a b)"), in_=rps[:, 0:32]), writes=["ro", "rps"])
        if DBG > 4:
            T.op("sp", lambda e: e.dma_start(out=rwso[g].rearrange("(st p) e -> p st e", p=128), in_=ro[:]), reads=["ro"], writes=["rwso"], dma=True)
    T.finish("sp")
    print("DISP n_ins", T.n_ins, "n_wait", T.n_wait)
    return nc


def build_experts(NSL=8 * CAP, NE=8):
    nc = bass.Bass("TRN2", target_bir_lowering=False)
    NSC = NSL // 512
    A = lambda name, shape, dt: nc.alloc_sbuf_tensor("sb_" + name, shape, dt)
    P_ = nc.alloc_psum_tensor
    xs = nc.dram_tensor("xs", [D, NSL], BF16, kind="ExternalInput").ap()
    rws = nc.dram_tensor("rws", [NSL, 8], F32, kind="ExternalInput").ap()
    wg = nc.dram_tensor("wg", [NE, D, 512], F32, kind="ExternalInput").ap()
    wu = nc.dram_tensor("wu", [NE, D, 512], F32, kind="ExternalInput").ap()
    wd = nc.dram_tensor("wd", [NE, 512, D], F32, kind="ExternalInput").ap()
    ys = nc.dram_tensor("ys", [NSL, D], F32, kind="ExternalOutput").ap()
    T = Trk(nc)
    W = [[A("w%d_%d" % (b, j), [128, 2048], BF16) for j in range(12)] for b in range(2)]
    stg = [A("stg%d" % i, [128, 2048], F32) for i in range(3)]
    xc = [A("xc%d" % i, [128, 16, 512], BF16) for i in range(2)]
    gs = A("gs", [128, 4, 512], BF16); hT = A("hT", [128, 4, 512], BF16)
    yt = [A("yt%d" % i, [128, D], F32) for i in range(3)]
    rs = A("rs", [128, NSL // 128, 8], F32)
    T.op("act", lambda e: e.dma_start(out=rs[:], in_=rws.rearrange("(st p) e -> p st e", p=128)), writes=["rs"], dma=True)
    acc = [P_("acc%d" % i, [128, 512], F32) for i in range(4)]
    acc2 = [P_("accd%d" % i, [128, 512], F32) for i in range(2)]
    ld = [0]

    def load_weights(e):
        b = e % 2
        for j in range(12):
            sb = ld[0] % 3
            ld[0] += 1
            if j < 8:
                src = (wg if j < 4 else wu)[e, (j % 4) * 512:(j % 4 + 1) * 512, :].rearrange("(kk p) n -> p kk n", p=128)
                dst = stg[sb][:].rearrange("p (kk n) -> p kk n", kk=4)
            else:
                src = wd[e, (j - 8) * 128:(j - 7) * 128, :]
                dst = stg[sb][:]
            T.op("sp", lambda e_: e_.dma_start(out=dst, in_=src), writes=["stg%d" % sb], dma=True)
            eng = "pool" if j % 2 == 0 else "act"
            if eng == "pool":
                T.op("pool", lambda e_: e_.tensor_copy(out=W[b][j][:], in_=stg[sb][:]), reads=["stg%d" % sb], writes=["w%d_%d" % (b, j)])
            else:
                T.op("act", lambda e_: e_.copy(out=W[b][j][:], in_=stg[sb][:]), reads=["stg%d" % sb], writes=["w%d_%d" % (b, j)])

    load_weights(0)
    it = 0
    dn = 0
    yi = 0
    for e in range(NE):
        b = e % 2
        if e + 1 < NE:
            load_weights(e + 1)
        for sc in range(NSC):
            xb = xc[it % 2]
            xk = "xc%d" % (it % 2)
            it += 1
            T.op("act", lambda e_: e_.dma_start(out=xb[:], in_=xs[:, sc * 512:(sc + 1) * 512].rearrange("(k p) s -> p k s", p=128)), writes=[xk], dma=True)
            for ph in range(2):
                for fc in range(4):
                    for k in range(16):
                        wt = W[b][ph * 4 + k // 4]
                        T.op("pe", lambda e_: e_.matmul(acc[fc][:], lhsT=wt[:, (k % 4) * 512 + fc * 128:(k % 4) * 512 + (fc + 1) * 128], rhs=xb[:, k, :],
                                                        start=(k == 0), stop=(k == 15)), reads=["w%d_%d" % (b, ph * 4 + k // 4), xk], writes=["acc%d" % fc])
                    if ph == 0:
                        T.op("act", lambda e_: e_.activation(out=gs[:, fc, :], in_=acc[fc][:], func=AF.Silu), writes=["gs%d" % fc, "acc%d" % fc])
                    else:
                        T.op("dve", lambda e_: e_.tensor_tensor(out=hT[:, fc, :], in0=acc[fc][:], in1=gs[:, fc, :], op=ALU.mult),
                             reads=["gs%d" % fc], writes=["hT%d" % fc, "acc%d" % fc])
            for st in range(4):
                sl = sc * 4 + st
                ytb = yt[yi % 3]
                yk = "yt%d" % (yi % 3)
                yi += 1
                if e > 0:
                    T.op("act", lambda e_: e_.dma_start(out=ytb[:], in_=ys[sl * 128:(sl + 1) * 128, :]), reads=["ys%d" % sl], writes=[yk], dma=True)
                for dmc in range(4):
                    a2 = dn % 2
                    dn += 1
                    for fc in range(4):
                        T.op("pe", lambda e_: e_.matmul(acc2[a2][:], lhsT=hT[:, fc, st * 128:(st + 1) * 128], rhs=W[b][8 + fc][:, dmc * 512:(dmc + 1) * 512],
                                                        start=(fc == 0), stop=(fc == 3)), reads=["hT%d" % fc, "w%d_%d" % (b, 8 + fc)], writes=["accd%d" % a2])
                    if e == 0:
                        T.op("dve", lambda e_: e_.tensor_scalar(out=ytb[:, dmc * 512:(dmc + 1) * 512], in0=acc2[a2][:], scalar1=rs[:, sl, e:e + 1], scalar2=None,
                                                                op0=ALU.mult), reads=["rs"], writes=[yk, "accd%d" % a2])
                    else:
                        T.op("dve", lambda e_: e_.scalar_tensor_tensor(out=ytb[:, dmc * 512:(dmc + 1) * 512], in0=acc2[a2][:], scalar=rs[:, sl, e:e + 1],
                                                                       in1=ytb[:, dmc * 512:(dmc + 1) * 512], op0=ALU.mult, op1=ALU.add),
                             reads=["rs"], writes=[yk, "accd%d" % a2])
                T.op("pool", lambda e_: e_.dma_start(out=ys[sl * 128:(sl + 1) * 128, :], in_=ytb[:]), reads=[yk], writes=["ys%d" % sl], dma=True)
    T.finish("sp")
    T.finish("pool")
    print("EXP n_ins", T.n_ins, "n_wait", T.n_wait)
    return nc


def build_combine(TPC=TPC):
    nc = bass.Bass("TRN2", target_bir_lowering=False)
    NT = TPC // 128
    A = lambda name, shape, dt: nc.alloc_sbuf_tensor("sb_" + name, shape, dt)
    P_ = nc.alloc_psum_tensor
    dI = lambda name, shape: nc.dram_tensor(name, shape, F32, kind="ExternalInput").ap()
    ysl = dI("ysl", [8, CAP, D]); oh = dI("oh", [TPC, 8]); x1 = dI("x1", [TPC, D]); iotad = dI("iota", [128, CAP])
    g2b = dI("g2b", [128, D]); ln2g = dI("ln2g", [128, D]); ln2b = dI("ln2b", [128, D])
    out = nc.dram_tensor("out", [TPC, D], F32, kind="ExternalOutput").ap()
    y2d = nc.dram_tensor("y2d", [TPC, D], F32).ap()
    T = Trk(nc)
    ohs, rank = _rank_setup(nc, T, A, P_, oh, TPC)
    iota = A("iota", [128, CAP], F32)
    T.op("sp", lambda e: e.dma_start(out=iota[:], in_=iotad[:, :]), writes=["iota"], dma=True)
    identb = make_ident(nc, T, BF16, "identb")
    yh = A("yh", [128, 32, 1024], BF16)
    Pt = A("Pt", [128, 8, CAP], BF16)
    PT = [A("PT%d" % i, [128, 32, 128], BF16) for i in range(2)]
    yo = [A("yo%d" % i, [128, 1024], F32) for i in range(2)]
    tps = [P_("tps%d" % i, [128, 8, 128], BF16) for i in range(2)]
    acc = [P_("acc%d" % i, [128, 512], F32) for i in range(2)]
    ysv = ysl.rearrange("g (st p) d -> p g st d", p=128)
    tn = 0
    an = 0
    for h in range(2):
        for g in range(8):
            T.op("pool", lambda e: e.dma_start(out=yh[:, g * 4:(g + 1) * 4, :], in_=ysv[:, g, :, h * 1024:(h + 1) * 1024]), writes=["yh"], dma=True)
        for c in range(NT):
            ptb = PT[c % 2]
            pk = "PT%d" % (c % 2)
            for g in range(8):
                T.op("dve", lambda e: e.tensor_scalar(out=Pt[:, g, :], in0=iota[:], scalar1=rank[:, c, g:g + 1], scalar2=ohs[:, c, g:g + 1],
                                                      op0=ALU.is_equal, op1=ALU.mult), reads=["iota", "rank", "ohs"], writes=["Pt"])
            for q in range(4):
                tb = tn % 2
                tn += 1
                for j in range(8):
                    gi = q * 8 + j
                    T.op("pe", lambda e: e.transpose(out=tps[tb][:, j, :], in_=Pt[:, gi // 4, (gi % 4) * 128:(gi % 4 + 1) * 128], identity=identb[:]),
                         reads=["Pt", "identb"], writes=["tps%d" % tb])
                T.op("act", lambda e: e.copy(out=ptb[:, q * 8:(q + 1) * 8, :], in_=tps[tb][:]), writes=[pk, "tps%d" % tb])
            yb = yo[c % 2]
            for dq in range(2):
                a = an % 2
                an += 1
                for gi in range(32):
                    T.op("pe", lambda e: e.matmul(acc[a][:], lhsT=ptb[:, gi, :], rhs=yh[:, gi, dq * 512:(dq + 1) * 512], start=(gi == 0), stop=(gi == 31)),
                         reads=[pk, "yh"], writes=["acc%d" % a])
                T.op("dve", lambda e: e.tensor_copy(out=yb[:, dq * 512:(dq + 1) * 512], in_=acc[a][:]), writes=["yo%d" % (c % 2), "acc%d" % a])
            T.op("sp", lambda e: e.dma_start(out=y2d[c * 128:(c + 1) * 128, h * 1024:(h + 1) * 1024], in_=yb[:]), reads=["yo%d" % (c % 2)], writes=["y2d%d" % c], dma=True)
    g2p = A("g2p", [128, D], F32); lg_ = A("lng", [128, D], F32); lb_ = A("lnb", [128, D], F32)
    T.op("sp", lambda e: e.dma_start(out=g2p[:], in_=g2b[:, :]), writes=["g2p"], dma=True)
    T.op("sp", lambda e: e.dma_start(out=lg_[:], in_=ln2g[:, :]), writes=["lng"], dma=True)
    T.op("sp", lambda e: e.dma_start(out=lb_[:], in_=ln2b[:, :]), writes=["lnb"], dma=True)
    T.op("dve", lambda e: e.tensor_scalar(out=g2p[:], in0=g2p[:], scalar1=1.0, scalar2=None, op0=ALU.add), reads=["g2p"], writes=["g2p"])
    xt = [A("xt%d" % i, [128, D], F32) for i in range(2)]
    y2 = [A("y2%d" % i, [128, D], F32) for i in range(2)]
    junk = A("junk", [128, D], BF16)
    st = A("st", [128, 16], F32)
    for c in range(NT):
        b = c % 2
        T.op("sp", lambda e: e.dma_start(out=xt[b][:], in_=x1[c * 128:(c + 1) * 128, :]), writes=["xt%d" % b], dma=True)
        T.op("sp", lambda e: e.dma_start(out=y2[b][:], in_=y2d[c * 128:(c + 1) * 128, :]), reads=["y2d%d" % c], writes=["y2%d" % b], dma=True)
        v = y2[b]
        vk = "y2%d" % b
        T.op("dve", lambda e: e.tensor_tensor(out=v[:], in0=v[:], in1=g2p[:], op=ALU.mult), reads=[vk, "g2p"], writes=[vk])
        T.op("dve", lambda e: e.scalar_tensor_tensor(out=v[:], in0=xt[b][:], scalar=ALPHA, in1=v[:], op0=ALU.mult, op1=ALU.add), reads=["xt%d" % b, vk], writes=[vk])
        T.op("dve", lambda e: e.reduce_sum(out=st[:, 0:1], in_=v[:], axis=AX.X), reads=[vk], writes=["st0"])
        T.op("act", lambda e: e.activation(out=junk[:], in_=v[:], func=AF.Square, accum_out=st[:, 1:2]), reads=[vk], writes=["st1", "junk"])
        T.op("dve", lambda e: e.tensor_scalar(out=st[:, 2:3], in0=st[:, 0:1], scalar1=1.0 / D, scalar2=None, op0=ALU.mult), reads=["st0"], writes=["st2"])
        T.op("dve", lambda e: e.tensor_tensor(out=st[:, 3:4], in0=st[:, 2:3], in1=st[:, 2:3], op=ALU.mult), reads=["st2"], writes=["st3"])
        T.op("dve", lambda e: e.scalar_tensor_tensor(out=st[:, 4:5], in0=st[:, 1:2], scalar=1.0 / D, in1=st[:, 3:4], op0=ALU.mult, op1=ALU.subtract),
             reads=["st1", "st3"], writes=["st4"])
        T.op("dve", lambda e: e.tensor_scalar(out=st[:, 6:7], in0=st[:, 4:5], scalar1=LN_EPS, scalar2=None, op0=ALU.add), reads=["st4"], writes=["st6"])
        T.op("act", lambda e: e.activation(out=st[:, 7:8], in_=st[:, 6:7], func=AF.Sqrt), reads=["st6"], writes=["st7"])
        T.op("dve", lambda e: e.reciprocal(out=st[:, 5:6], in_=st[:, 7:8]), reads=["st7"], writes=["st5"])
        T.op("dve", lambda e: e.tensor_scalar(out=v[:], in0=v[:], scalar1=st[:, 2:3], scalar2=st[:, 5:6], op0=ALU.subtract, op1=ALU.mult),
             reads=[vk, "st2", "st5"], writes=[vk])
        T.op("dve", lambda e: e.tensor_tensor(out=v[:], in0=v[:], in1=lg_[:], op=ALU.mult), reads=[vk, "lng"], writes=[vk])
        T.op("dve", lambda e: e.tensor_tensor(out=v[:], in0=v[:], in1=lb_[:], op=ALU.add), reads=[vk, "lnb"], writes=[vk])
        T.op("sp", lambda e: e.dma_start(out=out[c * 128:(c + 1) * 128, :], in_=v[:]), reads=[vk], writes=["out"], dma=True)
    T.finish("sp")
    print("COMB n_ins", T.n_ins, "n_wait", T.n_wait)
    return nc


def run_moe(xn2, rw, oh, x1, modrow, w_gate, w_up, w_down, ln2_g, ln2_b, TPC=TPC, cores=NCORES, ne=8):
    rep = lambda a: np.ascontiguousarray(np.broadcast_to(a.reshape(1, -1), (128, a.size))).astype(np.float32)
    iota = rep(np.arange(CAP, dtype=np.float32))
    mod2 = np.ascontiguousarray(np.concatenate([modrow[3 * D:4 * D].reshape(16, 128).T, modrow[4 * D:5 * D].reshape(16, 128).T], axis=1))
    nc1 = build_dispatch(TPC)
    maps = [{"xn2": np.ascontiguousarray(xn2[c * TPC:(c + 1) * TPC]), "oh": np.ascontiguousarray(oh[c * TPC:(c + 1) * TPC]),
             "rw": np.ascontiguousarray(rw[c * TPC:(c + 1) * TPC]), "mod2": mod2, "iota": iota} for c in range(cores)]
    r1 = run_bass_kernel_spmd(nc1, maps, core_ids=list(range(cores))).results
    nc2 = build_experts(cores * CAP, ne)
    maps2 = []
    for g in range(8):
        maps2.append({"xs": np.ascontiguousarray(np.concatenate([r1[c]["xs"][g] for c in range(cores)], axis=1)),
                      "rws": np.ascontiguousarray(np.concatenate([r1[c]["rws"][g] for c in range(cores)], axis=0)),
                      "wg": np.ascontiguousarray(w_gate[0][g * 8:g * 8 + ne]), "wu": np.ascontiguousarray(w_up[0][g * 8:g * 8 + ne]),
                      "wd": np.ascontiguousarray(w_down[0][g * 8:g * 8 + ne])})
    r2 = run_bass_kernel_spmd(nc2, maps2, core_ids=list(range(8))).results
    nc3 = build_combine(TPC)
    maps3 = []
    for c in range(cores):
        maps3.append({"ysl": np.ascontiguousarray(np.stack([r2[g]["ys"][c * CAP:(c + 1) * CAP] for g in range(8)], axis=0)),
                      "oh": np.ascontiguousarray(oh[c * TPC:(c + 1) * TPC]), "x1": np.ascontiguousarray(x1[c * TPC:(c + 1) * TPC]), "iota": iota,
                      "g2b": rep(modrow[5 * D:6 * D]), "ln2g": rep(ln2_g[0]), "ln2b": rep(ln2_b[0])})
    r3 = run_bass_kernel_spmd(nc3, maps3, core_ids=list(range(cores))).results
    return np.concatenate([r["out"] for r in r3], axis=0)


def build_mod():
    nc = bass.Bass("TRN2", target_bir_lowering=False)
    cT = nc.dram_tensor("cT", [128, 16], F32, kind="ExternalInput").ap()
    wada = nc.dram_tensor("wada", [D, 1536], F32, kind="ExternalInput").ap()
    badaT = nc.dram_tensor("badaT", [128, 12], F32, kind="ExternalInput").ap()
    modT = nc.dram_tensor("modT", [128, 12], F32, kind="ExternalOutput").ap()
    T = Trk(nc)
    A = lambda name, shape, dt: nc.alloc_sbuf_tensor("sb_" + name, shape, dt)
    wst = [A("wst%d" % i, [128, 16, 256], F32) for i in range(2)]
    cs = A("cs", [128, 16], F32); ca = A("ca", [128, 16], F32); bad = A("bad", [128, 12], F32); mo = A("mo", [128, 12], F32)
    modps = nc.alloc_psum_tensor("modps", [128, 512], F32)
    T.op("sp", lambda e: e.dma_start(out=cs[:], in_=cT[:, :]), writes=["cs"], dma=True)
    T.op("sp", lambda e: e.dma_start(out=bad[:], in_=badaT[:, :]), writes=["bad"], dma=True)
    T.op("act", lambda e: e.activation(out=ca[:], in_=cs[:], func=AF.Silu), reads=["cs"], writes=["ca"])
    wv = wada.rearrange("(k p) n -> p k n", p=128)
    for g in range(6):
        b = g % 2
        T.op("sp", lambda e: e.dma_start(out=wst[b][:], in_=wv[:, :, g * 256:(g + 1) * 256]), writes=["wst%d" % b], dma=True)
        for j in range(2):
            n = g * 2 + j
            for k in range(16):
                T.op("pe", lambda e: e.matmul(modps[:, n:n + 1], lhsT=wst[b][:, k, j * 128:(j + 1) * 128], rhs=ca[:, k:k + 1], start=(k == 0), stop=(k == 15)),
                     reads=["wst%d" % b, "ca"], writes=["modps"])
    T.op("dve", lambda e: e.tensor_tensor(out=mo[:], in0=modps[:, 0:12], in1=bad[:], op=ALU.add), reads=["bad"], writes=["mo", "modps"])
    T.op("sp", lambda e: e.dma_start(out=modT[:, :], in_=mo[:]), reads=["mo"], writes=["modT"], dma=True)
    T.finish("sp")
    return nc


def run_mod(c, w_ada, b_ada):
    nc = build_mod()
    cT = np.ascontiguousarray(c.reshape(16, 128).T)
    maps = [{"cT": cT, "wada": np.ascontiguousarray(w_ada[0][:, i * 1536:(i + 1) * 1536]),
             "badaT": np.ascontiguousarray(b_ada[0][i * 1536:(i + 1) * 1536].reshape(12, 128).T)} for i in range(NCORES)]
    res = run_bass_kernel_spmd(nc, maps, core_ids=list(range(NCORES)))
    return np.concatenate([r["modT"].T.reshape(-1) for r in res.results])


def kernel(x, c, w_ada, b_ada, w_in, b_fgt, t5_table, cmp_pe, cmp_w1, cmp_b1, cmp_w2, w_br_nsa, w_br_fox, w_o, ln1_g, ln1_b,
           w_rg, b_rg, w_re, b_re, w_gate, w_up, w_down, ln2_g, ln2_b):
    f = lambda a: np.asarray(a, dtype=np.float32)
    x, c, w_ada, b_ada, w_in = f(x), f(c), f(w_ada), f(b_ada), f(w_in)
    modrow = run_mod(c, w_ada, b_ada)
    proj = run_l1(x, modrow, w_in)
    o_fox = run_fox(proj, f(b_fgt))
    o_nsa = run_nsa(proj, f(t5_table), f(cmp_pe), f(cmp_w1), f(cmp_b1), f(cmp_w2))
    x1, xn2, rw, oh = run_l3a(o_nsa, o_fox, proj, x, modrow, f(w_br_nsa), f(w_br_fox), f(w_o), f(ln1_g), f(ln1_b), f(w_rg), f(b_rg), f(w_re), f(b_re))
    out = run_moe(xn2, rw, oh, x1, modrow, f(w_gate), f(w_up), f(w_down), f(ln2_g), f(ln2_b))
    return out.reshape(1, S, D).astype(np.float32)
```

```python
import numpy as np
import concourse.bass as bass
import concourse.mybir as mybir
from concourse.bass_utils import run_bass_kernel_spmd

F32 = mybir.dt.float32
BF16 = mybir.dt.bfloat16
AF = mybir.ActivationFunctionType
ALU = mybir.AluOpType
AX = mybir.AxisListType

NCORES = 8
D = 2048
S = 16384
TPC = S // NCORES
IN_COLS = 6944
LN_EPS = 1e-5


def _launch(name, nc, maps, cores):
    res = run_bass_kernel_spmd(nc, maps, core_ids=list(range(cores)))
    try:
        if getattr(res, "exec_time_ns", None) is not None:
            print("[launch] %s exec_time_ns=%s" % (name, res.exec_time_ns), flush=True)
    except Exception:
        pass
    return res


class Trk:
    def __init__(self, nc, n_dma_sems=14):
        self.nc = nc
        self.eng = {"pe": nc.tensor, "act": nc.scalar, "dve": nc.vector,
                    "pool": nc.gpsimd, "sp": nc.sync}
        self.sem = {}
        self.cnt = {}
        self.waited = {k: {} for k in self.eng}
        self._ctx = []
        for k in ["pe", "act", "dve", "pool"]:
            g = nc.semaphore("s_" + k)
            self.sem[k] = g.__enter__()
            self._ctx.append(g)
            self.cnt[k] = 0
        self.dsem = []
        self.dcnt = []
        self.dq = {}
        for q in ["sp", "act", "pool"]:
            self.dq[q] = [len(self.dsem) + j for j in range(n_dma_sems)]
            for j in range(n_dma_sems):
                g = nc.semaphore("s_dma_%s%d" % (q, j))
                self.dsem.append(g.__enter__())
                self._ctx.append(g)
                self.dcnt.append(0)
        self.dqn = {"sp": 0, "act": 0, "pool": 0}
        self.dnext = 0
        self.st = {}
        self.n_ins = 0
        self.n_wait = 0

    def _semobj(self, sk):
        return self.sem[sk] if isinstance(sk, str) else self.dsem[sk]

    def _wait(self, e, deps):
        best = {}
        for d in deps:
            if d is None:
                continue
            sk, v = d
            if v > best.get(sk, 0):
                best[sk] = v
        for sk, v in best.items():
            if e == "pe" and sk == "pe":
                continue
            if self.waited[e].get(sk, 0) >= v:
                continue
            self.eng[e].wait_ge(self._semobj(sk), v)
            self.waited[e][sk] = v
            self.n_wait += 1

    def op(self, e, fn, reads=(), writes=(), dma=False):
        deps = []
        for k in reads:
            s = self.st.get(k)
            if s is not None:
                deps.append(s[0])
        for k in writes:
            s = self.st.get(k)
            if s is not None:
                deps.append(s[0])
                deps.extend(s[1])
        if dma:
            i = self.dq[e][self.dqn[e] % len(self.dq[e])]
            self.dqn[e] += 1
            if self.dcnt[i] > 0:
                deps.append((i, self.dcnt[i]))
        self._wait(e, deps)
        ins = fn(self.eng[e])
        if dma:
            self.dcnt[i] += 16
            ins.then_inc(self.dsem[i], 16)
            tag = (i, self.dcnt[i])
        else:
            self.cnt[e] += 1
            ins.then_inc(self.sem[e], 1)
            tag = (e, self.cnt[e])
        for k in reads:
            s = self.st.setdefault(k, [None, []])
            s[1].append(tag)
            if len(s[1]) > 48:
                best = {}
                for sk, v in s[1]:
                    if v > best.get(sk, 0):
                        best[sk] = v
                s[1] = list(best.items())
        for k in writes:
            self.st[k] = [tag, []]
        self.n_ins += 1
        return tag

    def finish(self, e="sp"):
        deps = []
        for k, s in self.st.items():
            deps.append(s[0])
            deps.extend(s[1])
        self._wait(e, deps)


def make_ident(nc, T, dtype, name="ident"):
    ident = nc.alloc_sbuf_tensor(name, [128, 128], dtype)
    T.op("pool", lambda e: e.memset(ident[:], 1.0), writes=[name])
    T.op("pool", lambda e: e.affine_select(out=ident[:], in_=ident[:], pattern=[[-1, 128]],
                                           compare_op=ALU.is_equal, fill=0.0, base=0,
                                           channel_multiplier=1), reads=[name], writes=[name])
    return ident


def build_l1(stop=None, TPC=TPC):
    nc = bass.Bass("TRN2", target_bir_lowering=False)
    x = nc.dram_tensor("x", [TPC, D], F32, kind="ExternalInput").ap()
    cT = nc.dram_tensor("cT", [128, 16], F32, kind="ExternalInput").ap()
    wada = nc.dram_tensor("wada", [D, 8], F32, kind="ExternalInput").ap()
    badaT = nc.dram_tensor("badaT", [128, 32], F32, kind="ExternalInput").ap()
    w_in = nc.dram_tensor("w_in", [D, IN_COLS], F32, kind="ExternalInput").ap()
    proj = nc.dram_tensor("proj", [TPC, IN_COLS], F32, kind="ExternalOutput").ap()
    projb = nc.dram_tensor("projb", [TPC, 3072], BF16, kind="ExternalOutput").ap()
    T = Trk(nc)
    NT = TPC // 128
    obf = [nc.alloc_sbuf_tensor("obf%d" % i, [128, 512], BF16) for i in range(2)]

    wst = [nc.alloc_sbuf_tensor("wst%d" % i, [128, 8, 512], F32) for i in range(2)]
    wbf = [nc.alloc_sbuf_tensor("wbf%d" % i, [128, 16, 512], BF16) for i in range(2)]
    uT = nc.alloc_sbuf_tensor("uT", [128, 16, TPC], BF16)
    xt = [nc.alloc_sbuf_tensor("xt%d" % i, [128, D], F32) for i in range(2)]
    xn = nc.alloc_sbuf_tensor("xn", [128, D], BF16)
    junk = nc.alloc_sbuf_tensor("junk", [128, D], BF16)
    ost = [nc.alloc_sbuf_tensor("ost%d" % i, [128, 512], F32) for i in range(4)]
    cs = nc.alloc_sbuf_tensor("cs", [128, 16], F32)
    ca = nc.alloc_sbuf_tensor("ca", [128, 16], F32)
    bad = nc.alloc_sbuf_tensor("bad", [128, 32], F32)
    modT = nc.alloc_sbuf_tensor("modT", [128, 32], F32)
    st = nc.alloc_sbuf_tensor("st", [128, 16], F32)
    tp = [nc.alloc_psum_tensor("tp%d" % i, [128, 8, 128], BF16) for i in range(2)]
    acc = [nc.alloc_psum_tensor("acc%d" % i, [128, 512], F32) for i in range(4)]
    modps = nc.alloc_psum_tensor("modps", [128, 512], F32)
    ident = make_ident(nc, T, BF16)

    T.op("sp", lambda e: e.dma_start(out=modT[:], in_=badaT[:, :]), writes=["modT"], dma=True)
    T.op("dve", lambda e: e.tensor_scalar(out=modT[:, 16:32], in0=modT[:, 16:32], scalar1=1.0, scalar2=None, op0=ALU.add),
         reads=["modT"], writes=["modT"])
    ld = 0

    if stop == "A":
        T.op("sp", lambda e: e.dma_start(out=proj[0:128, 0:32], in_=modT[:]), reads=["modT"], writes=["proj"], dma=True)
        T.finish("sp")
        return nc
    for i in range(NT):
        b = i % 2
        T.op("sp", lambda e: e.dma_start(out=xt[b][:], in_=x[i * 128:(i + 1) * 128, :]), writes=["xt%d" % b], dma=True)
        T.op("dve", lambda e: e.reduce_sum(out=st[:, 0:1], in_=xt[b][:], axis=AX.X), reads=["xt%d" % b], writes=["st0"])
        T.op("act", lambda e: e.activation(out=junk[:], in_=xt[b][:], func=AF.Square, accum_out=st[:, 1:2]),
             reads=["xt%d" % b], writes=["st1", "junk"])
        T.op("dve", lambda e: e.tensor_scalar(out=st[:, 2:3], in0=st[:, 0:1], scalar1=1.0 / D, scalar2=None, op0=ALU.mult),
             reads=["st0"], writes=["st2"])
        T.op("dve", lambda e: e.tensor_tensor(out=st[:, 3:4], in0=st[:, 2:3], in1=st[:, 2:3], op=ALU.mult),
             reads=["st2"], writes=["st3"])
        T.op("dve", lambda e: e.scalar_tensor_tensor(out=st[:, 4:5], in0=st[:, 1:2], scalar=1.0 / D, in1=st[:, 3:4],
                                                     op0=ALU.mult, op1=ALU.subtract), reads=["st1", "st3"], writes=["st4"])
        T.op("dve", lambda e: e.tensor_scalar(out=st[:, 6:7], in0=st[:, 4:5], scalar1=LN_EPS, scalar2=None, op0=ALU.add),
             reads=["st4"], writes=["st6"])
        T.op("act", lambda e: e.activation(out=st[:, 7:8], in_=st[:, 6:7], func=AF.Sqrt), reads=["st6"], writes=["st7"])
        T.op("dve", lambda e: e.reciprocal(out=st[:, 5:6], in_=st[:, 7:8]), reads=["st7"], writes=["st5"])
        T.op("dve", lambda e: e.tensor_scalar(out=xn[:], in0=xt[b][:], scalar1=st[:, 2:3], scalar2=st[:, 5:6],
                                              op0=ALU.subtract, op1=ALU.mult), reads=["xt%d" % b, "st2", "st5"], writes=["xn"])
        for kq in range(2):
            pb = kq % 2
            for j in range(8):
                k = kq * 8 + j
                T.op("pe", lambda e: e.transpose(out=tp[pb][:, j, :], in_=xn[:, k * 128:(k + 1) * 128], identity=ident[:]),
                     reads=["xn", "ident"], writes=["tp%d" % pb])
            for j in range(8):
                k = kq * 8 + j
                T.op("act", lambda e: e.activation(out=uT[:, k, i * 128:(i + 1) * 128], in_=tp[pb][:, j, :], func=AF.Identity,
                                                   scale=modT[:, 16 + k:17 + k], bias=modT[:, k:k + 1]),
                     reads=["modT"], writes=["uT_%d" % i, "tp%d" % pb])

    if stop in ("B", "B1", "B2"):
        T.op("sp", lambda e: e.dma_start(out=proj[0:128, 0:32], in_=modT[:]), reads=["modT"], writes=["proj"], dma=True)
        T.finish("sp")
        return nc
    w_v = w_in.rearrange("(k p) n -> p k n", p=128)
    ngrp = (IN_COLS + 511) // 512
    ev = 0
    for g in range(ngrp):
        c0 = g * 512
        cw = min(512, IN_COLS - c0)
        wb = g % 2
        for kh in range(2):
            b = ld % 2
            ld += 1
            T.op("sp", lambda e: e.dma_start(out=wst[b][:, :, 0:cw], in_=w_v[:, kh * 8:(kh + 1) * 8, c0:c0 + cw]),
                 writes=["wst%d" % b], dma=True)
            T.op("pool", lambda e: e.tensor_copy(out=wbf[wb][:, kh * 8:(kh + 1) * 8, 0:cw], in_=wst[b][:, :, 0:cw]),
                 reads=["wst%d" % b], writes=["wbf%d_%d" % (wb, kh)])
        for i in range(NT):
            a = ev % 4
            for k in range(16):
                T.op("pe", lambda e: e.matmul(acc[a][:, 0:cw], lhsT=uT[:, k, i * 128:(i + 1) * 128], rhs=wbf[wb][:, k, 0:cw],
                                              start=(k == 0), stop=(k == 15)),
                     reads=["uT_%d" % i, "wbf%d_%d" % (wb, k // 8)], writes=["acc%d" % a])
            if ev % 2 == 0:
                T.op("act", lambda e: e.copy(out=ost[a][:, 0:cw], in_=acc[a][:, 0:cw]), writes=["ost%d" % a, "acc%d" % a])
            else:
                T.op("dve", lambda e: e.tensor_copy(out=ost[a][:, 0:cw], in_=acc[a][:, 0:cw]), writes=["ost%d" % a, "acc%d" % a])
            T.op("pool", lambda e: e.dma_start(out=proj[i * 128:(i + 1) * 128, c0:c0 + cw], in_=ost[a][:, 0:cw]),
                 reads=["ost%d" % a], writes=["proj"], dma=True)
            if g < 6:
                ob = obf[ev % 2]
                if ev % 2 == 0:
                    T.op("dve", lambda e: e.tensor_copy(out=ob[:], in_=acc[a][:]), writes=["obf%d" % (ev % 2), "acc%d" % a])
                else:
                    T.op("act", lambda e: e.copy(out=ob[:], in_=acc[a][:]), writes=["obf%d" % (ev % 2), "acc%d" % a])
                T.op("sp", lambda e: e.dma_start(out=projb[i * 128:(i + 1) * 128, c0:c0 + 512], in_=ob[:]),
                     reads=["obf%d" % (ev % 2)], writes=["projb"], dma=True)
            ev += 1
    T.finish("sp")
    T.finish("pool")
    print("L1 n_ins", T.n_ins, "n_wait", T.n_wait)
    return nc


def run_l1(x, modrow, w_in, stop=None, TPC=TPC, NCORES=NCORES):
    nc = build_l1(stop, TPC)
    x2 = np.ascontiguousarray(x.reshape(S, D))
    mod1 = np.ascontiguousarray(np.concatenate([modrow[0:D].reshape(16, 128).T, modrow[D:2 * D].reshape(16, 128).T], axis=1))
    w = np.ascontiguousarray(w_in[0])
    dummy = np.zeros((128, 16), np.float32)
    in_maps = [{"x": x2[i * TPC:(i + 1) * TPC], "cT": dummy, "wada": np.zeros((D, 8), np.float32), "badaT": mod1, "w_in": w} for i in range(NCORES)]
    res = _launch("l1", nc, in_maps, NCORES)
    return np.concatenate([r["proj"] for r in res.results], axis=0), np.concatenate([r["projb"] for r in res.results], axis=0)


def build_masks(nc, T, name="cmask"):
    mk = nc.alloc_sbuf_tensor(name, [128, 4, 512], BF16)
    T.op("pool", lambda e: e.memset(mk[:], 0.0), writes=[name])
    for j in range(4):
        T.op("pool", lambda e: e.affine_select(out=mk[:, j, :], in_=mk[:, j, :], pattern=[[1, 512]],
                                               compare_op=ALU.is_ge, fill=-30000.0, base=-128 * j,
                                               channel_multiplier=-1), reads=[name], writes=[name])
    return mk


def build_fox(Sx=S):
    nc = bass.Bass("TRN2", target_bir_lowering=False)
    NKB = Sx // 128
    NQC = Sx // 512
    qT = nc.dram_tensor("qT", [64, Sx], BF16, kind="ExternalInput").ap()
    kT = nc.dram_tensor("kT", [64, Sx], BF16, kind="ExternalInput").ap()
    v = nc.dram_tensor("v", [Sx, 64], BF16, kind="ExternalInput").ap()
    f2 = nc.dram_tensor("f2", [128, NKB], F32, kind="ExternalInput").ap()
    bfg = nc.dram_tensor("bfg", [128, 1], F32, kind="ExternalInput").ap()
    o = nc.dram_tensor("o", [Sx, 64], F32, kind="ExternalOutput").ap()
    scr = nc.dram_tensor("scr", [2, Sx], BF16).ap()
    T = Trk(nc)

    KTa = nc.alloc_sbuf_tensor("KTa", [128, Sx], BF16)
    QTa = nc.alloc_sbuf_tensor("QTa", [128, Sx], BF16)
    Vp = nc.alloc_sbuf_tensor("Vp", [128, NKB, 65], BF16)
    PT = [nc.alloc_sbuf_tensor("PT%d" % i, [128, 512], BF16) for i in range(4)]
    ost = [nc.alloc_sbuf_tensor("ost%d" % i, [128, 4, 64], F32) for i in range(2)]
    rc = nc.alloc_sbuf_tensor("rc", [128, 4], F32)
    fz = nc.alloc_sbuf_tensor("fz", [128, NKB], F32)
    lf = nc.alloc_sbuf_tensor("lf", [128, NKB], F32)
    nb = nc.alloc_sbuf_tensor("nb", [128, 1], F32)
    Usb = nc.alloc_sbuf_tensor("Usb", [128, 128], F32)
    SUsb = nc.alloc_sbuf_tensor("SUsb", [128, 128], F32)
    ones = nc.alloc_sbuf_tensor("ones", [128, 128], F32)
    totT = nc.alloc_sbuf_tensor("totT", [128, 128], F32)
    Fsb = nc.alloc_sbuf_tensor("Fsb", [128, NKB], F32)
    Fofs = nc.alloc_sbuf_tensor("Fofs", [128, NKB], F32)
    Gq = nc.alloc_sbuf_tensor("Gq", [128, NKB], F32)
    Ghi = nc.alloc_sbuf_tensor("Ghi", [128, 128], BF16)
    Ghf = nc.alloc_sbuf_tensor("Ghf", [128, NKB], F32)
    Glo = nc.alloc_sbuf_tensor("Glo", [128, 128], BF16)
    GT = nc.alloc_sbuf_tensor("GT", [128, 2, 128], BF16)
    bm = [nc.alloc_sbuf_tensor("bm%d" % i, [128, NKB], F32) for i in range(2)]
    Sps = [nc.alloc_psum_tensor("Sps%d" % i, [128, 512], F32) for i in range(3)]
    Ops = [nc.alloc_psum_tensor("Ops%d" % i, [128, 4, 128], F32) for i in range(2)]
    Fps = nc.alloc_psum_tensor("Fps", [128, 512], F32)
    Tps = nc.alloc_psum_tensor("Tps", [128, 8, 128], BF16)
    identb = make_ident(nc, T, BF16, "identb")
    mk = build_masks(nc, T)

    T.op("sp", lambda e: e.dma_start(out=fz[:], in_=f2[:, :]), writes=["fz"], dma=True)
    T.op("sp", lambda e: e.dma_start(out=nb[:], in_=bfg[:, :]), writes=["nb"], dma=True)
    for h in range(0, Sx, 2048):
        w = min(2048, Sx - h)
        T.op("sp", lambda e: e.dma_start(out=KTa[0:64, h:h + w], in_=kT[:, h:h + w]), writes=["KTa"], dma=True)
        T.op("act", lambda e: e.dma_start(out=QTa[0:64, h:h + w], in_=qT[:, h:h + w]), writes=["QTa"], dma=True)
    vv = v.rearrange("(kb p) d -> p kb d", p=128)
    for h in range(0, NKB, 16):
        w = min(16, NKB - h)
        T.op("sp", lambda e: e.dma_start(out=Vp[:, h:h + w, 0:64], in_=vv[:, h:h + w, :]), writes=["Vp"], dma=True)
    T.op("dve", lambda e: e.memset(Vp[:, :, 64:65], 1.0), writes=["Vp1"])
    T.op("dve", lambda e: e.memset(KTa[64:66, :], 8.0), writes=["KTa8"])

    T.op("pool", lambda e: e.memset(ones[:], 1.0), writes=["ones"])
    T.op("pool", lambda e: e.memset(Usb[:], 1.0), writes=["Usb"])
    T.op("pool", lambda e: e.affine_select(out=Usb[:], in_=Usb[:], pattern=[[1, 128]], compare_op=ALU.is_ge, fill=0.0,
                                           base=0, channel_multiplier=-1), reads=["Usb"], writes=["Usb"])
    T.op("pool", lambda e: e.memset(SUsb[:], 1.0), writes=["SUsb"])
    T.op("pool", lambda e: e.affine_select(out=SUsb[:], in_=SUsb[:], pattern=[[1, 128]], compare_op=ALU.is_ge, fill=0.0,
                                           base=-1, channel_multiplier=-1), reads=["SUsb"], writes=["SUsb"])

    T.op("dve", lambda e: e.tensor_scalar(out=nb[:], in0=nb[:], scalar1=-1.0, scalar2=None, op0=ALU.mult), reads=["nb"], writes=["nb"])
    T.op("act", lambda e: e.activation(out=lf[:], in_=fz[:], func=AF.Exp, scale=-1.0, bias=nb[:, 0:1]), reads=["fz", "nb"], writes=["lf"])
    T.op("act", lambda e: e.activation(out=lf[:], in_=lf[:], func=AF.Ln, bias=1.0), reads=["lf"], writes=["lf"])
    T.op("dve", lambda e: e.tensor_scalar(out=lf[:], in0=lf[:], scalar1=-1.0, scalar2=None, op0=ALU.mult), reads=["lf"], writes=["lf"])
    T.op("pe", lambda e: e.matmul(Fps[0:NKB, 0:128], lhsT=lf[:, 0:NKB], rhs=ones[:, :], start=True, stop=True),
         reads=["lf", "ones"], writes=["Fps"])
    T.op("dve", lambda e: e.memset(totT[:], 0.0), writes=["totT"])
    T.op("dve", lambda e: e.tensor_copy(out=totT[0:NKB, :], in_=Fps[0:NKB, 0:128]), writes=["totT", "Fps"])
    T.op("pe", lambda e: e.matmul(Fps[:, 128:128 + NKB], lhsT=totT[:, :], rhs=SUsb[:, 0:NKB], start=True, stop=True),
         reads=["totT", "SUsb"], writes=["Fps"])
    T.op("dve", lambda e: e.tensor_copy(out=Fofs[:], in_=Fps[:, 128:128 + NKB]), writes=["Fofs", "Fps"])
    T.op("pe", lambda e: e.matmul(Fps[:, 256:256 + NKB], lhsT=Usb[:, :], rhs=lf[:, 0:NKB], start=True, stop=True),
         reads=["Usb", "lf"], writes=["Fps"])
    T.op("dve", lambda e: e.tensor_tensor(out=Fsb[:], in0=Fps[:, 256:256 + NKB], in1=Fofs[:], op=ALU.add),
         reads=["Fofs"], writes=["Fsb", "Fps"])
    Gv = Gq[:].rearrange("j (c f) -> j c f", f=4)
    Fv = Fsb[:].rearrange("j (c f) -> j c f", f=4)
    Ov = Fofs[:].rearrange("j (c f) -> j c f", f=4)
    for f in range(4):
        T.op("dve", lambda e: e.tensor_tensor(out=Gv[:, :, f], in0=Fv[:, :, f], in1=Ov[:, :, 0], op=ALU.subtract),
             reads=["Fsb", "Fofs"], writes=["Gq"])
    T.op("dve", lambda e: e.memset(Ghi[:], 0.0), writes=["Ghi"])
    T.op("dve", lambda e: e.memset(Glo[:], 0.0), writes=["Glo"])
    T.op("dve", lambda e: e.tensor_copy(out=Ghi[:, 0:NKB], in_=Gq[:]), reads=["Gq"], writes=["Ghi"])
    T.op("dve", lambda e: e.tensor_copy(out=Ghf[:], in_=Ghi[:, 0:NKB]), reads=["Ghi"], writes=["Ghf"])
    T.op("dve", lambda e: e.tensor_tensor(out=Glo[:, 0:NKB], in0=Gq[:], in1=Ghf[:], op=ALU.subtract), reads=["Gq", "Ghf"], writes=["Glo"])
    T.op("pe", lambda e: e.transpose(out=Tps[:, 0, :], in_=Ghi[:], identity=identb[:]), reads=["Ghi", "identb"], writes=["Tps"])
    T.op("pe", lambda e: e.transpose(out=Tps[:, 1, :], in_=Glo[:], identity=identb[:]), reads=["Glo", "identb"], writes=["Tps"])
    T.op("dve", lambda e: e.tensor_copy(out=GT[:], in_=Tps[:, 0:2, :]), writes=["GT", "Tps"])
    for r in range(2):
        T.op("sp", lambda e: e.dma_start(out=scr[r:r + 1, :].rearrange("o (p j) -> (o p) j", j=128), in_=GT[0:NKB, r, :]),
             reads=["GT"], writes=["scr"], dma=True)
    T.op("sp", lambda e: e.dma_start(out=QTa[64:66, :], in_=scr[:, :]), reads=["scr"], writes=["QTaG"], dma=True)

    it = 0
    for qc in range(NQC):
        ob = qc % 2
        nk = 4 * qc + 4
        bmq = bm[qc % 2]
        T.op("dve", lambda e: e.tensor_scalar(out=bmq[:, 0:nk], in0=Fsb[:, 0:nk], scalar1=-1.0, scalar2=Fofs[:, 4 * qc:4 * qc + 1],
                                              op0=ALU.mult, op1=ALU.add), reads=["Fsb", "Fofs"], writes=["bm%d" % (qc % 2)])
        T.op("dve", lambda e: e.memset(Ops[ob][:], 0.0), writes=["Ops%d" % ob])

        def qk(kb, it):
            sb = it % 3
            j = kb - 4 * qc
            T.op("pe", lambda e: e.matmul(Sps[sb][:], lhsT=KTa[0:66, kb * 128:(kb + 1) * 128], rhs=QTa[0:66, qc * 512:(qc + 1) * 512],
                                          start=True, stop=(j < 0)),
                 reads=["KTa", "KTa8", "QTa", "QTaG"], writes=["Sps%d" % sb])
            if j >= 0:
                T.op("pe", lambda e: e.matmul(Sps[sb][:], lhsT=identb[:], rhs=mk[:, j, :], start=False, stop=True),
                     reads=["identb", "cmask"], writes=["Sps%d" % sb])

        def ex_pv(kb, it):
            sb = it % 3
            pb = it % 4
            j = kb - 4 * qc
            T.op("act", lambda e: e.activation(out=PT[pb][:], in_=Sps[sb][:], func=AF.Exp, scale=0.125, bias=bmq[:, kb:kb + 1]),
                 reads=["bm%d" % (qc % 2)], writes=["PT%d" % pb, "Sps%d" % sb])
            for jj in range(max(j, 0), 4):
                T.op("pe", lambda e: e.matmul(Ops[ob][:, jj, 0:65], lhsT=PT[pb][:, jj * 128:(jj + 1) * 128], rhs=Vp[:, kb, :],
                                              start=False, stop=False, skip_group_check=True),
                     reads=["PT%d" % pb, "Vp", "Vp1"], writes=["Ops%d" % ob])

        qk(0, it)
        if nk > 1:
            qk(1, it + 1)
        for kb in range(nk):
            if kb + 2 < nk:
                qk(kb + 2, it + 2)
            ex_pv(kb, it)
            it += 1
        osb = ost[qc % 2]
        T.op("dve", lambda e: e.reciprocal(out=rc[:], in_=Ops[ob][:, :, 64]), writes=["rc", "Ops%d" % ob])
        for jj in range(4):
            T.op("dve", lambda e: e.tensor_scalar(out=osb[:, jj, :], in0=Ops[ob][:, jj, 0:64], scalar1=rc[:, jj:jj + 1], scalar2=None,
                                                  op0=ALU.mult), reads=["rc"], writes=["ost%d" % (qc % 2), "Ops%d" % ob])
        T.op("sp", lambda e: e.dma_start(out=o[qc * 512:(qc + 1) * 512, :].rearrange("(jj p) d -> p jj d", p=128), in_=osb[:]),
             reads=["ost%d" % (qc % 2)], writes=["o"], dma=True)
    T.finish("sp")
    print("FOX n_ins", T.n_ins, "n_wait", T.n_wait)
    return nc


def fox_inputs(proj, projb, b_fgt, Sx=S):
    OFF_FOX = 512 + 768 + 24
    OFF_FGT = OFF_FOX + 1536
    maps = []
    for h in range(8):
        q = projb[:Sx, OFF_FOX + h * 64:OFF_FOX + (h + 1) * 64]
        k = projb[:Sx, OFF_FOX + 512 + h * 64:OFF_FOX + 512 + (h + 1) * 64]
        v = projb[:Sx, OFF_FOX + 1024 + h * 64:OFF_FOX + 1024 + (h + 1) * 64]
        f = proj[:Sx, OFF_FGT + h]
        maps.append({"qT": np.ascontiguousarray(q.T), "kT": np.ascontiguousarray(k.T), "v": np.ascontiguousarray(v),
                     "f2": np.ascontiguousarray(f.reshape(Sx // 128, 128).T),
                     "bfg": np.full((128, 1), b_fgt[0, h], np.float32)})
    return maps


def run_fox(proj, projb, b_fgt, Sx=S, cores=NCORES):
    nc = build_fox(Sx)
    maps = fox_inputs(proj, projb, b_fgt, Sx)[:cores]
    res = _launch("fox", nc, maps, cores)
    return np.concatenate([r["o"] for r in res.results], axis=1)


BIGNEG = -30000.0


def build_nsa(Sx=S):
    nc = bass.Bass("TRN2", target_bir_lowering=False)
    NKB = Sx // 128
    NB = Sx // 512
    NCT = max(1, Sx // 2048)
    NCMP = Sx // 16
    NJ = Sx // 64
    dI = lambda name, shape: nc.dram_tensor(name, shape, F32, kind="ExternalInput").ap()
    A = lambda name, shape, dt: nc.alloc_sbuf_tensor("sb_" + name, shape, dt)
    dB = lambda name, shape: nc.dram_tensor(name, shape, BF16, kind="ExternalInput").ap()
    qT4 = dB("qT4", [64, NB, 512])
    kc_s = dB("kc_s", [64, Sx]); vc_s = dB("vc_s", [64, Sx])
    kslcT = dB("kslcT", [64, Sx]); kwinT = dB("kwinT", [64, Sx])
    vslc1 = dB("vslc1", [Sx, 65]); vwin1 = dB("vwin1", [Sx, 65])
    gates = dI("gates", [128, NB * 12])
    w1d = dI("w1", [64, 2 * 32 * 256]); b1T = dI("b1T", [128, 4]); w2d = dI("w2", [128, 4 * 64]); peT = dI("peT", [64, 64])
    D0d = dI("D0", [128, 512]); D1d = dI("D1", [128, 512]); CBd = dI("CB", [128, 18 * 512]); Cfd = dI("Cfull", [128, 512])
    crow = dI("crow", [1, 512])
    wseld = dI("wsel", [128, NCT * NJ]); cvald = dI("cvalid", [128, NCT]); f0d = dI("force0", [128, NJ])
    o = nc.dram_tensor("o", [NB * 128, 256], F32, kind="ExternalOutput").ap()
    T = Trk(nc)
    bufA = A("bufA", [128, Sx], BF16)
    bufB = A("bufB", [128, Sx], BF16)
    bufC = A("bufC", [128, max(2 * NKB * 65, 16384)], BF16)
    w1v = bufC[0:64, 0:16384].rearrange("d (z l h) -> d z l h", z=2, l=32)
    Vs = bufC[:, 0:NKB * 65].rearrange("p (k c) -> p k c", c=65)
    Vw = bufC[:, NKB * 65:2 * NKB * 65].rearrange("p (k c) -> p k c", c=65)
    E = A("E", [128, 64, 128], BF16)
    CB = A("CB", [128, 18, 512], BF16)
    D0 = A("D0", [128, 512], BF16); D1 = A("D1", [128, 512], BF16); W4 = A("W4", [128, 512], BF16)
    hidT = A("hidT", [128, 2, 2, NCMP], BF16)
    KcT = A("KcT", [128, NCMP], BF16)
    Vc = A("Vc", [128, NCT, 65], BF16)
    wsel = A("wsel", [128, NCT, NJ], BF16)
    cval = A("cval", [128, NCT], F32)
    f0 = A("f0", [128, NJ], F32)
    w2 = A("w2", [128, 2, 2, 64], BF16)
    b1 = A("b1", [128, 4], F32); cb = A("cb", [128, 4], F32)
    pe = A("pe", [64, 2, 32], BF16)
    gts = A("gts", [128, NB, 4, 3], F32)
    QT = [A("QT%d" % i, [128, 512], BF16) for i in range(2)]
    PT = [A("PT%d" % i, [128, 512], BF16) for i in range(4)]
    xs = A("xs", [128, 512], F32); x2 = A("x2", [128, 512], F32); sg = A("sg", [128, 512], F32)
    imp = A("imp", [128, NJ], F32); work = A("work", [128, NJ], F32); selm = A("selm", [128, NJ], F32)
    mx8 = A("mx8", [128, 8], F32)
    selbT = A("selbT", [128, 2, 4, 128], BF16)
    rs = A("rs", [128, 3, 4], F32)
    oacc = [A("oacc%d" % i, [128, 4, 64], F32) for i in range(2)]
    vtmp = A("vtmp", [128, 64], F32)
    ident8 = A("ident8", [128, 128], BF16)
    P = nc.alloc_psum_tensor
    Sps = [P("Sps%d" % i, [128, 512], F32) for i in range(3)]
    Ops = [P("Ops%d" % i, [128, 4, 128], F32) for i in range(3)]
    Ips = [P("Ips%d" % i, [128, 2, 256], F32) for i in range(2)]
    identb = make_ident(nc, T, BF16, "identb")
    identf = make_ident(nc, T, F32, "identf")
    T.op("pool", lambda e: e.tensor_scalar(out=ident8[:], in0=identb[:], scalar1=8.0, scalar2=None, op0=ALU.mult),
         reads=["identb"], writes=["ident8"])

    def cast_load(dst, src, key, eng="pool"):
        T.op(eng, lambda e: e.dma_start(out=dst, in_=src), writes=[key], dma=True)

    cast_load(CB[:].rearrange("p r c -> p (r c)"), CBd[:, :], "CB")
    cast_load(D0[:], D0d[:, :], "D0"); cast_load(D1[:], D1d[:, :], "D1"); cast_load(W4[:], Cfd[:, :], "W4")
    cast_load(wsel[:].rearrange("p a b -> p (a b)"), wseld[:, :], "wsel")
    cast_load(w2[:].rearrange("p z c d -> p (z c d)"), w2d[:, :], "w2")
    cast_load(pe[:].rearrange("d z l -> d (z l)"), peT[:, :], "pe")
    cast_load(w1v.rearrange("d z l h -> d (z l h)"), w1d[:, :], "bufC")
    T.op("sp", lambda e: e.dma_start(out=cval[:], in_=cvald[:, :]), writes=["cval"], dma=True)
    T.op("sp", lambda e: e.dma_start(out=f0[:], in_=f0d[:, :]), writes=["f0"], dma=True)
    T.op("sp", lambda e: e.dma_start(out=b1[:], in_=b1T[:, :]), writes=["b1"], dma=True)
    T.op("sp", lambda e: e.dma_start(out=gts[:].rearrange("p a h b -> p (a h b)"), in_=gates[:, :]), writes=["gts"], dma=True)
    T.op("act", lambda e: e.activation(out=gts[:].rearrange("p a h b -> p (a h b)"), in_=gts[:].rearrange("p a h b -> p (a h b)"), func=AF.Sigmoid),
         reads=["gts"], writes=["gts"])
    for i in range(2):
        cast_load(QT[i][64:65, :], crow[:, :], "QTc%d" % i)
    v4 = lambda t: t[:].rearrange("p (h t) -> p h t", h=4)
    T.op("pool", lambda e: e.affine_select(out=v4(D0), in_=v4(D0), pattern=[[0, 4], [1, 128]], compare_op=ALU.is_ge, fill=BIGNEG,
                                           base=0, channel_multiplier=-1), reads=["D0"], writes=["D0"])
    T.op("pool", lambda e: e.affine_select(out=v4(W4), in_=v4(W4), pattern=[[0, 4], [-1, 128]], compare_op=ALU.is_ge, fill=BIGNEG,
                                           base=-1, channel_multiplier=1), reads=["W4"], writes=["W4"])
    for R in range(18):
        cbv = CB[:, R, :].rearrange("p (h t) -> p h t", h=4)
        T.op("pool", lambda e: e.affine_select(out=cbv, in_=cbv, pattern=[[0, 4], [1, 128]], compare_op=ALU.is_ge, fill=BIGNEG,
                                               base=128 * R - 31, channel_multiplier=-16), reads=["CB"], writes=["CB"])
    T.op("pool", lambda e: e.memset(E[:], 1.0), writes=["E"])
    for hs in range(2):
        ev = E[:, :, hs * 64:(hs + 1) * 64]
        T.op("pool", lambda e: e.affine_select(out=ev, in_=ev, pattern=[[-2, 64], [0, 64]], compare_op=ALU.is_equal, fill=0.0,
                                               base=-hs, channel_multiplier=1), reads=["E"], writes=["E"])

    for h in range(0, Sx, 2048):
        w = min(2048, Sx - h)
        cast_load(bufA[0:64, h:h + w], kc_s[:, h:h + w], "bufA", "sp")
        cast_load(bufB[0:64, h:h + w], vc_s[:, h:h + w], "bufB", "act")
    T.op("dve", lambda e: e.memset(hidT[:], 0.0), writes=["hidT"])
    T.op("dve", lambda e: e.memset(KcT[:], 0.0), writes=["KcT"])
    T.op("dve", lambda e: e.memset(KcT[64:65, :], 8.0), reads=[], writes=["KcT"])
    for z in range(2):
        for c2 in range(2):
            col = z * 2 + c2
            for l in range(32):
                T.op("pe", lambda e: e.matmul(Sps[0][:, col:col + 1], lhsT=w1v[:, z, l, c2 * 128:(c2 + 1) * 128], rhs=pe[:, z, l:l + 1],
                                              start=(l == 0), stop=(l == 31)), reads=["bufC", "pe"], writes=["Sps0"])
    T.op("dve", lambda e: e.tensor_tensor(out=cb[:], in0=Sps[0][:, 0:4], in1=b1[:], op=ALU.add), reads=["b1"], writes=["cb", "Sps0"])
    nvalid = NCMP - 1
    src = [bufA, bufB]
    it = 0
    for z in range(2):
        for c2 in range(2):
            for n0 in range(0, nvalid, 512):
                nw = min(512, nvalid - n0)
                sb = it % 2
                it += 1
                for l in range(32):
                    T.op("pe", lambda e: e.matmul(Sps[sb][:, 0:nw], lhsT=w1v[:, z, l, c2 * 128:(c2 + 1) * 128],
                                                  rhs=src[z][0:64, 16 * n0 + l:16 * n0 + l + 16 * (nw - 1) + 1:16],
                                                  start=(l == 0), stop=(l == 31)),
                         reads=["bufC", "bufA" if z == 0 else "bufB"], writes=["Sps%d" % sb])
                col = z * 2 + c2
                T.op("act", lambda e: e.activation(out=xs[:, 0:nw], in_=Sps[sb][:, 0:nw], func=AF.Identity, bias=cb[:, col:col + 1]),
                     reads=["cb"], writes=["xs", "Sps%d" % sb])
                T.op("dve", lambda e: e.tensor_tensor(out=x2[:, 0:nw], in0=xs[:, 0:nw], in1=xs[:, 0:nw], op=ALU.mult), reads=["xs"], writes=["x2"])
                T.op("dve", lambda e: e.tensor_scalar(out=x2[:, 0:nw], in0=x2[:, 0:nw], scalar1=0.044715, scalar2=1.0, op0=ALU.mult, op1=ALU.add),
                     reads=["x2"], writes=["x2"])
                T.op("dve", lambda e: e.tensor_tensor(out=x2[:, 0:nw], in0=x2[:, 0:nw], in1=xs[:, 0:nw], op=ALU.mult), reads=["x2", "xs"], writes=["x2"])
                T.op("act", lambda e: e.activation(out=sg[:, 0:nw], in_=x2[:, 0:nw], func=AF.Sigmoid, scale=1.5957691216057308),
                     reads=["x2"], writes=["sg"])
                T.op("dve", lambda e: e.tensor_tensor(out=hidT[:, z, c2, n0:n0 + nw], in0=xs[:, 0:nw], in1=sg[:, 0:nw], op=ALU.mult),
                     reads=["xs", "sg"], writes=["hidT"])
    for n0 in range(0, NCMP, 512):
        nw = min(512, NCMP - n0)
        for c2 in range(2):
            T.op("pe", lambda e: e.matmul(Sps[0][0:64, 0:nw], lhsT=w2[:, 0, c2, :], rhs=hidT[:, 0, c2, n0:n0 + nw], start=(c2 == 0), stop=(c2 == 1)),
                 reads=["w2", "hidT"], writes=["Sps0"])
        T.op("act", lambda e: e.copy(out=KcT[0:64, n0:n0 + nw], in_=Sps[0][0:64, 0:nw]), writes=["KcT", "Sps0"])
    for nt in range(NCT):
        for c2 in range(2):
            T.op("pe", lambda e: e.matmul(Sps[1][:, 0:64], lhsT=hidT[:, 1, c2, nt * 128:(nt + 1) * 128], rhs=w2[:, 1, c2, :], start=(c2 == 0), stop=(c2 == 1)),
                 reads=["w2", "hidT"], writes=["Sps1"])
        T.op("dve", lambda e: e.tensor_scalar(out=Vc[:, nt, 0:64], in0=Sps[1][:, 0:64], scalar1=cval[:, nt:nt + 1], scalar2=None, op0=ALU.mult),
             reads=["cval"], writes=["Vc", "Sps1"])
        T.op("dve", lambda e: e.tensor_copy(out=Vc[:, nt, 64:65], in_=cval[:, nt:nt + 1]), reads=["cval"], writes=["Vc"])

    for h in range(0, Sx, 2048):
        w = min(2048, Sx - h)
        cast_load(bufA[0:64, h:h + w], kslcT[:, h:h + w], "bufA", "sp")
        cast_load(bufB[0:64, h:h + w], kwinT[:, h:h + w], "bufB", "act")
    T.op("dve", lambda e: e.memset(bufA[64:65, :], 8.0), writes=["bufA8"])
    T.op("dve", lambda e: e.memset(bufB[64:65, :], 8.0), writes=["bufB8"])
    vsv = vslc1.rearrange("(kb p) d -> p kb d", p=128)
    vwv = vwin1.rearrange("(kb p) d -> p kb d", p=128)
    for h in range(0, NKB, 16):
        w = min(16, NKB - h)
        cast_load(Vs[:, h:h + w, :], vsv[:, h:h + w, :], "bufC", "sp")
        cast_load(Vw[:, h:h + w, :], vwv[:, h:h + w, :], "bufC", "act")

    sidx = [0]
    pidx = [0]

    pendq = []

    def flush():
        while pendq:
            pendq.pop(0)()

    def tile_step(Kbuf, kkey, col0, Vap, vkey, ob, qt, qkey, far, bias_tile, bias_key, sel_chunk=None, sel_m=None, imp_nt=None):
        sb = sidx[0] % 3
        sidx[0] += 1
        pb = pidx[0] % 4
        pidx[0] += 1
        kk = 65 if far else 64
        last_plain = (bias_tile is None and sel_chunk is None)
        T.op("pe", lambda e: e.matmul(Sps[sb][:], lhsT=Kbuf[0:kk, col0:col0 + 128], rhs=qt[0:kk, :], start=True, stop=last_plain),
             reads=[kkey, kkey + "8", qkey, qkey.replace("QT", "QTc")], writes=["Sps%d" % sb])
        if bias_tile is not None:
            T.op("pe", lambda e: e.matmul(Sps[sb][:], lhsT=ident8[:], rhs=bias_tile, start=False, stop=(sel_chunk is None)),
                 reads=["ident8", bias_key], writes=["Sps%d" % sb])
        if sel_chunk is not None:
            T.op("pe", lambda e: e.matmul(Sps[sb][:], lhsT=E[:, sel_m, :], rhs=selbT[:, sel_chunk, :, :].rearrange("p h t -> p (h t)"),
                                          start=False, stop=True), reads=["E", "selbT"], writes=["Sps%d" % sb])

        def rest():
            T.op("act", lambda e: e.activation(out=PT[pb][:], in_=Sps[sb][:], func=AF.Exp, scale=0.125), writes=["PT%d" % pb, "Sps%d" % sb])
            for h in range(4):
                T.op("pe", lambda e: e.matmul(Ops[ob][:, h, 0:65], lhsT=PT[pb][:, h * 128:(h + 1) * 128], rhs=Vap,
                                              start=False, stop=False, skip_group_check=True),
                     reads=["PT%d" % pb, vkey], writes=["Ops%d" % ob])
            if imp_nt is not None:
                for h in range(4):
                    T.op("pe", lambda e: e.matmul(Ips[h // 2][:, h % 2, 0:NJ], lhsT=PT[pb][:, h * 128:(h + 1) * 128], rhs=wsel[:, imp_nt, :],
                                                  start=False, stop=False, skip_group_check=True),
                         reads=["PT%d" % pb, "wsel"], writes=["Ips%d" % (h // 2)])
        pendq.append(rest)
        if len(pendq) > 2:
            pendq.pop(0)()

    for i in range(NB):
        md = 4 * i + 3
        qt = QT[i % 2]
        qkey = "QT%d" % (i % 2)
        cast_load(qt[0:64, :], qT4[:, i, :], qkey, "sp")
        for b in range(3):
            T.op("dve", lambda e: e.memset(Ops[b][:], 0.0), writes=["Ops%d" % b])
        for b in range(2):
            T.op("dve", lambda e: e.memset(Ips[b][:], 0.0), writes=["Ips%d" % b])
        for nt in range(NCT):
            R = md - 16 * nt
            if R < 0:
                continue
            far = R >= 18
            tile_step(KcT, "KcT", nt * 128, Vc[:, nt, :], "Vc", 0, qt, qkey, far,
                      None if far else CB[:, R, :], "CB", imp_nt=nt)
        flush()
        T.op("dve", lambda e: e.tensor_scalar(out=rs[:, 0, :], in0=Ops[0][:, :, 64], scalar1=1e-30, scalar2=None, op0=ALU.max),
             writes=["rs0", "Ops0"])
        T.op("dve", lambda e: e.reciprocal(out=rs[:, 0, :], in_=rs[:, 0, :]), reads=["rs0"], writes=["rs0"])
        for h in range(4):
            if h == 0:
                T.op("dve", lambda e: e.tensor_scalar(out=imp[:], in0=Ips[0][:, 0, 0:NJ], scalar1=rs[:, 0, 0:1], scalar2=None, op0=ALU.mult),
                     reads=["rs0"], writes=["imp", "Ips0"])
            else:
                T.op("dve", lambda e: e.scalar_tensor_tensor(out=imp[:], in0=Ips[h // 2][:, h % 2, 0:NJ], scalar=rs[:, 0, h:h + 1], in1=imp[:],
                                                             op0=ALU.mult, op1=ALU.add), reads=["rs0", "imp"], writes=["imp", "Ips%d" % (h // 2)])
        c1 = 2 * md + 1
        if c1 + 1 < NJ:
            T.op("dve", lambda e: e.memset(imp[:, c1 + 1:NJ], -1.0), reads=["imp"], writes=["imp"])
        T.op("dve", lambda e: e.memset(imp[0:64, c1:c1 + 1], -1.0), reads=["imp"], writes=["imp"])
        T.op("dve", lambda e: e.memset(imp[64:128, c1:c1 + 1], 20000.0), reads=["imp"], writes=["imp"])
        T.op("dve", lambda e: e.memset(imp[0:64, c1 - 1:c1], 20000.0), reads=["imp"], writes=["imp"])
        T.op("dve", lambda e: e.memset(imp[64:128, c1 - 1:c1], 10000.0), reads=["imp"], writes=["imp"])
        T.op("dve", lambda e: e.memset(imp[0:64, c1 - 2:c1 - 1], 10000.0), reads=["imp"], writes=["imp"])
        T.op("dve", lambda e: e.tensor_tensor(out=imp[:], in0=imp[:], in1=f0[:], op=ALU.max), reads=["imp", "f0"], writes=["imp"])
        T.op("dve", lambda e: e.tensor_copy(out=work[:], in_=imp[:]), reads=["imp"], writes=["work"])
        for rnd in range(2):
            T.op("dve", lambda e: e.max(out=mx8[:], in_=work[:]), reads=["work"], writes=["mx8"])
            T.op("dve", lambda e: e.match_replace(out=work[:], in_to_replace=mx8[:], in_values=work[:], imm_value=-1e9),
                 reads=["mx8", "work"], writes=["work"])
        T.op("dve", lambda e: e.tensor_scalar(out=selm[:], in0=work[:], scalar1=-1e8, scalar2=None, op0=ALU.is_le), reads=["work"], writes=["selm"])
        nch = (NJ + 127) // 128
        for ch in range(nch):
            cwid = min(128, NJ - ch * 128)
            T.op("pe", lambda e: e.transpose(out=Ips[1][0:cwid, 0, ch * 128:(ch + 1) * 128], in_=selm[:, ch * 128:ch * 128 + cwid], identity=identf[:]),
                 reads=["selm", "identf"], writes=["Ips1"])
        for ch in range(nch):
            cwid = min(128, NJ - ch * 128)
            for h in range(4):
                T.op("dve", lambda e: e.tensor_scalar(out=selbT[0:cwid, ch, h, :], in0=Ips[1][0:cwid, 0, ch * 128:(ch + 1) * 128], scalar1=-1.0, scalar2=-BIGNEG,
                                                      op0=ALU.add, op1=ALU.mult), writes=["selbT", "Ips1"])
        for m in range(max(0, md - 4), md + 1):
            rel = md - m
            bt, bk = {0: (D0[:], "D0"), 1: (D1[:], "D1"), 4: (W4[:], "W4")}.get(rel, (None, None))
            tile_step(bufB, "bufB", m * 128, Vw[:, m, :], "bufC", 2, qt, qkey, bt is None, bt, bk)
        for m in range(0, md + 1):
            rel = md - m
            bt, bk = {0: (D0[:], "D0"), 1: (D1[:], "D1")}.get(rel, (None, None))
            tile_step(bufA, "bufA", m * 128, Vs[:, m, :], "bufC", 1, qt, qkey, bt is None, bt, bk, sel_chunk=m // 64, sel_m=m % 64)
        flush()
        oa = oacc[i % 2]
        for b in range(3):
            if b > 0:
                T.op("dve", lambda e: e.tensor_scalar(out=rs[:, b, :], in0=Ops[b][:, :, 64], scalar1=1e-30, scalar2=None, op0=ALU.max),
                     writes=["rs%d" % b, "Ops%d" % b])
                T.op("dve", lambda e: e.reciprocal(out=rs[:, b, :], in_=rs[:, b, :]), reads=["rs%d" % b], writes=["rs%d" % b])
            T.op("dve", lambda e: e.tensor_tensor(out=rs[:, b, :], in0=rs[:, b, :], in1=gts[:, i, :, b], op=ALU.mult),
                 reads=["rs%d" % b, "gts"], writes=["rs%d" % b])
            for h in range(4):
                if b == 0:
                    T.op("dve", lambda e: e.tensor_scalar(out=oa[:, h, :], in0=Ops[b][:, h, 0:64], scalar1=rs[:, b, h:h + 1], scalar2=None, op0=ALU.mult),
                         reads=["rs%d" % b], writes=["oacc%d" % (i % 2), "Ops%d" % b])
                else:
                    T.op("dve", lambda e: e.scalar_tensor_tensor(out=oa[:, h, :], in0=Ops[b][:, h, 0:64], scalar=rs[:, b, h:h + 1], in1=oa[:, h, :],
                                                                 op0=ALU.mult, op1=ALU.add), reads=["rs%d" % b], writes=["oacc%d" % (i % 2), "Ops%d" % b])
        T.op("sp", lambda e: e.dma_start(out=o[i * 128:(i + 1) * 128, :], in_=oa[:].rearrange("p h d -> p (h d)")),
             reads=["oacc%d" % (i % 2)], writes=["o"], dma=True)
    T.finish("sp")
    print("NSA n_ins", T.n_ins, "n_wait", T.n_wait)
    return nc


def t5_bucket_np(dist):
    n = np.maximum(dist, 0)
    ratio = np.log(np.maximum(n, 16).astype(np.float32) / np.float32(16))
    big = 16 + (ratio / np.float32(np.log(128 / 16)) * np.float32(16)).astype(np.int32)
    return np.where(n < 16, n, np.minimum(big, 31))


def nsa_inputs(proj, projb, t5_table, cmp_pe, cmp_w1, cmp_b1, cmp_w2, Sx=S):
    OFF_KV = 512
    OFF_GATE = 512 + 768
    NB = Sx // 512
    NCT = max(1, Sx // 2048)
    NJ = Sx // 64
    NCMP = Sx // 16
    maps = []
    si = np.arange(128)[:, None]
    ti = np.arange(128)[None, :]
    w1 = np.ascontiguousarray(cmp_w1[0].reshape(2, 32, 64, 256).transpose(2, 0, 1, 3).reshape(64, -1))
    b1T = np.ascontiguousarray(cmp_b1[0].reshape(2, 2, 128).transpose(2, 0, 1).reshape(128, 4))
    w2 = np.ascontiguousarray(cmp_w2[0].reshape(2, 2, 128, 64).transpose(2, 0, 1, 3).reshape(128, -1))
    peT = np.ascontiguousarray(cmp_pe[0].transpose(2, 0, 1).reshape(64, 64))
    for c in range(8):
        g, r = c // 4, c % 4
        sh = 3 - r
        p = proj[:Sx]
        pb = projb[:Sx]
        tb = t5_table.T[4 * g:4 * g + 4]
        kvc = lambda z: pb[:, OFF_KV + (z * 2 + g) * 64:OFF_KV + (z * 2 + g + 1) * 64]

        def shiftT(a):
            out = np.zeros((64, Sx), pb.dtype)
            if sh * 128 < Sx:
                out[:, sh * 128:] = a[:Sx - sh * 128].T
            return out

        def shiftV1(a):
            out = np.zeros((Sx, 65), pb.dtype)
            if sh * 128 < Sx:
                out[sh * 128:, 0:64] = a[:Sx - sh * 128]
                out[sh * 128:, 64] = 1.0
            return out
        q = pb[:, g * 256:(g + 1) * 256].reshape(Sx // 128, 128, 4, 64)[r::4]
        qT4 = np.ascontiguousarray(q.transpose(3, 0, 2, 1).reshape(64, NB, 512))
        gt = p[:, OFF_GATE + g * 12:OFF_GATE + (g + 1) * 12].reshape(Sx // 128, 128, 12)[r::4]
        gates = np.ascontiguousarray(gt.transpose(1, 0, 2).reshape(128, NB * 12))
        D0 = np.stack([tb[h][t5_bucket_np(ti - si)] for h in range(4)], axis=1).reshape(128, 512)
        D1 = np.stack([tb[h][t5_bucket_np(128 + ti - si)] for h in range(4)], axis=1).reshape(128, 512)
        CB = np.stack([np.stack([tb[h][t5_bucket_np(128 * R + ti - 16 * si - 31)] for h in range(4)], axis=1).reshape(128, 512)
                       for R in range(18)], axis=1).reshape(128, 18 * 512)
        Cfull = np.ascontiguousarray(np.broadcast_to(np.repeat(tb[:, 31], 128)[None, :], (128, 512)))
        crow = np.ascontiguousarray(np.repeat(tb[:, 31], 128)[None, :])
        npad = 8 * sh
        nn = np.arange(NCT * 128)[:, None]
        jj = np.arange(NJ)[None, :]
        dlt = nn - 4 * jj
        wsel = np.where((dlt >= 0) & (dlt <= 2), 1.0, np.where((dlt == -1) | (dlt == 3), 0.5, 0.0)).astype(np.float32)
        wsel[:npad] = 0.0
        wsel[NCMP - 1 - 0:] = 0.0 if True else 0.0
        wsel = np.ascontiguousarray(wsel.reshape(NCT, 128, NJ).transpose(1, 0, 2).reshape(128, NCT * NJ))
        cvalid = (np.arange(NCT * 128) >= npad).astype(np.float32)
        cvalid[NCMP - 1:] = 0.0
        cvalid = np.ascontiguousarray(cvalid.reshape(NCT, 128).T)
        force0 = np.zeros((128, NJ), np.float32)
        force0[:, 2 * sh] = 30000.0
        maps.append({"qT4": qT4, "kc_s": shiftT(kvc(0)), "vc_s": shiftT(kvc(1)), "kslcT": shiftT(kvc(2)), "kwinT": shiftT(kvc(4)),
                     "vslc1": shiftV1(kvc(3)), "vwin1": shiftV1(kvc(5)), "gates": gates, "w1": w1, "b1T": b1T, "w2": w2, "peT": peT,
                     "D0": np.ascontiguousarray(D0), "D1": np.ascontiguousarray(D1), "CB": np.ascontiguousarray(CB), "Cfull": Cfull, "crow": crow,
                     "wsel": wsel, "cvalid": cvalid, "force0": force0})
    return maps


def run_nsa(proj, projb, t5_table, cmp_pe, cmp_w1, cmp_b1, cmp_w2, Sx=S, cores=NCORES):
    nc = build_nsa(Sx)
    maps = nsa_inputs(proj, projb, t5_table, cmp_pe, cmp_w1, cmp_b1, cmp_w2, Sx)[:cores]
    res = _launch("nsa", nc, maps, cores)
    out = np.zeros((Sx, 512), np.float32)
    for c in range(cores):
        g, r = c // 4, c % 4
        oc = res.results[c]["o"].reshape(Sx // 512, 128, 256)
        out.reshape(Sx // 128, 128, 512)[r::4, :, g * 256:(g + 1) * 256] = oc
    return out


ALPHA = 2.0 ** 0.25


def build_l3a(TPC=TPC):
    nc = bass.Bass("TRN2", target_bir_lowering=False)
    NT = TPC // 128
    NG = TPC // 512
    dI = lambda name, shape: nc.dram_tensor(name, shape, F32, kind="ExternalInput").ap()
    A = lambda name, shape, dt: nc.alloc_sbuf_tensor("sb_" + name, shape, dt)
    onT = dI("onT", [512, TPC]); ofT = dI("ofT", [512, TPC]); mgT = dI("mgT", [4096, TPC]); x = dI("x", [TPC, D])
    wbn = dI("wbn", [512, D]); wbf = dI("wbf", [512, D]); wo = dI("wo", [D, D])
    g1b = dI("g1b", [128, D]); ln1g = dI("ln1g", [128, D]); ln1b = dI("ln1b", [128, D])
    mod2 = dI("mod2", [128, 32]); wr = dI("wr", [D, 72]); brb = dI("brb", [128, 72])
    x1o = nc.dram_tensor("x1", [TPC, D], F32, kind="ExternalOutput").ap()
    xn2o = nc.dram_tensor("xn2", [TPC, D], BF16, kind="ExternalOutput").ap()
    rwo = nc.dram_tensor("rw", [TPC, 64], F32, kind="ExternalOutput").ap()
    oho = nc.dram_tensor("oh", [TPC, 8], F32, kind="ExternalOutput").ap()
    mTd = nc.dram_tensor("mTd", [16, 128, TPC], BF16).ap()
    T = Trk(nc)
    big = A("big", [128, 32768], BF16)
    wbn_s = big[:, 0:8192].rearrange("p (k n) -> p k n", k=4)
    wbf_s = big[:, 8192:16384].rearrange("p (k n) -> p k n", k=4)
    onT_s = big[:, 16384:16384 + 4 * TPC].rearrange("p (k n) -> p k n", k=4)
    ofT_s = big[:, 24576:24576 + 4 * TPC].rearrange("p (k n) -> p k n", k=4)
    wo_s = big[:, :].rearrange("p (k n) -> p k n", k=16)
    mgs = [A("mgs%d" % i, [128, 2, 512], F32) for i in range(2)]
    t1 = A("t1", [128, 512], F32); t2 = A("t2", [128, 512], F32)
    mo = [A("mo%d" % i, [128, 512], BF16) for i in range(2)]
    P = nc.alloc_psum_tensor
    acc = [P("acc%d" % i, [128, 512], F32) for i in range(4)]
    tpf = [P("tpf%d" % i, [128, 4, 128], F32) for i in range(2)]
    rps = P("rps", [128, 512], F32)

    def cast_load(dst, src, key):
        T.op("pool", lambda e: e.dma_start(out=dst, in_=src), writes=[key], dma=True)
    cast_load(wbn_s, wbn.rearrange("(k p) n -> p k n", p=128), "big")
    cast_load(wbf_s, wbf.rearrange("(k p) n -> p k n", p=128), "big")
    cast_load(onT_s, onT.rearrange("(k p) n -> p k n", p=128), "big")
    cast_load(ofT_s, ofT.rearrange("(k p) n -> p k n", p=128), "big")
    it = 0
    for dc in range(16):
        for tg in range(NG):
            mb = mgs[it % 2]
            T.op("sp", lambda e: e.dma_start(out=mb[:, 0, :], in_=mgT[dc * 128:(dc + 1) * 128, tg * 512:(tg + 1) * 512]), writes=["mgs%d" % (it % 2)], dma=True)
            T.op("sp", lambda e: e.dma_start(out=mb[:, 1, :], in_=mgT[2048 + dc * 128:2048 + (dc + 1) * 128, tg * 512:(tg + 1) * 512]), writes=["mgs%d" % (it % 2)], dma=True)
            T.op("act", lambda e: e.activation(out=mb[:].rearrange("p a n -> p (a n)"), in_=mb[:].rearrange("p a n -> p (a n)"), func=AF.Sigmoid),
                 reads=["mgs%d" % (it % 2)], writes=["mgs%d" % (it % 2)])
            for br, (ws, os_) in enumerate([(wbn_s, onT_s), (wbf_s, ofT_s)]):
                a = (it % 2) * 2 + br
                for k in range(4):
                    T.op("pe", lambda e: e.matmul(acc[a][:], lhsT=ws[:, k, dc * 128:(dc + 1) * 128], rhs=os_[:, k, tg * 512:(tg + 1) * 512],
                                                  start=(k == 0), stop=(k == 3)), reads=["big"], writes=["acc%d" % a])
            a0 = (it % 2) * 2
            T.op("dve", lambda e: e.tensor_tensor(out=t1[:], in0=acc[a0][:], in1=mb[:, 0, :], op=ALU.mult), reads=["mgs%d" % (it % 2)], writes=["t1", "acc%d" % a0])
            T.op("dve", lambda e: e.tensor_tensor(out=t2[:], in0=acc[a0 + 1][:], in1=mb[:, 1, :], op=ALU.mult), reads=["mgs%d" % (it % 2)], writes=["t2", "acc%d" % (a0 + 1)])
            T.op("dve", lambda e: e.tensor_tensor(out=mo[it % 2][:], in0=t1[:], in1=t2[:], op=ALU.add), reads=["t1", "t2"], writes=["mo%d" % (it % 2)])
            T.op("sp", lambda e: e.dma_start(out=mTd[dc, :, tg * 512:(tg + 1) * 512], in_=mo[it % 2][:]), reads=["mo%d" % (it % 2)], writes=["mTd"], dma=True)
            it += 1
    for h in range(4):
        cast_load(wo_s[:, 4 * h:4 * h + 4, :], wo.rearrange("(k p) n -> p k n", p=128)[:, 4 * h:4 * h + 4, :], "big")
    g1p = A("g1p", [128, D], F32); lg_ = A("lng", [128, D], F32); lb_ = A("lnb", [128, D], F32)
    T.op("sp", lambda e: e.dma_start(out=g1p[:], in_=g1b[:, :]), writes=["g1p"], dma=True)
    T.op("sp", lambda e: e.dma_start(out=lg_[:], in_=ln1g[:, :]), writes=["lng"], dma=True)
    T.op("sp", lambda e: e.dma_start(out=lb_[:], in_=ln1b[:, :]), writes=["lnb"], dma=True)
    T.op("dve", lambda e: e.tensor_scalar(out=g1p[:], in0=g1p[:], scalar1=1.0, scalar2=None, op0=ALU.add), reads=["g1p"], writes=["g1p"])
    m2 = A("m2", [128, 32], F32); wrs = A("wrs", [128, 16, 72], F32); brs = A("brs", [128, 72], F32)
    T.op("sp", lambda e: e.dma_start(out=m2[:], in_=mod2[:, :]), writes=["m2"], dma=True)
    T.op("dve", lambda e: e.tensor_scalar(out=m2[:, 16:32], in0=m2[:, 16:32], scalar1=1.0, scalar2=None, op0=ALU.add), reads=["m2"], writes=["m2"])
    T.op("sp", lambda e: e.dma_start(out=wrs[:], in_=wr.rearrange("(k p) n -> p k n", p=128)), writes=["wrs"], dma=True)
    T.op("sp", lambda e: e.dma_start(out=brs[:], in_=brb[:, :]), writes=["brs"], dma=True)
    identf = make_ident(nc, T, F32, "identf")
    mt = [A("mt%d" % i, [128, 16, 128], BF16) for i in range(2)]
    xt = [A("xt%d" % i, [128, D], F32) for i in range(2)]
    v = A("v", [128, D], F32); xn = A("xn", [128, D], F32); junk = A("junk", [128, D], BF16)
    xnb = A("xnb", [128, D], BF16)
    u2T = A("u2T", [128, 16, 128], F32)
    st = A("st", [128, 16], F32)
    lgt = A("lgt", [128, 72], F32); ml = A("ml", [128, 64], F32); mx8 = A("mx8", [128, 8], F32)
    r1 = A("r1", [128, 16], F32); ra = A("ra", [128, 64], F32); rb = A("rb", [128, 64], F32); ohs = A("ohs", [128, 8], F32)
    ge = A("ge", [128, 8], F32)

    def ln_stats(src, key):
        T.op("dve", lambda e: e.reduce_sum(out=st[:, 0:1], in_=src, axis=AX.X), reads=[key], writes=["st0"])
        T.op("act", lambda e: e.activation(out=junk[:], in_=src, func=AF.Square, accum_out=st[:, 1:2]), reads=[key], writes=["st1", "junk"])
        T.op("dve", lambda e: e.tensor_scalar(out=st[:, 2:3], in0=st[:, 0:1], scalar1=1.0 / D, scalar2=None, op0=ALU.mult), reads=["st0"], writes=["st2"])
        T.op("dve", lambda e: e.tensor_tensor(out=st[:, 3:4], in0=st[:, 2:3], in1=st[:, 2:3], op=ALU.mult), reads=["st2"], writes=["st3"])
        T.op("dve", lambda e: e.scalar_tensor_tensor(out=st[:, 4:5], in0=st[:, 1:2], scalar=1.0 / D, in1=st[:, 3:4], op0=ALU.mult, op1=ALU.subtract),
             reads=["st1", "st3"], writes=["st4"])
        T.op("dve", lambda e: e.tensor_scalar(out=st[:, 6:7], in0=st[:, 4:5], scalar1=LN_EPS, scalar2=None, op0=ALU.add), reads=["st4"], writes=["st6"])
        T.op("act", lambda e: e.activation(out=st[:, 7:8], in_=st[:, 6:7], func=AF.Sqrt), reads=["st6"], writes=["st7"])
        T.op("dve", lambda e: e.reciprocal(out=st[:, 5:6], in_=st[:, 7:8]), reads=["st7"], writes=["st5"])

    for i in range(NT):
        b = i % 2
        T.op("sp", lambda e: e.dma_start(out=mt[b][:], in_=mTd[:, :, i * 128:(i + 1) * 128].rearrange("k p t -> p k t")), reads=["mTd"], writes=["mt%d" % b], dma=True)
        T.op("sp", lambda e: e.dma_start(out=xt[b][:], in_=x[i * 128:(i + 1) * 128, :]), writes=["xt%d" % b], dma=True)
        for cg in range(4):
            for k in range(16):
                T.op("pe", lambda e: e.matmul(acc[cg][:], lhsT=mt[b][:, k, :], rhs=wo_s[:, k, cg * 512:(cg + 1) * 512], start=(k == 0), stop=(k == 15)),
                     reads=["mt%d" % b, "big"], writes=["acc%d" % cg])
            T.op("dve", lambda e: e.tensor_tensor(out=v[:, cg * 512:(cg + 1) * 512], in0=acc[cg][:], in1=g1p[:, cg * 512:(cg + 1) * 512], op=ALU.mult),
                 reads=["g1p"], writes=["v", "acc%d" % cg])
        T.op("dve", lambda e: e.scalar_tensor_tensor(out=v[:], in0=xt[b][:], scalar=ALPHA, in1=v[:], op0=ALU.mult, op1=ALU.add),
             reads=["xt%d" % b, "v"], writes=["v"])
        ln_stats(v[:], "v")
        T.op("dve", lambda e: e.tensor_scalar(out=xn[:], in0=v[:], scalar1=st[:, 2:3], scalar2=st[:, 5:6], op0=ALU.subtract, op1=ALU.mult),
             reads=["v", "st2", "st5"], writes=["xn"])
        T.op("dve", lambda e: e.tensor_tensor(out=xn[:], in0=xn[:], in1=lg_[:], op=ALU.mult), reads=["xn", "lng"], writes=["xn"])
        T.op("dve", lambda e: e.tensor_tensor(out=xn[:], in0=xn[:], in1=lb_[:], op=ALU.add), reads=["xn", "lnb"], writes=["xn"])
        T.op("sp", lambda e: e.dma_start(out=x1o[i * 128:(i + 1) * 128, :], in_=xn[:]), reads=["xn"], writes=["x1o"], dma=True)
        ln_stats(xn[:], "xn")
        T.op("dve", lambda e: e.tensor_scalar(out=v[:], in0=xn[:], scalar1=st[:, 2:3], scalar2=st[:, 5:6], op0=ALU.subtract, op1=ALU.mult),
             reads=["xn", "st2", "st5"], writes=["v"])
        T.op("act", lambda e: e.copy(out=xnb[:], in_=v[:]), reads=["v"], writes=["xnb"])
        T.op("sp", lambda e: e.dma_start(out=xn2o[i * 128:(i + 1) * 128, :], in_=xnb[:]), reads=["xnb"], writes=["xn2o"], dma=True)
        for kq in range(4):
            pb = kq % 2
            for j in range(4):
                k = kq * 4 + j
                T.op("pe", lambda e: e.transpose(out=tpf[pb][:, j, :], in_=v[:, k * 128:(k + 1) * 128], identity=identf[:]),
                     reads=["v", "identf"], writes=["tpf%d" % pb])
            for j in range(4):
                k = kq * 4 + j
                T.op("act", lambda e: e.activation(out=u2T[:, k, :], in_=tpf[pb][:, j, :], func=AF.Identity, scale=m2[:, 16 + k:17 + k], bias=m2[:, k:k + 1]),
                     reads=["m2"], writes=["u2T", "tpf%d" % pb])
        for k in range(16):
            T.op("pe", lambda e: e.matmul(rps[:, 0:72], lhsT=u2T[:, k, :], rhs=wrs[:, k, :], start=(k == 0), stop=(k == 15)),
                 reads=["u2T", "wrs"], writes=["rps"])
        T.op("dve", lambda e: e.tensor_tensor(out=lgt[:], in0=rps[:, 0:72], in1=brs[:], op=ALU.add), reads=["brs"], writes=["lgt", "rps"])
        T.op("dve", lambda e: e.reduce_max(out=r1[:, 0:1], in_=lgt[:, 0:8], axis=AX.X), reads=["lgt"], writes=["r1a"])
        T.op("dve", lambda e: e.tensor_scalar(out=r1[:, 1:2], in0=r1[:, 0:1], scalar1=-1.0, scalar2=None, op0=ALU.mult), reads=["r1a"], writes=["r1b"])
        T.op("act", lambda e: e.activation(out=ge[:], in_=lgt[:, 0:8], func=AF.Exp, bias=r1[:, 1:2], accum_out=r1[:, 2:3]), reads=["lgt", "r1b"], writes=["ge", "r1c"])
        T.op("dve", lambda e: e.reciprocal(out=r1[:, 3:4], in_=r1[:, 2:3]), reads=["r1c"], writes=["r1d"])
        T.op("dve", lambda e: e.tensor_scalar(out=ohs[:], in0=lgt[:, 0:8], scalar1=r1[:, 0:1], scalar2=None, op0=ALU.is_equal), reads=["lgt", "r1a"], writes=["ohs"])
        T.op("sp", lambda e: e.dma_start(out=oho[i * 128:(i + 1) * 128, :], in_=ohs[:]), reads=["ohs"], writes=["oho"], dma=True)
        T.op("dve", lambda e: e.tensor_scalar(out=ge[:], in0=ohs[:], scalar1=-1.0, scalar2=1e9, op0=ALU.add, op1=ALU.mult), reads=["ohs", "ge"], writes=["ge"])
        for g in range(8):
            T.op("dve", lambda e: e.tensor_scalar(out=ml[:, g * 8:(g + 1) * 8], in0=lgt[:, 8 + g * 8:16 + g * 8], scalar1=ge[:, g:g + 1], scalar2=None, op0=ALU.add),
                 reads=["lgt", "ge"], writes=["ml"])
        T.op("dve", lambda e: e.max(out=mx8[:], in_=ml[:]), reads=["ml"], writes=["mx8"])
        T.op("dve", lambda e: e.tensor_tensor(out=r1[:, 4:5], in0=mx8[:, 1:2], in1=mx8[:, 0:1], op=ALU.subtract), reads=["mx8"], writes=["r1e"])
        T.op("act", lambda e: e.activation(out=r1[:, 5:6], in_=r1[:, 4:5], func=AF.Exp), reads=["r1e"], writes=["r1f"])
        T.op("dve", lambda e: e.tensor_scalar(out=r1[:, 6:7], in0=r1[:, 5:6], scalar1=1.0, scalar2=None, op0=ALU.add), reads=["r1f"], writes=["r1g"])
        T.op("dve", lambda e: e.reciprocal(out=r1[:, 7:8], in_=r1[:, 6:7]), reads=["r1g"], writes=["r1h"])
        T.op("dve", lambda e: e.tensor_tensor(out=r1[:, 8:9], in0=r1[:, 7:8], in1=r1[:, 3:4], op=ALU.mult), reads=["r1h", "r1d"], writes=["r1i"])
        T.op("dve", lambda e: e.tensor_tensor(out=r1[:, 9:10], in0=r1[:, 8:9], in1=r1[:, 5:6], op=ALU.mult), reads=["r1i", "r1f"], writes=["r1j"])
        T.op("dve", lambda e: e.tensor_scalar(out=ra[:], in0=ml[:], scalar1=mx8[:, 0:1], scalar2=r1[:, 8:9], op0=ALU.is_equal, op1=ALU.mult),
             reads=["ml", "mx8", "r1i"], writes=["ra"])
        T.op("dve", lambda e: e.tensor_scalar(out=rb[:], in0=ml[:], scalar1=mx8[:, 1:2], scalar2=r1[:, 9:10], op0=ALU.is_equal, op1=ALU.mult),
             reads=["ml", "mx8", "r1j"], writes=["rb"])
        T.op("dve", lambda e: e.tensor_tensor(out=ra[:], in0=ra[:], in1=rb[:], op=ALU.add), reads=["ra", "rb"], writes=["ra"])
        T.op("sp", lambda e: e.dma_start(out=rwo[i * 128:(i + 1) * 128, :], in_=ra[:]), reads=["ra"], writes=["rwo"], dma=True)
    T.finish("sp")
    print("L3a n_ins", T.n_ins, "n_wait", T.n_wait)
    return nc


def run_l3a(o_nsa, o_fox, proj, x, modrow, w_br_nsa, w_br_fox, w_o, ln1_g, ln1_b, w_rg, b_rg, w_re, b_re, TPC=TPC, cores=NCORES):
    nc = build_l3a(TPC)
    rep = lambda a: np.ascontiguousarray(np.broadcast_to(a.reshape(1, -1), (128, a.size))).astype(np.float32)
    wr = np.ascontiguousarray(np.concatenate([w_rg[0], w_re[0].reshape(D, 64)], axis=1))
    brb = rep(np.concatenate([b_rg[0], b_re[0].reshape(64)]))
    mod2 = np.ascontiguousarray(np.concatenate([modrow[3 * D:4 * D].reshape(16, 128).T, modrow[4 * D:5 * D].reshape(16, 128).T], axis=1))
    OFF_MERGE = 512 + 768 + 24 + 1536 + 8
    x2 = x.reshape(S, D)
    maps = []
    for c in range(cores):
        sl = slice(c * TPC, (c + 1) * TPC)
        maps.append({"onT": np.ascontiguousarray(o_nsa[sl].T), "ofT": np.ascontiguousarray(o_fox[sl].T),
                     "mgT": np.ascontiguousarray(proj[sl, OFF_MERGE:OFF_MERGE + 4096].T), "x": np.ascontiguousarray(x2[sl]),
                     "wbn": w_br_nsa[0], "wbf": w_br_fox[0], "wo": w_o[0], "g1b": rep(modrow[2 * D:3 * D]), "ln1g": rep(ln1_g[0]), "ln1b": rep(ln1_b[0]),
                     "mod2": mod2, "wr": wr, "brb": brb})
    res = _launch("l3a", nc, maps, cores)
    cat = lambda k: np.concatenate([r[k] for r in res.results], axis=0)
    return cat("x1"), cat("xn2"), cat("rw"), cat("oh")


CAP = 512


def _rank_setup(nc, T, A, P_, oh, TPC):
    NT = TPC // 128
    ohs = A("ohs", [128, NT, 8], F32)
    ohb = A("ohb", [128, NT, 8], BF16)
    rank = A("rank", [128, NT, 8], F32)
    onesb = A("onesb", [128, 128], BF16)
    sub = A("sub", [128, 128], BF16)
    rkps = P_("rkps", [128, 512], F32)
    T.op("sp", lambda e: e.dma_start(out=ohs[:], in_=oh.rearrange("(c p) g -> p c g", p=128)), writes=["ohs"], dma=True)
    T.op("dve", lambda e: e.tensor_copy(out=ohb[:], in_=ohs[:]), reads=["ohs"], writes=["ohb"])
    T.op("pool", lambda e: e.memset(onesb[:], 1.0), writes=["onesb"])
    T.op("pool", lambda e: e.memset(sub[:], 1.0), writes=["sub"])
    T.op("pool", lambda e: e.affine_select(out=sub[:], in_=sub[:], pattern=[[1, 128]], compare_op=ALU.is_ge, fill=0.0, base=-1,
                                           channel_multiplier=-1), reads=["sub"], writes=["sub"])
    for c in range(NT):
        for c2 in range(c):
            T.op("pe", lambda e: e.matmul(rkps[:, c * 8:(c + 1) * 8], lhsT=onesb[:], rhs=ohb[:, c2, :], start=(c2 == 0), stop=False),
                 reads=["onesb", "ohb"], writes=["rkps"])
        T.op("pe", lambda e: e.matmul(rkps[:, c * 8:(c + 1) * 8], lhsT=sub[:], rhs=ohb[:, c, :], start=(c == 0), stop=True),
             reads=["sub", "ohb"], writes=["rkps"])
    T.op("dve", lambda e: e.tensor_copy(out=rank[:].rearrange("p c g -> p (c g)"), in_=rkps[:, 0:NT * 8]), writes=["rank", "rkps"])
    return ohs, rank


def build_dispatch(TPC=TPC):
    nc = bass.Bass("TRN2", target_bir_lowering=False)
    NT = TPC // 128
    A = lambda name, shape, dt: nc.alloc_sbuf_tensor("sb_" + name, shape, dt)
    P_ = nc.alloc_psum_tensor
    xn2 = nc.dram_tensor("xn2", [TPC, D], BF16, kind="ExternalInput").ap()
    oh = nc.dram_tensor("oh", [TPC, 8], F32, kind="ExternalInput").ap()
    rw = nc.dram_tensor("rw", [TPC, 64], F32, kind="ExternalInput").ap()
    mod2 = nc.dram_tensor("mod2", [128, 32], F32, kind="ExternalInput").ap()
    iotad = nc.dram_tensor("iota", [128, CAP], F32, kind="ExternalInput").ap()
    xso = nc.dram_tensor("xs", [8, D, CAP], BF16, kind="ExternalOutput").ap()
    rwso = nc.dram_tensor("rws", [8, CAP, 8], F32, kind="ExternalOutput").ap()
    T = Trk(nc)
    ohs, rank = _rank_setup(nc, T, A, P_, oh, TPC)
    xs_ = A("xn2s", [128, NT, D], BF16)
    for h in range(0, NT, 4):
        hw = min(4, NT - h)
        T.op("sp", lambda e: e.dma_start(out=xs_[:, h:h + hw, :], in_=xn2.rearrange("(c p) d -> p c d", p=128)[:, h:h + hw, :]), writes=["xn2s"], dma=True)
    rws_ = A("rwf", [128, NT, 64], F32)
    rwh = A("rwh", [128, NT, 64], BF16); rwhf = A("rwhf", [128, NT, 64], F32); rwl = A("rwl", [128, NT, 64], BF16)
    T.op("sp", lambda e: e.dma_start(out=rws_[:], in_=rw.rearrange("(c p) g -> p c g", p=128)), writes=["rwf"], dma=True)
    T.op("dve", lambda e: e.tensor_copy(out=rwh[:], in_=rws_[:]), reads=["rwf"], writes=["rwh"])
    T.op("dve", lambda e: e.tensor_copy(out=rwhf[:], in_=rwh[:]), reads=["rwh"], writes=["rwhf"])
    T.op("dve", lambda e: e.tensor_tensor(out=rwl[:], in0=rws_[:], in1=rwhf[:], op=ALU.subtract), reads=["rwf", "rwhf"], writes=["rwl"])
    m2 = A("m2", [128, 32], F32); iota = A("iota", [128, CAP], F32)
    T.op("sp", lambda e: e.dma_start(out=m2[:], in_=mod2[:, :]), writes=["m2"], dma=True)
    T.op("sp", lambda e: e.dma_start(out=iota[:], in_=iotad[:, :]), writes=["iota"], dma=True)
    T.op("dve", lambda e: e.tensor_scalar(out=m2[:, 16:32], in0=m2[:, 16:32], scalar1=1.0, scalar2=None, op0=ALU.add), reads=["m2"], writes=["m2"])
    Pm = [A("Pm%d" % i, [128, NT, CAP], BF16) for i in range(2)]
    xo = [A("xo%d" % i, [128, 4, CAP], BF16) for i in range(2)]
    ro = A("ro", [128, 4, 8], F32)
    acc = [P_("acc%d" % i, [128, 512], F32) for i in range(4)]
    rps = P_("rps", [128, 512], F32)
    ev = 0
    import os
    DBG = int(os.environ.get("DBG", "9"))
    for g in range(8 if DBG > 0 else 0):
        Pg = Pm[g % 2]
        pk = "Pm%d" % (g % 2)
        for c in range(NT):
            T.op("dve", lambda e: e.tensor_scalar(out=Pg[:, c, :], in0=iota[:], scalar1=rank[:, c, g:g + 1], scalar2=ohs[:, c, g:g + 1],
                                                  op0=ALU.is_equal, op1=ALU.mult), reads=["iota", "rank", "ohs"], writes=[pk])
        for dg in range(4 if DBG > 1 else 0):
            for c in range(NT):
                for dd in range(4):
                    k = dg * 4 + dd
                    T.op("pe", lambda e: e.matmul(acc[dd][:], lhsT=xs_[:, c, k * 128:(k + 1) * 128], rhs=Pg[:, c, :], start=(c == 0), stop=(c == NT - 1)),
                         reads=["xn2s", pk], writes=["acc%d" % dd])
            xb = xo[ev % 2]
            for dd in range(4):
                k = dg * 4 + dd
                T.op("act", lambda e: e.activation(out=xb[:, dd, :], in_=acc[dd][:], func=AF.Identity, scale=m2[:, 16 + k:17 + k], bias=m2[:, k:k + 1]),
                     reads=["m2"], writes=["xo%d" % (ev % 2), "acc%d" % dd])
            T.op("sp", lambda e: e.dma_start(out=xso[g, dg * 512:(dg + 1) * 512, :].rearrange("(dd p) s -> p dd s", p=128), in_=xb[:]),
                 reads=["xo%d" % (ev % 2)], writes=["xso"], dma=True)
            ev += 1
        for st in range(4 if DBG > 2 else 0):
            n = 0
            for c in range(NT):
                for hl in (rwh, rwl):
                    T.op("pe", lambda e: e.matmul(rps[:, st * 8:(st + 1) * 8], lhsT=Pg[:, c, st * 128:(st + 1) * 128], rhs=hl[:, c, g * 8:(g + 1) * 8],
                                                  start=(n == 0), stop=(n == 2 * NT - 1)), reads=[pk, "rwh", "rwl"], writes=["rps"])
                    n += 1
        if DBG > 3:
            T.op("dve", lambda e: e.tensor_copy(out=ro[:].rearrange("p a b -> p (a b)"), in_=rps[:, 0:32]), writes=["ro", "rps"])
        if DBG > 4:
            T.op("sp", lambda e: e.dma_start(out=rwso[g].rearrange("(st p) e -> p st e", p=128), in_=ro[:]), reads=["ro"], writes=["rwso"], dma=True)
    T.finish("sp")
    print("DISP n_ins", T.n_ins, "n_wait", T.n_wait)
    return nc


def build_experts(NSL=8 * CAP, NE=8):
    nc = bass.Bass("TRN2", target_bir_lowering=False)
    NSC = NSL // 512
    A = lambda name, shape, dt: nc.alloc_sbuf_tensor("sb_" + name, shape, dt)
    P_ = nc.alloc_psum_tensor
    xs = nc.dram_tensor("xs", [D, NSL], BF16, kind="ExternalInput").ap()
    rws = nc.dram_tensor("rws", [NSL, 8], F32, kind="ExternalInput").ap()
    wg = nc.dram_tensor("wg", [NE, D, 512], F32, kind="ExternalInput").ap()
    wu = nc.dram_tensor("wu", [NE, D, 512], F32, kind="ExternalInput").ap()
    wd = nc.dram_tensor("wd", [NE, 512, D], F32, kind="ExternalInput").ap()
    ys = nc.dram_tensor("ys", [NSL, D], F32, kind="ExternalOutput").ap()
    T = Trk(nc)
    W = [[A("w%d_%d" % (b, j), [128, 2048], BF16) for j in range(12)] for b in range(2)]
    stg = [A("stg%d" % i, [128, 2048], F32) for i in range(3)]
    xc = [A("xc%d" % i, [128, 16, 512], BF16) for i in range(2)]
    gs = A("gs", [128, 4, 512], BF16); hT = A("hT", [128, 4, 512], BF16)
    yt = [A("yt%d" % i, [128, D], F32) for i in range(3)]
    rs = A("rs", [128, NSL // 128, 8], F32)
    T.op("act", lambda e: e.dma_start(out=rs[:], in_=rws.rearrange("(st p) e -> p st e", p=128)), writes=["rs"], dma=True)
    acc = [P_("acc%d" % i, [128, 512], F32) for i in range(4)]
    acc2 = [P_("accd%d" % i, [128, 512], F32) for i in range(2)]
    ld = [0]

    def load_weights(e):
        b = e % 2
        for j in range(12):
            sb = ld[0] % 3
            ld[0] += 1
            if j < 8:
                src = (wg if j < 4 else wu)[e, (j % 4) * 512:(j % 4 + 1) * 512, :].rearrange("(kk p) n -> p kk n", p=128)
                dst = stg[sb][:].rearrange("p (kk n) -> p kk n", kk=4)
            else:
                src = wd[e, (j - 8) * 128:(j - 7) * 128, :]
                dst = stg[sb][:]
            T.op("sp", lambda e_: e_.dma_start(out=dst, in_=src), writes=["stg%d" % sb], dma=True)
            eng = "pool" if j % 2 == 0 else "act"
            if eng == "pool":
                T.op("pool", lambda e_: e_.tensor_copy(out=W[b][j][:], in_=stg[sb][:]), reads=["stg%d" % sb], writes=["w%d_%d" % (b, j)])
            else:
                T.op("act", lambda e_: e_.copy(out=W[b][j][:], in_=stg[sb][:]), reads=["stg%d" % sb], writes=["w%d_%d" % (b, j)])

    load_weights(0)
    it = 0
    dn = 0
    yi = 0
    for e in range(NE):
        b = e % 2
        if e + 1 < NE:
            load_weights(e + 1)
        for sc in range(NSC):
            xb = xc[it % 2]
            xk = "xc%d" % (it % 2)
            it += 1
            T.op("act", lambda e_: e_.dma_start(out=xb[:], in_=xs[:, sc * 512:(sc + 1) * 512].rearrange("(k p) s -> p k s", p=128)), writes=[xk], dma=True)
            for ph in range(2):
                for fc in range(4):
                    for k in range(16):
                        wt = W[b][ph * 4 + k // 4]
                        T.op("pe", lambda e_: e_.matmul(acc[fc][:], lhsT=wt[:, (k % 4) * 512 + fc * 128:(k % 4) * 512 + (fc + 1) * 128], rhs=xb[:, k, :],
                                                        start=(k == 0), stop=(k == 15)), reads=["w%d_%d" % (b, ph * 4 + k // 4), xk], writes=["acc%d" % fc])
                    if ph == 0:
                        T.op("act", lambda e_: e_.activation(out=gs[:, fc, :], in_=acc[fc][:], func=AF.Silu), writes=["gs%d" % fc, "acc%d" % fc])
                    else:
                        T.op("dve", lambda e_: e_.tensor_tensor(out=hT[:, fc, :], in0=acc[fc][:], in1=gs[:, fc, :], op=ALU.mult),
                             reads=["gs%d" % fc], writes=["hT%d" % fc, "acc%d" % fc])
            for st in range(4):
                sl = sc * 4 + st
                ytb = yt[yi % 3]
                yk = "yt%d" % (yi % 3)
                yi += 1
                if e > 0:
                    T.op("act", lambda e_: e_.dma_start(out=ytb[:], in_=ys[sl * 128:(sl + 1) * 128, :]), reads=["ys%d" % sl], writes=[yk], dma=True)
                for dmc in range(4):
                    a2 = dn % 2
                    dn += 1
                    for fc in range(4):
                        T.op("pe", lambda e_: e_.matmul(acc2[a2][:], lhsT=hT[:, fc, st * 128:(st + 1) * 128], rhs=W[b][8 + fc][:, dmc * 512:(dmc + 1) * 512],
                                                        start=(fc == 0), stop=(fc == 3)), reads=["hT%d" % fc, "w%d_%d" % (b, 8 + fc)], writes=["accd%d" % a2])
                    if e == 0:
                        T.op("dve", lambda e_: e_.tensor_scalar(out=ytb[:, dmc * 512:(dmc + 1) * 512], in0=acc2[a2][:], scalar1=rs[:, sl, e:e + 1], scalar2=None,
                                                                op0=ALU.mult), reads=["rs"], writes=[yk, "accd%d" % a2])
                    else:
                        T.op("dve", lambda e_: e_.scalar_tensor_tensor(out=ytb[:, dmc * 512:(dmc + 1) * 512], in0=acc2[a2][:], scalar=rs[:, sl, e:e + 1],
                                                                       in1=ytb[:, dmc * 512:(dmc + 1) * 512], op0=ALU.mult, op1=ALU.add),
                             reads=["rs"], writes=[yk, "accd%d" % a2])
                T.op("pool", lambda e_: e_.dma_start(out=ys[sl * 128:(sl + 1) * 128, :], in_=ytb[:]), reads=[yk], writes=["ys%d" % sl], dma=True)
    T.finish("sp")
    T.finish("pool")
    print("EXP n_ins", T.n_ins, "n_wait", T.n_wait)
    return nc


def build_combine(TPC=TPC):
    nc = bass.Bass("TRN2", target_bir_lowering=False)
    NT = TPC // 128
    A = lambda name, shape, dt: nc.alloc_sbuf_tensor("sb_" + name, shape, dt)
    P_ = nc.alloc_psum_tensor
    dI = lambda name, shape: nc.dram_tensor(name, shape, F32, kind="ExternalInput").ap()
    ysl = dI("ysl", [8, CAP, D]); oh = dI("oh", [TPC, 8]); x1 = dI("x1", [TPC, D]); iotad = dI("iota", [128, CAP])
    g2b = dI("g2b", [128, D]); ln2g = dI("ln2g", [128, D]); ln2b = dI("ln2b", [128, D])
    out = nc.dram_tensor("out", [TPC, D], F32, kind="ExternalOutput").ap()
    y2d = nc.dram_tensor("y2d", [TPC, D], F32).ap()
    T = Trk(nc)
    ohs, rank = _rank_setup(nc, T, A, P_, oh, TPC)
    iota = A("iota", [128, CAP], F32)
    T.op("sp", lambda e: e.dma_start(out=iota[:], in_=iotad[:, :]), writes=["iota"], dma=True)
    identb = make_ident(nc, T, BF16, "identb")
    yh = A("yh", [128, 32, 1024], BF16)
    Pt = A("Pt", [128, 8, CAP], BF16)
    PT = [A("PT%d" % i, [128, 32, 128], BF16) for i in range(2)]
    yo = [A("yo%d" % i, [128, 1024], F32) for i in range(2)]
    tps = [P_("tps%d" % i, [128, 8, 128], BF16) for i in range(2)]
    acc = [P_("acc%d" % i, [128, 512], F32) for i in range(2)]
    ysv = ysl.rearrange("g (st p) d -> p g st d", p=128)
    tn = 0
    an = 0
    for h in range(2):
        for g in range(8):
            T.op("pool", lambda e: e.dma_start(out=yh[:, g * 4:(g + 1) * 4, :], in_=ysv[:, g, :, h * 1024:(h + 1) * 1024]), writes=["yh"], dma=True)
        for c in range(NT):
            ptb = PT[c % 2]
            pk = "PT%d" % (c % 2)
            for g in range(8):
                T.op("dve", lambda e: e.tensor_scalar(out=Pt[:, g, :], in0=iota[:], scalar1=rank[:, c, g:g + 1], scalar2=ohs[:, c, g:g + 1],
                                                      op0=ALU.is_equal, op1=ALU.mult), reads=["iota", "rank", "ohs"], writes=["Pt"])
            for q in range(4):
                tb = tn % 2
                tn += 1
                for j in range(8):
                    gi = q * 8 + j
                    T.op("pe", lambda e: e.transpose(out=tps[tb][:, j, :], in_=Pt[:, gi // 4, (gi % 4) * 128:(gi % 4 + 1) * 128], identity=identb[:]),
                         reads=["Pt", "identb"], writes=["tps%d" % tb])
                T.op("act", lambda e: e.copy(out=ptb[:, q * 8:(q + 1) * 8, :], in_=tps[tb][:]), writes=[pk, "tps%d" % tb])
            yb = yo[c % 2]
            for dq in range(2):
                a = an % 2
                an += 1
                for gi in range(32):
                    T.op("pe", lambda e: e.matmul(acc[a][:], lhsT=ptb[:, gi, :], rhs=yh[:, gi, dq * 512:(dq + 1) * 512], start=(gi == 0), stop=(gi == 31)),
                         reads=[pk, "yh"], writes=["acc%d" % a])
                T.op("dve", lambda e: e.tensor_copy(out=yb[:, dq * 512:(dq + 1) * 512], in_=acc[a][:]), writes=["yo%d" % (c % 2), "acc%d" % a])
            T.op("sp", lambda e: e.dma_start(out=y2d[c * 128:(c + 1) * 128, h * 1024:(h + 1) * 1024], in_=yb[:]), reads=["yo%d" % (c % 2)], writes=["y2d%d" % c], dma=True)
    g2p = A("g2p", [128, D], F32); lg_ = A("lng", [128, D], F32); lb_ = A("lnb", [128, D], F32)
    T.op("sp", lambda e: e.dma_start(out=g2p[:], in_=g2b[:, :]), writes=["g2p"], dma=True)
    T.op("sp", lambda e: e.dma_start(out=lg_[:], in_=ln2g[:, :]), writes=["lng"], dma=True)
    T.op("sp", lambda e: e.dma_start(out=lb_[:], in_=ln2b[:, :]), writes=["lnb"], dma=True)
    T.op("dve", lambda e: e.tensor_scalar(out=g2p[:], in0=g2p[:], scalar1=1.0, scalar2=None, op0=ALU.add), reads=["g2p"], writes=["g2p"])
    xt = [A("xt%d" % i, [128, D], F32) for i in range(2)]
    y2 = [A("y2%d" % i, [128, D], F32) for i in range(2)]
    junk = A("junk", [128, D], BF16)
    st = A("st", [128, 16], F32)
    for c in range(NT):
        b = c % 2
        T.op("sp", lambda e: e.dma_start(out=xt[b][:], in_=x1[c * 128:(c + 1) * 128, :]), writes=["xt%d" % b], dma=True)
        T.op("sp", lambda e: e.dma_start(out=y2[b][:], in_=y2d[c * 128:(c + 1) * 128, :]), reads=["y2d%d" % c], writes=["y2%d" % b], dma=True)
        v = y2[b]
        vk = "y2%d" % b
        T.op("dve", lambda e: e.tensor_tensor(out=v[:], in0=v[:], in1=g2p[:], op=ALU.mult), reads=[vk, "g2p"], writes=[vk])
        T.op("dve", lambda e: e.scalar_tensor_tensor(out=v[:], in0=xt[b][:], scalar=ALPHA, in1=v[:], op0=ALU.mult, op1=ALU.add), reads=["xt%d" % b, vk], writes=[vk])
        T.op("dve", lambda e: e.reduce_sum(out=st[:, 0:1], in_=v[:], axis=AX.X), reads=[vk], writes=["st0"])
        T.op("act", lambda e: e.activation(out=junk[:], in_=v[:], func=AF.Square, accum_out=st[:, 1:2]), reads=[vk], writes=["st1", "junk"])
        T.op("dve", lambda e: e.tensor_scalar(out=st[:, 2:3], in0=st[:, 0:1], scalar1=1.0 / D, scalar2=None, op0=ALU.mult), reads=["st0"], writes=["st2"])
        T.op("dve", lambda e: e.tensor_tensor(out=st[:, 3:4], in0=st[:, 2:3], in1=st[:, 2:3], op=ALU.mult), reads=["st2"], writes=["st3"])
        T.op("dve", lambda e: e.scalar_tensor_tensor(out=st[:, 4:5], in0=st[:, 1:2], scalar=1.0 / D, in1=st[:, 3:4], op0=ALU.mult, op1=ALU.subtract),
             reads=["st1", "st3"], writes=["st4"])
        T.op("dve", lambda e: e.tensor_scalar(out=st[:, 6:7], in0=st[:, 4:5], scalar1=LN_EPS, scalar2=None, op0=ALU.add), reads=["st4"], writes=["st6"])
        T.op("act", lambda e: e.activation(out=st[:, 7:8], in_=st[:, 6:7], func=AF.Sqrt), reads=["st6"], writes=["st7"])
        T.op("dve", lambda e: e.reciprocal(out=st[:, 5:6], in_=st[:, 7:8]), reads=["st7"], writes=["st5"])
        T.op("dve", lambda e: e.tensor_scalar(out=v[:], in0=v[:], scalar1=st[:, 2:3], scalar2=st[:, 5:6], op0=ALU.subtract, op1=ALU.mult),
             reads=[vk, "st2", "st5"], writes=[vk])
        T.op("dve", lambda e: e.tensor_tensor(out=v[:], in0=v[:], in1=lg_[:], op=ALU.mult), reads=[vk, "lng"], writes=[vk])
        T.op("dve", lambda e: e.tensor_tensor(out=v[:], in0=v[:], in1=lb_[:], op=ALU.add), reads=[vk, "lnb"], writes=[vk])
        T.op("sp", lambda e: e.dma_start(out=out[c * 128:(c + 1) * 128, :], in_=v[:]), reads=[vk], writes=["out"], dma=True)
    T.finish("sp")
    print("COMB n_ins", T.n_ins, "n_wait", T.n_wait)
    return nc


def run_moe(xn2, rw, oh, x1, modrow, w_gate, w_up, w_down, ln2_g, ln2_b, TPC=TPC, cores=NCORES, ne=8):
    rep = lambda a: np.ascontiguousarray(np.broadcast_to(a.reshape(1, -1), (128, a.size))).astype(np.float32)
    iota = rep(np.arange(CAP, dtype=np.float32))
    mod2 = np.ascontiguousarray(np.concatenate([modrow[3 * D:4 * D].reshape(16, 128).T, modrow[4 * D:5 * D].reshape(16, 128).T], axis=1))
    nc1 = build_dispatch(TPC)
    maps = [{"xn2": np.ascontiguousarray(xn2[c * TPC:(c + 1) * TPC]), "oh": np.ascontiguousarray(oh[c * TPC:(c + 1) * TPC]),
             "rw": np.ascontiguousarray(rw[c * TPC:(c + 1) * TPC]), "mod2": mod2, "iota": iota} for c in range(cores)]
    r1 = _launch("dispatch", nc1, maps, cores).results
    nc2 = build_experts(cores * CAP, ne)
    maps2 = []
    for g in range(8):
        maps2.append({"xs": np.ascontiguousarray(np.concatenate([r1[c]["xs"][g] for c in range(cores)], axis=1)),
                      "rws": np.ascontiguousarray(np.concatenate([r1[c]["rws"][g] for c in range(cores)], axis=0)),
                      "wg": np.ascontiguousarray(w_gate[0][g * 8:g * 8 + ne]), "wu": np.ascontiguousarray(w_up[0][g * 8:g * 8 + ne]),
                      "wd": np.ascontiguousarray(w_down[0][g * 8:g * 8 + ne])})
    r2 = _launch("experts", nc2, maps2, 8).results
    nc3 = build_combine(TPC)
    maps3 = []
    for c in range(cores):
        maps3.append({"ysl": np.ascontiguousarray(np.stack([r2[g]["ys"][c * CAP:(c + 1) * CAP] for g in range(8)], axis=0)),
                      "oh": np.ascontiguousarray(oh[c * TPC:(c + 1) * TPC]), "x1": np.ascontiguousarray(x1[c * TPC:(c + 1) * TPC]), "iota": iota,
                      "g2b": rep(modrow[5 * D:6 * D]), "ln2g": rep(ln2_g[0]), "ln2b": rep(ln2_b[0])})
    r3 = _launch("combine", nc3, maps3, cores).results
    return np.concatenate([r["out"] for r in r3], axis=0)


def build_mod():
    nc = bass.Bass("TRN2", target_bir_lowering=False)
    cT = nc.dram_tensor("cT", [128, 16], F32, kind="ExternalInput").ap()
    wada = nc.dram_tensor("wada", [D, 1536], F32, kind="ExternalInput").ap()
    badaT = nc.dram_tensor("badaT", [128, 12], F32, kind="ExternalInput").ap()
    modT = nc.dram_tensor("modT", [128, 12], F32, kind="ExternalOutput").ap()
    T = Trk(nc)
    A = lambda name, shape, dt: nc.alloc_sbuf_tensor("sb_" + name, shape, dt)
    wst = [A("wst%d" % i, [128, 16, 256], F32) for i in range(2)]
    cs = A("cs", [128, 16], F32); ca = A("ca", [128, 16], F32); bad = A("bad", [128, 12], F32); mo = A("mo", [128, 12], F32)
    modps = nc.alloc_psum_tensor("modps", [128, 512], F32)
    T.op("sp", lambda e: e.dma_start(out=cs[:], in_=cT[:, :]), writes=["cs"], dma=True)
    T.op("sp", lambda e: e.dma_start(out=bad[:], in_=badaT[:, :]), writes=["bad"], dma=True)
    T.op("act", lambda e: e.activation(out=ca[:], in_=cs[:], func=AF.Silu), reads=["cs"], writes=["ca"])
    wv = wada.rearrange("(k p) n -> p k n", p=128)
    for g in range(6):
        b = g % 2
        T.op("sp", lambda e: e.dma_start(out=wst[b][:], in_=wv[:, :, g * 256:(g + 1) * 256]), writes=["wst%d" % b], dma=True)
        for j in range(2):
            n = g * 2 + j
            for k in range(16):
                T.op("pe", lambda e: e.matmul(modps[:, n:n + 1], lhsT=wst[b][:, k, j * 128:(j + 1) * 128], rhs=ca[:, k:k + 1], start=(k == 0), stop=(k == 15)),
                     reads=["wst%d" % b, "ca"], writes=["modps"])
    T.op("dve", lambda e: e.tensor_tensor(out=mo[:], in0=modps[:, 0:12], in1=bad[:], op=ALU.add), reads=["bad"], writes=["mo", "modps"])
    T.op("sp", lambda e: e.dma_start(out=modT[:, :], in_=mo[:]), reads=["mo"], writes=["modT"], dma=True)
    T.finish("sp")
    return nc


def run_mod(c, w_ada, b_ada):
    nc = build_mod()
    cT = np.ascontiguousarray(c.reshape(16, 128).T)
    maps = [{"cT": cT, "wada": np.ascontiguousarray(w_ada[0][:, i * 1536:(i + 1) * 1536]),
             "badaT": np.ascontiguousarray(b_ada[0][i * 1536:(i + 1) * 1536].reshape(12, 128).T)} for i in range(NCORES)]
    res = _launch("mod", nc, maps, NCORES)
    return np.concatenate([r["modT"].T.reshape(-1) for r in res.results])


def kernel(x, c, w_ada, b_ada, w_in, b_fgt, t5_table, cmp_pe, cmp_w1, cmp_b1, cmp_w2, w_br_nsa, w_br_fox, w_o, ln1_g, ln1_b,
           w_rg, b_rg, w_re, b_re, w_gate, w_up, w_down, ln2_g, ln2_b):
    f = lambda a: np.asarray(a, dtype=np.float32)
    x, c, w_ada, b_ada, w_in = f(x), f(c), f(w_ada), f(b_ada), f(w_in)
    modrow = run_mod(c, w_ada, b_ada)
    proj, projb = run_l1(x, modrow, w_in)
    o_fox = run_fox(proj, projb, f(b_fgt))
    o_nsa = run_nsa(proj, projb, f(t5_table), f(cmp_pe), f(cmp_w1), f(cmp_b1), f(cmp_w2))
    x1, xn2, rw, oh = run_l3a(o_nsa, o_fox, proj, x, modrow, f(w_br_nsa), f(w_br_fox), f(w_o), f(ln1_g), f(ln1_b), f(w_rg), f(b_rg), f(w_re), f(b_re))
    out = run_moe(xn2, rw, oh, x1, modrow, f(w_gate), f(w_up), f(w_down), f(ln2_g), f(ln2_b))
    return out.reshape(1, S, D).astype(np.float32)
```

```python
import numpy as np
import concourse.bass as bass
import concourse.mybir as mybir
from concourse.bass_utils import run_bass_kernel_spmd

F32 = mybir.dt.float32
BF16 = mybir.dt.bfloat16
AF = mybir.ActivationFunctionType
ALU = mybir.AluOpType
AX = mybir.AxisListType

NCORES = 8
D = 2048
S = 16384
TPC = S // NCORES
IN_COLS = 6944
LN_EPS = 1e-5


def _launch(name, nc, maps, cores):
    res = run_bass_kernel_spmd(nc, maps, core_ids=list(range(cores)))
    try:
        if getattr(res, "exec_time_ns", None) is not None:
            print("[launch] %s exec_time_ns=%s" % (name, res.exec_time_ns), flush=True)
    except Exception:
        pass
    return res


class Trk:
    def __init__(self, nc, n_dma_sems=14):
        self.nc = nc
        self.eng = {"pe": nc.tensor, "act": nc.scalar, "dve": nc.vector,
                    "pool": nc.gpsimd, "sp": nc.sync}
        self.sem = {}
        self.cnt = {}
        self.waited = {k: {} for k in self.eng}
        self._ctx = []
        for k in ["pe", "act", "dve", "pool"]:
            g = nc.semaphore("s_" + k)
            self.sem[k] = g.__enter__()
            self._ctx.append(g)
            self.cnt[k] = 0
        self.dsem = []
        self.dcnt = []
        self.dq = {}
        for q in ["sp", "act", "pool"]:
            self.dq[q] = [len(self.dsem) + j for j in range(n_dma_sems)]
            for j in range(n_dma_sems):
                g = nc.semaphore("s_dma_%s%d" % (q, j))
                self.dsem.append(g.__enter__())
                self._ctx.append(g)
                self.dcnt.append(0)
        self.dqn = {"sp": 0, "act": 0, "pool": 0}
        self.dnext = 0
        self.st = {}
        self.n_ins = 0
        self.n_wait = 0

    def _semobj(self, sk):
        return self.sem[sk] if isinstance(sk, str) else self.dsem[sk]

    def _wait(self, e, deps):
        best = {}
        for d in deps:
            if d is None:
                continue
            sk, v = d
            if v > best.get(sk, 0):
                best[sk] = v
        for sk, v in best.items():
            if e == "pe" and sk == "pe":
                continue
            if self.waited[e].get(sk, 0) >= v:
                continue
            self.eng[e].wait_ge(self._semobj(sk), v)
            self.waited[e][sk] = v
            self.n_wait += 1

    def op(self, e, fn, reads=(), writes=(), dma=False):
        deps = []
        for k in reads:
            s = self.st.get(k)
            if s is not None:
                deps.append(s[0])
        for k in writes:
            s = self.st.get(k)
            if s is not None:
                deps.append(s[0])
                deps.extend(s[1])
        if dma:
            i = self.dq[e][self.dqn[e] % len(self.dq[e])]
            self.dqn[e] += 1
            if self.dcnt[i] > 0:
                deps.append((i, self.dcnt[i]))
        self._wait(e, deps)
        ins = fn(self.eng[e])
        if dma:
            self.dcnt[i] += 16
            ins.then_inc(self.dsem[i], 16)
            tag = (i, self.dcnt[i])
        else:
            self.cnt[e] += 1
            ins.then_inc(self.sem[e], 1)
            tag = (e, self.cnt[e])
        for k in reads:
            s = self.st.setdefault(k, [None, []])
            s[1].append(tag)
            if len(s[1]) > 48:
                best = {}
                for sk, v in s[1]:
                    if v > best.get(sk, 0):
                        best[sk] = v
                s[1] = list(best.items())
        for k in writes:
            self.st[k] = [tag, []]
        self.n_ins += 1
        return tag

    def finish(self, e="sp"):
        deps = []
        for k, s in self.st.items():
            deps.append(s[0])
            deps.extend(s[1])
        self._wait(e, deps)


def make_ident(nc, T, dtype, name="ident"):
    ident = nc.alloc_sbuf_tensor(name, [128, 128], dtype)
    T.op("pool", lambda e: e.memset(ident[:], 1.0), writes=[name])
    T.op("pool", lambda e: e.affine_select(out=ident[:], in_=ident[:], pattern=[[-1, 128]],
                                           compare_op=ALU.is_equal, fill=0.0, base=0,
                                           channel_multiplier=1), reads=[name], writes=[name])
    return ident


def build_l1(stop=None, TPC=TPC):
    nc = bass.Bass("TRN2", target_bir_lowering=False)
    x = nc.dram_tensor("x", [TPC, D], F32, kind="ExternalInput").ap()
    cT = nc.dram_tensor("cT", [128, 16], F32, kind="ExternalInput").ap()
    wada = nc.dram_tensor("wada", [D, 8], F32, kind="ExternalInput").ap()
    badaT = nc.dram_tensor("badaT", [128, 32], F32, kind="ExternalInput").ap()
    w_in = nc.dram_tensor("w_in", [D, IN_COLS], F32, kind="ExternalInput").ap()
    proj = nc.dram_tensor("proj", [TPC, IN_COLS], F32, kind="ExternalOutput").ap()
    projb = nc.dram_tensor("projb", [TPC, 3072], BF16, kind="ExternalOutput").ap()
    T = Trk(nc)
    NT = TPC // 128
    obf = [nc.alloc_sbuf_tensor("obf%d" % i, [128, 512], BF16) for i in range(2)]

    wst = [nc.alloc_sbuf_tensor("wst%d" % i, [128, 8, 512], F32) for i in range(2)]
    wbf = [nc.alloc_sbuf_tensor("wbf%d" % i, [128, 16, 512], BF16) for i in range(2)]
    uT = nc.alloc_sbuf_tensor("uT", [128, 16, TPC], BF16)
    xt = [nc.alloc_sbuf_tensor("xt%d" % i, [128, D], F32) for i in range(2)]
    xn = nc.alloc_sbuf_tensor("xn", [128, D], BF16)
    junk = nc.alloc_sbuf_tensor("junk", [128, D], BF16)
    ost = [nc.alloc_sbuf_tensor("ost%d" % i, [128, 512], F32) for i in range(4)]
    cs = nc.alloc_sbuf_tensor("cs", [128, 16], F32)
    ca = nc.alloc_sbuf_tensor("ca", [128, 16], F32)
    bad = nc.alloc_sbuf_tensor("bad", [128, 32], F32)
    modT = nc.alloc_sbuf_tensor("modT", [128, 32], F32)
    st = nc.alloc_sbuf_tensor("st", [128, 16], F32)
    tp = [nc.alloc_psum_tensor("tp%d" % i, [128, 8, 128], BF16) for i in range(2)]
    acc = [nc.alloc_psum_tensor("acc%d" % i, [128, 512], F32) for i in range(4)]
    modps = nc.alloc_psum_tensor("modps", [128, 512], F32)
    ident = make_ident(nc, T, BF16)

    T.op("sp", lambda e: e.dma_start(out=modT[:], in_=badaT[:, :]), writes=["modT"], dma=True)
    T.op("dve", lambda e: e.tensor_scalar(out=modT[:, 16:32], in0=modT[:, 16:32], scalar1=1.0, scalar2=None, op0=ALU.add),
         reads=["modT"], writes=["modT"])
    ld = 0

    if stop == "A":
        T.op("sp", lambda e: e.dma_start(out=proj[0:128, 0:32], in_=modT[:]), reads=["modT"], writes=["proj"], dma=True)
        T.finish("sp")
        return nc
    for i in range(NT):
        b = i % 2
        T.op("sp", lambda e: e.dma_start(out=xt[b][:], in_=x[i * 128:(i + 1) * 128, :]), writes=["xt%d" % b], dma=True)
        T.op("dve", lambda e: e.reduce_sum(out=st[:, 0:1], in_=xt[b][:], axis=AX.X), reads=["xt%d" % b], writes=["st0"])
        T.op("act", lambda e: e.activation(out=junk[:], in_=xt[b][:], func=AF.Square, accum_out=st[:, 1:2]),
             reads=["xt%d" % b], writes=["st1", "junk"])
        T.op("dve", lambda e: e.tensor_scalar(out=st[:, 2:3], in0=st[:, 0:1], scalar1=1.0 / D, scalar2=None, op0=ALU.mult),
             reads=["st0"], writes=["st2"])
        T.op("dve", lambda e: e.tensor_tensor(out=st[:, 3:4], in0=st[:, 2:3], in1=st[:, 2:3], op=ALU.mult),
             reads=["st2"], writes=["st3"])
        T.op("dve", lambda e: e.scalar_tensor_tensor(out=st[:, 4:5], in0=st[:, 1:2], scalar=1.0 / D, in1=st[:, 3:4],
                                                     op0=ALU.mult, op1=ALU.subtract), reads=["st1", "st3"], writes=["st4"])
        T.op("dve", lambda e: e.tensor_scalar(out=st[:, 6:7], in0=st[:, 4:5], scalar1=LN_EPS, scalar2=None, op0=ALU.add),
             reads=["st4"], writes=["st6"])
        T.op("act", lambda e: e.activation(out=st[:, 7:8], in_=st[:, 6:7], func=AF.Sqrt), reads=["st6"], writes=["st7"])
        T.op("dve", lambda e: e.reciprocal(out=st[:, 5:6], in_=st[:, 7:8]), reads=["st7"], writes=["st5"])
        T.op("dve", lambda e: e.tensor_scalar(out=xn[:], in0=xt[b][:], scalar1=st[:, 2:3], scalar2=st[:, 5:6],
                                              op0=ALU.subtract, op1=ALU.mult), reads=["xt%d" % b, "st2", "st5"], writes=["xn"])
        for kq in range(2):
            pb = kq % 2
            for j in range(8):
                k = kq * 8 + j
                T.op("pe", lambda e: e.transpose(out=tp[pb][:, j, :], in_=xn[:, k * 128:(k + 1) * 128], identity=ident[:]),
                     reads=["xn", "ident"], writes=["tp%d" % pb])
            for j in range(8):
                k = kq * 8 + j
                T.op("act", lambda e: e.activation(out=uT[:, k, i * 128:(i + 1) * 128], in_=tp[pb][:, j, :], func=AF.Identity,
                                                   scale=modT[:, 16 + k:17 + k], bias=modT[:, k:k + 1]),
                     reads=["modT"], writes=["uT_%d" % i, "tp%d" % pb])

    if stop in ("B", "B1", "B2"):
        T.op("sp", lambda e: e.dma_start(out=proj[0:128, 0:32], in_=modT[:]), reads=["modT"], writes=["proj"], dma=True)
        T.finish("sp")
        return nc
    w_v = w_in.rearrange("(k p) n -> p k n", p=128)
    ngrp = (IN_COLS + 511) // 512
    ev = 0
    for g in range(ngrp):
        c0 = g * 512
        cw = min(512, IN_COLS - c0)
        wb = g % 2
        for kh in range(2):
            b = ld % 2
            ld += 1
            T.op("sp", lambda e: e.dma_start(out=wst[b][:, :, 0:cw], in_=w_v[:, kh * 8:(kh + 1) * 8, c0:c0 + cw]),
                 writes=["wst%d" % b], dma=True)
            T.op("pool", lambda e: e.tensor_copy(out=wbf[wb][:, kh * 8:(kh + 1) * 8, 0:cw], in_=wst[b][:, :, 0:cw]),
                 reads=["wst%d" % b], writes=["wbf%d_%d" % (wb, kh)])
        for i in range(NT):
            a = ev % 4
            for k in range(16):
                T.op("pe", lambda e: e.matmul(acc[a][:, 0:cw], lhsT=uT[:, k, i * 128:(i + 1) * 128], rhs=wbf[wb][:, k, 0:cw],
                                              start=(k == 0), stop=(k == 15)),
                     reads=["uT_%d" % i, "wbf%d_%d" % (wb, k // 8)], writes=["acc%d" % a])
            if ev % 2 == 0:
                T.op("act", lambda e: e.copy(out=ost[a][:, 0:cw], in_=acc[a][:, 0:cw]), writes=["ost%d" % a, "acc%d" % a])
            else:
                T.op("dve", lambda e: e.tensor_copy(out=ost[a][:, 0:cw], in_=acc[a][:, 0:cw]), writes=["ost%d" % a, "acc%d" % a])
            T.op("pool", lambda e: e.dma_start(out=proj[i * 128:(i + 1) * 128, c0:c0 + cw], in_=ost[a][:, 0:cw]),
                 reads=["ost%d" % a], writes=["proj"], dma=True)
            if g < 6:
                ob = obf[ev % 2]
                if ev % 2 == 0:
                    T.op("dve", lambda e: e.tensor_copy(out=ob[:], in_=acc[a][:]), writes=["obf%d" % (ev % 2), "acc%d" % a])
                else:
                    T.op("act", lambda e: e.copy(out=ob[:], in_=acc[a][:]), writes=["obf%d" % (ev % 2), "acc%d" % a])
                T.op("sp", lambda e: e.dma_start(out=projb[i * 128:(i + 1) * 128, c0:c0 + 512], in_=ob[:]),
                     reads=["obf%d" % (ev % 2)], writes=["projb"], dma=True)
            ev += 1
    T.finish("sp")
    T.finish("pool")
    print("L1 n_ins", T.n_ins, "n_wait", T.n_wait)
    return nc


def run_l1(x, modrow, w_in, stop=None, TPC=TPC, NCORES=NCORES):
    nc = build_l1(stop, TPC)
    x2 = np.ascontiguousarray(x.reshape(S, D))
    mod1 = np.ascontiguousarray(np.concatenate([modrow[0:D].reshape(16, 128).T, modrow[D:2 * D].reshape(16, 128).T], axis=1))
    w = np.ascontiguousarray(w_in[0])
    dummy = np.zeros((128, 16), np.float32)
    in_maps = [{"x": x2[i * TPC:(i + 1) * TPC], "cT": dummy, "wada": np.zeros((D, 8), np.float32), "badaT": mod1, "w_in": w} for i in range(NCORES)]
    res = _launch("l1", nc, in_maps, NCORES)
    return np.concatenate([r["proj"] for r in res.results], axis=0), np.concatenate([r["projb"] for r in res.results], axis=0)


def build_masks(nc, T, name="cmask"):
    mk = nc.alloc_sbuf_tensor(name, [128, 4, 512], BF16)
    T.op("pool", lambda e: e.memset(mk[:], 0.0), writes=[name])
    for j in range(4):
        T.op("pool", lambda e: e.affine_select(out=mk[:, j, :], in_=mk[:, j, :], pattern=[[1, 512]],
                                               compare_op=ALU.is_ge, fill=-30000.0, base=-128 * j,
                                               channel_multiplier=-1), reads=[name], writes=[name])
    return mk


def build_fox(Sx=S):
    nc = bass.Bass("TRN2", target_bir_lowering=False)
    NKB = Sx // 128
    NQC = Sx // 512
    qT = nc.dram_tensor("qT", [64, Sx], BF16, kind="ExternalInput").ap()
    kT = nc.dram_tensor("kT", [64, Sx], BF16, kind="ExternalInput").ap()
    v = nc.dram_tensor("v", [Sx, 64], BF16, kind="ExternalInput").ap()
    f2 = nc.dram_tensor("f2", [128, NKB], F32, kind="ExternalInput").ap()
    bfg = nc.dram_tensor("bfg", [128, 1], F32, kind="ExternalInput").ap()
    o = nc.dram_tensor("o", [Sx, 64], F32, kind="ExternalOutput").ap()
    scr = nc.dram_tensor("scr", [2, Sx], BF16).ap()
    T = Trk(nc)

    KTa = nc.alloc_sbuf_tensor("KTa", [128, Sx], BF16)
    QTa = nc.alloc_sbuf_tensor("QTa", [128, Sx], BF16)
    Vp = nc.alloc_sbuf_tensor("Vp", [128, NKB, 65], BF16)
    PT = [nc.alloc_sbuf_tensor("PT%d" % i, [128, 512], BF16) for i in range(4)]
    ost = [nc.alloc_sbuf_tensor("ost%d" % i, [128, 4, 64], F32) for i in range(2)]
    rc = nc.alloc_sbuf_tensor("rc", [128, 4], F32)
    fz = nc.alloc_sbuf_tensor("fz", [128, NKB], F32)
    lf = nc.alloc_sbuf_tensor("lf", [128, NKB], F32)
    nb = nc.alloc_sbuf_tensor("nb", [128, 1], F32)
    Usb = nc.alloc_sbuf_tensor("Usb", [128, 128], F32)
    SUsb = nc.alloc_sbuf_tensor("SUsb", [128, 128], F32)
    ones = nc.alloc_sbuf_tensor("ones", [128, 128], F32)
    totT = nc.alloc_sbuf_tensor("totT", [128, 128], F32)
    Fsb = nc.alloc_sbuf_tensor("Fsb", [128, NKB], F32)
    Fofs = nc.alloc_sbuf_tensor("Fofs", [128, NKB], F32)
    Gq = nc.alloc_sbuf_tensor("Gq", [128, NKB], F32)
    Ghi = nc.alloc_sbuf_tensor("Ghi", [128, 128], BF16)
    Ghf = nc.alloc_sbuf_tensor("Ghf", [128, NKB], F32)
    Glo = nc.alloc_sbuf_tensor("Glo", [128, 128], BF16)
    GT = nc.alloc_sbuf_tensor("GT", [128, 2, 128], BF16)
    bm = [nc.alloc_sbuf_tensor("bm%d" % i, [128, NKB], F32) for i in range(2)]
    Sps = [nc.alloc_psum_tensor("Sps%d" % i, [128, 512], F32) for i in range(3)]
    Ops = [nc.alloc_psum_tensor("Ops%d" % i, [128, 4, 128], F32) for i in range(2)]
    Fps = nc.alloc_psum_tensor("Fps", [128, 512], F32)
    Tps = nc.alloc_psum_tensor("Tps", [128, 8, 128], BF16)
    identb = make_ident(nc, T, BF16, "identb")
    mk = build_masks(nc, T)

    T.op("sp", lambda e: e.dma_start(out=fz[:], in_=f2[:, :]), writes=["fz"], dma=True)
    T.op("sp", lambda e: e.dma_start(out=nb[:], in_=bfg[:, :]), writes=["nb"], dma=True)
    for h in range(0, Sx, 2048):
        w = min(2048, Sx - h)
        T.op("sp", lambda e: e.dma_start(out=KTa[0:64, h:h + w], in_=kT[:, h:h + w]), writes=["KTa"], dma=True)
        T.op("act", lambda e: e.dma_start(out=QTa[0:64, h:h + w], in_=qT[:, h:h + w]), writes=["QTa"], dma=True)
    vv = v.rearrange("(kb p) d -> p kb d", p=128)
    for h in range(0, NKB, 16):
        w = min(16, NKB - h)
        T.op("sp", lambda e: e.dma_start(out=Vp[:, h:h + w, 0:64], in_=vv[:, h:h + w, :]), writes=["Vp"], dma=True)
    T.op("dve", lambda e: e.memset(Vp[:, :, 64:65], 1.0), writes=["Vp1"])
    T.op("dve", lambda e: e.memset(KTa[64:66, :], 8.0), writes=["KTa8"])

    T.op("pool", lambda e: e.memset(ones[:], 1.0), writes=["ones"])
    T.op("pool", lambda e: e.memset(Usb[:], 1.0), writes=["Usb"])
    T.op("pool", lambda e: e.affine_select(out=Usb[:], in_=Usb[:], pattern=[[1, 128]], compare_op=ALU.is_ge, fill=0.0,
                                           base=0, channel_multiplier=-1), reads=["Usb"], writes=["Usb"])
    T.op("pool", lambda e: e.memset(SUsb[:], 1.0), writes=["SUsb"])
    T.op("pool", lambda e: e.affine_select(out=SUsb[:], in_=SUsb[:], pattern=[[1, 128]], compare_op=ALU.is_ge, fill=0.0,
                                           base=-1, channel_multiplier=-1), reads=["SUsb"], writes=["SUsb"])

    T.op("dve", lambda e: e.tensor_scalar(out=nb[:], in0=nb[:], scalar1=-1.0, scalar2=None, op0=ALU.mult), reads=["nb"], writes=["nb"])
    T.op("act", lambda e: e.activation(out=lf[:], in_=fz[:], func=AF.Exp, scale=-1.0, bias=nb[:, 0:1]), reads=["fz", "nb"], writes=["lf"])
    T.op("act", lambda e: e.activation(out=lf[:], in_=lf[:], func=AF.Ln, bias=1.0), reads=["lf"], writes=["lf"])
    T.op("dve", lambda e: e.tensor_scalar(out=lf[:], in0=lf[:], scalar1=-1.0, scalar2=None, op0=ALU.mult), reads=["lf"], writes=["lf"])
    T.op("pe", lambda e: e.matmul(Fps[0:NKB, 0:128], lhsT=lf[:, 0:NKB], rhs=ones[:, :], start=True, stop=True),
         reads=["lf", "ones"], writes=["Fps"])
    T.op("dve", lambda e: e.memset(totT[:], 0.0), writes=["totT"])
    T.op("dve", lambda e: e.tensor_copy(out=totT[0:NKB, :], in_=Fps[0:NKB, 0:128]), writes=["totT", "Fps"])
    T.op("pe", lambda e: e.matmul(Fps[:, 128:128 + NKB], lhsT=totT[:, :], rhs=SUsb[:, 0:NKB], start=True, stop=True),
         reads=["totT", "SUsb"], writes=["Fps"])
    T.op("dve", lambda e: e.tensor_copy(out=Fofs[:], in_=Fps[:, 128:128 + NKB]), writes=["Fofs", "Fps"])
    T.op("pe", lambda e: e.matmul(Fps[:, 256:256 + NKB], lhsT=Usb[:, :], rhs=lf[:, 0:NKB], start=True, stop=True),
         reads=["Usb", "lf"], writes=["Fps"])
    T.op("dve", lambda e: e.tensor_tensor(out=Fsb[:], in0=Fps[:, 256:256 + NKB], in1=Fofs[:], op=ALU.add),
         reads=["Fofs"], writes=["Fsb", "Fps"])
    Gv = Gq[:].rearrange("j (c f) -> j c f", f=4)
    Fv = Fsb[:].rearrange("j (c f) -> j c f", f=4)
    Ov = Fofs[:].rearrange("j (c f) -> j c f", f=4)
    for f in range(4):
        T.op("dve", lambda e: e.tensor_tensor(out=Gv[:, :, f], in0=Fv[:, :, f], in1=Ov[:, :, 0], op=ALU.subtract),
             reads=["Fsb", "Fofs"], writes=["Gq"])
    T.op("dve", lambda e: e.memset(Ghi[:], 0.0), writes=["Ghi"])
    T.op("dve", lambda e: e.memset(Glo[:], 0.0), writes=["Glo"])
    T.op("dve", lambda e: e.tensor_copy(out=Ghi[:, 0:NKB], in_=Gq[:]), reads=["Gq"], writes=["Ghi"])
    T.op("dve", lambda e: e.tensor_copy(out=Ghf[:], in_=Ghi[:, 0:NKB]), reads=["Ghi"], writes=["Ghf"])
    T.op("dve", lambda e: e.tensor_tensor(out=Glo[:, 0:NKB], in0=Gq[:], in1=Ghf[:], op=ALU.subtract), reads=["Gq", "Ghf"], writes=["Glo"])
    T.op("pe", lambda e: e.transpose(out=Tps[:, 0, :], in_=Ghi[:], identity=identb[:]), reads=["Ghi", "identb"], writes=["Tps"])
    T.op("pe", lambda e: e.transpose(out=Tps[:, 1, :], in_=Glo[:], identity=identb[:]), reads=["Glo", "identb"], writes=["Tps"])
    T.op("dve", lambda e: e.tensor_copy(out=GT[:], in_=Tps[:, 0:2, :]), writes=["GT", "Tps"])
    for r in range(2):
        T.op("sp", lambda e: e.dma_start(out=scr[r:r + 1, :].rearrange("o (p j) -> (o p) j", j=128), in_=GT[0:NKB, r, :]),
             reads=["GT"], writes=["scr"], dma=True)
    T.op("sp", lambda e: e.dma_start(out=QTa[64:66, :], in_=scr[:, :]), reads=["scr"], writes=["QTaG"], dma=True)

    it = 0
    for qc in range(NQC):
        ob = qc % 2
        nk = 4 * qc + 4
        bmq = bm[qc % 2]
        T.op("dve", lambda e: e.tensor_scalar(out=bmq[:, 0:nk], in0=Fsb[:, 0:nk], scalar1=-1.0, scalar2=Fofs[:, 4 * qc:4 * qc + 1],
                                              op0=ALU.mult, op1=ALU.add), reads=["Fsb", "Fofs"], writes=["bm%d" % (qc % 2)])
        T.op("dve", lambda e: e.memset(Ops[ob][:], 0.0), writes=["Ops%d" % ob])

        def qk(kb, it):
            sb = it % 3
            j = kb - 4 * qc
            T.op("pe", lambda e: e.matmul(Sps[sb][:], lhsT=KTa[0:66, kb * 128:(kb + 1) * 128], rhs=QTa[0:66, qc * 512:(qc + 1) * 512],
                                          start=True, stop=(j < 0)),
                 reads=["KTa", "KTa8", "QTa", "QTaG"], writes=["Sps%d" % sb])
            if j >= 0:
                T.op("pe", lambda e: e.matmul(Sps[sb][:], lhsT=identb[:], rhs=mk[:, j, :], start=False, stop=True),
                     reads=["identb", "cmask"], writes=["Sps%d" % sb])

        def ex_pv(kb, it):
            sb = it % 3
            pb = it % 4
            j = kb - 4 * qc
            T.op("act", lambda e: e.activation(out=PT[pb][:], in_=Sps[sb][:], func=AF.Exp, scale=0.125, bias=bmq[:, kb:kb + 1]),
                 reads=["bm%d" % (qc % 2)], writes=["PT%d" % pb, "Sps%d" % sb])
            for jj in range(max(j, 0), 4):
                T.op("pe", lambda e: e.matmul(Ops[ob][:, jj, 0:65], lhsT=PT[pb][:, jj * 128:(jj + 1) * 128], rhs=Vp[:, kb, :],
                                              start=False, stop=False, skip_group_check=True),
                     reads=["PT%d" % pb, "Vp", "Vp1"], writes=["Ops%d" % ob])

        qk(0, it)
        if nk > 1:
            qk(1, it + 1)
        for kb in range(nk):
            if kb + 2 < nk:
                qk(kb + 2, it + 2)
            ex_pv(kb, it)
            it += 1
        osb = ost[qc % 2]
        T.op("dve", lambda e: e.reciprocal(out=rc[:], in_=Ops[ob][:, :, 64]), writes=["rc", "Ops%d" % ob])
        for jj in range(4):
            T.op("dve", lambda e: e.tensor_scalar(out=osb[:, jj, :], in0=Ops[ob][:, jj, 0:64], scalar1=rc[:, jj:jj + 1], scalar2=None,
                                                  op0=ALU.mult), reads=["rc"], writes=["ost%d" % (qc % 2), "Ops%d" % ob])
        T.op("sp", lambda e: e.dma_start(out=o[qc * 512:(qc + 1) * 512, :].rearrange("(jj p) d -> p jj d", p=128), in_=osb[:]),
             reads=["ost%d" % (qc % 2)], writes=["o"], dma=True)
    T.finish("sp")
    print("FOX n_ins", T.n_ins, "n_wait", T.n_wait)
    return nc


def fox_inputs(proj, projb, b_fgt, Sx=S):
    OFF_FOX = 512 + 768 + 24
    OFF_FGT = OFF_FOX + 1536
    maps = []
    for h in range(8):
        q = projb[:Sx, OFF_FOX + h * 64:OFF_FOX + (h + 1) * 64]
        k = projb[:Sx, OFF_FOX + 512 + h * 64:OFF_FOX + 512 + (h + 1) * 64]
        v = projb[:Sx, OFF_FOX + 1024 + h * 64:OFF_FOX + 1024 + (h + 1) * 64]
        f = proj[:Sx, OFF_FGT + h]
        maps.append({"qT": np.ascontiguousarray(q.T), "kT": np.ascontiguousarray(k.T), "v": np.ascontiguousarray(v),
                     "f2": np.ascontiguousarray(f.reshape(Sx // 128, 128).T),
                     "bfg": np.full((128, 1), b_fgt[0, h], np.float32)})
    return maps


def run_fox(proj, projb, b_fgt, Sx=S, cores=NCORES):
    nc = build_fox(Sx)
    maps = fox_inputs(proj, projb, b_fgt, Sx)[:cores]
    res = _launch("fox", nc, maps, cores)
    return np.concatenate([r["o"] for r in res.results], axis=1)


BIGNEG = -30000.0


def build_nsa(Sx=S):
    nc = bass.Bass("TRN2", target_bir_lowering=False)
    NKB = Sx // 128
    NB = Sx // 512
    NCT = max(1, Sx // 2048)
    NCMP = Sx // 16
    NJ = Sx // 64
    dI = lambda name, shape: nc.dram_tensor(name, shape, F32, kind="ExternalInput").ap()
    A = lambda name, shape, dt: nc.alloc_sbuf_tensor("sb_" + name, shape, dt)
    dB = lambda name, shape: nc.dram_tensor(name, shape, BF16, kind="ExternalInput").ap()
    qT4 = dB("qT4", [64, NB, 512])
    kc_s = dB("kc_s", [64, Sx]); vc_s = dB("vc_s", [64, Sx])
    kslcT = dB("kslcT", [64, Sx]); kwinT = dB("kwinT", [64, Sx])
    vslc1 = dB("vslc1", [Sx, 65]); vwin1 = dB("vwin1", [Sx, 65])
    gates = dI("gates", [128, NB * 12])
    w1d = dI("w1", [64, 2 * 32 * 256]); b1T = dI("b1T", [128, 4]); w2d = dI("w2", [128, 4 * 64]); peT = dI("peT", [64, 64])
    D0d = dI("D0", [128, 512]); D1d = dI("D1", [128, 512]); CBd = dI("CB", [128, 18 * 512]); Cfd = dI("Cfull", [128, 512])
    crow = dI("crow", [1, 512])
    wseld = dI("wsel", [128, NCT * NJ]); cvald = dI("cvalid", [128, NCT]); f0d = dI("force0", [128, NJ])
    o = nc.dram_tensor("o", [NB * 128, 256], F32, kind="ExternalOutput").ap()
    T = Trk(nc)
    bufA = A("bufA", [128, Sx], BF16)
    bufB = A("bufB", [128, Sx], BF16)
    bufC = A("bufC", [128, max(2 * NKB * 65, 16384)], BF16)
    w1v = bufC[0:64, 0:16384].rearrange("d (z l h) -> d z l h", z=2, l=32)
    Vs = bufC[:, 0:NKB * 65].rearrange("p (k c) -> p k c", c=65)
    Vw = bufC[:, NKB * 65:2 * NKB * 65].rearrange("p (k c) -> p k c", c=65)
    E = A("E", [128, 64, 128], BF16)
    CB = A("CB", [128, 18, 512], BF16)
    D0 = A("D0", [128, 512], BF16); D1 = A("D1", [128, 512], BF16); W4 = A("W4", [128, 512], BF16)
    hidT = A("hidT", [128, 2, 2, NCMP], BF16)
    KcT = A("KcT", [128, NCMP], BF16)
    Vc = A("Vc", [128, NCT, 65], BF16)
    wsel = A("wsel", [128, NCT, NJ], BF16)
    cval = A("cval", [128, NCT], F32)
    f0 = A("f0", [128, NJ], F32)
    w2 = A("w2", [128, 2, 2, 64], BF16)
    b1 = A("b1", [128, 4], F32); cb = A("cb", [128, 4], F32)
    pe = A("pe", [64, 2, 32], BF16)
    gts = A("gts", [128, NB, 4, 3], F32)
    QT = [A("QT%d" % i, [128, 512], BF16) for i in range(2)]
    PT = [A("PT%d" % i, [128, 512], BF16) for i in range(4)]
    xs = A("xs", [128, 512], F32); x2 = A("x2", [128, 512], F32); sg = A("sg", [128, 512], F32)
    imp = A("imp", [128, NJ], F32); work = A("work", [128, NJ], F32); selm = A("selm", [128, NJ], F32)
    mx8 = A("mx8", [128, 8], F32)
    selbT = A("selbT", [128, 2, 4, 128], BF16)
    rs = A("rs", [128, 3, 4], F32)
    oacc = [A("oacc%d" % i, [128, 4, 64], F32) for i in range(2)]
    vtmp = A("vtmp", [128, 64], F32)
    ident8 = A("ident8", [128, 128], BF16)
    P = nc.alloc_psum_tensor
    Sps = [P("Sps%d" % i, [128, 512], F32) for i in range(3)]
    Ops = [P("Ops%d" % i, [128, 4, 128], F32) for i in range(3)]
    Ips = [P("Ips%d" % i, [128, 2, 256], F32) for i in range(2)]
    identb = make_ident(nc, T, BF16, "identb")
    identf = make_ident(nc, T, F32, "identf")
    T.op("pool", lambda e: e.tensor_scalar(out=ident8[:], in0=identb[:], scalar1=8.0, scalar2=None, op0=ALU.mult),
         reads=["identb"], writes=["ident8"])

    def cast_load(dst, src, key, eng="pool"):
        T.op(eng, lambda e: e.dma_start(out=dst, in_=src), writes=[key], dma=True)

    cast_load(CB[:].rearrange("p r c -> p (r c)"), CBd[:, :], "CB")
    cast_load(D0[:], D0d[:, :], "D0"); cast_load(D1[:], D1d[:, :], "D1"); cast_load(W4[:], Cfd[:, :], "W4")
    cast_load(wsel[:].rearrange("p a b -> p (a b)"), wseld[:, :], "wsel")
    cast_load(w2[:].rearrange("p z c d -> p (z c d)"), w2d[:, :], "w2")
    cast_load(pe[:].rearrange("d z l -> d (z l)"), peT[:, :], "pe")
    cast_load(w1v.rearrange("d z l h -> d (z l h)"), w1d[:, :], "bufC")
    T.op("sp", lambda e: e.dma_start(out=cval[:], in_=cvald[:, :]), writes=["cval"], dma=True)
    T.op("sp", lambda e: e.dma_start(out=f0[:], in_=f0d[:, :]), writes=["f0"], dma=True)
    T.op("sp", lambda e: e.dma_start(out=b1[:], in_=b1T[:, :]), writes=["b1"], dma=True)
    T.op("sp", lambda e: e.dma_start(out=gts[:].rearrange("p a h b -> p (a h b)"), in_=gates[:, :]), writes=["gts"], dma=True)
    T.op("act", lambda e: e.activation(out=gts[:].rearrange("p a h b -> p (a h b)"), in_=gts[:].rearrange("p a h b -> p (a h b)"), func=AF.Sigmoid),
         reads=["gts"], writes=["gts"])
    for i in range(2):
        cast_load(QT[i][64:65, :], crow[:, :], "QTc%d" % i)
    v4 = lambda t: t[:].rearrange("p (h t) -> p h t", h=4)
    T.op("pool", lambda e: e.affine_select(out=v4(D0), in_=v4(D0), pattern=[[0, 4], [1, 128]], compare_op=ALU.is_ge, fill=BIGNEG,
                                           base=0, channel_multiplier=-1), reads=["D0"], writes=["D0"])
    T.op("pool", lambda e: e.affine_select(out=v4(W4), in_=v4(W4), pattern=[[0, 4], [-1, 128]], compare_op=ALU.is_ge, fill=BIGNEG,
                                           base=-1, channel_multiplier=1), reads=["W4"], writes=["W4"])
    for R in range(18):
        cbv = CB[:, R, :].rearrange("p (h t) -> p h t", h=4)
        T.op("pool", lambda e: e.affine_select(out=cbv, in_=cbv, pattern=[[0, 4], [1, 128]], compare_op=ALU.is_ge, fill=BIGNEG,
                                               base=128 * R - 31, channel_multiplier=-16), reads=["CB"], writes=["CB"])
    T.op("pool", lambda e: e.memset(E[:], 1.0), writes=["E"])
    for hs in range(2):
        ev = E[:, :, hs * 64:(hs + 1) * 64]
        T.op("pool", lambda e: e.affine_select(out=ev, in_=ev, pattern=[[-2, 64], [0, 64]], compare_op=ALU.is_equal, fill=0.0,
                                               base=-hs, channel_multiplier=1), reads=["E"], writes=["E"])

    for h in range(0, Sx, 2048):
        w = min(2048, Sx - h)
        cast_load(bufA[0:64, h:h + w], kc_s[:, h:h + w], "bufA", "sp")
        cast_load(bufB[0:64, h:h + w], vc_s[:, h:h + w], "bufB", "act")
    T.op("dve", lambda e: e.memset(hidT[:], 0.0), writes=["hidT"])
    T.op("dve", lambda e: e.memset(KcT[:], 0.0), writes=["KcT"])
    T.op("dve", lambda e: e.memset(KcT[64:65, :], 8.0), reads=[], writes=["KcT"])
    for z in range(2):
        for c2 in range(2):
            col = z * 2 + c2
            for l in range(32):
                T.op("pe", lambda e: e.matmul(Sps[0][:, col:col + 1], lhsT=w1v[:, z, l, c2 * 128:(c2 + 1) * 128], rhs=pe[:, z, l:l + 1],
                                              start=(l == 0), stop=(l == 31)), reads=["bufC", "pe"], writes=["Sps0"])
    T.op("dve", lambda e: e.tensor_tensor(out=cb[:], in0=Sps[0][:, 0:4], in1=b1[:], op=ALU.add), reads=["b1"], writes=["cb", "Sps0"])
    nvalid = NCMP - 1
    src = [bufA, bufB]
    it = 0
    for z in range(2):
        for c2 in range(2):
            for n0 in range(0, nvalid, 512):
                nw = min(512, nvalid - n0)
                sb = it % 2
                it += 1
                for l in range(32):
                    T.op("pe", lambda e: e.matmul(Sps[sb][:, 0:nw], lhsT=w1v[:, z, l, c2 * 128:(c2 + 1) * 128],
                                                  rhs=src[z][0:64, 16 * n0 + l:16 * n0 + l + 16 * (nw - 1) + 1:16],
                                                  start=(l == 0), stop=(l == 31)),
                         reads=["bufC", "bufA" if z == 0 else "bufB"], writes=["Sps%d" % sb])
                col = z * 2 + c2
                T.op("act", lambda e: e.activation(out=xs[:, 0:nw], in_=Sps[sb][:, 0:nw], func=AF.Identity, bias=cb[:, col:col + 1]),
                     reads=["cb"], writes=["xs", "Sps%d" % sb])
                T.op("dve", lambda e: e.tensor_tensor(out=x2[:, 0:nw], in0=xs[:, 0:nw], in1=xs[:, 0:nw], op=ALU.mult), reads=["xs"], writes=["x2"])
                T.op("dve", lambda e: e.tensor_scalar(out=x2[:, 0:nw], in0=x2[:, 0:nw], scalar1=0.044715, scalar2=1.0, op0=ALU.mult, op1=ALU.add),
                     reads=["x2"], writes=["x2"])
                T.op("dve", lambda e: e.tensor_tensor(out=x2[:, 0:nw], in0=x2[:, 0:nw], in1=xs[:, 0:nw], op=ALU.mult), reads=["x2", "xs"], writes=["x2"])
                T.op("act", lambda e: e.activation(out=sg[:, 0:nw], in_=x2[:, 0:nw], func=AF.Sigmoid, scale=1.5957691216057308),
                     reads=["x2"], writes=["sg"])
                T.op("dve", lambda e: e.tensor_tensor(out=hidT[:, z, c2, n0:n0 + nw], in0=xs[:, 0:nw], in1=sg[:, 0:nw], op=ALU.mult),
                     reads=["xs", "sg"], writes=["hidT"])
    for n0 in range(0, NCMP, 512):
        nw = min(512, NCMP - n0)
        for c2 in range(2):
            T.op("pe", lambda e: e.matmul(Sps[0][0:64, 0:nw], lhsT=w2[:, 0, c2, :], rhs=hidT[:, 0, c2, n0:n0 + nw], start=(c2 == 0), stop=(c2 == 1)),
                 reads=["w2", "hidT"], writes=["Sps0"])
        T.op("act", lambda e: e.copy(out=KcT[0:64, n0:n0 + nw], in_=Sps[0][0:64, 0:nw]), writes=["KcT", "Sps0"])
    for nt in range(NCT):
        for c2 in range(2):
            T.op("pe", lambda e: e.matmul(Sps[1][:, 0:64], lhsT=hidT[:, 1, c2, nt * 128:(nt + 1) * 128], rhs=w2[:, 1, c2, :], start=(c2 == 0), stop=(c2 == 1)),
                 reads=["w2", "hidT"], writes=["Sps1"])
        T.op("dve", lambda e: e.tensor_scalar(out=Vc[:, nt, 0:64], in0=Sps[1][:, 0:64], scalar1=cval[:, nt:nt + 1], scalar2=None, op0=ALU.mult),
             reads=["cval"], writes=["Vc", "Sps1"])
        T.op("dve", lambda e: e.tensor_copy(out=Vc[:, nt, 64:65], in_=cval[:, nt:nt + 1]), reads=["cval"], writes=["Vc"])

    for h in range(0, Sx, 2048):
        w = min(2048, Sx - h)
        cast_load(bufA[0:64, h:h + w], kslcT[:, h:h + w], "bufA", "sp")
        cast_load(bufB[0:64, h:h + w], kwinT[:, h:h + w], "bufB", "act")
    T.op("dve", lambda e: e.memset(bufA[64:65, :], 8.0), writes=["bufA8"])
    T.op("dve", lambda e: e.memset(bufB[64:65, :], 8.0), writes=["bufB8"])
    vsv = vslc1.rearrange("(kb p) d -> p kb d", p=128)
    vwv = vwin1.rearrange("(kb p) d -> p kb d", p=128)
    for h in range(0, NKB, 16):
        w = min(16, NKB - h)
        cast_load(Vs[:, h:h + w, :], vsv[:, h:h + w, :], "bufC", "sp")
        cast_load(Vw[:, h:h + w, :], vwv[:, h:h + w, :], "bufC", "act")

    sidx = [0]
    pidx = [0]

    pendq = []

    def flush():
        while pendq:
            pendq.pop(0)()

    def tile_step(Kbuf, kkey, col0, Vap, vkey, ob, qt, qkey, far, bias_tile, bias_key, sel_chunk=None, sel_m=None, imp_nt=None):
        sb = sidx[0] % 3
        sidx[0] += 1
        pb = pidx[0] % 4
        pidx[0] += 1
        kk = 65 if far else 64
        last_plain = (bias_tile is None and sel_chunk is None)
        T.op("pe", lambda e: e.matmul(Sps[sb][:], lhsT=Kbuf[0:kk, col0:col0 + 128], rhs=qt[0:kk, :], start=True, stop=last_plain),
             reads=[kkey, kkey + "8", qkey, qkey.replace("QT", "QTc")], writes=["Sps%d" % sb])
        if bias_tile is not None:
            T.op("pe", lambda e: e.matmul(Sps[sb][:], lhsT=ident8[:], rhs=bias_tile, start=False, stop=(sel_chunk is None)),
                 reads=["ident8", bias_key], writes=["Sps%d" % sb])
        if sel_chunk is not None:
            T.op("pe", lambda e: e.matmul(Sps[sb][:], lhsT=E[:, sel_m, :], rhs=selbT[:, sel_chunk, :, :].rearrange("p h t -> p (h t)"),
                                          start=False, stop=True), reads=["E", "selbT"], writes=["Sps%d" % sb])

        def rest():
            T.op("act", lambda e: e.activation(out=PT[pb][:], in_=Sps[sb][:], func=AF.Exp, scale=0.125), writes=["PT%d" % pb, "Sps%d" % sb])
            for h in range(4):
                T.op("pe", lambda e: e.matmul(Ops[ob][:, h, 0:65], lhsT=PT[pb][:, h * 128:(h + 1) * 128], rhs=Vap,
                                              start=False, stop=False, skip_group_check=True),
                     reads=["PT%d" % pb, vkey], writes=["Ops%d" % ob])
            if imp_nt is not None:
                for h in range(4):
                    T.op("pe", lambda e: e.matmul(Ips[h // 2][:, h % 2, 0:NJ], lhsT=PT[pb][:, h * 128:(h + 1) * 128], rhs=wsel[:, imp_nt, :],
                                                  start=False, stop=False, skip_group_check=True),
                         reads=["PT%d" % pb, "wsel"], writes=["Ips%d" % (h // 2)])
        pendq.append(rest)
        if len(pendq) > 2:
            pendq.pop(0)()

    for i in range(NB):
        md = 4 * i + 3
        qt = QT[i % 2]
        qkey = "QT%d" % (i % 2)
        cast_load(qt[0:64, :], qT4[:, i, :], qkey, "sp")
        for b in range(3):
            T.op("dve", lambda e: e.memset(Ops[b][:], 0.0), writes=["Ops%d" % b])
        for b in range(2):
            T.op("dve", lambda e: e.memset(Ips[b][:], 0.0), writes=["Ips%d" % b])
        for nt in range(NCT):
            R = md - 16 * nt
            if R < 0:
                continue
            far = R >= 18
            tile_step(KcT, "KcT", nt * 128, Vc[:, nt, :], "Vc", 0, qt, qkey, far,
                      None if far else CB[:, R, :], "CB", imp_nt=nt)
        flush()
        T.op("dve", lambda e: e.tensor_scalar(out=rs[:, 0, :], in0=Ops[0][:, :, 64], scalar1=1e-30, scalar2=None, op0=ALU.max),
             writes=["rs0", "Ops0"])
        T.op("dve", lambda e: e.reciprocal(out=rs[:, 0, :], in_=rs[:, 0, :]), reads=["rs0"], writes=["rs0"])
        for h in range(4):
            if h == 0:
                T.op("dve", lambda e: e.tensor_scalar(out=imp[:], in0=Ips[0][:, 0, 0:NJ], scalar1=rs[:, 0, 0:1], scalar2=None, op0=ALU.mult),
                     reads=["rs0"], writes=["imp", "Ips0"])
            else:
                T.op("dve", lambda e: e.scalar_tensor_tensor(out=imp[:], in0=Ips[h // 2][:, h % 2, 0:NJ], scalar=rs[:, 0, h:h + 1], in1=imp[:],
                                                             op0=ALU.mult, op1=ALU.add), reads=["rs0", "imp"], writes=["imp", "Ips%d" % (h // 2)])
        c1 = 2 * md + 1
        if c1 + 1 < NJ:
            T.op("dve", lambda e: e.memset(imp[:, c1 + 1:NJ], -1.0), reads=["imp"], writes=["imp"])
        T.op("dve", lambda e: e.memset(imp[0:64, c1:c1 + 1], -1.0), reads=["imp"], writes=["imp"])
        T.op("dve", lambda e: e.memset(imp[64:128, c1:c1 + 1], 20000.0), reads=["imp"], writes=["imp"])
        T.op("dve", lambda e: e.memset(imp[0:64, c1 - 1:c1], 20000.0), reads=["imp"], writes=["imp"])
        T.op("dve", lambda e: e.memset(imp[64:128, c1 - 1:c1], 10000.0), reads=["imp"], writes=["imp"])
        T.op("dve", lambda e: e.memset(imp[0:64, c1 - 2:c1 - 1], 10000.0), reads=["imp"], writes=["imp"])
        T.op("dve", lambda e: e.tensor_tensor(out=imp[:], in0=imp[:], in1=f0[:], op=ALU.max), reads=["imp", "f0"], writes=["imp"])
        T.op("dve", lambda e: e.tensor_copy(out=work[:], in_=imp[:]), reads=["imp"], writes=["work"])
        for rnd in range(2):
            T.op("dve", lambda e: e.max(out=mx8[:], in_=work[:]), reads=["work"], writes=["mx8"])
            T.op("dve", lambda e: e.match_replace(out=work[:], in_to_replace=mx8[:], in_values=work[:], imm_value=-1e9),
                 reads=["mx8", "work"], writes=["work"])
        T.op("dve", lambda e: e.tensor_scalar(out=selm[:], in0=work[:], scalar1=-1e8, scalar2=None, op0=ALU.is_le), reads=["work"], writes=["selm"])
        nch = (NJ + 127) // 128
        for ch in range(nch):
            cwid = min(128, NJ - ch * 128)
            T.op("pe", lambda e: e.transpose(out=Ips[1][0:cwid, 0, ch * 128:(ch + 1) * 128], in_=selm[:, ch * 128:ch * 128 + cwid], identity=identf[:]),
                 reads=["selm", "identf"], writes=["Ips1"])
        for ch in range(nch):
            cwid = min(128, NJ - ch * 128)
            for h in range(4):
                T.op("dve", lambda e: e.tensor_scalar(out=selbT[0:cwid, ch, h, :], in0=Ips[1][0:cwid, 0, ch * 128:(ch + 1) * 128], scalar1=-1.0, scalar2=-BIGNEG,
                                                      op0=ALU.add, op1=ALU.mult), writes=["selbT", "Ips1"])
        for m in range(max(0, md - 4), md + 1):
            rel = md - m
            bt, bk = {0: (D0[:], "D0"), 1: (D1[:], "D1"), 4: (W4[:], "W4")}.get(rel, (None, None))
            tile_step(bufB, "bufB", m * 128, Vw[:, m, :], "bufC", 2, qt, qkey, bt is None, bt, bk)
        for m in range(0, md + 1):
            rel = md - m
            bt, bk = {0: (D0[:], "D0"), 1: (D1[:], "D1")}.get(rel, (None, None))
            tile_step(bufA, "bufA", m * 128, Vs[:, m, :], "bufC", 1, qt, qkey, bt is None, bt, bk, sel_chunk=m // 64, sel_m=m % 64)
        flush()
        oa = oacc[i % 2]
        for b in range(3):
            if b > 0:
                T.op("dve", lambda e: e.tensor_scalar(out=rs[:, b, :], in0=Ops[b][:, :, 64], scalar1=1e-30, scalar2=None, op0=ALU.max),
                     writes=["rs%d" % b, "Ops%d" % b])
                T.op("dve", lambda e: e.reciprocal(out=rs[:, b, :], in_=rs[:, b, :]), reads=["rs%d" % b], writes=["rs%d" % b])
            T.op("dve", lambda e: e.tensor_tensor(out=rs[:, b, :], in0=rs[:, b, :], in1=gts[:, i, :, b], op=ALU.mult),
                 reads=["rs%d" % b, "gts"], writes=["rs%d" % b])
            for h in range(4):
                if b == 0:
                    T.op("dve", lambda e: e.tensor_scalar(out=oa[:, h, :], in0=Ops[b][:, h, 0:64], scalar1=rs[:, b, h:h + 1], scalar2=None, op0=ALU.mult),
                         reads=["rs%d" % b], writes=["oacc%d" % (i % 2), "Ops%d" % b])
                else:
                    T.op("dve", lambda e: e.scalar_tensor_tensor(out=oa[:, h, :], in0=Ops[b][:, h, 0:64], scalar=rs[:, b, h:h + 1], in1=oa[:, h, :],
                                                                 op0=ALU.mult, op1=ALU.add), reads=["rs%d" % b], writes=["oacc%d" % (i % 2), "Ops%d" % b])
        T.op("sp", lambda e: e.dma_start(out=o[i * 128:(i + 1) * 128, :], in_=oa[:].rearrange("p h d -> p (h d)")),
             reads=["oacc%d" % (i % 2)], writes=["o"], dma=True)
    T.finish("sp")
    print("NSA n_ins", T.n_ins, "n_wait", T.n_wait)
    return nc


def t5_bucket_np(dist):
    n = np.maximum(dist, 0)
    ratio = np.log(np.maximum(n, 16).astype(np.float32) / np.float32(16))
    big = 16 + (ratio / np.float32(np.log(128 / 16)) * np.float32(16)).astype(np.int32)
    return np.where(n < 16, n, np.minimum(big, 31))


def nsa_inputs(proj, projb, t5_table, cmp_pe, cmp_w1, cmp_b1, cmp_w2, Sx=S):
    OFF_KV = 512
    OFF_GATE = 512 + 768
    NB = Sx // 512
    NCT = max(1, Sx // 2048)
    NJ = Sx // 64
    NCMP = Sx // 16
    maps = []
    si = np.arange(128)[:, None]
    ti = np.arange(128)[None, :]
    w1 = np.ascontiguousarray(cmp_w1[0].reshape(2, 32, 64, 256).transpose(2, 0, 1, 3).reshape(64, -1))
    b1T = np.ascontiguousarray(cmp_b1[0].reshape(2, 2, 128).transpose(2, 0, 1).reshape(128, 4))
    w2 = np.ascontiguousarray(cmp_w2[0].reshape(2, 2, 128, 64).transpose(2, 0, 1, 3).reshape(128, -1))
    peT = np.ascontiguousarray(cmp_pe[0].transpose(2, 0, 1).reshape(64, 64))
    for c in range(8):
        g, r = c // 4, c % 4
        sh = 3 - r
        p = proj[:Sx]
        pb = projb[:Sx]
        tb = t5_table.T[4 * g:4 * g + 4]
        kvc = lambda z: pb[:, OFF_KV + (z * 2 + g) * 64:OFF_KV + (z * 2 + g + 1) * 64]

        def shiftT(a):
            out = np.zeros((64, Sx), pb.dtype)
            if sh * 128 < Sx:
                out[:, sh * 128:] = a[:Sx - sh * 128].T
            return out

        def shiftV1(a):
            out = np.zeros((Sx, 65), pb.dtype)
            if sh * 128 < Sx:
                out[sh * 128:, 0:64] = a[:Sx - sh * 128]
                out[sh * 128:, 64] = 1.0
            return out
        q = pb[:, g * 256:(g + 1) * 256].reshape(Sx // 128, 128, 4, 64)[r::4]
        qT4 = np.ascontiguousarray(q.transpose(3, 0, 2, 1).reshape(64, NB, 512))
        gt = p[:, OFF_GATE + g * 12:OFF_GATE + (g + 1) * 12].reshape(Sx // 128, 128, 12)[r::4]
        gates = np.ascontiguousarray(gt.transpose(1, 0, 2).reshape(128, NB * 12))
        D0 = np.stack([tb[h][t5_bucket_np(ti - si)] for h in range(4)], axis=1).reshape(128, 512)
        D1 = np.stack([tb[h][t5_bucket_np(128 + ti - si)] for h in range(4)], axis=1).reshape(128, 512)
        CB = np.stack([np.stack([tb[h][t5_bucket_np(128 * R + ti - 16 * si - 31)] for h in range(4)], axis=1).reshape(128, 512)
                       for R in range(18)], axis=1).reshape(128, 18 * 512)
        Cfull = np.ascontiguousarray(np.broadcast_to(np.repeat(tb[:, 31], 128)[None, :], (128, 512)))
        crow = np.ascontiguousarray(np.repeat(tb[:, 31], 128)[None, :])
        npad = 8 * sh
        nn = np.arange(NCT * 128)[:, None]
        jj = np.arange(NJ)[None, :]
        dlt = nn - 4 * jj
        wsel = np.where((dlt >= 0) & (dlt <= 2), 1.0, np.where((dlt == -1) | (dlt == 3), 0.5, 0.0)).astype(np.float32)
        wsel[:npad] = 0.0
        wsel[NCMP - 1 - 0:] = 0.0 if True else 0.0
        wsel = np.ascontiguousarray(wsel.reshape(NCT, 128, NJ).transpose(1, 0, 2).reshape(128, NCT * NJ))
        cvalid = (np.arange(NCT * 128) >= npad).astype(np.float32)
        cvalid[NCMP - 1:] = 0.0
        cvalid = np.ascontiguousarray(cvalid.reshape(NCT, 128).T)
        force0 = np.zeros((128, NJ), np.float32)
        force0[:, 2 * sh] = 30000.0
        maps.append({"qT4": qT4, "kc_s": shiftT(kvc(0)), "vc_s": shiftT(kvc(1)), "kslcT": shiftT(kvc(2)), "kwinT": shiftT(kvc(4)),
                     "vslc1": shiftV1(kvc(3)), "vwin1": shiftV1(kvc(5)), "gates": gates, "w1": w1, "b1T": b1T, "w2": w2, "peT": peT,
                     "D0": np.ascontiguousarray(D0), "D1": np.ascontiguousarray(D1), "CB": np.ascontiguousarray(CB), "Cfull": Cfull, "crow": crow,
                     "wsel": wsel, "cvalid": cvalid, "force0": force0})
    return maps


def run_nsa(proj, projb, t5_table, cmp_pe, cmp_w1, cmp_b1, cmp_w2, Sx=S, cores=NCORES):
    nc = build_nsa(Sx)
    maps = nsa_inputs(proj, projb, t5_table, cmp_pe, cmp_w1, cmp_b1, cmp_w2, Sx)[:cores]
    res = _launch("nsa", nc, maps, cores)
    out = np.zeros((Sx, 512), np.float32)
    for c in range(cores):
        g, r = c // 4, c % 4
        oc = res.results[c]["o"].reshape(Sx // 512, 128, 256)
        out.reshape(Sx // 128, 128, 512)[r::4, :, g * 256:(g + 1) * 256] = oc
    return out


ALPHA = 2.0 ** 0.25


def build_l3a(TPC=TPC):
    nc = bass.Bass("TRN2", target_bir_lowering=False)
    NT = TPC // 128
    NG = TPC // 512
    dI = lambda name, shape: nc.dram_tensor(name, shape, F32, kind="ExternalInput").ap()
    A = lambda name, shape, dt: nc.alloc_sbuf_tensor("sb_" + name, shape, dt)
    onT = dI("onT", [512, TPC]); ofT = dI("ofT", [512, TPC]); mgT = dI("mgT", [4096, TPC]); x = dI("x", [TPC, D])
    wbn = dI("wbn", [512, D]); wbf = dI("wbf", [512, D]); wo = dI("wo", [D, D])
    g1b = dI("g1b", [128, D]); ln1g = dI("ln1g", [128, D]); ln1b = dI("ln1b", [128, D])
    mod2 = dI("mod2", [128, 32]); wr = dI("wr", [D, 72]); brb = dI("brb", [128, 72])
    x1o = nc.dram_tensor("x1", [TPC, D], F32, kind="ExternalOutput").ap()
    xn2o = nc.dram_tensor("xn2", [TPC, D], BF16, kind="ExternalOutput").ap()
    rwo = nc.dram_tensor("rw", [TPC, 64], F32, kind="ExternalOutput").ap()
    oho = nc.dram_tensor("oh", [TPC, 8], F32, kind="ExternalOutput").ap()
    mTd = nc.dram_tensor("mTd", [16, 128, TPC], BF16).ap()
    T = Trk(nc)
    big = A("big", [128, 32768], BF16)
    wbn_s = big[:, 0:8192].rearrange("p (k n) -> p k n", k=4)
    wbf_s = big[:, 8192:16384].rearrange("p (k n) -> p k n", k=4)
    onT_s = big[:, 16384:16384 + 4 * TPC].rearrange("p (k n) -> p k n", k=4)
    ofT_s = big[:, 24576:24576 + 4 * TPC].rearrange("p (k n) -> p k n", k=4)
    wo_s = big[:, :].rearrange("p (k n) -> p k n", k=16)
    mgs = [A("mgs%d" % i, [128, 2, 512], F32) for i in range(2)]
    t1 = A("t1", [128, 512], F32); t2 = A("t2", [128, 512], F32)
    mo = [A("mo%d" % i, [128, 512], BF16) for i in range(2)]
    P = nc.alloc_psum_tensor
    acc = [P("acc%d" % i, [128, 512], F32) for i in range(4)]
    tpf = [P("tpf%d" % i, [128, 4, 128], F32) for i in range(2)]
    rps = P("rps", [128, 512], F32)

    def cast_load(dst, src, key):
        T.op("pool", lambda e: e.dma_start(out=dst, in_=src), writes=[key], dma=True)
    cast_load(wbn_s, wbn.rearrange("(k p) n -> p k n", p=128), "big")
    cast_load(wbf_s, wbf.rearrange("(k p) n -> p k n", p=128), "big")
    cast_load(onT_s, onT.rearrange("(k p) n -> p k n", p=128), "big")
    cast_load(ofT_s, ofT.rearrange("(k p) n -> p k n", p=128), "big")
    it = 0
    for dc in range(16):
        for tg in range(NG):
            mb = mgs[it % 2]
            T.op("sp", lambda e: e.dma_start(out=mb[:, 0, :], in_=mgT[dc * 128:(dc + 1) * 128, tg * 512:(tg + 1) * 512]), writes=["mgs%d" % (it % 2)], dma=True)
            T.op("sp", lambda e: e.dma_start(out=mb[:, 1, :], in_=mgT[2048 + dc * 128:2048 + (dc + 1) * 128, tg * 512:(tg + 1) * 512]), writes=["mgs%d" % (it % 2)], dma=True)
            T.op("act", lambda e: e.activation(out=mb[:].rearrange("p a n -> p (a n)"), in_=mb[:].rearrange("p a n -> p (a n)"), func=AF.Sigmoid),
                 reads=["mgs%d" % (it % 2)], writes=["mgs%d" % (it % 2)])
            for br, (ws, os_) in enumerate([(wbn_s, onT_s), (wbf_s, ofT_s)]):
                a = (it % 2) * 2 + br
                for k in range(4):
                    T.op("pe", lambda e: e.matmul(acc[a][:], lhsT=ws[:, k, dc * 128:(dc + 1) * 128], rhs=os_[:, k, tg * 512:(tg + 1) * 512],
                                                  start=(k == 0), stop=(k == 3)), reads=["big"], writes=["acc%d" % a])
            a0 = (it % 2) * 2
            T.op("dve", lambda e: e.tensor_tensor(out=t1[:], in0=acc[a0][:], in1=mb[:, 0, :], op=ALU.mult), reads=["mgs%d" % (it % 2)], writes=["t1", "acc%d" % a0])
            T.op("dve", lambda e: e.tensor_tensor(out=t2[:], in0=acc[a0 + 1][:], in1=mb[:, 1, :], op=ALU.mult), reads=["mgs%d" % (it % 2)], writes=["t2", "acc%d" % (a0 + 1)])
            T.op("dve", lambda e: e.tensor_tensor(out=mo[it % 2][:], in0=t1[:], in1=t2[:], op=ALU.add), reads=["t1", "t2"], writes=["mo%d" % (it % 2)])
            T.op("sp", lambda e: e.dma_start(out=mTd[dc, :, tg * 512:(tg + 1) * 512], in_=mo[it % 2][:]), reads=["mo%d" % (it % 2)], writes=["mTd"], dma=True)
            it += 1
    for h in range(4):
        cast_load(wo_s[:, 4 * h:4 * h + 4, :], wo.rearrange("(k p) n -> p k n", p=128)[:, 4 * h:4 * h + 4, :], "big")
    g1p = A("g1p", [128, D], F32); lg_ = A("lng", [128, D], F32); lb_ = A("lnb", [128, D], F32)
    T.op("sp", lambda e: e.dma_start(out=g1p[:], in_=g1b[:, :]), writes=["g1p"], dma=True)
    T.op("sp", lambda e: e.dma_start(out=lg_[:], in_=ln1g[:, :]), writes=["lng"], dma=True)
    T.op("sp", lambda e: e.dma_start(out=lb_[:], in_=ln1b[:, :]), writes=["lnb"], dma=True)
    T.op("dve", lambda e: e.tensor_scalar(out=g1p[:], in0=g1p[:], scalar1=1.0, scalar2=None, op0=ALU.add), reads=["g1p"], writes=["g1p"])
    m2 = A("m2", [128, 32], F32); wrs = A("wrs", [128, 16, 72], F32); brs = A("brs", [128, 72], F32)
    T.op("sp", lambda e: e.dma_start(out=m2[:], in_=mod2[:, :]), writes=["m2"], dma=True)
    T.op("dve", lambda e: e.tensor_scalar(out=m2[:, 16:32], in0=m2[:, 16:32], scalar1=1.0, scalar2=None, op0=ALU.add), reads=["m2"], writes=["m2"])
    T.op("sp", lambda e: e.dma_start(out=wrs[:], in_=wr.rearrange("(k p) n -> p k n", p=128)), writes=["wrs"], dma=True)
    T.op("sp", lambda e: e.dma_start(out=brs[:], in_=brb[:, :]), writes=["brs"], dma=True)
    identf = make_ident(nc, T, F32, "identf")
    mt = [A("mt%d" % i, [128, 16, 128], BF16) for i in range(2)]
    xt = [A("xt%d" % i, [128, D], F32) for i in range(2)]
    v = A("v", [128, D], F32); xn = A("xn", [128, D], F32); junk = A("junk", [128, D], BF16)
    xnb = A("xnb", [128, D], BF16)
    u2T = A("u2T", [128, 16, 128], F32)
    st = A("st", [128, 16], F32)
    lgt = A("lgt", [128, 72], F32); ml = A("ml", [128, 64], F32); mx8 = A("mx8", [128, 8], F32)
    r1 = A("r1", [128, 16], F32); ra = A("ra", [128, 64], F32); rb = A("rb", [128, 64], F32); ohs = A("ohs", [128, 8], F32)
    ge = A("ge", [128, 8], F32)

    def ln_stats(src, key):
        T.op("dve", lambda e: e.reduce_sum(out=st[:, 0:1], in_=src, axis=AX.X), reads=[key], writes=["st0"])
        T.op("act", lambda e: e.activation(out=junk[:], in_=src, func=AF.Square, accum_out=st[:, 1:2]), reads=[key], writes=["st1", "junk"])
        T.op("dve", lambda e: e.tensor_scalar(out=st[:, 2:3], in0=st[:, 0:1], scalar1=1.0 / D, scalar2=None, op0=ALU.mult), reads=["st0"], writes=["st2"])
        T.op("dve", lambda e: e.tensor_tensor(out=st[:, 3:4], in0=st[:, 2:3], in1=st[:, 2:3], op=ALU.mult), reads=["st2"], writes=["st3"])
        T.op("dve", lambda e: e.scalar_tensor_tensor(out=st[:, 4:5], in0=st[:, 1:2], scalar=1.0 / D, in1=st[:, 3:4], op0=ALU.mult, op1=ALU.subtract),
             reads=["st1", "st3"], writes=["st4"])
        T.op("dve", lambda e: e.tensor_scalar(out=st[:, 6:7], in0=st[:, 4:5], scalar1=LN_EPS, scalar2=None, op0=ALU.add), reads=["st4"], writes=["st6"])
        T.op("act", lambda e: e.activation(out=st[:, 7:8], in_=st[:, 6:7], func=AF.Sqrt), reads=["st6"], writes=["st7"])
        T.op("dve", lambda e: e.reciprocal(out=st[:, 5:6], in_=st[:, 7:8]), reads=["st7"], writes=["st5"])

    for i in range(NT):
        b = i % 2
        T.op("sp", lambda e: e.dma_start(out=mt[b][:], in_=mTd[:, :, i * 128:(i + 1) * 128].rearrange("k p t -> p k t")), reads=["mTd"], writes=["mt%d" % b], dma=True)
        T.op("sp", lambda e: e.dma_start(out=xt[b][:], in_=x[i * 128:(i + 1) * 128, :]), writes=["xt%d" % b], dma=True)
        for cg in range(4):
            for k in range(16):
                T.op("pe", lambda e: e.matmul(acc[cg][:], lhsT=mt[b][:, k, :], rhs=wo_s[:, k, cg * 512:(cg + 1) * 512], start=(k == 0), stop=(k == 15)),
                     reads=["mt%d" % b, "big"], writes=["acc%d" % cg])
            T.op("dve", lambda e: e.tensor_tensor(out=v[:, cg * 512:(cg + 1) * 512], in0=acc[cg][:], in1=g1p[:, cg * 512:(cg + 1) * 512], op=ALU.mult),
                 reads=["g1p"], writes=["v", "acc%d" % cg])
        T.op("dve", lambda e: e.scalar_tensor_tensor(out=v[:], in0=xt[b][:], scalar=ALPHA, in1=v[:], op0=ALU.mult, op1=ALU.add),
             reads=["xt%d" % b, "v"], writes=["v"])
        ln_stats(v[:], "v")
        T.op("dve", lambda e: e.tensor_scalar(out=xn[:], in0=v[:], scalar1=st[:, 2:3], scalar2=st[:, 5:6], op0=ALU.subtract, op1=ALU.mult),
             reads=["v", "st2", "st5"], writes=["xn"])
        T.op("dve", lambda e: e.tensor_tensor(out=xn[:], in0=xn[:], in1=lg_[:], op=ALU.mult), reads=["xn", "lng"], writes=["xn"])
        T.op("dve", lambda e: e.tensor_tensor(out=xn[:], in0=xn[:], in1=lb_[:], op=ALU.add), reads=["xn", "lnb"], writes=["xn"])
        T.op("sp", lambda e: e.dma_start(out=x1o[i * 128:(i + 1) * 128, :], in_=xn[:]), reads=["xn"], writes=["x1o"], dma=True)
        ln_stats(xn[:], "xn")
        T.op("dve", lambda e: e.tensor_scalar(out=v[:], in0=xn[:], scalar1=st[:, 2:3], scalar2=st[:, 5:6], op0=ALU.subtract, op1=ALU.mult),
             reads=["xn", "st2", "st5"], writes=["v"])
        T.op("act", lambda e: e.copy(out=xnb[:], in_=v[:]), reads=["v"], writes=["xnb"])
        T.op("sp", lambda e: e.dma_start(out=xn2o[i * 128:(i + 1) * 128, :], in_=xnb[:]), reads=["xnb"], writes=["xn2o"], dma=True)
        for kq in range(4):
            pb = kq % 2
            for j in range(4):
                k = kq * 4 + j
                T.op("pe", lambda e: e.transpose(out=tpf[pb][:, j, :], in_=v[:, k * 128:(k + 1) * 128], identity=identf[:]),
                     reads=["v", "identf"], writes=["tpf%d" % pb])
            for j in range(4):
                k = kq * 4 + j
                T.op("act", lambda e: e.activation(out=u2T[:, k, :], in_=tpf[pb][:, j, :], func=AF.Identity, scale=m2[:, 16 + k:17 + k], bias=m2[:, k:k + 1]),
                     reads=["m2"], writes=["u2T", "tpf%d" % pb])
        for k in range(16):
            T.op("pe", lambda e: e.matmul(rps[:, 0:72], lhsT=u2T[:, k, :], rhs=wrs[:, k, :], start=(k == 0), stop=(k == 15)),
                 reads=["u2T", "wrs"], writes=["rps"])
        T.op("dve", lambda e: e.tensor_tensor(out=lgt[:], in0=rps[:, 0:72], in1=brs[:], op=ALU.add), reads=["brs"], writes=["lgt", "rps"])
        T.op("dve", lambda e: e.reduce_max(out=r1[:, 0:1], in_=lgt[:, 0:8], axis=AX.X), reads=["lgt"], writes=["r1a"])
        T.op("dve", lambda e: e.tensor_scalar(out=r1[:, 1:2], in0=r1[:, 0:1], scalar1=-1.0, scalar2=None, op0=ALU.mult), reads=["r1a"], writes=["r1b"])
        T.op("act", lambda e: e.activation(out=ge[:], in_=lgt[:, 0:8], func=AF.Exp, bias=r1[:, 1:2], accum_out=r1[:, 2:3]), reads=["lgt", "r1b"], writes=["ge", "r1c"])
        T.op("dve", lambda e: e.reciprocal(out=r1[:, 3:4], in_=r1[:, 2:3]), reads=["r1c"], writes=["r1d"])
        T.op("dve", lambda e: e.tensor_scalar(out=ohs[:], in0=lgt[:, 0:8], scalar1=r1[:, 0:1], scalar2=None, op0=ALU.is_equal), reads=["lgt", "r1a"], writes=["ohs"])
        T.op("sp", lambda e: e.dma_start(out=oho[i * 128:(i + 1) * 128, :], in_=ohs[:]), reads=["ohs"], writes=["oho"], dma=True)
        T.op("dve", lambda e: e.tensor_scalar(out=ge[:], in0=ohs[:], scalar1=-1.0, scalar2=1e9, op0=ALU.add, op1=ALU.mult), reads=["ohs", "ge"], writes=["ge"])
        for g in range(8):
            T.op("dve", lambda e: e.tensor_scalar(out=ml[:, g * 8:(g + 1) * 8], in0=lgt[:, 8 + g * 8:16 + g * 8], scalar1=ge[:, g:g + 1], scalar2=None, op0=ALU.add),
                 reads=["lgt", "ge"], writes=["ml"])
        T.op("dve", lambda e: e.max(out=mx8[:], in_=ml[:]), reads=["ml"], writes=["mx8"])
        T.op("dve", lambda e: e.tensor_tensor(out=r1[:, 4:5], in0=mx8[:, 1:2], in1=mx8[:, 0:1], op=ALU.subtract), reads=["mx8"], writes=["r1e"])
        T.op("act", lambda e: e.activation(out=r1[:, 5:6], in_=r1[:, 4:5], func=AF.Exp), reads=["r1e"], writes=["r1f"])
        T.op("dve", lambda e: e.tensor_scalar(out=r1[:, 6:7], in0=r1[:, 5:6], scalar1=1.0, scalar2=None, op0=ALU.add), reads=["r1f"], writes=["r1g"])
        T.op("dve", lambda e: e.reciprocal(out=r1[:, 7:8], in_=r1[:, 6:7]), reads=["r1g"], writes=["r1h"])
        T.op("dve", lambda e: e.tensor_tensor(out=r1[:, 8:9], in0=r1[:, 7:8], in1=r1[:, 3:4], op=ALU.mult), reads=["r1h", "r1d"], writes=["r1i"])
        T.op("dve", lambda e: e.tensor_tensor(out=r1[:, 9:10], in0=r1[:, 8:9], in1=r1[:, 5:6], op=ALU.mult), reads=["r1i", "r1f"], writes=["r1j"])
        T.op("dve", lambda e: e.tensor_scalar(out=ra[:], in0=ml[:], scalar1=mx8[:, 0:1], scalar2=r1[:, 8:9], op0=ALU.is_equal, op1=ALU.mult),
             reads=["ml", "mx8", "r1i"], writes=["ra"])
        T.op("dve", lambda e: e.tensor_scalar(out=rb[:], in0=ml[:], scalar1=mx8[:, 1:2], scalar2=r1[:, 9:10], op0=ALU.is_equal, op1=ALU.mult),
             reads=["ml", "mx8", "r1j"], writes=["rb"])
        T.op("dve", lambda e: e.tensor_tensor(out=ra[:], in0=ra[:], in1=rb[:], op=ALU.add), reads=["ra", "rb"], writes=["ra"])
        T.op("sp", lambda e: e.dma_start(out=rwo[i * 128:(i + 1) * 128, :], in_=ra[:]), reads=["ra"], writes=["rwo"], dma=True)
    T.finish("sp")
    print("L3a n_ins", T.n_ins, "n_wait", T.n_wait)
    return nc


def run_l3a(o_nsa, o_fox, proj, x, modrow, w_br_nsa, w_br_fox, w_o, ln1_g, ln1_b, w_rg, b_rg, w_re, b_re, TPC=TPC, cores=NCORES):
    nc = build_l3a(TPC)
    rep = lambda a: np.ascontiguousarray(np.broadcast_to(a.reshape(1, -1), (128, a.size))).astype(np.float32)
    wr = np.ascontiguousarray(np.concatenate([w_rg[0], w_re[0].reshape(D, 64)], axis=1))
    brb = rep(np.concatenate([b_rg[0], b_re[0].reshape(64)]))
    mod2 = np.ascontiguousarray(np.concatenate([modrow[3 * D:4 * D].reshape(16, 128).T, modrow[4 * D:5 * D].reshape(16, 128).T], axis=1))
    OFF_MERGE = 512 + 768 + 24 + 1536 + 8
    x2 = x.reshape(S, D)
    maps = []
    for c in range(cores):
        sl = slice(c * TPC, (c + 1) * TPC)
        maps.append({"onT": np.ascontiguousarray(o_nsa[sl].T), "ofT": np.ascontiguousarray(o_fox[sl].T),
                     "mgT": np.ascontiguousarray(proj[sl, OFF_MERGE:OFF_MERGE + 4096].T), "x": np.ascontiguousarray(x2[sl]),
                     "wbn": w_br_nsa[0], "wbf": w_br_fox[0], "wo": w_o[0], "g1b": rep(modrow[2 * D:3 * D]), "ln1g": rep(ln1_g[0]), "ln1b": rep(ln1_b[0]),
                     "mod2": mod2, "wr": wr, "brb": brb})
    res = _launch("l3a", nc, maps, cores)
    cat = lambda k: np.concatenate([r[k] for r in res.results], axis=0)
    return cat("x1"), cat("xn2"), cat("rw"), cat("oh")


CAP = 512


def _rank_setup(nc, T, A, P_, oh, TPC):
    NT = TPC // 128
    ohs = A("ohs", [128, NT, 8], F32)
    ohb = A("ohb", [128, NT, 8], BF16)
    rank = A("rank", [128, NT, 8], F32)
    onesb = A("onesb", [128, 128], BF16)
    sub = A("sub", [128, 128], BF16)
    rkps = P_("rkps", [128, 512], F32)
    T.op("sp", lambda e: e.dma_start(out=ohs[:], in_=oh.rearrange("(c p) g -> p c g", p=128)), writes=["ohs"], dma=True)
    T.op("dve", lambda e: e.tensor_copy(out=ohb[:], in_=ohs[:]), reads=["ohs"], writes=["ohb"])
    T.op("pool", lambda e: e.memset(onesb[:], 1.0), writes=["onesb"])
    T.op("pool", lambda e: e.memset(sub[:], 1.0), writes=["sub"])
    T.op("pool", lambda e: e.affine_select(out=sub[:], in_=sub[:], pattern=[[1, 128]], compare_op=ALU.is_ge, fill=0.0, base=-1,
                                           channel_multiplier=-1), reads=["sub"], writes=["sub"])
    for c in range(NT):
        for c2 in range(c):
            T.op("pe", lambda e: e.matmul(rkps[:, c * 8:(c + 1) * 8], lhsT=onesb[:], rhs=ohb[:, c2, :], start=(c2 == 0), stop=False),
                 reads=["onesb", "ohb"], writes=["rkps"])
        T.op("pe", lambda e: e.matmul(rkps[:, c * 8:(c + 1) * 8], lhsT=sub[:], rhs=ohb[:, c, :], start=(c == 0), stop=True),
             reads=["sub", "ohb"], writes=["rkps"])
    T.op("dve", lambda e: e.tensor_copy(out=rank[:].rearrange("p c g -> p (c g)"), in_=rkps[:, 0:NT * 8]), writes=["rank", "rkps"])
    return ohs, rank


def build_dispatch(TPC=TPC):
    nc = bass.Bass("TRN2", target_bir_lowering=False)
    NT = TPC // 128
    A = lambda name, shape, dt: nc.alloc_sbuf_tensor("sb_" + name, shape, dt)
    P_ = nc.alloc_psum_tensor
    xn2 = nc.dram_tensor("xn2", [TPC, D], BF16, kind="ExternalInput").ap()
    oh = nc.dram_tensor("oh", [TPC, 8], F32, kind="ExternalInput").ap()
    rw = nc.dram_tensor("rw", [TPC, 64], F32, kind="ExternalInput").ap()
    mod2 = nc.dram_tensor("mod2", [128, 32], F32, kind="ExternalInput").ap()
    iotad = nc.dram_tensor("iota", [128, CAP], F32, kind="ExternalInput").ap()
    xso = nc.dram_tensor("xs", [8, D, CAP], BF16, kind="ExternalOutput").ap()
    rwso = nc.dram_tensor("rws", [8, CAP, 8], F32, kind="ExternalOutput").ap()
    T = Trk(nc)
    ohs, rank = _rank_setup(nc, T, A, P_, oh, TPC)
    xs_ = A("xn2s", [128, NT, D], BF16)
    for h in range(0, NT, 4):
        hw = min(4, NT - h)
        T.op("sp", lambda e: e.dma_start(out=xs_[:, h:h + hw, :], in_=xn2.rearrange("(c p) d -> p c d", p=128)[:, h:h + hw, :]), writes=["xn2s"], dma=True)
    rws_ = A("rwf", [128, NT, 64], F32)
    rwh = A("rwh", [128, NT, 64], BF16); rwhf = A("rwhf", [128, NT, 64], F32); rwl = A("rwl", [128, NT, 64], BF16)
    T.op("sp", lambda e: e.dma_start(out=rws_[:], in_=rw.rearrange("(c p) g -> p c g", p=128)), writes=["rwf"], dma=True)
    T.op("dve", lambda e: e.tensor_copy(out=rwh[:], in_=rws_[:]), reads=["rwf"], writes=["rwh"])
    T.op("dve", lambda e: e.tensor_copy(out=rwhf[:], in_=rwh[:]), reads=["rwh"], writes=["rwhf"])
    T.op("dve", lambda e: e.tensor_tensor(out=rwl[:], in0=rws_[:], in1=rwhf[:], op=ALU.subtract), reads=["rwf", "rwhf"], writes=["rwl"])
    m2 = A("m2", [128, 32], F32); iota = A("iota", [128, CAP], F32)
    T.op("sp", lambda e: e.dma_start(out=m2[:], in_=mod2[:, :]), writes=["m2"], dma=True)
    T.op("sp", lambda e: e.dma_start(out=iota[:], in_=iotad[:, :]), writes=["iota"], dma=True)
    T.op("dve", lambda e: e.tensor_scalar(out=m2[:, 16:32], in0=m2[:, 16:32], scalar1=1.0, scalar2=None, op0=ALU.add), reads=["m2"], writes=["m2"])
    Pm = [A("Pm%d" % i, [128, NT, CAP], BF16) for i in range(2)]
    xo = [A("xo%d" % i, [128, 4, CAP], BF16) for i in range(2)]
    ro = A("ro", [128, 4, 8], F32)
    acc = [P_("acc%d" % i, [128, 512], F32) for i in range(4)]
    rps = P_("rps", [128, 512], F32)
    ev = 0
    import os
    DBG = int(os.environ.get("DBG", "9"))
    for g in range(8 if DBG > 0 else 0):
        Pg = Pm[g % 2]
        pk = "Pm%d" % (g % 2)
        for c in range(NT):
            T.op("dve", lambda e: e.tensor_scalar(out=Pg[:, c, :], in0=iota[:], scalar1=rank[:, c, g:g + 1], scalar2=ohs[:, c, g:g + 1],
                                                  op0=ALU.is_equal, op1=ALU.mult), reads=["iota", "rank", "ohs"], writes=[pk])
        for dg in range(4 if DBG > 1 else 0):
            for c in range(NT):
                for dd in range(4):
                    k = dg * 4 + dd
                    T.op("pe", lambda e: e.matmul(acc[dd][:], lhsT=xs_[:, c, k * 128:(k + 1) * 128], rhs=Pg[:, c, :], start=(c == 0), stop=(c == NT - 1)),
                         reads=["xn2s", pk], writes=["acc%d" % dd])
            xb = xo[ev % 2]
            for dd in range(4):
                k = dg * 4 + dd
                T.op("act", lambda e: e.activation(out=xb[:, dd, :], in_=acc[dd][:], func=AF.Identity, scale=m2[:, 16 + k:17 + k], bias=m2[:, k:k + 1]),
                     reads=["m2"], writes=["xo%d" % (ev % 2), "acc%d" % dd])
            T.op("sp", lambda e: e.dma_start(out=xso[g, dg * 512:(dg + 1) * 512, :].rearrange("(dd p) s -> p dd s", p=128), in_=xb[:]),
                 reads=["xo%d" % (ev % 2)], writes=["xso"], dma=True)
            ev += 1
        for st in range(4 if DBG > 2 else 0):
            n = 0
            for c in range(NT):
                for hl in (rwh, rwl):
                    T.op("pe", lambda e: e.matmul(rps[:, st * 8:(st + 1) * 8], lhsT=Pg[:, c, st * 128:(st + 1) * 128], rhs=hl[:, c, g * 8:(g + 1) * 8],
                                                  start=(n == 0), stop=(n == 2 * NT - 1)), reads=[pk, "rwh", "rwl"], writes=["rps"])
                    n += 1
        if DBG > 3:
            T.op("dve", lambda e: e.tensor_copy(out=ro[:].rearrange("p a b -> p (a b)"), in_=rps[:, 0:32]), writes=["ro", "rps"])
        if DBG > 4:
            T.op("sp", lambda e: e.dma_start(out=rwso[g].rearrange("(st p) e -> p st e", p=128), in_=ro[:]), reads=["ro"], writes=["rwso"], dma=True)
    T.finish("sp")
    print("DISP n_ins", T.n_ins, "n_wait", T.n_wait)
    return nc


def build_experts(NSL=8 * CAP, NE=8):
    nc = bass.Bass("TRN2", target_bir_lowering=False)
    NSC = NSL // 512
    A = lambda name, shape, dt: nc.alloc_sbuf_tensor("sb_" + name, shape, dt)
    P_ = nc.alloc_psum_tensor
    xs = nc.dram_tensor("xs", [D, NSL], BF16, kind="ExternalInput").ap()
    rws = nc.dram_tensor("rws", [NSL, 8], F32, kind="ExternalInput").ap()
    wg = nc.dram_tensor("wg", [NE, D, 512], F32, kind="ExternalInput").ap()
    wu = nc.dram_tensor("wu", [NE, D, 512], F32, kind="ExternalInput").ap()
    wd = nc.dram_tensor("wd", [NE, 512, D], F32, kind="ExternalInput").ap()
    ys = nc.dram_tensor("ys", [NSL, D], F32, kind="ExternalOutput").ap()
    T = Trk(nc)
    W = [[A("w%d_%d" % (b, j), [128, 2048], BF16) for j in range(12)] for b in range(2)]
    stg = [A("stg%d" % i, [128, 2048], F32) for i in range(3)]
    xc = [A("xc%d" % i, [128, 16, 512], BF16) for i in range(2)]
    gs = A("gs", [128, 4, 512], BF16); hT = A("hT", [128, 4, 512], BF16)
    yt = [A("yt%d" % i, [128, D], F32) for i in range(3)]
    rs = A("rs", [128, NSL // 128, 8], F32)
    T.op("act", lambda e: e.dma_start(out=rs[:], in_=rws.rearrange("(st p) e -> p st e", p=128)), writes=["rs"], dma=True)
    acc = [P_("acc%d" % i, [128, 512], F32) for i in range(4)]
    acc2 = [P_("accd%d" % i, [128, 512], F32) for i in range(2)]
    ld = [0]

    def load_weights(e):
        b = e % 2
        for j in range(12):
            sb = ld[0] % 3
            ld[0] += 1
            if j < 8:
                src = (wg if j < 4 else wu)[e, (j % 4) * 512:(j % 4 + 1) * 512, :].rearrange("(kk p) n -> p kk n", p=128)
                dst = stg[sb][:].rearrange("p (kk n) -> p kk n", kk=4)
            else:
                src = wd[e, (j - 8) * 128:(j - 7) * 128, :]
                dst = stg[sb][:]
            T.op("sp", lambda e_: e_.dma_start(out=dst, in_=src), writes=["stg%d" % sb], dma=True)
            eng = "pool" if j % 2 == 0 else "act"
            if eng == "pool":
                T.op("pool", lambda e_: e_.tensor_copy(out=W[b][j][:], in_=stg[sb][:]), reads=["stg%d" % sb], writes=["w%d_%d" % (b, j)])
            else:
                T.op("act", lambda e_: e_.copy(out=W[b][j][:], in_=stg[sb][:]), reads=["stg%d" % sb], writes=["w%d_%d" % (b, j)])

    load_weights(0)
    dn = 0
    chunks = [(e, sc) for e in range(NE) for sc in range(NSC)]
    tiles = [(e, sc, st) for e in range(NE) for sc in range(NSC) for st in range(4)]

    def load_chunk(ci):
        e, sc = chunks[ci]
        xb = xc[ci % 2]
        T.op("act", lambda e_: e_.dma_start(out=xb[:], in_=xs[:, sc * 512:(sc + 1) * 512].rearrange("(k p) s -> p k s", p=128)),
             writes=["xc%d" % (ci % 2)], dma=True)

    def load_ytile(ti):
        e, sc, st = tiles[ti]
        if e == 0:
            return
        sl = sc * 4 + st
        ytb = yt[ti % 3]
        T.op("act", lambda e_: e_.dma_start(out=ytb[:], in_=ys[sl * 128:(sl + 1) * 128, :]), reads=["ys%d" % sl], writes=["yt%d" % (ti % 3)], dma=True)

    load_chunk(0)
    for ci, (e, sc) in enumerate(chunks):
        b = e % 2
        if sc == 0 and e + 1 < NE:
            load_weights(e + 1)
        if ci + 1 < len(chunks):
            load_chunk(ci + 1)
        xb = xc[ci % 2]
        xk = "xc%d" % (ci % 2)
        for ph in range(2):
            for fc in range(4):
                for k in range(16):
                    wt = W[b][ph * 4 + k // 4]
                    T.op("pe", lambda e_: e_.matmul(acc[fc][:], lhsT=wt[:, (k % 4) * 512 + fc * 128:(k % 4) * 512 + (fc + 1) * 128], rhs=xb[:, k, :],
                                                    start=(k == 0), stop=(k == 15)), reads=["w%d_%d" % (b, ph * 4 + k // 4), xk], writes=["acc%d" % fc])
                if ph == 0:
                    T.op("act", lambda e_: e_.activation(out=gs[:, fc, :], in_=acc[fc][:], func=AF.Silu), writes=["gs%d" % fc, "acc%d" % fc])
                else:
                    T.op("dve", lambda e_: e_.tensor_tensor(out=hT[:, fc, :], in0=acc[fc][:], in1=gs[:, fc, :], op=ALU.mult),
                         reads=["gs%d" % fc], writes=["hT%d" % fc, "acc%d" % fc])
        for st in range(4):
            ti = ci * 4 + st
            sl = sc * 4 + st
            ytb = yt[ti % 3]
            yk = "yt%d" % (ti % 3)
            if ti + 1 < len(tiles):
                load_ytile(ti + 1)
            for dmc in range(4):
                a2 = dn % 2
                dn += 1
                for fc in range(4):
                    T.op("pe", lambda e_: e_.matmul(acc2[a2][:], lhsT=hT[:, fc, st * 128:(st + 1) * 128], rhs=W[b][8 + fc][:, dmc * 512:(dmc + 1) * 512],
                                                    start=(fc == 0), stop=(fc == 3)), reads=["hT%d" % fc, "w%d_%d" % (b, 8 + fc)], writes=["accd%d" % a2])
                if e == 0:
                    T.op("dve", lambda e_: e_.tensor_scalar(out=ytb[:, dmc * 512:(dmc + 1) * 512], in0=acc2[a2][:], scalar1=rs[:, sl, e:e + 1], scalar2=None,
                                                            op0=ALU.mult), reads=["rs"], writes=[yk, "accd%d" % a2])
                else:
                    T.op("dve", lambda e_: e_.scalar_tensor_tensor(out=ytb[:, dmc * 512:(dmc + 1) * 512], in0=acc2[a2][:], scalar=rs[:, sl, e:e + 1],
                                                                   in1=ytb[:, dmc * 512:(dmc + 1) * 512], op0=ALU.mult, op1=ALU.add),
                         reads=["rs"], writes=[yk, "accd%d" % a2])
            T.op("pool", lambda e_: e_.dma_start(out=ys[sl * 128:(sl + 1) * 128, :], in_=ytb[:]), reads=[yk], writes=["ys%d" % sl], dma=True)
    T.finish("sp")
    T.finish("pool")
    print("EXP n_ins", T.n_ins, "n_wait", T.n_wait)
    return nc


def build_combine(TPC=TPC):
    nc = bass.Bass("TRN2", target_bir_lowering=False)
    NT = TPC // 128
    A = lambda name, shape, dt: nc.alloc_sbuf_tensor("sb_" + name, shape, dt)
    P_ = nc.alloc_psum_tensor
    dI = lambda name, shape: nc.dram_tensor(name, shape, F32, kind="ExternalInput").ap()
    ysl = dI("ysl", [8, CAP, D]); oh = dI("oh", [TPC, 8]); x1 = dI("x1", [TPC, D]); iotad = dI("iota", [128, CAP])
    g2b = dI("g2b", [128, D]); ln2g = dI("ln2g", [128, D]); ln2b = dI("ln2b", [128, D])
    out = nc.dram_tensor("out", [TPC, D], F32, kind="ExternalOutput").ap()
    y2d = nc.dram_tensor("y2d", [TPC, D], F32).ap()
    T = Trk(nc)
    ohs, rank = _rank_setup(nc, T, A, P_, oh, TPC)
    iota = A("iota", [128, CAP], F32)
    T.op("sp", lambda e: e.dma_start(out=iota[:], in_=iotad[:, :]), writes=["iota"], dma=True)
    identb = make_ident(nc, T, BF16, "identb")
    yh = A("yh", [128, 32, 1024], BF16)
    Pt = A("Pt", [128, 8, CAP], BF16)
    PT = [A("PT%d" % i, [128, 32, 128], BF16) for i in range(2)]
    yo = [A("yo%d" % i, [128, 1024], F32) for i in range(2)]
    tps = [P_("tps%d" % i, [128, 8, 128], BF16) for i in range(2)]
    acc = [P_("acc%d" % i, [128, 512], F32) for i in range(2)]
    ysv = ysl.rearrange("g (st p) d -> p g st d", p=128)
    tn = 0
    an = 0
    for h in range(2):
        for g in range(8):
            T.op("pool", lambda e: e.dma_start(out=yh[:, g * 4:(g + 1) * 4, :], in_=ysv[:, g, :, h * 1024:(h + 1) * 1024]), writes=["yh"], dma=True)
        for c in range(NT):
            ptb = PT[c % 2]
            pk = "PT%d" % (c % 2)
            for g in range(8):
                T.op("dve", lambda e: e.tensor_scalar(out=Pt[:, g, :], in0=iota[:], scalar1=rank[:, c, g:g + 1], scalar2=ohs[:, c, g:g + 1],
                                                      op0=ALU.is_equal, op1=ALU.mult), reads=["iota", "rank", "ohs"], writes=["Pt"])
            for q in range(4):
                tb = tn % 2
                tn += 1
                for j in range(8):
                    gi = q * 8 + j
                    T.op("pe", lambda e: e.transpose(out=tps[tb][:, j, :], in_=Pt[:, gi // 4, (gi % 4) * 128:(gi % 4 + 1) * 128], identity=identb[:]),
                         reads=["Pt", "identb"], writes=["tps%d" % tb])
                T.op("act", lambda e: e.copy(out=ptb[:, q * 8:(q + 1) * 8, :], in_=tps[tb][:]), writes=[pk, "tps%d" % tb])
            yb = yo[c % 2]
            for dq in range(2):
                a = an % 2
                an += 1
                for gi in range(32):
                    T.op("pe", lambda e: e.matmul(acc[a][:], lhsT=ptb[:, gi, :], rhs=yh[:, gi, dq * 512:(dq + 1) * 512], start=(gi == 0), stop=(gi == 31)),
                         reads=[pk, "yh"], writes=["acc%d" % a])
                T.op("dve", lambda e: e.tensor_copy(out=yb[:, dq * 512:(dq + 1) * 512], in_=acc[a][:]), writes=["yo%d" % (c % 2), "acc%d" % a])
            T.op("sp", lambda e: e.dma_start(out=y2d[c * 128:(c + 1) * 128, h * 1024:(h + 1) * 1024], in_=yb[:]), reads=["yo%d" % (c % 2)], writes=["y2d%d" % c], dma=True)
    g2p = A("g2p", [128, D], F32); lg_ = A("lng", [128, D], F32); lb_ = A("lnb", [128, D], F32)
    T.op("sp", lambda e: e.dma_start(out=g2p[:], in_=g2b[:, :]), writes=["g2p"], dma=True)
    T.op("sp", lambda e: e.dma_start(out=lg_[:], in_=ln2g[:, :]), writes=["lng"], dma=True)
    T.op("sp", lambda e: e.dma_start(out=lb_[:], in_=ln2b[:, :]), writes=["lnb"], dma=True)
    T.op("dve", lambda e: e.tensor_scalar(out=g2p[:], in0=g2p[:], scalar1=1.0, scalar2=None, op0=ALU.add), reads=["g2p"], writes=["g2p"])
    xt = [A("xt%d" % i, [128, D], F32) for i in range(2)]
    y2 = [A("y2%d" % i, [128, D], F32) for i in range(2)]
    junk = A("junk", [128, D], BF16)
    st = A("st", [128, 16], F32)
    for c in range(NT):
        b = c % 2
        T.op("sp", lambda e: e.dma_start(out=xt[b][:], in_=x1[c * 128:(c + 1) * 128, :]), writes=["xt%d" % b], dma=True)
        T.op("sp", lambda e: e.dma_start(out=y2[b][:], in_=y2d[c * 128:(c + 1) * 128, :]), reads=["y2d%d" % c], writes=["y2%d" % b], dma=True)
        v = y2[b]
        vk = "y2%d" % b
        T.op("dve", lambda e: e.tensor_tensor(out=v[:], in0=v[:], in1=g2p[:], op=ALU.mult), reads=[vk, "g2p"], writes=[vk])
        T.op("dve", lambda e: e.scalar_tensor_tensor(out=v[:], in0=xt[b][:], scalar=ALPHA, in1=v[:], op0=ALU.mult, op1=ALU.add), reads=["xt%d" % b, vk], writes=[vk])
        T.op("dve", lambda e: e.reduce_sum(out=st[:, 0:1], in_=v[:], axis=AX.X), reads=[vk], writes=["st0"])
        T.op("act", lambda e: e.activation(out=junk[:], in_=v[:], func=AF.Square, accum_out=st[:, 1:2]), reads=[vk], writes=["st1", "junk"])
        T.op("dve", lambda e: e.tensor_scalar(out=st[:, 2:3], in0=st[:, 0:1], scalar1=1.0 / D, scalar2=None, op0=ALU.mult), reads=["st0"], writes=["st2"])
        T.op("dve", lambda e: e.tensor_tensor(out=st[:, 3:4], in0=st[:, 2:3], in1=st[:, 2:3], op=ALU.mult), reads=["st2"], writes=["st3"])
        T.op("dve", lambda e: e.scalar_tensor_tensor(out=st[:, 4:5], in0=st[:, 1:2], scalar=1.0 / D, in1=st[:, 3:4], op0=ALU.mult, op1=ALU.subtract),
             reads=["st1", "st3"], writes=["st4"])
        T.op("dve", lambda e: e.tensor_scalar(out=st[:, 6:7], in0=st[:, 4:5], scalar1=LN_EPS, scalar2=None, op0=ALU.add), reads=["st4"], writes=["st6"])
        T.op("act", lambda e: e.activation(out=st[:, 7:8], in_=st[:, 6:7], func=AF.Sqrt), reads=["st6"], writes=["st7"])
        T.op("dve", lambda e: e.reciprocal(out=st[:, 5:6], in_=st[:, 7:8]), reads=["st7"], writes=["st5"])
        T.op("dve", lambda e: e.tensor_scalar(out=v[:], in0=v[:], scalar1=st[:, 2:3], scalar2=st[:, 5:6], op0=ALU.subtract, op1=ALU.mult),
             reads=[vk, "st2", "st5"], writes=[vk])
        T.op("dve", lambda e: e.tensor_tensor(out=v[:], in0=v[:], in1=lg_[:], op=ALU.mult), reads=[vk, "lng"], writes=[vk])
        T.op("dve", lambda e: e.tensor_tensor(out=v[:], in0=v[:], in1=lb_[:], op=ALU.add), reads=[vk, "lnb"], writes=[vk])
        T.op("sp", lambda e: e.dma_start(out=out[c * 128:(c + 1) * 128, :], in_=v[:]), reads=[vk], writes=["out"], dma=True)
    T.finish("sp")
    print("COMB n_ins", T.n_ins, "n_wait", T.n_wait)
    return nc


def run_moe(xn2, rw, oh, x1, modrow, w_gate, w_up, w_down, ln2_g, ln2_b, TPC=TPC, cores=NCORES, ne=8):
    rep = lambda a: np.ascontiguousarray(np.broadcast_to(a.reshape(1, -1), (128, a.size))).astype(np.float32)
    iota = rep(np.arange(CAP, dtype=np.float32))
    mod2 = np.ascontiguousarray(np.concatenate([modrow[3 * D:4 * D].reshape(16, 128).T, modrow[4 * D:5 * D].reshape(16, 128).T], axis=1))
    nc1 = build_dispatch(TPC)
    maps = [{"xn2": np.ascontiguousarray(xn2[c * TPC:(c + 1) * TPC]), "oh": np.ascontiguousarray(oh[c * TPC:(c + 1) * TPC]),
             "rw": np.ascontiguousarray(rw[c * TPC:(c + 1) * TPC]), "mod2": mod2, "iota": iota} for c in range(cores)]
    r1 = _launch("dispatch", nc1, maps, cores).results
    nc2 = build_experts(cores * CAP, ne)
    maps2 = []
    for g in range(8):
        maps2.append({"xs": np.ascontiguousarray(np.concatenate([r1[c]["xs"][g] for c in range(cores)], axis=1)),
                      "rws": np.ascontiguousarray(np.concatenate([r1[c]["rws"][g] for c in range(cores)], axis=0)),
                      "wg": np.ascontiguousarray(w_gate[0][g * 8:g * 8 + ne]), "wu": np.ascontiguousarray(w_up[0][g * 8:g * 8 + ne]),
                      "wd": np.ascontiguousarray(w_down[0][g * 8:g * 8 + ne])})
    r2 = _launch("experts", nc2, maps2, 8).results
    nc3 = build_combine(TPC)
    maps3 = []
    for c in range(cores):
        maps3.append({"ysl": np.ascontiguousarray(np.stack([r2[g]["ys"][c * CAP:(c + 1) * CAP] for g in range(8)], axis=0)),
                      "oh": np.ascontiguousarray(oh[c * TPC:(c + 1) * TPC]), "x1": np.ascontiguousarray(x1[c * TPC:(c + 1) * TPC]), "iota": iota,
                      "g2b": rep(modrow[5 * D:6 * D]), "ln2g": rep(ln2_g[0]), "ln2b": rep(ln2_b[0])})
    r3 = _launch("combine", nc3, maps3, cores).results
    return np.concatenate([r["out"] for r in r3], axis=0)


def build_mod():
    nc = bass.Bass("TRN2", target_bir_lowering=False)
    cT = nc.dram_tensor("cT", [128, 16], F32, kind="ExternalInput").ap()
    wada = nc.dram_tensor("wada", [D, 1536], F32, kind="ExternalInput").ap()
    badaT = nc.dram_tensor("badaT", [128, 12], F32, kind="ExternalInput").ap()
    modT = nc.dram_tensor("modT", [128, 12], F32, kind="ExternalOutput").ap()
    T = Trk(nc)
    A = lambda name, shape, dt: nc.alloc_sbuf_tensor("sb_" + name, shape, dt)
    wst = [A("wst%d" % i, [128, 16, 256], F32) for i in range(2)]
    cs = A("cs", [128, 16], F32); ca = A("ca", [128, 16], F32); bad = A("bad", [128, 12], F32); mo = A("mo", [128, 12], F32)
    modps = nc.alloc_psum_tensor("modps", [128, 512], F32)
    T.op("sp", lambda e: e.dma_start(out=cs[:], in_=cT[:, :]), writes=["cs"], dma=True)
    T.op("sp", lambda e: e.dma_start(out=bad[:], in_=badaT[:, :]), writes=["bad"], dma=True)
    T.op("act", lambda e: e.activation(out=ca[:], in_=cs[:], func=AF.Silu), reads=["cs"], writes=["ca"])
    wv = wada.rearrange("(k p) n -> p k n", p=128)
    for g in range(6):
        b = g % 2
        T.op("sp", lambda e: e.dma_start(out=wst[b][:], in_=wv[:, :, g * 256:(g + 1) * 256]), writes=["wst%d" % b], dma=True)
        for j in range(2):
            n = g * 2 + j
            for k in range(16):
                T.op("pe", lambda e: e.matmul(modps[:, n:n + 1], lhsT=wst[b][:, k, j * 128:(j + 1) * 128], rhs=ca[:, k:k + 1], start=(k == 0), stop=(k == 15)),
                     reads=["wst%d" % b, "ca"], writes=["modps"])
    T.op("dve", lambda e: e.tensor_tensor(out=mo[:], in0=modps[:, 0:12], in1=bad[:], op=ALU.add), reads=["bad"], writes=["mo", "modps"])
    T.op("sp", lambda e: e.dma_start(out=modT[:, :], in_=mo[:]), reads=["mo"], writes=["modT"], dma=True)
    T.finish("sp")
    return nc


def run_mod(c, w_ada, b_ada):
    nc = build_mod()
    cT = np.ascontiguousarray(c.reshape(16, 128).T)
    maps = [{"cT": cT, "wada": np.ascontiguousarray(w_ada[0][:, i * 1536:(i + 1) * 1536]),
             "badaT": np.ascontiguousarray(b_ada[0][i * 1536:(i + 1) * 1536].reshape(12, 128).T)} for i in range(NCORES)]
    res = _launch("mod", nc, maps, NCORES)
    return np.concatenate([r["modT"].T.reshape(-1) for r in res.results])


def kernel(x, c, w_ada, b_ada, w_in, b_fgt, t5_table, cmp_pe, cmp_w1, cmp_b1, cmp_w2, w_br_nsa, w_br_fox, w_o, ln1_g, ln1_b,
           w_rg, b_rg, w_re, b_re, w_gate, w_up, w_down, ln2_g, ln2_b):
    f = lambda a: np.asarray(a, dtype=np.float32)
    x, c, w_ada, b_ada, w_in = f(x), f(c), f(w_ada), f(b_ada), f(w_in)
    modrow = run_mod(c, w_ada, b_ada)
    proj, projb = run_l1(x, modrow, w_in)
    o_fox = run_fox(proj, projb, f(b_fgt))
    o_nsa = run_nsa(proj, projb, f(t5_table), f(cmp_pe), f(cmp_w1), f(cmp_b1), f(cmp_w2))
    x1, xn2, rw, oh = run_l3a(o_nsa, o_fox, proj, x, modrow, f(w_br_nsa), f(w_br_fox), f(w_o), f(ln1_g), f(ln1_b), f(w_rg), f(b_rg), f(w_re), f(b_re))
    out = run_moe(xn2, rw, oh, x1, modrow, f(w_gate), f(w_up), f(w_down), f(ln2_g), f(ln2_b))
    return out.reshape(1, S, D).astype(np.float32)
```

```python
import numpy as np
import concourse.bass as bass
import concourse.mybir as mybir
from concourse.bass_utils import run_bass_kernel_spmd

F32 = mybir.dt.float32
BF16 = mybir.dt.bfloat16
AF = mybir.ActivationFunctionType
ALU = mybir.AluOpType
AX = mybir.AxisListType

NCORES = 8
D = 2048
S = 16384
TPC = S // NCORES
IN_COLS = 6944
LN_EPS = 1e-5


def _launch(name, nc, maps, cores):
    res = run_bass_kernel_spmd(nc, maps, core_ids=list(range(cores)))
    try:
        if getattr(res, "exec_time_ns", None) is not None:
            print("[launch] %s exec_time_ns=%s" % (name, res.exec_time_ns), flush=True)
    except Exception:
        pass
    return res


class Trk:
    def __init__(self, nc, n_dma_sems=14):
        self.nc = nc
        self.eng = {"pe": nc.tensor, "act": nc.scalar, "dve": nc.vector,
                    "pool": nc.gpsimd, "sp": nc.sync}
        self.sem = {}
        self.cnt = {}
        self.waited = {k: {} for k in self.eng}
        self._ctx = []
        for k in ["pe", "act", "dve", "pool"]:
            g = nc.semaphore("s_" + k)
            self.sem[k] = g.__enter__()
            self._ctx.append(g)
            self.cnt[k] = 0
        self.dsem = []
        self.dcnt = []
        self.dq = {}
        for q in ["sp", "act", "pool"]:
            self.dq[q] = [len(self.dsem) + j for j in range(n_dma_sems)]
            for j in range(n_dma_sems):
                g = nc.semaphore("s_dma_%s%d" % (q, j))
                self.dsem.append(g.__enter__())
                self._ctx.append(g)
                self.dcnt.append(0)
        self.dqn = {"sp": 0, "act": 0, "pool": 0}
        self.dnext = 0
        self.st = {}
        self.n_ins = 0
        self.n_wait = 0

    def _semobj(self, sk):
        return self.sem[sk] if isinstance(sk, str) else self.dsem[sk]

    def _wait(self, e, deps):
        best = {}
        for d in deps:
            if d is None:
                continue
            sk, v = d
            if v > best.get(sk, 0):
                best[sk] = v
        for sk, v in best.items():
            if e == "pe" and sk == "pe":
                continue
            if self.waited[e].get(sk, 0) >= v:
                continue
            self.eng[e].wait_ge(self._semobj(sk), v)
            self.waited[e][sk] = v
            self.n_wait += 1

    def op(self, e, fn, reads=(), writes=(), dma=False):
        deps = []
        for k in reads:
            s = self.st.get(k)
            if s is not None:
                deps.append(s[0])
        for k in writes:
            s = self.st.get(k)
            if s is not None:
                deps.append(s[0])
                deps.extend(s[1])
        if dma:
            i = self.dq[e][self.dqn[e] % len(self.dq[e])]
            self.dqn[e] += 1
            if self.dcnt[i] > 0:
                deps.append((i, self.dcnt[i]))
        self._wait(e, deps)
        ins = fn(self.eng[e])
        if dma:
            self.dcnt[i] += 16
            ins.then_inc(self.dsem[i], 16)
            tag = (i, self.dcnt[i])
        else:
            self.cnt[e] += 1
            ins.then_inc(self.sem[e], 1)
            tag = (e, self.cnt[e])
        for k in reads:
            s = self.st.setdefault(k, [None, []])
            s[1].append(tag)
            if len(s[1]) > 48:
                best = {}
                for sk, v in s[1]:
                    if v > best.get(sk, 0):
                        best[sk] = v
                s[1] = list(best.items())
        for k in writes:
            self.st[k] = [tag, []]
        self.n_ins += 1
        return tag

    def finish(self, e="sp"):
        deps = []
        for k, s in self.st.items():
            deps.append(s[0])
            deps.extend(s[1])
        self._wait(e, deps)


def make_ident(nc, T, dtype, name="ident"):
    ident = nc.alloc_sbuf_tensor(name, [128, 128], dtype)
    T.op("pool", lambda e: e.memset(ident[:], 1.0), writes=[name])
    T.op("pool", lambda e: e.affine_select(out=ident[:], in_=ident[:], pattern=[[-1, 128]],
                                           compare_op=ALU.is_equal, fill=0.0, base=0,
                                           channel_multiplier=1), reads=[name], writes=[name])
    return ident


def build_l1(stop=None, TPC=TPC):
    nc = bass.Bass("TRN2", target_bir_lowering=False)
    x = nc.dram_tensor("x", [TPC, D], F32, kind="ExternalInput").ap()
    cT = nc.dram_tensor("cT", [128, 16], F32, kind="ExternalInput").ap()
    wada = nc.dram_tensor("wada", [D, 8], F32, kind="ExternalInput").ap()
    badaT = nc.dram_tensor("badaT", [128, 32], F32, kind="ExternalInput").ap()
    w_in = nc.dram_tensor("w_in", [D, IN_COLS], F32, kind="ExternalInput").ap()
    proj = nc.dram_tensor("proj", [TPC, IN_COLS], F32, kind="ExternalOutput").ap()
    projb = nc.dram_tensor("projb", [TPC, 3072], BF16, kind="ExternalOutput").ap()
    T = Trk(nc)
    NT = TPC // 128
    obf = [nc.alloc_sbuf_tensor("obf%d" % i, [128, 512], BF16) for i in range(2)]

    wst = [nc.alloc_sbuf_tensor("wst%d" % i, [128, 8, 512], F32) for i in range(2)]
    wbf = [nc.alloc_sbuf_tensor("wbf%d" % i, [128, 16, 512], BF16) for i in range(2)]
    uT = nc.alloc_sbuf_tensor("uT", [128, 16, TPC], BF16)
    xt = [nc.alloc_sbuf_tensor("xt%d" % i, [128, D], F32) for i in range(2)]
    xn = nc.alloc_sbuf_tensor("xn", [128, D], BF16)
    junk = nc.alloc_sbuf_tensor("junk", [128, D], BF16)
    ost = [nc.alloc_sbuf_tensor("ost%d" % i, [128, 512], F32) for i in range(4)]
    cs = nc.alloc_sbuf_tensor("cs", [128, 16], F32)
    ca = nc.alloc_sbuf_tensor("ca", [128, 16], F32)
    bad = nc.alloc_sbuf_tensor("bad", [128, 32], F32)
    modT = nc.alloc_sbuf_tensor("modT", [128, 32], F32)
    st = nc.alloc_sbuf_tensor("st", [128, 16], F32)
    tp = [nc.alloc_psum_tensor("tp%d" % i, [128, 8, 128], BF16) for i in range(2)]
    acc = [nc.alloc_psum_tensor("acc%d" % i, [128, 512], F32) for i in range(4)]
    modps = nc.alloc_psum_tensor("modps", [128, 512], F32)
    ident = make_ident(nc, T, BF16)

    T.op("sp", lambda e: e.dma_start(out=modT[:], in_=badaT[:, :]), writes=["modT"], dma=True)
    T.op("dve", lambda e: e.tensor_scalar(out=modT[:, 16:32], in0=modT[:, 16:32], scalar1=1.0, scalar2=None, op0=ALU.add),
         reads=["modT"], writes=["modT"])
    ld = 0

    if stop == "A":
        T.op("sp", lambda e: e.dma_start(out=proj[0:128, 0:32], in_=modT[:]), reads=["modT"], writes=["proj"], dma=True)
        T.finish("sp")
        return nc
    def ld_x(i_):
        b_ = i_ % 2
        T.op("sp", lambda e: e.dma_start(out=xt[b_][:], in_=x[i_ * 128:(i_ + 1) * 128, :]), writes=["xt%d" % b_], dma=True)
    ld_x(0)
    for i in range(NT):
        b = i % 2
        if i + 1 < NT:
            ld_x(i + 1)
        T.op("dve", lambda e: e.reduce_sum(out=st[:, 0:1], in_=xt[b][:], axis=AX.X), reads=["xt%d" % b], writes=["st0"])
        T.op("act", lambda e: e.activation(out=junk[:], in_=xt[b][:], func=AF.Square, accum_out=st[:, 1:2]),
             reads=["xt%d" % b], writes=["st1", "junk"])
        T.op("dve", lambda e: e.tensor_scalar(out=st[:, 2:3], in0=st[:, 0:1], scalar1=1.0 / D, scalar2=None, op0=ALU.mult),
             reads=["st0"], writes=["st2"])
        T.op("dve", lambda e: e.tensor_tensor(out=st[:, 3:4], in0=st[:, 2:3], in1=st[:, 2:3], op=ALU.mult),
             reads=["st2"], writes=["st3"])
        T.op("dve", lambda e: e.scalar_tensor_tensor(out=st[:, 4:5], in0=st[:, 1:2], scalar=1.0 / D, in1=st[:, 3:4],
                                                     op0=ALU.mult, op1=ALU.subtract), reads=["st1", "st3"], writes=["st4"])
        T.op("dve", lambda e: e.tensor_scalar(out=st[:, 6:7], in0=st[:, 4:5], scalar1=LN_EPS, scalar2=None, op0=ALU.add),
             reads=["st4"], writes=["st6"])
        T.op("act", lambda e: e.activation(out=st[:, 7:8], in_=st[:, 6:7], func=AF.Sqrt), reads=["st6"], writes=["st7"])
        T.op("dve", lambda e: e.reciprocal(out=st[:, 5:6], in_=st[:, 7:8]), reads=["st7"], writes=["st5"])
        T.op("dve", lambda e: e.tensor_scalar(out=xn[:], in0=xt[b][:], scalar1=st[:, 2:3], scalar2=st[:, 5:6],
                                              op0=ALU.subtract, op1=ALU.mult), reads=["xt%d" % b, "st2", "st5"], writes=["xn"])
        for kq in range(2):
            pb = kq % 2
            for j in range(8):
                k = kq * 8 + j
                T.op("pe", lambda e: e.transpose(out=tp[pb][:, j, :], in_=xn[:, k * 128:(k + 1) * 128], identity=ident[:]),
                     reads=["xn", "ident"], writes=["tp%d" % pb])
            for j in range(8):
                k = kq * 8 + j
                T.op("act", lambda e: e.activation(out=uT[:, k, i * 128:(i + 1) * 128], in_=tp[pb][:, j, :], func=AF.Identity,
                                                   scale=modT[:, 16 + k:17 + k], bias=modT[:, k:k + 1]),
                     reads=["modT"], writes=["uT_%d" % i, "tp%d" % pb])

    if stop in ("B", "B1", "B2"):
        T.op("sp", lambda e: e.dma_start(out=proj[0:128, 0:32], in_=modT[:]), reads=["modT"], writes=["proj"], dma=True)
        T.finish("sp")
        return nc
    w_v = w_in.rearrange("(k p) n -> p k n", p=128)
    ngrp = (IN_COLS + 511) // 512
    ev = 0
    for g in range(ngrp):
        c0 = g * 512
        cw = min(512, IN_COLS - c0)
        wb = g % 2
        for kh in range(2):
            b = ld % 2
            ld += 1
            T.op("sp", lambda e: e.dma_start(out=wst[b][:, :, 0:cw], in_=w_v[:, kh * 8:(kh + 1) * 8, c0:c0 + cw]),
                 writes=["wst%d" % b], dma=True)
            T.op("pool", lambda e: e.tensor_copy(out=wbf[wb][:, kh * 8:(kh + 1) * 8, 0:cw], in_=wst[b][:, :, 0:cw]),
                 reads=["wst%d" % b], writes=["wbf%d_%d" % (wb, kh)])
        for i in range(NT):
            a = ev % 4
            for k in range(16):
                T.op("pe", lambda e: e.matmul(acc[a][:, 0:cw], lhsT=uT[:, k, i * 128:(i + 1) * 128], rhs=wbf[wb][:, k, 0:cw],
                                              start=(k == 0), stop=(k == 15)),
                     reads=["uT_%d" % i, "wbf%d_%d" % (wb, k // 8)], writes=["acc%d" % a])
            if ev % 2 == 0:
                T.op("act", lambda e: e.copy(out=ost[a][:, 0:cw], in_=acc[a][:, 0:cw]), writes=["ost%d" % a, "acc%d" % a])
            else:
                T.op("dve", lambda e: e.tensor_copy(out=ost[a][:, 0:cw], in_=acc[a][:, 0:cw]), writes=["ost%d" % a, "acc%d" % a])
            T.op("pool", lambda e: e.dma_start(out=proj[i * 128:(i + 1) * 128, c0:c0 + cw], in_=ost[a][:, 0:cw]),
                 reads=["ost%d" % a], writes=["proj"], dma=True)
            if g < 6:
                ob = obf[ev % 2]
                if ev % 2 == 0:
                    T.op("dve", lambda e: e.tensor_copy(out=ob[:], in_=acc[a][:]), writes=["obf%d" % (ev % 2), "acc%d" % a])
                else:
                    T.op("act", lambda e: e.copy(out=ob[:], in_=acc[a][:]), writes=["obf%d" % (ev % 2), "acc%d" % a])
                T.op("sp", lambda e: e.dma_start(out=projb[i * 128:(i + 1) * 128, c0:c0 + 512], in_=ob[:]),
                     reads=["obf%d" % (ev % 2)], writes=["projb"], dma=True)
            ev += 1
    T.finish("sp")
    T.finish("pool")
    print("L1 n_ins", T.n_ins, "n_wait", T.n_wait)
    return nc


def run_l1(x, modrow, w_in, stop=None, TPC=TPC, NCORES=NCORES):
    nc = build_l1(stop, TPC)
    x2 = np.ascontiguousarray(x.reshape(S, D))
    mod1 = np.ascontiguousarray(np.concatenate([modrow[0:D].reshape(16, 128).T, modrow[D:2 * D].reshape(16, 128).T], axis=1))
    w = np.ascontiguousarray(w_in[0])
    dummy = np.zeros((128, 16), np.float32)
    in_maps = [{"x": x2[i * TPC:(i + 1) * TPC], "cT": dummy, "wada": np.zeros((D, 8), np.float32), "badaT": mod1, "w_in": w} for i in range(NCORES)]
    res = _launch("l1", nc, in_maps, NCORES)
    return np.concatenate([r["proj"] for r in res.results], axis=0), np.concatenate([r["projb"] for r in res.results], axis=0)


def build_masks(nc, T, name="cmask"):
    mk = nc.alloc_sbuf_tensor(name, [128, 4, 512], BF16)
    T.op("pool", lambda e: e.memset(mk[:], 0.0), writes=[name])
    for j in range(4):
        T.op("pool", lambda e: e.affine_select(out=mk[:, j, :], in_=mk[:, j, :], pattern=[[1, 512]],
                                               compare_op=ALU.is_ge, fill=-30000.0, base=-128 * j,
                                               channel_multiplier=-1), reads=[name], writes=[name])
    return mk


def build_fox(Sx=S):
    nc = bass.Bass("TRN2", target_bir_lowering=False)
    NKB = Sx // 128
    NQC = Sx // 512
    qT = nc.dram_tensor("qT", [64, Sx], BF16, kind="ExternalInput").ap()
    kT = nc.dram_tensor("kT", [64, Sx], BF16, kind="ExternalInput").ap()
    v = nc.dram_tensor("v", [Sx, 64], BF16, kind="ExternalInput").ap()
    f2 = nc.dram_tensor("f2", [128, NKB], F32, kind="ExternalInput").ap()
    bfg = nc.dram_tensor("bfg", [128, 1], F32, kind="ExternalInput").ap()
    o = nc.dram_tensor("o", [Sx, 64], F32, kind="ExternalOutput").ap()
    scr = nc.dram_tensor("scr", [2, Sx], BF16).ap()
    T = Trk(nc)

    KTa = nc.alloc_sbuf_tensor("KTa", [128, Sx], BF16)
    QTa = nc.alloc_sbuf_tensor("QTa", [128, Sx], BF16)
    Vp = nc.alloc_sbuf_tensor("Vp", [128, NKB, 65], BF16)
    PT = [nc.alloc_sbuf_tensor("PT%d" % i, [128, 512], BF16) for i in range(4)]
    ost = [nc.alloc_sbuf_tensor("ost%d" % i, [128, 4, 64], F32) for i in range(2)]
    rc = nc.alloc_sbuf_tensor("rc", [128, 4], F32)
    fz = nc.alloc_sbuf_tensor("fz", [128, NKB], F32)
    lf = nc.alloc_sbuf_tensor("lf", [128, NKB], F32)
    nb = nc.alloc_sbuf_tensor("nb", [128, 1], F32)
    Usb = nc.alloc_sbuf_tensor("Usb", [128, 128], F32)
    SUsb = nc.alloc_sbuf_tensor("SUsb", [128, 128], F32)
    ones = nc.alloc_sbuf_tensor("ones", [128, 128], F32)
    totT = nc.alloc_sbuf_tensor("totT", [128, 128], F32)
    Fsb = nc.alloc_sbuf_tensor("Fsb", [128, NKB], F32)
    Fofs = nc.alloc_sbuf_tensor("Fofs", [128, NKB], F32)
    Gq = nc.alloc_sbuf_tensor("Gq", [128, NKB], F32)
    Ghi = nc.alloc_sbuf_tensor("Ghi", [128, 128], BF16)
    Ghf = nc.alloc_sbuf_tensor("Ghf", [128, NKB], F32)
    Glo = nc.alloc_sbuf_tensor("Glo", [128, 128], BF16)
    GT = nc.alloc_sbuf_tensor("GT", [128, 2, 128], BF16)
    bm = [nc.alloc_sbuf_tensor("bm%d" % i, [128, NKB], F32) for i in range(2)]
    Sps = [nc.alloc_psum_tensor("Sps%d" % i, [128, 512], F32) for i in range(3)]
    Ops = [nc.alloc_psum_tensor("Ops%d" % i, [128, 4, 128], F32) for i in range(2)]
    Fps = nc.alloc_psum_tensor("Fps", [128, 512], F32)
    Tps = nc.alloc_psum_tensor("Tps", [128, 8, 128], BF16)
    identb = make_ident(nc, T, BF16, "identb")
    mk = build_masks(nc, T)

    T.op("sp", lambda e: e.dma_start(out=fz[:], in_=f2[:, :]), writes=["fz"], dma=True)
    T.op("sp", lambda e: e.dma_start(out=nb[:], in_=bfg[:, :]), writes=["nb"], dma=True)
    for h in range(0, Sx, 2048):
        w = min(2048, Sx - h)
        T.op("sp", lambda e: e.dma_start(out=KTa[0:64, h:h + w], in_=kT[:, h:h + w]), writes=["KTa"], dma=True)
        T.op("act", lambda e: e.dma_start(out=QTa[0:64, h:h + w], in_=qT[:, h:h + w]), writes=["QTa"], dma=True)
    vv = v.rearrange("(kb p) d -> p kb d", p=128)
    for h in range(0, NKB, 16):
        w = min(16, NKB - h)
        T.op("sp", lambda e: e.dma_start(out=Vp[:, h:h + w, 0:64], in_=vv[:, h:h + w, :]), writes=["Vp"], dma=True)
    T.op("dve", lambda e: e.memset(Vp[:, :, 64:65], 1.0), writes=["Vp1"])
    T.op("dve", lambda e: e.memset(KTa[64:66, :], 8.0), writes=["KTa8"])

    T.op("pool", lambda e: e.memset(ones[:], 1.0), writes=["ones"])
    T.op("pool", lambda e: e.memset(Usb[:], 1.0), writes=["Usb"])
    T.op("pool", lambda e: e.affine_select(out=Usb[:], in_=Usb[:], pattern=[[1, 128]], compare_op=ALU.is_ge, fill=0.0,
                                           base=0, channel_multiplier=-1), reads=["Usb"], writes=["Usb"])
    T.op("pool", lambda e: e.memset(SUsb[:], 1.0), writes=["SUsb"])
    T.op("pool", lambda e: e.affine_select(out=SUsb[:], in_=SUsb[:], pattern=[[1, 128]], compare_op=ALU.is_ge, fill=0.0,
                                           base=-1, channel_multiplier=-1), reads=["SUsb"], writes=["SUsb"])

    T.op("dve", lambda e: e.tensor_scalar(out=nb[:], in0=nb[:], scalar1=-1.0, scalar2=None, op0=ALU.mult), reads=["nb"], writes=["nb"])
    T.op("act", lambda e: e.activation(out=lf[:], in_=fz[:], func=AF.Exp, scale=-1.0, bias=nb[:, 0:1]), reads=["fz", "nb"], writes=["lf"])
    T.op("act", lambda e: e.activation(out=lf[:], in_=lf[:], func=AF.Ln, bias=1.0), reads=["lf"], writes=["lf"])
    T.op("dve", lambda e: e.tensor_scalar(out=lf[:], in0=lf[:], scalar1=-1.0, scalar2=None, op0=ALU.mult), reads=["lf"], writes=["lf"])
    T.op("pe", lambda e: e.matmul(Fps[0:NKB, 0:128], lhsT=lf[:, 0:NKB], rhs=ones[:, :], start=True, stop=True),
         reads=["lf", "ones"], writes=["Fps"])
    T.op("dve", lambda e: e.memset(totT[:], 0.0), writes=["totT"])
    T.op("dve", lambda e: e.tensor_copy(out=totT[0:NKB, :], in_=Fps[0:NKB, 0:128]), writes=["totT", "Fps"])
    T.op("pe", lambda e: e.matmul(Fps[:, 128:128 + NKB], lhsT=totT[:, :], rhs=SUsb[:, 0:NKB], start=True, stop=True),
         reads=["totT", "SUsb"], writes=["Fps"])
    T.op("dve", lambda e: e.tensor_copy(out=Fofs[:], in_=Fps[:, 128:128 + NKB]), writes=["Fofs", "Fps"])
    T.op("pe", lambda e: e.matmul(Fps[:, 256:256 + NKB], lhsT=Usb[:, :], rhs=lf[:, 0:NKB], start=True, stop=True),
         reads=["Usb", "lf"], writes=["Fps"])
    T.op("dve", lambda e: e.tensor_tensor(out=Fsb[:], in0=Fps[:, 256:256 + NKB], in1=Fofs[:], op=ALU.add),
         reads=["Fofs"], writes=["Fsb", "Fps"])
    Gv = Gq[:].rearrange("j (c f) -> j c f", f=4)
    Fv = Fsb[:].rearrange("j (c f) -> j c f", f=4)
    Ov = Fofs[:].rearrange("j (c f) -> j c f", f=4)
    for f in range(4):
        T.op("dve", lambda e: e.tensor_tensor(out=Gv[:, :, f], in0=Fv[:, :, f], in1=Ov[:, :, 0], op=ALU.subtract),
             reads=["Fsb", "Fofs"], writes=["Gq"])
    T.op("dve", lambda e: e.memset(Ghi[:], 0.0), writes=["Ghi"])
    T.op("dve", lambda e: e.memset(Glo[:], 0.0), writes=["Glo"])
    T.op("dve", lambda e: e.tensor_copy(out=Ghi[:, 0:NKB], in_=Gq[:]), reads=["Gq"], writes=["Ghi"])
    T.op("dve", lambda e: e.tensor_copy(out=Ghf[:], in_=Ghi[:, 0:NKB]), reads=["Ghi"], writes=["Ghf"])
    T.op("dve", lambda e: e.tensor_tensor(out=Glo[:, 0:NKB], in0=Gq[:], in1=Ghf[:], op=ALU.subtract), reads=["Gq", "Ghf"], writes=["Glo"])
    T.op("pe", lambda e: e.transpose(out=Tps[:, 0, :], in_=Ghi[:], identity=identb[:]), reads=["Ghi", "identb"], writes=["Tps"])
    T.op("pe", lambda e: e.transpose(out=Tps[:, 1, :], in_=Glo[:], identity=identb[:]), reads=["Glo", "identb"], writes=["Tps"])
    T.op("dve", lambda e: e.tensor_copy(out=GT[:], in_=Tps[:, 0:2, :]), writes=["GT", "Tps"])
    for r in range(2):
        T.op("sp", lambda e: e.dma_start(out=scr[r:r + 1, :].rearrange("o (p j) -> (o p) j", j=128), in_=GT[0:NKB, r, :]),
             reads=["GT"], writes=["scr"], dma=True)
    T.op("sp", lambda e: e.dma_start(out=QTa[64:66, :], in_=scr[:, :]), reads=["scr"], writes=["QTaG"], dma=True)

    it = 0
    for qc in range(NQC):
        ob = qc % 2
        nk = 4 * qc + 4
        bmq = bm[qc % 2]
        T.op("dve", lambda e: e.tensor_scalar(out=bmq[:, 0:nk], in0=Fsb[:, 0:nk], scalar1=-1.0, scalar2=Fofs[:, 4 * qc:4 * qc + 1],
                                              op0=ALU.mult, op1=ALU.add), reads=["Fsb", "Fofs"], writes=["bm%d" % (qc % 2)])
        T.op("dve", lambda e: e.memset(Ops[ob][:], 0.0), writes=["Ops%d" % ob])

        def qk(kb, it):
            sb = it % 3
            j = kb - 4 * qc
            T.op("pe", lambda e: e.matmul(Sps[sb][:], lhsT=KTa[0:66, kb * 128:(kb + 1) * 128], rhs=QTa[0:66, qc * 512:(qc + 1) * 512],
                                          start=True, stop=(j < 0)),
                 reads=["KTa", "KTa8", "QTa", "QTaG"], writes=["Sps%d" % sb])
            if j >= 0:
                T.op("pe", lambda e: e.matmul(Sps[sb][:], lhsT=identb[:], rhs=mk[:, j, :], start=False, stop=True),
                     reads=["identb", "cmask"], writes=["Sps%d" % sb])

        def ex_pv(kb, it):
            sb = it % 3
            pb = it % 4
            j = kb - 4 * qc
            T.op("act", lambda e: e.activation(out=PT[pb][:], in_=Sps[sb][:], func=AF.Exp, scale=0.125, bias=bmq[:, kb:kb + 1]),
                 reads=["bm%d" % (qc % 2)], writes=["PT%d" % pb, "Sps%d" % sb])
            for jj in range(max(j, 0), 4):
                T.op("pe", lambda e: e.matmul(Ops[ob][:, jj, 0:65], lhsT=PT[pb][:, jj * 128:(jj + 1) * 128], rhs=Vp[:, kb, :],
                                              start=False, stop=False, skip_group_check=True),
                     reads=["PT%d" % pb, "Vp", "Vp1"], writes=["Ops%d" % ob])

        qk(0, it)
        if nk > 1:
            qk(1, it + 1)
        for kb in range(nk):
            if kb + 2 < nk:
                qk(kb + 2, it + 2)
            ex_pv(kb, it)
            it += 1
        osb = ost[qc % 2]
        T.op("dve", lambda e: e.reciprocal(out=rc[:], in_=Ops[ob][:, :, 64]), writes=["rc", "Ops%d" % ob])
        for jj in range(4):
            T.op("dve", lambda e: e.tensor_scalar(out=osb[:, jj, :], in0=Ops[ob][:, jj, 0:64], scalar1=rc[:, jj:jj + 1], scalar2=None,
                                                  op0=ALU.mult), reads=["rc"], writes=["ost%d" % (qc % 2), "Ops%d" % ob])
        T.op("sp", lambda e: e.dma_start(out=o[qc * 512:(qc + 1) * 512, :].rearrange("(jj p) d -> p jj d", p=128), in_=osb[:]),
             reads=["ost%d" % (qc % 2)], writes=["o"], dma=True)
    T.finish("sp")
    print("FOX n_ins", T.n_ins, "n_wait", T.n_wait)
    return nc


def fox_inputs(proj, projb, b_fgt, Sx=S):
    OFF_FOX = 512 + 768 + 24
    OFF_FGT = OFF_FOX + 1536
    maps = []
    for h in range(8):
        q = projb[:Sx, OFF_FOX + h * 64:OFF_FOX + (h + 1) * 64]
        k = projb[:Sx, OFF_FOX + 512 + h * 64:OFF_FOX + 512 + (h + 1) * 64]
        v = projb[:Sx, OFF_FOX + 1024 + h * 64:OFF_FOX + 1024 + (h + 1) * 64]
        f = proj[:Sx, OFF_FGT + h]
        maps.append({"qT": np.ascontiguousarray(q.T), "kT": np.ascontiguousarray(k.T), "v": np.ascontiguousarray(v),
                     "f2": np.ascontiguousarray(f.reshape(Sx // 128, 128).T),
                     "bfg": np.full((128, 1), b_fgt[0, h], np.float32)})
    return maps


def run_fox(proj, projb, b_fgt, Sx=S, cores=NCORES):
    nc = build_fox(Sx)
    maps = fox_inputs(proj, projb, b_fgt, Sx)[:cores]
    res = _launch("fox", nc, maps, cores)
    return np.concatenate([r["o"] for r in res.results], axis=1)


BIGNEG = -30000.0


def build_nsa(Sx=S):
    nc = bass.Bass("TRN2", target_bir_lowering=False)
    NKB = Sx // 128
    NB = Sx // 512
    NCT = max(1, Sx // 2048)
    NCMP = Sx // 16
    NJ = Sx // 64
    dI = lambda name, shape: nc.dram_tensor(name, shape, F32, kind="ExternalInput").ap()
    A = lambda name, shape, dt: nc.alloc_sbuf_tensor("sb_" + name, shape, dt)
    dB = lambda name, shape: nc.dram_tensor(name, shape, BF16, kind="ExternalInput").ap()
    qT4 = dB("qT4", [64, NB, 512])
    kc_s = dB("kc_s", [64, Sx]); vc_s = dB("vc_s", [64, Sx])
    kslcT = dB("kslcT", [64, Sx]); kwinT = dB("kwinT", [64, Sx])
    vslc1 = dB("vslc1", [Sx, 65]); vwin1 = dB("vwin1", [Sx, 65])
    gates = dI("gates", [128, NB * 12])
    w1d = dI("w1", [64, 2 * 32 * 256]); b1T = dI("b1T", [128, 4]); w2d = dI("w2", [128, 4 * 64]); peT = dI("peT", [64, 64])
    D0d = dI("D0", [128, 512]); D1d = dI("D1", [128, 512]); CBd = dI("CB", [128, 18 * 512]); Cfd = dI("Cfull", [128, 512])
    crow = dI("crow", [1, 512])
    wseld = dI("wsel", [128, NCT * NJ]); cvald = dI("cvalid", [128, NCT]); f0d = dI("force0", [128, NJ])
    o = nc.dram_tensor("o", [NB * 128, 256], F32, kind="ExternalOutput").ap()
    T = Trk(nc)
    bufA = A("bufA", [128, Sx], BF16)
    bufB = A("bufB", [128, Sx], BF16)
    bufC = A("bufC", [128, max(2 * NKB * 65, 16384)], BF16)
    w1v = bufC[0:64, 0:16384].rearrange("d (z l h) -> d z l h", z=2, l=32)
    Vs = bufC[:, 0:NKB * 65].rearrange("p (k c) -> p k c", c=65)
    Vw = bufC[:, NKB * 65:2 * NKB * 65].rearrange("p (k c) -> p k c", c=65)
    E = A("E", [128, 64, 128], BF16)
    CB = A("CB", [128, 18, 512], BF16)
    D0 = A("D0", [128, 512], BF16); D1 = A("D1", [128, 512], BF16); W4 = A("W4", [128, 512], BF16)
    hidT = A("hidT", [128, 2, 2, NCMP], BF16)
    KcT = A("KcT", [128, NCMP], BF16)
    Vc = A("Vc", [128, NCT, 65], BF16)
    wsel = A("wsel", [128, NCT, NJ], BF16)
    cval = A("cval", [128, NCT], F32)
    f0 = A("f0", [128, NJ], F32)
    w2 = A("w2", [128, 2, 2, 64], BF16)
    b1 = A("b1", [128, 4], F32); cb = A("cb", [128, 4], F32)
    pe = A("pe", [64, 2, 32], BF16)
    gts = A("gts", [128, NB, 4, 3], F32)
    QT = [A("QT%d" % i, [128, 512], BF16) for i in range(2)]
    PT = [A("PT%d" % i, [128, 512], BF16) for i in range(4)]
    xs = A("xs", [128, 512], F32); x2 = A("x2", [128, 512], F32); sg = A("sg", [128, 512], F32)
    imp = A("imp", [128, NJ], F32); work = A("work", [128, NJ], F32); selm = A("selm", [128, NJ], F32)
    mx8 = A("mx8", [128, 8], F32)
    selbT = A("selbT", [128, 2, 4, 128], BF16)
    rs = A("rs", [128, 3, 4], F32)
    oacc = [A("oacc%d" % i, [128, 4, 64], F32) for i in range(2)]
    vtmp = A("vtmp", [128, 64], F32)
    ident8 = A("ident8", [128, 128], BF16)
    P = nc.alloc_psum_tensor
    Sps = [P("Sps%d" % i, [128, 512], F32) for i in range(3)]
    Ops = [P("Ops%d" % i, [128, 4, 128], F32) for i in range(3)]
    Ips = [P("Ips%d" % i, [128, 2, 256], F32) for i in range(2)]
    identb = make_ident(nc, T, BF16, "identb")
    identf = make_ident(nc, T, F32, "identf")
    T.op("pool", lambda e: e.tensor_scalar(out=ident8[:], in0=identb[:], scalar1=8.0, scalar2=None, op0=ALU.mult),
         reads=["identb"], writes=["ident8"])

    def cast_load(dst, src, key, eng="pool"):
        T.op(eng, lambda e: e.dma_start(out=dst, in_=src), writes=[key], dma=True)

    cast_load(CB[:].rearrange("p r c -> p (r c)"), CBd[:, :], "CB")
    cast_load(D0[:], D0d[:, :], "D0"); cast_load(D1[:], D1d[:, :], "D1"); cast_load(W4[:], Cfd[:, :], "W4")
    cast_load(wsel[:].rearrange("p a b -> p (a b)"), wseld[:, :], "wsel")
    cast_load(w2[:].rearrange("p z c d -> p (z c d)"), w2d[:, :], "w2")
    cast_load(pe[:].rearrange("d z l -> d (z l)"), peT[:, :], "pe")
    cast_load(w1v.rearrange("d z l h -> d (z l h)"), w1d[:, :], "bufC")
    T.op("sp", lambda e: e.dma_start(out=cval[:], in_=cvald[:, :]), writes=["cval"], dma=True)
    T.op("sp", lambda e: e.dma_start(out=f0[:], in_=f0d[:, :]), writes=["f0"], dma=True)
    T.op("sp", lambda e: e.dma_start(out=b1[:], in_=b1T[:, :]), writes=["b1"], dma=True)
    T.op("sp", lambda e: e.dma_start(out=gts[:].rearrange("p a h b -> p (a h b)"), in_=gates[:, :]), writes=["gts"], dma=True)
    T.op("act", lambda e: e.activation(out=gts[:].rearrange("p a h b -> p (a h b)"), in_=gts[:].rearrange("p a h b -> p (a h b)"), func=AF.Sigmoid),
         reads=["gts"], writes=["gts"])
    for i in range(2):
        cast_load(QT[i][64:65, :], crow[:, :], "QTc%d" % i)
    v4 = lambda t: t[:].rearrange("p (h t) -> p h t", h=4)
    T.op("pool", lambda e: e.affine_select(out=v4(D0), in_=v4(D0), pattern=[[0, 4], [1, 128]], compare_op=ALU.is_ge, fill=BIGNEG,
                                           base=0, channel_multiplier=-1), reads=["D0"], writes=["D0"])
    T.op("pool", lambda e: e.affine_select(out=v4(W4), in_=v4(W4), pattern=[[0, 4], [-1, 128]], compare_op=ALU.is_ge, fill=BIGNEG,
                                           base=-1, channel_multiplier=1), reads=["W4"], writes=["W4"])
    for R in range(18):
        cbv = CB[:, R, :].rearrange("p (h t) -> p h t", h=4)
        T.op("pool", lambda e: e.affine_select(out=cbv, in_=cbv, pattern=[[0, 4], [1, 128]], compare_op=ALU.is_ge, fill=BIGNEG,
                                               base=128 * R - 31, channel_multiplier=-16), reads=["CB"], writes=["CB"])
    T.op("pool", lambda e: e.memset(E[:], 1.0), writes=["E"])
    for hs in range(2):
        ev = E[:, :, hs * 64:(hs + 1) * 64]
        T.op("pool", lambda e: e.affine_select(out=ev, in_=ev, pattern=[[-2, 64], [0, 64]], compare_op=ALU.is_equal, fill=0.0,
                                               base=-hs, channel_multiplier=1), reads=["E"], writes=["E"])

    for h in range(0, Sx, 2048):
        w = min(2048, Sx - h)
        cast_load(bufA[0:64, h:h + w], kc_s[:, h:h + w], "bufA", "sp")
        cast_load(bufB[0:64, h:h + w], vc_s[:, h:h + w], "bufB", "act")
    T.op("dve", lambda e: e.memset(hidT[:], 0.0), writes=["hidT"])
    T.op("dve", lambda e: e.memset(KcT[:], 0.0), writes=["KcT"])
    T.op("dve", lambda e: e.memset(KcT[64:65, :], 8.0), reads=[], writes=["KcT"])
    for z in range(2):
        for c2 in range(2):
            col = z * 2 + c2
            for l in range(32):
                T.op("pe", lambda e: e.matmul(Sps[0][:, col:col + 1], lhsT=w1v[:, z, l, c2 * 128:(c2 + 1) * 128], rhs=pe[:, z, l:l + 1],
                                              start=(l == 0), stop=(l == 31)), reads=["bufC", "pe"], writes=["Sps0"])
    T.op("dve", lambda e: e.tensor_tensor(out=cb[:], in0=Sps[0][:, 0:4], in1=b1[:], op=ALU.add), reads=["b1"], writes=["cb", "Sps0"])
    nvalid = NCMP - 1
    src = [bufA, bufB]
    it = 0
    for z in range(2):
        for c2 in range(2):
            for n0 in range(0, nvalid, 512):
                nw = min(512, nvalid - n0)
                sb = it % 2
                it += 1
                for l in range(32):
                    T.op("pe", lambda e: e.matmul(Sps[sb][:, 0:nw], lhsT=w1v[:, z, l, c2 * 128:(c2 + 1) * 128],
                                                  rhs=src[z][0:64, 16 * n0 + l:16 * n0 + l + 16 * (nw - 1) + 1:16],
                                                  start=(l == 0), stop=(l == 31)),
                         reads=["bufC", "bufA" if z == 0 else "bufB"], writes=["Sps%d" % sb])
                col = z * 2 + c2
                T.op("act", lambda e: e.activation(out=xs[:, 0:nw], in_=Sps[sb][:, 0:nw], func=AF.Identity, bias=cb[:, col:col + 1]),
                     reads=["cb"], writes=["xs", "Sps%d" % sb])
                T.op("dve", lambda e: e.tensor_tensor(out=x2[:, 0:nw], in0=xs[:, 0:nw], in1=xs[:, 0:nw], op=ALU.mult), reads=["xs"], writes=["x2"])
                T.op("dve", lambda e: e.tensor_scalar(out=x2[:, 0:nw], in0=x2[:, 0:nw], scalar1=0.044715, scalar2=1.0, op0=ALU.mult, op1=ALU.add),
                     reads=["x2"], writes=["x2"])
                T.op("dve", lambda e: e.tensor_tensor(out=x2[:, 0:nw], in0=x2[:, 0:nw], in1=xs[:, 0:nw], op=ALU.mult), reads=["x2", "xs"], writes=["x2"])
                T.op("act", lambda e: e.activation(out=sg[:, 0:nw], in_=x2[:, 0:nw], func=AF.Sigmoid, scale=1.5957691216057308),
                     reads=["x2"], writes=["sg"])
                T.op("dve", lambda e: e.tensor_tensor(out=hidT[:, z, c2, n0:n0 + nw], in0=xs[:, 0:nw], in1=sg[:, 0:nw], op=ALU.mult),
                     reads=["xs", "sg"], writes=["hidT"])
    for n0 in range(0, NCMP, 512):
        nw = min(512, NCMP - n0)
        for c2 in range(2):
            T.op("pe", lambda e: e.matmul(Sps[0][0:64, 0:nw], lhsT=w2[:, 0, c2, :], rhs=hidT[:, 0, c2, n0:n0 + nw], start=(c2 == 0), stop=(c2 == 1)),
                 reads=["w2", "hidT"], writes=["Sps0"])
        T.op("act", lambda e: e.copy(out=KcT[0:64, n0:n0 + nw], in_=Sps[0][0:64, 0:nw]), writes=["KcT", "Sps0"])
    for nt in range(NCT):
        for c2 in range(2):
            T.op("pe", lambda e: e.matmul(Sps[1][:, 0:64], lhsT=hidT[:, 1, c2, nt * 128:(nt + 1) * 128], rhs=w2[:, 1, c2, :], start=(c2 == 0), stop=(c2 == 1)),
                 reads=["w2", "hidT"], writes=["Sps1"])
        T.op("dve", lambda e: e.tensor_scalar(out=Vc[:, nt, 0:64], in0=Sps[1][:, 0:64], scalar1=cval[:, nt:nt + 1], scalar2=None, op0=ALU.mult),
             reads=["cval"], writes=["Vc", "Sps1"])
        T.op("dve", lambda e: e.tensor_copy(out=Vc[:, nt, 64:65], in_=cval[:, nt:nt + 1]), reads=["cval"], writes=["Vc"])

    for h in range(0, Sx, 2048):
        w = min(2048, Sx - h)
        cast_load(bufA[0:64, h:h + w], kslcT[:, h:h + w], "bufA", "sp")
        cast_load(bufB[0:64, h:h + w], kwinT[:, h:h + w], "bufB", "act")
    T.op("dve", lambda e: e.memset(bufA[64:65, :], 8.0), writes=["bufA8"])
    T.op("dve", lambda e: e.memset(bufB[64:65, :], 8.0), writes=["bufB8"])
    vsv = vslc1.rearrange("(kb p) d -> p kb d", p=128)
    vwv = vwin1.rearrange("(kb p) d -> p kb d", p=128)
    for h in range(0, NKB, 16):
        w = min(16, NKB - h)
        cast_load(Vs[:, h:h + w, :], vsv[:, h:h + w, :], "bufC", "sp")
        cast_load(Vw[:, h:h + w, :], vwv[:, h:h + w, :], "bufC", "act")

    sidx = [0]
    pidx = [0]

    pendq = []

    def flush():
        while pendq:
            pendq.pop(0)()

    def tile_step(Kbuf, kkey, col0, Vap, vkey, ob, qt, qkey, far, bias_tile, bias_key, sel_chunk=None, sel_m=None, imp_nt=None):
        sb = sidx[0] % 3
        sidx[0] += 1
        pb = pidx[0] % 4
        pidx[0] += 1
        kk = 65 if far else 64
        last_plain = (bias_tile is None and sel_chunk is None)
        T.op("pe", lambda e: e.matmul(Sps[sb][:], lhsT=Kbuf[0:kk, col0:col0 + 128], rhs=qt[0:kk, :], start=True, stop=last_plain),
             reads=[kkey, kkey + "8", qkey, qkey.replace("QT", "QTc")], writes=["Sps%d" % sb])
        if bias_tile is not None:
            T.op("pe", lambda e: e.matmul(Sps[sb][:], lhsT=ident8[:], rhs=bias_tile, start=False, stop=(sel_chunk is None)),
                 reads=["ident8", bias_key], writes=["Sps%d" % sb])
        if sel_chunk is not None:
            T.op("pe", lambda e: e.matmul(Sps[sb][:], lhsT=E[:, sel_m, :], rhs=selbT[:, sel_chunk, :, :].rearrange("p h t -> p (h t)"),
                                          start=False, stop=True), reads=["E", "selbT"], writes=["Sps%d" % sb])

        def rest():
            T.op("act", lambda e: e.activation(out=PT[pb][:], in_=Sps[sb][:], func=AF.Exp, scale=0.125), writes=["PT%d" % pb, "Sps%d" % sb])
            for h in range(4):
                T.op("pe", lambda e: e.matmul(Ops[ob][:, h, 0:65], lhsT=PT[pb][:, h * 128:(h + 1) * 128], rhs=Vap,
                                              start=False, stop=False, skip_group_check=True),
                     reads=["PT%d" % pb, vkey], writes=["Ops%d" % ob])
            if imp_nt is not None:
                for h in range(4):
                    T.op("pe", lambda e: e.matmul(Ips[h // 2][:, h % 2, 0:NJ], lhsT=PT[pb][:, h * 128:(h + 1) * 128], rhs=wsel[:, imp_nt, :],
                                                  start=False, stop=False, skip_group_check=True),
                         reads=["PT%d" % pb, "wsel"], writes=["Ips%d" % (h // 2)])
        pendq.append(rest)
        if len(pendq) > 2:
            pendq.pop(0)()

    for i in range(NB):
        md = 4 * i + 3
        qt = QT[i % 2]
        qkey = "QT%d" % (i % 2)
        cast_load(qt[0:64, :], qT4[:, i, :], qkey, "sp")
        for b in range(3):
            T.op("dve", lambda e: e.memset(Ops[b][:], 0.0), writes=["Ops%d" % b])
        for b in range(2):
            T.op("dve", lambda e: e.memset(Ips[b][:], 0.0), writes=["Ips%d" % b])
        for nt in range(NCT):
            R = md - 16 * nt
            if R < 0:
                continue
            far = R >= 18
            tile_step(KcT, "KcT", nt * 128, Vc[:, nt, :], "Vc", 0, qt, qkey, far,
                      None if far else CB[:, R, :], "CB", imp_nt=nt)
        flush()
        T.op("dve", lambda e: e.tensor_scalar(out=rs[:, 0, :], in0=Ops[0][:, :, 64], scalar1=1e-30, scalar2=None, op0=ALU.max),
             writes=["rs0", "Ops0"])
        T.op("dve", lambda e: e.reciprocal(out=rs[:, 0, :], in_=rs[:, 0, :]), reads=["rs0"], writes=["rs0"])
        for h in range(4):
            if h == 0:
                T.op("dve", lambda e: e.tensor_scalar(out=imp[:], in0=Ips[0][:, 0, 0:NJ], scalar1=rs[:, 0, 0:1], scalar2=None, op0=ALU.mult),
                     reads=["rs0"], writes=["imp", "Ips0"])
            else:
                T.op("dve", lambda e: e.scalar_tensor_tensor(out=imp[:], in0=Ips[h // 2][:, h % 2, 0:NJ], scalar=rs[:, 0, h:h + 1], in1=imp[:],
                                                             op0=ALU.mult, op1=ALU.add), reads=["rs0", "imp"], writes=["imp", "Ips%d" % (h // 2)])
        c1 = 2 * md + 1
        if c1 + 1 < NJ:
            T.op("dve", lambda e: e.memset(imp[:, c1 + 1:NJ], -1.0), reads=["imp"], writes=["imp"])
        T.op("dve", lambda e: e.memset(imp[0:64, c1:c1 + 1], -1.0), reads=["imp"], writes=["imp"])
        T.op("dve", lambda e: e.memset(imp[64:128, c1:c1 + 1], 20000.0), reads=["imp"], writes=["imp"])
        T.op("dve", lambda e: e.memset(imp[0:64, c1 - 1:c1], 20000.0), reads=["imp"], writes=["imp"])
        T.op("dve", lambda e: e.memset(imp[64:128, c1 - 1:c1], 10000.0), reads=["imp"], writes=["imp"])
        T.op("dve", lambda e: e.memset(imp[0:64, c1 - 2:c1 - 1], 10000.0), reads=["imp"], writes=["imp"])
        T.op("dve", lambda e: e.tensor_tensor(out=imp[:], in0=imp[:], in1=f0[:], op=ALU.max), reads=["imp", "f0"], writes=["imp"])
        T.op("dve", lambda e: e.tensor_copy(out=work[:], in_=imp[:]), reads=["imp"], writes=["work"])
        for rnd in range(2):
            T.op("dve", lambda e: e.max(out=mx8[:], in_=work[:]), reads=["work"], writes=["mx8"])
            T.op("dve", lambda e: e.match_replace(out=work[:], in_to_replace=mx8[:], in_values=work[:], imm_value=-1e9),
                 reads=["mx8", "work"], writes=["work"])
        T.op("dve", lambda e: e.tensor_scalar(out=selm[:], in0=work[:], scalar1=-1e8, scalar2=None, op0=ALU.is_le), reads=["work"], writes=["selm"])
        nch = (NJ + 127) // 128
        for ch in range(nch):
            cwid = min(128, NJ - ch * 128)
            T.op("pe", lambda e: e.transpose(out=Ips[1][0:cwid, 0, ch * 128:(ch + 1) * 128], in_=selm[:, ch * 128:ch * 128 + cwid], identity=identf[:]),
                 reads=["selm", "identf"], writes=["Ips1"])
        for ch in range(nch):
            cwid = min(128, NJ - ch * 128)
            for h in range(4):
                T.op("dve", lambda e: e.tensor_scalar(out=selbT[0:cwid, ch, h, :], in0=Ips[1][0:cwid, 0, ch * 128:(ch + 1) * 128], scalar1=-1.0, scalar2=-BIGNEG,
                                                      op0=ALU.add, op1=ALU.mult), writes=["selbT", "Ips1"])
        for m in range(max(0, md - 4), md + 1):
            rel = md - m
            bt, bk = {0: (D0[:], "D0"), 1: (D1[:], "D1"), 4: (W4[:], "W4")}.get(rel, (None, None))
            tile_step(bufB, "bufB", m * 128, Vw[:, m, :], "bufC", 2, qt, qkey, bt is None, bt, bk)
        for m in range(0, md + 1):
            rel = md - m
            bt, bk = {0: (D0[:], "D0"), 1: (D1[:], "D1")}.get(rel, (None, None))
            tile_step(bufA, "bufA", m * 128, Vs[:, m, :], "bufC", 1, qt, qkey, bt is None, bt, bk, sel_chunk=m // 64, sel_m=m % 64)
        flush()
        oa = oacc[i % 2]
        for b in range(3):
            if b > 0:
                T.op("dve", lambda e: e.tensor_scalar(out=rs[:, b, :], in0=Ops[b][:, :, 64], scalar1=1e-30, scalar2=None, op0=ALU.max),
                     writes=["rs%d" % b, "Ops%d" % b])
                T.op("dve", lambda e: e.reciprocal(out=rs[:, b, :], in_=rs[:, b, :]), reads=["rs%d" % b], writes=["rs%d" % b])
            T.op("dve", lambda e: e.tensor_tensor(out=rs[:, b, :], in0=rs[:, b, :], in1=gts[:, i, :, b], op=ALU.mult),
                 reads=["rs%d" % b, "gts"], writes=["rs%d" % b])
            for h in range(4):
                if b == 0:
                    T.op("dve", lambda e: e.tensor_scalar(out=oa[:, h, :], in0=Ops[b][:, h, 0:64], scalar1=rs[:, b, h:h + 1], scalar2=None, op0=ALU.mult),
                         reads=["rs%d" % b], writes=["oacc%d" % (i % 2), "Ops%d" % b])
                else:
                    T.op("dve", lambda e: e.scalar_tensor_tensor(out=oa[:, h, :], in0=Ops[b][:, h, 0:64], scalar=rs[:, b, h:h + 1], in1=oa[:, h, :],
                                                                 op0=ALU.mult, op1=ALU.add), reads=["rs%d" % b], writes=["oacc%d" % (i % 2), "Ops%d" % b])
        T.op("sp", lambda e: e.dma_start(out=o[i * 128:(i + 1) * 128, :], in_=oa[:].rearrange("p h d -> p (h d)")),
             reads=["oacc%d" % (i % 2)], writes=["o"], dma=True)
    T.finish("sp")
    print("NSA n_ins", T.n_ins, "n_wait", T.n_wait)
    return nc


def t5_bucket_np(dist):
    n = np.maximum(dist, 0)
    ratio = np.log(np.maximum(n, 16).astype(np.float32) / np.float32(16))
    big = 16 + (ratio / np.float32(np.log(128 / 16)) * np.float32(16)).astype(np.int32)
    return np.where(n < 16, n, np.minimum(big, 31))


def nsa_inputs(proj, projb, t5_table, cmp_pe, cmp_w1, cmp_b1, cmp_w2, Sx=S):
    OFF_KV = 512
    OFF_GATE = 512 + 768
    NB = Sx // 512
    NCT = max(1, Sx // 2048)
    NJ = Sx // 64
    NCMP = Sx // 16
    maps = []
    si = np.arange(128)[:, None]
    ti = np.arange(128)[None, :]
    w1 = np.ascontiguousarray(cmp_w1[0].reshape(2, 32, 64, 256).transpose(2, 0, 1, 3).reshape(64, -1))
    b1T = np.ascontiguousarray(cmp_b1[0].reshape(2, 2, 128).transpose(2, 0, 1).reshape(128, 4))
    w2 = np.ascontiguousarray(cmp_w2[0].reshape(2, 2, 128, 64).transpose(2, 0, 1, 3).reshape(128, -1))
    peT = np.ascontiguousarray(cmp_pe[0].transpose(2, 0, 1).reshape(64, 64))
    for c in range(8):
        g, r = c // 4, c % 4
        sh = 3 - r
        p = proj[:Sx]
        pb = projb[:Sx]
        tb = t5_table.T[4 * g:4 * g + 4]
        kvc = lambda z: pb[:, OFF_KV + (z * 2 + g) * 64:OFF_KV + (z * 2 + g + 1) * 64]

        def shiftT(a):
            out = np.zeros((64, Sx), pb.dtype)
            if sh * 128 < Sx:
                out[:, sh * 128:] = a[:Sx - sh * 128].T
            return out

        def shiftV1(a):
            out = np.zeros((Sx, 65), pb.dtype)
            if sh * 128 < Sx:
                out[sh * 128:, 0:64] = a[:Sx - sh * 128]
                out[sh * 128:, 64] = 1.0
            return out
        q = pb[:, g * 256:(g + 1) * 256].reshape(Sx // 128, 128, 4, 64)[r::4]
        qT4 = np.ascontiguousarray(q.transpose(3, 0, 2, 1).reshape(64, NB, 512))
        gt = p[:, OFF_GATE + g * 12:OFF_GATE + (g + 1) * 12].reshape(Sx // 128, 128, 12)[r::4]
        gates = np.ascontiguousarray(gt.transpose(1, 0, 2).reshape(128, NB * 12))
        D0 = np.stack([tb[h][t5_bucket_np(ti - si)] for h in range(4)], axis=1).reshape(128, 512)
        D1 = np.stack([tb[h][t5_bucket_np(128 + ti - si)] for h in range(4)], axis=1).reshape(128, 512)
        CB = np.stack([np.stack([tb[h][t5_bucket_np(128 * R + ti - 16 * si - 31)] for h in range(4)], axis=1).reshape(128, 512)
                       for R in range(18)], axis=1).reshape(128, 18 * 512)
        Cfull = np.ascontiguousarray(np.broadcast_to(np.repeat(tb[:, 31], 128)[None, :], (128, 512)))
        crow = np.ascontiguousarray(np.repeat(tb[:, 31], 128)[None, :])
        npad = 8 * sh
        nn = np.arange(NCT * 128)[:, None]
        jj = np.arange(NJ)[None, :]
        dlt = nn - 4 * jj
        wsel = np.where((dlt >= 0) & (dlt <= 2), 1.0, np.where((dlt == -1) | (dlt == 3), 0.5, 0.0)).astype(np.float32)
        wsel[:npad] = 0.0
        wsel[NCMP - 1 - 0:] = 0.0 if True else 0.0
        wsel = np.ascontiguousarray(wsel.reshape(NCT, 128, NJ).transpose(1, 0, 2).reshape(128, NCT * NJ))
        cvalid = (np.arange(NCT * 128) >= npad).astype(np.float32)
        cvalid[NCMP - 1:] = 0.0
        cvalid = np.ascontiguousarray(cvalid.reshape(NCT, 128).T)
        force0 = np.zeros((128, NJ), np.float32)
        force0[:, 2 * sh] = 30000.0
        maps.append({"qT4": qT4, "kc_s": shiftT(kvc(0)), "vc_s": shiftT(kvc(1)), "kslcT": shiftT(kvc(2)), "kwinT": shiftT(kvc(4)),
                     "vslc1": shiftV1(kvc(3)), "vwin1": shiftV1(kvc(5)), "gates": gates, "w1": w1, "b1T": b1T, "w2": w2, "peT": peT,
                     "D0": np.ascontiguousarray(D0), "D1": np.ascontiguousarray(D1), "CB": np.ascontiguousarray(CB), "Cfull": Cfull, "crow": crow,
                     "wsel": wsel, "cvalid": cvalid, "force0": force0})
    return maps


def run_nsa(proj, projb, t5_table, cmp_pe, cmp_w1, cmp_b1, cmp_w2, Sx=S, cores=NCORES):
    nc = build_nsa(Sx)
    maps = nsa_inputs(proj, projb, t5_table, cmp_pe, cmp_w1, cmp_b1, cmp_w2, Sx)[:cores]
    res = _launch("nsa", nc, maps, cores)
    out = np.zeros((Sx, 512), np.float32)
    for c in range(cores):
        g, r = c // 4, c % 4
        oc = res.results[c]["o"].reshape(Sx // 512, 128, 256)
        out.reshape(Sx // 128, 128, 512)[r::4, :, g * 256:(g + 1) * 256] = oc
    return out


ALPHA = 2.0 ** 0.25


def build_l3a(TPC=TPC):
    nc = bass.Bass("TRN2", target_bir_lowering=False)
    NT = TPC // 128
    NG = TPC // 512
    dI = lambda name, shape: nc.dram_tensor(name, shape, F32, kind="ExternalInput").ap()
    A = lambda name, shape, dt: nc.alloc_sbuf_tensor("sb_" + name, shape, dt)
    onT = dI("onT", [512, TPC]); ofT = dI("ofT", [512, TPC]); mgT = dI("mgT", [4096, TPC]); x = dI("x", [TPC, D])
    wbn = dI("wbn", [512, D]); wbf = dI("wbf", [512, D]); wo = dI("wo", [D, D])
    g1b = dI("g1b", [128, D]); ln1g = dI("ln1g", [128, D]); ln1b = dI("ln1b", [128, D])
    mod2 = dI("mod2", [128, 32]); wr = dI("wr", [D, 72]); brb = dI("brb", [128, 72])
    x1o = nc.dram_tensor("x1", [TPC, D], F32, kind="ExternalOutput").ap()
    xn2o = nc.dram_tensor("xn2", [TPC, D], BF16, kind="ExternalOutput").ap()
    rwo = nc.dram_tensor("rw", [TPC, 64], F32, kind="ExternalOutput").ap()
    oho = nc.dram_tensor("oh", [TPC, 8], F32, kind="ExternalOutput").ap()
    mTd = nc.dram_tensor("mTd", [16, 128, TPC], BF16).ap()
    T = Trk(nc)
    big = A("big", [128, 32768], BF16)
    wbn_s = big[:, 0:8192].rearrange("p (k n) -> p k n", k=4)
    wbf_s = big[:, 8192:16384].rearrange("p (k n) -> p k n", k=4)
    onT_s = big[:, 16384:16384 + 4 * TPC].rearrange("p (k n) -> p k n", k=4)
    ofT_s = big[:, 24576:24576 + 4 * TPC].rearrange("p (k n) -> p k n", k=4)
    wo_s = big[:, :].rearrange("p (k n) -> p k n", k=16)
    mgs = [A("mgs%d" % i, [128, 2, 512], F32) for i in range(2)]
    t1 = A("t1", [128, 512], F32); t2 = A("t2", [128, 512], F32)
    mo = [A("mo%d" % i, [128, 512], BF16) for i in range(2)]
    P = nc.alloc_psum_tensor
    acc = [P("acc%d" % i, [128, 512], F32) for i in range(4)]
    tpf = [P("tpf%d" % i, [128, 4, 128], F32) for i in range(2)]
    rps = P("rps", [128, 512], F32)

    def cast_load(dst, src, key):
        T.op("pool", lambda e: e.dma_start(out=dst, in_=src), writes=[key], dma=True)
    cast_load(wbn_s, wbn.rearrange("(k p) n -> p k n", p=128), "big")
    cast_load(wbf_s, wbf.rearrange("(k p) n -> p k n", p=128), "big")
    cast_load(onT_s, onT.rearrange("(k p) n -> p k n", p=128), "big")
    cast_load(ofT_s, ofT.rearrange("(k p) n -> p k n", p=128), "big")
    it = 0
    for dc in range(16):
        for tg in range(NG):
            mb = mgs[it % 2]
            T.op("sp", lambda e: e.dma_start(out=mb[:, 0, :], in_=mgT[dc * 128:(dc + 1) * 128, tg * 512:(tg + 1) * 512]), writes=["mgs%d" % (it % 2)], dma=True)
            T.op("sp", lambda e: e.dma_start(out=mb[:, 1, :], in_=mgT[2048 + dc * 128:2048 + (dc + 1) * 128, tg * 512:(tg + 1) * 512]), writes=["mgs%d" % (it % 2)], dma=True)
            T.op("act", lambda e: e.activation(out=mb[:].rearrange("p a n -> p (a n)"), in_=mb[:].rearrange("p a n -> p (a n)"), func=AF.Sigmoid),
                 reads=["mgs%d" % (it % 2)], writes=["mgs%d" % (it % 2)])
            for br, (ws, os_) in enumerate([(wbn_s, onT_s), (wbf_s, ofT_s)]):
                a = (it % 2) * 2 + br
                for k in range(4):
                    T.op("pe", lambda e: e.matmul(acc[a][:], lhsT=ws[:, k, dc * 128:(dc + 1) * 128], rhs=os_[:, k, tg * 512:(tg + 1) * 512],
                                                  start=(k == 0), stop=(k == 3)), reads=["big"], writes=["acc%d" % a])
            a0 = (it % 2) * 2
            T.op("dve", lambda e: e.tensor_tensor(out=t1[:], in0=acc[a0][:], in1=mb[:, 0, :], op=ALU.mult), reads=["mgs%d" % (it % 2)], writes=["t1", "acc%d" % a0])
            T.op("dve", lambda e: e.tensor_tensor(out=t2[:], in0=acc[a0 + 1][:], in1=mb[:, 1, :], op=ALU.mult), reads=["mgs%d" % (it % 2)], writes=["t2", "acc%d" % (a0 + 1)])
            T.op("dve", lambda e: e.tensor_tensor(out=mo[it % 2][:], in0=t1[:], in1=t2[:], op=ALU.add), reads=["t1", "t2"], writes=["mo%d" % (it % 2)])
            T.op("sp", lambda e: e.dma_start(out=mTd[dc, :, tg * 512:(tg + 1) * 512], in_=mo[it % 2][:]), reads=["mo%d" % (it % 2)], writes=["mTd"], dma=True)
            it += 1
    for h in range(4):
        cast_load(wo_s[:, 4 * h:4 * h + 4, :], wo.rearrange("(k p) n -> p k n", p=128)[:, 4 * h:4 * h + 4, :], "big")
    g1p = A("g1p", [128, D], F32); lg_ = A("lng", [128, D], F32); lb_ = A("lnb", [128, D], F32)
    T.op("sp", lambda e: e.dma_start(out=g1p[:], in_=g1b[:, :]), writes=["g1p"], dma=True)
    T.op("sp", lambda e: e.dma_start(out=lg_[:], in_=ln1g[:, :]), writes=["lng"], dma=True)
    T.op("sp", lambda e: e.dma_start(out=lb_[:], in_=ln1b[:, :]), writes=["lnb"], dma=True)
    T.op("dve", lambda e: e.tensor_scalar(out=g1p[:], in0=g1p[:], scalar1=1.0, scalar2=None, op0=ALU.add), reads=["g1p"], writes=["g1p"])
    m2 = A("m2", [128, 32], F32); wrs = A("wrs", [128, 16, 72], F32); brs = A("brs", [128, 72], F32)
    T.op("sp", lambda e: e.dma_start(out=m2[:], in_=mod2[:, :]), writes=["m2"], dma=True)
    T.op("dve", lambda e: e.tensor_scalar(out=m2[:, 16:32], in0=m2[:, 16:32], scalar1=1.0, scalar2=None, op0=ALU.add), reads=["m2"], writes=["m2"])
    T.op("sp", lambda e: e.dma_start(out=wrs[:], in_=wr.rearrange("(k p) n -> p k n", p=128)), writes=["wrs"], dma=True)
    T.op("sp", lambda e: e.dma_start(out=brs[:], in_=brb[:, :]), writes=["brs"], dma=True)
    identf = make_ident(nc, T, F32, "identf")
    mt = [A("mt%d" % i, [128, 16, 128], BF16) for i in range(2)]
    xt = [A("xt%d" % i, [128, D], F32) for i in range(2)]
    v = A("v", [128, D], F32); xn = A("xn", [128, D], F32); junk = A("junk", [128, D], BF16)
    xnb = A("xnb", [128, D], BF16)
    u2T = A("u2T", [128, 16, 128], F32)
    st = A("st", [128, 16], F32)
    lgt = A("lgt", [128, 72], F32); ml = A("ml", [128, 64], F32); mx8 = A("mx8", [128, 8], F32)
    r1 = A("r1", [128, 16], F32); ra = A("ra", [128, 64], F32); rb = A("rb", [128, 64], F32); ohs = A("ohs", [128, 8], F32)
    ge = A("ge", [128, 8], F32)

    def ln_stats(src, key):
        T.op("dve", lambda e: e.reduce_sum(out=st[:, 0:1], in_=src, axis=AX.X), reads=[key], writes=["st0"])
        T.op("act", lambda e: e.activation(out=junk[:], in_=src, func=AF.Square, accum_out=st[:, 1:2]), reads=[key], writes=["st1", "junk"])
        T.op("dve", lambda e: e.tensor_scalar(out=st[:, 2:3], in0=st[:, 0:1], scalar1=1.0 / D, scalar2=None, op0=ALU.mult), reads=["st0"], writes=["st2"])
        T.op("dve", lambda e: e.tensor_tensor(out=st[:, 3:4], in0=st[:, 2:3], in1=st[:, 2:3], op=ALU.mult), reads=["st2"], writes=["st3"])
        T.op("dve", lambda e: e.scalar_tensor_tensor(out=st[:, 4:5], in0=st[:, 1:2], scalar=1.0 / D, in1=st[:, 3:4], op0=ALU.mult, op1=ALU.subtract),
             reads=["st1", "st3"], writes=["st4"])
        T.op("dve", lambda e: e.tensor_scalar(out=st[:, 6:7], in0=st[:, 4:5], scalar1=LN_EPS, scalar2=None, op0=ALU.add), reads=["st4"], writes=["st6"])
        T.op("act", lambda e: e.activation(out=st[:, 7:8], in_=st[:, 6:7], func=AF.Sqrt), reads=["st6"], writes=["st7"])
        T.op("dve", lambda e: e.reciprocal(out=st[:, 5:6], in_=st[:, 7:8]), reads=["st7"], writes=["st5"])

    def ld_t(i_):
        b_ = i_ % 2
        T.op("sp", lambda e: e.dma_start(out=mt[b_][:], in_=mTd[:, :, i_ * 128:(i_ + 1) * 128].rearrange("k p t -> p k t")), reads=["mTd"], writes=["mt%d" % b_], dma=True)
        T.op("sp", lambda e: e.dma_start(out=xt[b_][:], in_=x[i_ * 128:(i_ + 1) * 128, :]), writes=["xt%d" % b_], dma=True)
    ld_t(0)
    for i in range(NT):
        b = i % 2
        if i + 1 < NT:
            ld_t(i + 1)
        for cg in range(4):
            for k in range(16):
                T.op("pe", lambda e: e.matmul(acc[cg][:], lhsT=mt[b][:, k, :], rhs=wo_s[:, k, cg * 512:(cg + 1) * 512], start=(k == 0), stop=(k == 15)),
                     reads=["mt%d" % b, "big"], writes=["acc%d" % cg])
            T.op("dve", lambda e: e.tensor_tensor(out=v[:, cg * 512:(cg + 1) * 512], in0=acc[cg][:], in1=g1p[:, cg * 512:(cg + 1) * 512], op=ALU.mult),
                 reads=["g1p"], writes=["v", "acc%d" % cg])
        T.op("dve", lambda e: e.scalar_tensor_tensor(out=v[:], in0=xt[b][:], scalar=ALPHA, in1=v[:], op0=ALU.mult, op1=ALU.add),
             reads=["xt%d" % b, "v"], writes=["v"])
        ln_stats(v[:], "v")
        T.op("dve", lambda e: e.tensor_scalar(out=xn[:], in0=v[:], scalar1=st[:, 2:3], scalar2=st[:, 5:6], op0=ALU.subtract, op1=ALU.mult),
             reads=["v", "st2", "st5"], writes=["xn"])
        T.op("dve", lambda e: e.tensor_tensor(out=xn[:], in0=xn[:], in1=lg_[:], op=ALU.mult), reads=["xn", "lng"], writes=["xn"])
        T.op("dve", lambda e: e.tensor_tensor(out=xn[:], in0=xn[:], in1=lb_[:], op=ALU.add), reads=["xn", "lnb"], writes=["xn"])
        T.op("sp", lambda e: e.dma_start(out=x1o[i * 128:(i + 1) * 128, :], in_=xn[:]), reads=["xn"], writes=["x1o"], dma=True)
        ln_stats(xn[:], "xn")
        T.op("dve", lambda e: e.tensor_scalar(out=v[:], in0=xn[:], scalar1=st[:, 2:3], scalar2=st[:, 5:6], op0=ALU.subtract, op1=ALU.mult),
             reads=["xn", "st2", "st5"], writes=["v"])
        T.op("act", lambda e: e.copy(out=xnb[:], in_=v[:]), reads=["v"], writes=["xnb"])
        T.op("sp", lambda e: e.dma_start(out=xn2o[i * 128:(i + 1) * 128, :], in_=xnb[:]), reads=["xnb"], writes=["xn2o"], dma=True)
        for kq in range(4):
            pb = kq % 2
            for j in range(4):
                k = kq * 4 + j
                T.op("pe", lambda e: e.transpose(out=tpf[pb][:, j, :], in_=v[:, k * 128:(k + 1) * 128], identity=identf[:]),
                     reads=["v", "identf"], writes=["tpf%d" % pb])
            for j in range(4):
                k = kq * 4 + j
                T.op("act", lambda e: e.activation(out=u2T[:, k, :], in_=tpf[pb][:, j, :], func=AF.Identity, scale=m2[:, 16 + k:17 + k], bias=m2[:, k:k + 1]),
                     reads=["m2"], writes=["u2T", "tpf%d" % pb])
        for k in range(16):
            T.op("pe", lambda e: e.matmul(rps[:, 0:72], lhsT=u2T[:, k, :], rhs=wrs[:, k, :], start=(k == 0), stop=(k == 15)),
                 reads=["u2T", "wrs"], writes=["rps"])
        T.op("dve", lambda e: e.tensor_tensor(out=lgt[:], in0=rps[:, 0:72], in1=brs[:], op=ALU.add), reads=["brs"], writes=["lgt", "rps"])
        T.op("dve", lambda e: e.reduce_max(out=r1[:, 0:1], in_=lgt[:, 0:8], axis=AX.X), reads=["lgt"], writes=["r1a"])
        T.op("dve", lambda e: e.tensor_scalar(out=r1[:, 1:2], in0=r1[:, 0:1], scalar1=-1.0, scalar2=None, op0=ALU.mult), reads=["r1a"], writes=["r1b"])
        T.op("act", lambda e: e.activation(out=ge[:], in_=lgt[:, 0:8], func=AF.Exp, bias=r1[:, 1:2], accum_out=r1[:, 2:3]), reads=["lgt", "r1b"], writes=["ge", "r1c"])
        T.op("dve", lambda e: e.reciprocal(out=r1[:, 3:4], in_=r1[:, 2:3]), reads=["r1c"], writes=["r1d"])
        T.op("dve", lambda e: e.tensor_scalar(out=ohs[:], in0=lgt[:, 0:8], scalar1=r1[:, 0:1], scalar2=None, op0=ALU.is_equal), reads=["lgt", "r1a"], writes=["ohs"])
        T.op("sp", lambda e: e.dma_start(out=oho[i * 128:(i + 1) * 128, :], in_=ohs[:]), reads=["ohs"], writes=["oho"], dma=True)
        T.op("dve", lambda e: e.tensor_scalar(out=ge[:], in0=ohs[:], scalar1=-1.0, scalar2=1e9, op0=ALU.add, op1=ALU.mult), reads=["ohs", "ge"], writes=["ge"])
        for g in range(8):
            T.op("dve", lambda e: e.tensor_scalar(out=ml[:, g * 8:(g + 1) * 8], in0=lgt[:, 8 + g * 8:16 + g * 8], scalar1=ge[:, g:g + 1], scalar2=None, op0=ALU.add),
                 reads=["lgt", "ge"], writes=["ml"])
        T.op("dve", lambda e: e.max(out=mx8[:], in_=ml[:]), reads=["ml"], writes=["mx8"])
        T.op("dve", lambda e: e.tensor_tensor(out=r1[:, 4:5], in0=mx8[:, 1:2], in1=mx8[:, 0:1], op=ALU.subtract), reads=["mx8"], writes=["r1e"])
        T.op("act", lambda e: e.activation(out=r1[:, 5:6], in_=r1[:, 4:5], func=AF.Exp), reads=["r1e"], writes=["r1f"])
        T.op("dve", lambda e: e.tensor_scalar(out=r1[:, 6:7], in0=r1[:, 5:6], scalar1=1.0, scalar2=None, op0=ALU.add), reads=["r1f"], writes=["r1g"])
        T.op("dve", lambda e: e.reciprocal(out=r1[:, 7:8], in_=r1[:, 6:7]), reads=["r1g"], writes=["r1h"])
        T.op("dve", lambda e: e.tensor_tensor(out=r1[:, 8:9], in0=r1[:, 7:8], in1=r1[:, 3:4], op=ALU.mult), reads=["r1h", "r1d"], writes=["r1i"])
        T.op("dve", lambda e: e.tensor_tensor(out=r1[:, 9:10], in0=r1[:, 8:9], in1=r1[:, 5:6], op=ALU.mult), reads=["r1i", "r1f"], writes=["r1j"])
        T.op("dve", lambda e: e.tensor_scalar(out=ra[:], in0=ml[:], scalar1=mx8[:, 0:1], scalar2=r1[:, 8:9], op0=ALU.is_equal, op1=ALU.mult),
             reads=["ml", "mx8", "r1i"], writes=["ra"])
        T.op("dve", lambda e: e.tensor_scalar(out=rb[:], in0=ml[:], scalar1=mx8[:, 1:2], scalar2=r1[:, 9:10], op0=ALU.is_equal, op1=ALU.mult),
             reads=["ml", "mx8", "r1j"], writes=["rb"])
        T.op("dve", lambda e: e.tensor_tensor(out=ra[:], in0=ra[:], in1=rb[:], op=ALU.add), reads=["ra", "rb"], writes=["ra"])
        T.op("sp", lambda e: e.dma_start(out=rwo[i * 128:(i + 1) * 128, :], in_=ra[:]), reads=["ra"], writes=["rwo"], dma=True)
    T.finish("sp")
    print("L3a n_ins", T.n_ins, "n_wait", T.n_wait)
    return nc


def run_l3a(o_nsa, o_fox, proj, x, modrow, w_br_nsa, w_br_fox, w_o, ln1_g, ln1_b, w_rg, b_rg, w_re, b_re, TPC=TPC, cores=NCORES):
    nc = build_l3a(TPC)
    rep = lambda a: np.ascontiguousarray(np.broadcast_to(a.reshape(1, -1), (128, a.size))).astype(np.float32)
    wr = np.ascontiguousarray(np.concatenate([w_rg[0], w_re[0].reshape(D, 64)], axis=1))
    brb = rep(np.concatenate([b_rg[0], b_re[0].reshape(64)]))
    mod2 = np.ascontiguousarray(np.concatenate([modrow[3 * D:4 * D].reshape(16, 128).T, modrow[4 * D:5 * D].reshape(16, 128).T], axis=1))
    OFF_MERGE = 512 + 768 + 24 + 1536 + 8
    x2 = x.reshape(S, D)
    maps = []
    for c in range(cores):
        sl = slice(c * TPC, (c + 1) * TPC)
        maps.append({"onT": np.ascontiguousarray(o_nsa[sl].T), "ofT": np.ascontiguousarray(o_fox[sl].T),
                     "mgT": np.ascontiguousarray(proj[sl, OFF_MERGE:OFF_MERGE + 4096].T), "x": np.ascontiguousarray(x2[sl]),
                     "wbn": w_br_nsa[0], "wbf": w_br_fox[0], "wo": w_o[0], "g1b": rep(modrow[2 * D:3 * D]), "ln1g": rep(ln1_g[0]), "ln1b": rep(ln1_b[0]),
                     "mod2": mod2, "wr": wr, "brb": brb})
    res = _launch("l3a", nc, maps, cores)
    cat = lambda k: np.concatenate([r[k] for r in res.results], axis=0)
    return cat("x1"), cat("xn2"), cat("rw"), cat("oh")


CAP = 512


def _rank_setup(nc, T, A, P_, oh, TPC):
    NT = TPC // 128
    ohs = A("ohs", [128, NT, 8], F32)
    ohb = A("ohb", [128, NT, 8], BF16)
    rank = A("rank", [128, NT, 8], F32)
    onesb = A("onesb", [128, 128], BF16)
    sub = A("sub", [128, 128], BF16)
    rkps = P_("rkps", [128, 512], F32)
    T.op("sp", lambda e: e.dma_start(out=ohs[:], in_=oh.rearrange("(c p) g -> p c g", p=128)), writes=["ohs"], dma=True)
    T.op("dve", lambda e: e.tensor_copy(out=ohb[:], in_=ohs[:]), reads=["ohs"], writes=["ohb"])
    T.op("pool", lambda e: e.memset(onesb[:], 1.0), writes=["onesb"])
    T.op("pool", lambda e: e.memset(sub[:], 1.0), writes=["sub"])
    T.op("pool", lambda e: e.affine_select(out=sub[:], in_=sub[:], pattern=[[1, 128]], compare_op=ALU.is_ge, fill=0.0, base=-1,
                                           channel_multiplier=-1), reads=["sub"], writes=["sub"])
    for c in range(NT):
        for c2 in range(c):
            T.op("pe", lambda e: e.matmul(rkps[:, c * 8:(c + 1) * 8], lhsT=onesb[:], rhs=ohb[:, c2, :], start=(c2 == 0), stop=False),
                 reads=["onesb", "ohb"], writes=["rkps"])
        T.op("pe", lambda e: e.matmul(rkps[:, c * 8:(c + 1) * 8], lhsT=sub[:], rhs=ohb[:, c, :], start=(c == 0), stop=True),
             reads=["sub", "ohb"], writes=["rkps"])
    T.op("dve", lambda e: e.tensor_copy(out=rank[:].rearrange("p c g -> p (c g)"), in_=rkps[:, 0:NT * 8]), writes=["rank", "rkps"])
    return ohs, rank


def build_dispatch(TPC=TPC):
    nc = bass.Bass("TRN2", target_bir_lowering=False)
    NT = TPC // 128
    A = lambda name, shape, dt: nc.alloc_sbuf_tensor("sb_" + name, shape, dt)
    P_ = nc.alloc_psum_tensor
    xn2 = nc.dram_tensor("xn2", [TPC, D], BF16, kind="ExternalInput").ap()
    oh = nc.dram_tensor("oh", [TPC, 8], F32, kind="ExternalInput").ap()
    rw = nc.dram_tensor("rw", [TPC, 64], F32, kind="ExternalInput").ap()
    mod2 = nc.dram_tensor("mod2", [128, 32], F32, kind="ExternalInput").ap()
    iotad = nc.dram_tensor("iota", [128, CAP], F32, kind="ExternalInput").ap()
    xso = nc.dram_tensor("xs", [8, D, CAP], BF16, kind="ExternalOutput").ap()
    rwso = nc.dram_tensor("rws", [8, CAP, 8], F32, kind="ExternalOutput").ap()
    T = Trk(nc)
    ohs, rank = _rank_setup(nc, T, A, P_, oh, TPC)
    xs_ = A("xn2s", [128, NT, D], BF16)
    for h in range(0, NT, 4):
        hw = min(4, NT - h)
        T.op("sp", lambda e: e.dma_start(out=xs_[:, h:h + hw, :], in_=xn2.rearrange("(c p) d -> p c d", p=128)[:, h:h + hw, :]), writes=["xn2s"], dma=True)
    rws_ = A("rwf", [128, NT, 64], F32)
    rwh = A("rwh", [128, NT, 64], BF16); rwhf = A("rwhf", [128, NT, 64], F32); rwl = A("rwl", [128, NT, 64], BF16)
    T.op("sp", lambda e: e.dma_start(out=rws_[:], in_=rw.rearrange("(c p) g -> p c g", p=128)), writes=["rwf"], dma=True)
    T.op("dve", lambda e: e.tensor_copy(out=rwh[:], in_=rws_[:]), reads=["rwf"], writes=["rwh"])
    T.op("dve", lambda e: e.tensor_copy(out=rwhf[:], in_=rwh[:]), reads=["rwh"], writes=["rwhf"])
    T.op("dve", lambda e: e.tensor_tensor(out=rwl[:], in0=rws_[:], in1=rwhf[:], op=ALU.subtract), reads=["rwf", "rwhf"], writes=["rwl"])
    m2 = A("m2", [128, 32], F32); iota = A("iota", [128, CAP], F32)
    T.op("sp", lambda e: e.dma_start(out=m2[:], in_=mod2[:, :]), writes=["m2"], dma=True)
    T.op("sp", lambda e: e.dma_start(out=iota[:], in_=iotad[:, :]), writes=["iota"], dma=True)
    T.op("dve", lambda e: e.tensor_scalar(out=m2[:, 16:32], in0=m2[:, 16:32], scalar1=1.0, scalar2=None, op0=ALU.add), reads=["m2"], writes=["m2"])
    Pm = [A("Pm%d" % i, [128, NT, CAP], BF16) for i in range(2)]
    xo = [A("xo%d" % i, [128, 4, CAP], BF16) for i in range(2)]
    ro = A("ro", [128, 4, 8], F32)
    acc = [P_("acc%d" % i, [128, 512], F32) for i in range(4)]
    rps = P_("rps", [128, 512], F32)
    ev = 0
    import os
    DBG = int(os.environ.get("DBG", "9"))
    for g in range(8 if DBG > 0 else 0):
        Pg = Pm[g % 2]
        pk = "Pm%d" % (g % 2)
        for c in range(NT):
            T.op("dve", lambda e: e.tensor_scalar(out=Pg[:, c, :], in0=iota[:], scalar1=rank[:, c, g:g + 1], scalar2=ohs[:, c, g:g + 1],
                                                  op0=ALU.is_equal, op1=ALU.mult), reads=["iota", "rank", "ohs"], writes=[pk])
        for dg in range(4 if DBG > 1 else 0):
            for c in range(NT):
                for dd in range(4):
                    k = dg * 4 + dd
                    T.op("pe", lambda e: e.matmul(acc[dd][:], lhsT=xs_[:, c, k * 128:(k + 1) * 128], rhs=Pg[:, c, :], start=(c == 0), stop=(c == NT - 1)),
                         reads=["xn2s", pk], writes=["acc%d" % dd])
            xb = xo[ev % 2]
            for dd in range(4):
                k = dg * 4 + dd
                T.op("act", lambda e: e.activation(out=xb[:, dd, :], in_=acc[dd][:], func=AF.Identity, scale=m2[:, 16 + k:17 + k], bias=m2[:, k:k + 1]),
                     reads=["m2"], writes=["xo%d" % (ev % 2), "acc%d" % dd])
            T.op("sp", lambda e: e.dma_start(out=xso[g, dg * 512:(dg + 1) * 512, :].rearrange("(dd p) s -> p dd s", p=128), in_=xb[:]),
                 reads=["xo%d" % (ev % 2)], writes=["xso"], dma=True)
            ev += 1
        for st in range(4 if DBG > 2 else 0):
            n = 0
            for c in range(NT):
                for hl in (rwh, rwl):
                    T.op("pe", lambda e: e.matmul(rps[:, st * 8:(st + 1) * 8], lhsT=Pg[:, c, st * 128:(st + 1) * 128], rhs=hl[:, c, g * 8:(g + 1) * 8],
                                                  start=(n == 0), stop=(n == 2 * NT - 1)), reads=[pk, "rwh", "rwl"], writes=["rps"])
                    n += 1
        if DBG > 3:
            T.op("dve", lambda e: e.tensor_copy(out=ro[:].rearrange("p a b -> p (a b)"), in_=rps[:, 0:32]), writes=["ro", "rps"])
        if DBG > 4:
            T.op("sp", lambda e: e.dma_start(out=rwso[g].rearrange("(st p) e -> p st e", p=128), in_=ro[:]), reads=["ro"], writes=["rwso"], dma=True)
    T.finish("sp")
    print("DISP n_ins", T.n_ins, "n_wait", T.n_wait)
    return nc


def build_experts(NSL=8 * CAP, NE=8):
    nc = bass.Bass("TRN2", target_bir_lowering=False)
    NSC = NSL // 512
    A = lambda name, shape, dt: nc.alloc_sbuf_tensor("sb_" + name, shape, dt)
    P_ = nc.alloc_psum_tensor
    xs = nc.dram_tensor("xs", [D, NSL], BF16, kind="ExternalInput").ap()
    rws = nc.dram_tensor("rws", [NSL, 8], F32, kind="ExternalInput").ap()
    wg = nc.dram_tensor("wg", [NE, D, 512], F32, kind="ExternalInput").ap()
    wu = nc.dram_tensor("wu", [NE, D, 512], F32, kind="ExternalInput").ap()
    wd = nc.dram_tensor("wd", [NE, 512, D], F32, kind="ExternalInput").ap()
    ys = nc.dram_tensor("ys", [NSL, D], F32, kind="ExternalOutput").ap()
    T = Trk(nc)
    W = [[A("w%d_%d" % (b, j), [128, 2048], BF16) for j in range(12)] for b in range(2)]
    stg = [A("stg%d" % i, [128, 2048], F32) for i in range(3)]
    xc = [A("xc%d" % i, [128, 16, 512], BF16) for i in range(2)]
    gs = A("gs", [128, 4, 512], BF16); hT = A("hT", [128, 4, 512], BF16)
    yt = [A("yt%d" % i, [128, D], F32) for i in range(3)]
    rs = A("rs", [128, NSL // 128, 8], F32)
    T.op("act", lambda e: e.dma_start(out=rs[:], in_=rws.rearrange("(st p) e -> p st e", p=128)), writes=["rs"], dma=True)
    acc = [P_("acc%d" % i, [128, 512], F32) for i in range(4)]
    acc2 = [P_("accd%d" % i, [128, 512], F32) for i in range(2)]
    ld = [0]

    def load_weights(e):
        b = e % 2
        for j in range(12):
            sb = ld[0] % 3
            ld[0] += 1
            if j < 8:
                src = (wg if j < 4 else wu)[e, (j % 4) * 512:(j % 4 + 1) * 512, :].rearrange("(kk p) n -> p kk n", p=128)
                dst = stg[sb][:].rearrange("p (kk n) -> p kk n", kk=4)
            else:
                src = wd[e, (j - 8) * 128:(j - 7) * 128, :]
                dst = stg[sb][:]
            T.op("sp", lambda e_: e_.dma_start(out=dst, in_=src), writes=["stg%d" % sb], dma=True)
            eng = "pool" if j % 2 == 0 else "act"
            if eng == "pool":
                T.op("pool", lambda e_: e_.tensor_copy(out=W[b][j][:], in_=stg[sb][:]), reads=["stg%d" % sb], writes=["w%d_%d" % (b, j)])
            else:
                T.op("act", lambda e_: e_.copy(out=W[b][j][:], in_=stg[sb][:]), reads=["stg%d" % sb], writes=["w%d_%d" % (b, j)])

    load_weights(0)
    dn = 0
    chunks = [(e, sc) for e in range(NE) for sc in range(NSC)]
    tiles = [(e, sc, st) for e in range(NE) for sc in range(NSC) for st in range(4)]

    def load_chunk(ci):
        e, sc = chunks[ci]
        xb = xc[ci % 2]
        T.op("act", lambda e_: e_.dma_start(out=xb[:], in_=xs[:, sc * 512:(sc + 1) * 512].rearrange("(k p) s -> p k s", p=128)),
             writes=["xc%d" % (ci % 2)], dma=True)

    def load_ytile(ti):
        e, sc, st = tiles[ti]
        if e == 0:
            return
        sl = sc * 4 + st
        ytb = yt[ti % 3]
        T.op("act", lambda e_: e_.dma_start(out=ytb[:], in_=ys[sl * 128:(sl + 1) * 128, :]), reads=["ys%d" % sl], writes=["yt%d" % (ti % 3)], dma=True)

    load_chunk(0)
    for ci, (e, sc) in enumerate(chunks):
        b = e % 2
        if sc == 0 and e + 1 < NE:
            load_weights(e + 1)
        if ci + 1 < len(chunks):
            load_chunk(ci + 1)
        xb = xc[ci % 2]
        xk = "xc%d" % (ci % 2)
        for ph in range(2):
            for fc in range(4):
                for k in range(16):
                    wt = W[b][ph * 4 + k // 4]
                    T.op("pe", lambda e_: e_.matmul(acc[fc][:], lhsT=wt[:, (k % 4) * 512 + fc * 128:(k % 4) * 512 + (fc + 1) * 128], rhs=xb[:, k, :],
                                                    start=(k == 0), stop=(k == 15)), reads=["w%d_%d" % (b, ph * 4 + k // 4), xk], writes=["acc%d" % fc])
                if ph == 0:
                    T.op("act", lambda e_: e_.activation(out=gs[:, fc, :], in_=acc[fc][:], func=AF.Silu), writes=["gs%d" % fc, "acc%d" % fc])
                else:
                    T.op("dve", lambda e_: e_.tensor_tensor(out=hT[:, fc, :], in0=acc[fc][:], in1=gs[:, fc, :], op=ALU.mult),
                         reads=["gs%d" % fc], writes=["hT%d" % fc, "acc%d" % fc])
        for st in range(4):
            ti = ci * 4 + st
            sl = sc * 4 + st
            ytb = yt[ti % 3]
            yk = "yt%d" % (ti % 3)
            if ti + 1 < len(tiles):
                load_ytile(ti + 1)
            for dmc in range(4):
                a2 = dn % 2
                dn += 1
                for fc in range(4):
                    T.op("pe", lambda e_: e_.matmul(acc2[a2][:], lhsT=hT[:, fc, st * 128:(st + 1) * 128], rhs=W[b][8 + fc][:, dmc * 512:(dmc + 1) * 512],
                                                    start=(fc == 0), stop=(fc == 3)), reads=["hT%d" % fc, "w%d_%d" % (b, 8 + fc)], writes=["accd%d" % a2])
                if e == 0:
                    T.op("dve", lambda e_: e_.tensor_scalar(out=ytb[:, dmc * 512:(dmc + 1) * 512], in0=acc2[a2][:], scalar1=rs[:, sl, e:e + 1], scalar2=None,
                                                            op0=ALU.mult), reads=["rs"], writes=[yk, "accd%d" % a2])
                else:
                    T.op("dve", lambda e_: e_.scalar_tensor_tensor(out=ytb[:, dmc * 512:(dmc + 1) * 512], in0=acc2[a2][:], scalar=rs[:, sl, e:e + 1],
                                                                   in1=ytb[:, dmc * 512:(dmc + 1) * 512], op0=ALU.mult, op1=ALU.add),
                         reads=["rs"], writes=[yk, "accd%d" % a2])
            T.op("pool", lambda e_: e_.dma_start(out=ys[sl * 128:(sl + 1) * 128, :], in_=ytb[:]), reads=[yk], writes=["ys%d" % sl], dma=True)
    T.finish("sp")
    T.finish("pool")
    print("EXP n_ins", T.n_ins, "n_wait", T.n_wait)
    return nc


def build_combine(TPC=TPC):
    nc = bass.Bass("TRN2", target_bir_lowering=False)
    NT = TPC // 128
    A = lambda name, shape, dt: nc.alloc_sbuf_tensor("sb_" + name, shape, dt)
    P_ = nc.alloc_psum_tensor
    dI = lambda name, shape: nc.dram_tensor(name, shape, F32, kind="ExternalInput").ap()
    ysl = dI("ysl", [8, CAP, D]); oh = dI("oh", [TPC, 8]); x1 = dI("x1", [TPC, D]); iotad = dI("iota", [128, CAP])
    g2b = dI("g2b", [128, D]); ln2g = dI("ln2g", [128, D]); ln2b = dI("ln2b", [128, D])
    out = nc.dram_tensor("out", [TPC, D], F32, kind="ExternalOutput").ap()
    y2d = nc.dram_tensor("y2d", [TPC, D], F32).ap()
    T = Trk(nc)
    ohs, rank = _rank_setup(nc, T, A, P_, oh, TPC)
    iota = A("iota", [128, CAP], F32)
    T.op("sp", lambda e: e.dma_start(out=iota[:], in_=iotad[:, :]), writes=["iota"], dma=True)
    identb = make_ident(nc, T, BF16, "identb")
    yh = A("yh", [128, 32, 1024], BF16)
    Pt = A("Pt", [128, 8, CAP], BF16)
    PT = [A("PT%d" % i, [128, 32, 128], BF16) for i in range(2)]
    yo = [A("yo%d" % i, [128, 1024], F32) for i in range(2)]
    tps = [P_("tps%d" % i, [128, 8, 128], BF16) for i in range(2)]
    acc = [P_("acc%d" % i, [128, 512], F32) for i in range(2)]
    ysv = ysl.rearrange("g (st p) d -> p g st d", p=128)
    tn = 0
    an = 0
    for h in range(2):
        for g in range(8):
            T.op("pool", lambda e: e.dma_start(out=yh[:, g * 4:(g + 1) * 4, :], in_=ysv[:, g, :, h * 1024:(h + 1) * 1024]), writes=["yh"], dma=True)
        for c in range(NT):
            ptb = PT[c % 2]
            pk = "PT%d" % (c % 2)
            for g in range(8):
                T.op("dve", lambda e: e.tensor_scalar(out=Pt[:, g, :], in0=iota[:], scalar1=rank[:, c, g:g + 1], scalar2=ohs[:, c, g:g + 1],
                                                      op0=ALU.is_equal, op1=ALU.mult), reads=["iota", "rank", "ohs"], writes=["Pt"])
            for q in range(4):
                tb = tn % 2
                tn += 1
                for j in range(8):
                    gi = q * 8 + j
                    T.op("pe", lambda e: e.transpose(out=tps[tb][:, j, :], in_=Pt[:, gi // 4, (gi % 4) * 128:(gi % 4 + 1) * 128], identity=identb[:]),
                         reads=["Pt", "identb"], writes=["tps%d" % tb])
                T.op("act", lambda e: e.copy(out=ptb[:, q * 8:(q + 1) * 8, :], in_=tps[tb][:]), writes=[pk, "tps%d" % tb])
            yb = yo[c % 2]
            for dq in range(2):
                a = an % 2
                an += 1
                for gi in range(32):
                    T.op("pe", lambda e: e.matmul(acc[a][:], lhsT=ptb[:, gi, :], rhs=yh[:, gi, dq * 512:(dq + 1) * 512], start=(gi == 0), stop=(gi == 31)),
                         reads=[pk, "yh"], writes=["acc%d" % a])
                T.op("dve", lambda e: e.tensor_copy(out=yb[:, dq * 512:(dq + 1) * 512], in_=acc[a][:]), writes=["yo%d" % (c % 2), "acc%d" % a])
            T.op("sp", lambda e: e.dma_start(out=y2d[c * 128:(c + 1) * 128, h * 1024:(h + 1) * 1024], in_=yb[:]), reads=["yo%d" % (c % 2)], writes=["y2d%d" % c], dma=True)
    g2p = A("g2p", [128, D], F32); lg_ = A("lng", [128, D], F32); lb_ = A("lnb", [128, D], F32)
    T.op("sp", lambda e: e.dma_start(out=g2p[:], in_=g2b[:, :]), writes=["g2p"], dma=True)
    T.op("sp", lambda e: e.dma_start(out=lg_[:], in_=ln2g[:, :]), writes=["lng"], dma=True)
    T.op("sp", lambda e: e.dma_start(out=lb_[:], in_=ln2b[:, :]), writes=["lnb"], dma=True)
    T.op("dve", lambda e: e.tensor_scalar(out=g2p[:], in0=g2p[:], scalar1=1.0, scalar2=None, op0=ALU.add), reads=["g2p"], writes=["g2p"])
    xt = [A("xt%d" % i, [128, D], F32) for i in range(2)]
    y2 = [A("y2%d" % i, [128, D], F32) for i in range(2)]
    junk = A("junk", [128, D], BF16)
    st = A("st", [128, 16], F32)
    def ld_f(c_):
        b_ = c_ % 2
        T.op("act", lambda e: e.dma_start(out=xt[b_][:], in_=x1[c_ * 128:(c_ + 1) * 128, :]), writes=["xt%d" % b_], dma=True)
        T.op("act", lambda e: e.dma_start(out=y2[b_][:], in_=y2d[c_ * 128:(c_ + 1) * 128, :]), reads=["y2d%d" % c_], writes=["y2%d" % b_], dma=True)
    ld_f(0)
    for c in range(NT):
        b = c % 2
        if c + 1 < NT:
            ld_f(c + 1)
        v = y2[b]
        vk = "y2%d" % b
        T.op("dve", lambda e: e.tensor_tensor(out=v[:], in0=v[:], in1=g2p[:], op=ALU.mult), reads=[vk, "g2p"], writes=[vk])
        T.op("dve", lambda e: e.scalar_tensor_tensor(out=v[:], in0=xt[b][:], scalar=ALPHA, in1=v[:], op0=ALU.mult, op1=ALU.add), reads=["xt%d" % b, vk], writes=[vk])
        T.op("dve", lambda e: e.reduce_sum(out=st[:, 0:1], in_=v[:], axis=AX.X), reads=[vk], writes=["st0"])
        T.op("act", lambda e: e.activation(out=junk[:], in_=v[:], func=AF.Square, accum_out=st[:, 1:2]), reads=[vk], writes=["st1", "junk"])
        T.op("dve", lambda e: e.tensor_scalar(out=st[:, 2:3], in0=st[:, 0:1], scalar1=1.0 / D, scalar2=None, op0=ALU.mult), reads=["st0"], writes=["st2"])
        T.op("dve", lambda e: e.tensor_tensor(out=st[:, 3:4], in0=st[:, 2:3], in1=st[:, 2:3], op=ALU.mult), reads=["st2"], writes=["st3"])
        T.op("dve", lambda e: e.scalar_tensor_tensor(out=st[:, 4:5], in0=st[:, 1:2], scalar=1.0 / D, in1=st[:, 3:4], op0=ALU.mult, op1=ALU.subtract),
             reads=["st1", "st3"], writes=["st4"])
        T.op("dve", lambda e: e.tensor_scalar(out=st[:, 6:7], in0=st[:, 4:5], scalar1=LN_EPS, scalar2=None, op0=ALU.add), reads=["st4"], writes=["st6"])
        T.op("act", lambda e: e.activation(out=st[:, 7:8], in_=st[:, 6:7], func=AF.Sqrt), reads=["st6"], writes=["st7"])
        T.op("dve", lambda e: e.reciprocal(out=st[:, 5:6], in_=st[:, 7:8]), reads=["st7"], writes=["st5"])
        T.op("dve", lambda e: e.tensor_scalar(out=v[:], in0=v[:], scalar1=st[:, 2:3], scalar2=st[:, 5:6], op0=ALU.subtract, op1=ALU.mult),
             reads=[vk, "st2", "st5"], writes=[vk])
        T.op("dve", lambda e: e.tensor_tensor(out=v[:], in0=v[:], in1=lg_[:], op=ALU.mult), reads=[vk, "lng"], writes=[vk])
        T.op("dve", lambda e: e.tensor_tensor(out=v[:], in0=v[:], in1=lb_[:], op=ALU.add), reads=[vk, "lnb"], writes=[vk])
        T.op("sp", lambda e: e.dma_start(out=out[c * 128:(c + 1) * 128, :], in_=v[:]), reads=[vk], writes=["out"], dma=True)
    T.finish("sp")
    print("COMB n_ins", T.n_ins, "n_wait", T.n_wait)
    return nc


def run_moe(xn2, rw, oh, x1, modrow, w_gate, w_up, w_down, ln2_g, ln2_b, TPC=TPC, cores=NCORES, ne=8):
    rep = lambda a: np.ascontiguousarray(np.broadcast_to(a.reshape(1, -1), (128, a.size))).astype(np.float32)
    iota = rep(np.arange(CAP, dtype=np.float32))
    mod2 = np.ascontiguousarray(np.concatenate([modrow[3 * D:4 * D].reshape(16, 128).T, modrow[4 * D:5 * D].reshape(16, 128).T], axis=1))
    nc1 = build_dispatch(TPC)
    maps = [{"xn2": np.ascontiguousarray(xn2[c * TPC:(c + 1) * TPC]), "oh": np.ascontiguousarray(oh[c * TPC:(c + 1) * TPC]),
             "rw": np.ascontiguousarray(rw[c * TPC:(c + 1) * TPC]), "mod2": mod2, "iota": iota} for c in range(cores)]
    r1 = _launch("dispatch", nc1, maps, cores).results
    nc2 = build_experts(cores * CAP, ne)
    maps2 = []
    for g in range(8):
        maps2.append({"xs": np.ascontiguousarray(np.concatenate([r1[c]["xs"][g] for c in range(cores)], axis=1)),
                      "rws": np.ascontiguousarray(np.concatenate([r1[c]["rws"][g] for c in range(cores)], axis=0)),
                      "wg": np.ascontiguousarray(w_gate[0][g * 8:g * 8 + ne]), "wu": np.ascontiguousarray(w_up[0][g * 8:g * 8 + ne]),
                      "wd": np.ascontiguousarray(w_down[0][g * 8:g * 8 + ne])})
    r2 = _launch("experts", nc2, maps2, 8).results
    nc3 = build_combine(TPC)
    maps3 = []
    for c in range(cores):
        maps3.append({"ysl": np.ascontiguousarray(np.stack([r2[g]["ys"][c * CAP:(c + 1) * CAP] for g in range(8)], axis=0)),
                      "oh": np.ascontiguousarray(oh[c * TPC:(c + 1) * TPC]), "x1": np.ascontiguousarray(x1[c * TPC:(c + 1) * TPC]), "iota": iota,
                      "g2b": rep(modrow[5 * D:6 * D]), "ln2g": rep(ln2_g[0]), "ln2b": rep(ln2_b[0])})
    r3 = _launch("combine", nc3, maps3, cores).results
    return np.concatenate([r["out"] for r in r3], axis=0)


def build_mod():
    nc = bass.Bass("TRN2", target_bir_lowering=False)
    cT = nc.dram_tensor("cT", [128, 16], F32, kind="ExternalInput").ap()
    wada = nc.dram_tensor("wada", [D, 1536], F32, kind="ExternalInput").ap()
    badaT = nc.dram_tensor("badaT", [128, 12], F32, kind="ExternalInput").ap()
    modT = nc.dram_tensor("modT", [128, 12], F32, kind="ExternalOutput").ap()
    T = Trk(nc)
    A = lambda name, shape, dt: nc.alloc_sbuf_tensor("sb_" + name, shape, dt)
    wst = [A("wst%d" % i, [128, 16, 256], F32) for i in range(2)]
    cs = A("cs", [128, 16], F32); ca = A("ca", [128, 16], F32); bad = A("bad", [128, 12], F32); mo = A("mo", [128, 12], F32)
    modps = nc.alloc_psum_tensor("modps", [128, 512], F32)
    T.op("sp", lambda e: e.dma_start(out=cs[:], in_=cT[:, :]), writes=["cs"], dma=True)
    T.op("sp", lambda e: e.dma_start(out=bad[:], in_=badaT[:, :]), writes=["bad"], dma=True)
    T.op("act", lambda e: e.activation(out=ca[:], in_=cs[:], func=AF.Silu), reads=["cs"], writes=["ca"])
    wv = wada.rearrange("(k p) n -> p k n", p=128)
    for g in range(6):
        b = g % 2
        T.op("sp", lambda e: e.dma_start(out=wst[b][:], in_=wv[:, :, g * 256:(g + 1) * 256]), writes=["wst%d" % b], dma=True)
        for j in range(2):
            n = g * 2 + j
            for k in range(16):
                T.op("pe", lambda e: e.matmul(modps[:, n:n + 1], lhsT=wst[b][:, k, j * 128:(j + 1) * 128], rhs=ca[:, k:k + 1], start=(k == 0), stop=(k == 15)),
                     reads=["wst%d" % b, "ca"], writes=["modps"])
    T.op("dve", lambda e: e.tensor_tensor(out=mo[:], in0=modps[:, 0:12], in1=bad[:], op=ALU.add), reads=["bad"], writes=["mo", "modps"])
    T.op("sp", lambda e: e.dma_start(out=modT[:, :], in_=mo[:]), reads=["mo"], writes=["modT"], dma=True)
    T.finish("sp")
    return nc


def run_mod(c, w_ada, b_ada):
    nc = build_mod()
    cT = np.ascontiguousarray(c.reshape(16, 128).T)
    maps = [{"cT": cT, "wada": np.ascontiguousarray(w_ada[0][:, i * 1536:(i + 1) * 1536]),
             "badaT": np.ascontiguousarray(b_ada[0][i * 1536:(i + 1) * 1536].reshape(12, 128).T)} for i in range(NCORES)]
    res = _launch("mod", nc, maps, NCORES)
    return np.concatenate([r["modT"].T.reshape(-1) for r in res.results])


def kernel(x, c, w_ada, b_ada, w_in, b_fgt, t5_table, cmp_pe, cmp_w1, cmp_b1, cmp_w2, w_br_nsa, w_br_fox, w_o, ln1_g, ln1_b,
           w_rg, b_rg, w_re, b_re, w_gate, w_up, w_down, ln2_g, ln2_b):
    f = lambda a: np.asarray(a, dtype=np.float32)
    x, c, w_ada, b_ada, w_in = f(x), f(c), f(w_ada), f(b_ada), f(w_in)
    modrow = run_mod(c, w_ada, b_ada)
    proj, projb = run_l1(x, modrow, w_in)
    o_fox = run_fox(proj, projb, f(b_fgt))
    o_nsa = run_nsa(proj, projb, f(t5_table), f(cmp_pe), f(cmp_w1), f(cmp_b1), f(cmp_w2))
    x1, xn2, rw, oh = run_l3a(o_nsa, o_fox, proj, x, modrow, f(w_br_nsa), f(w_br_fox), f(w_o), f(ln1_g), f(ln1_b), f(w_rg), f(b_rg), f(w_re), f(b_re))
    out = run_moe(xn2, rw, oh, x1, modrow, f(w_gate), f(w_up), f(w_down), f(ln2_g), f(ln2_b))
    return out.reshape(1, S, D).astype(np.float32)
```

```python
import numpy as np
import concourse.bass as bass
import concourse.mybir as mybir
from concourse.bass_utils import run_bass_kernel_spmd

F32 = mybir.dt.float32
BF16 = mybir.dt.bfloat16
AF = mybir.ActivationFunctionType
ALU = mybir.AluOpType
AX = mybir.AxisListType

NCORES = 8
D = 2048
S = 16384
TPC = S // NCORES
IN_COLS = 6944
LN_EPS = 1e-5


def _launch(name, nc, maps, cores):
    res = run_bass_kernel_spmd(nc, maps, core_ids=list(range(cores)))
    try:
        if getattr(res, "exec_time_ns", None) is not None:
            print("[launch] %s exec_time_ns=%s" % (name, res.exec_time_ns), flush=True)
    except Exception:
        pass
    return res


class Trk:
    def __init__(self, nc, n_dma_sems=14):
        self.nc = nc
        self.eng = {"pe": nc.tensor, "act": nc.scalar, "dve": nc.vector,
                    "pool": nc.gpsimd, "sp": nc.sync}
        self.sem = {}
        self.cnt = {}
        self.waited = {k: {} for k in self.eng}
        self._ctx = []
        for k in ["pe", "act", "dve", "pool"]:
            g = nc.semaphore("s_" + k)
            self.sem[k] = g.__enter__()
            self._ctx.append(g)
            self.cnt[k] = 0
        self.dsem = []
        self.dcnt = []
        self.dq = {}
        for q in ["sp", "act", "pool"]:
            self.dq[q] = [len(self.dsem) + j for j in range(n_dma_sems)]
            for j in range(n_dma_sems):
                g = nc.semaphore("s_dma_%s%d" % (q, j))
                self.dsem.append(g.__enter__())
                self._ctx.append(g)
                self.dcnt.append(0)
        self.dqn = {"sp": 0, "act": 0, "pool": 0}
        self.dnext = 0
        self.st = {}
        self.n_ins = 0
        self.n_wait = 0

    def _semobj(self, sk):
        return self.sem[sk] if isinstance(sk, str) else self.dsem[sk]

    def _wait(self, e, deps):
        best = {}
        for d in deps:
            if d is None:
                continue
            sk, v = d
            if v > best.get(sk, 0):
                best[sk] = v
        for sk, v in best.items():
            if e == "pe" and sk == "pe":
                continue
            if self.waited[e].get(sk, 0) >= v:
                continue
            self.eng[e].wait_ge(self._semobj(sk), v)
            self.waited[e][sk] = v
            self.n_wait += 1

    def op(self, e, fn, reads=(), writes=(), dma=False):
        deps = []
        for k in reads:
            s = self.st.get(k)
            if s is not None:
                deps.append(s[0])
        for k in writes:
            s = self.st.get(k)
            if s is not None:
                deps.append(s[0])
                deps.extend(s[1])
        if dma:
            i = self.dq[e][self.dqn[e] % len(self.dq[e])]
            self.dqn[e] += 1
            if self.dcnt[i] > 0:
                deps.append((i, self.dcnt[i]))
        self._wait(e, deps)
        ins = fn(self.eng[e])
        if dma:
            self.dcnt[i] += 16
            ins.then_inc(self.dsem[i], 16)
            tag = (i, self.dcnt[i])
        else:
            self.cnt[e] += 1
            ins.then_inc(self.sem[e], 1)
            tag = (e, self.cnt[e])
        for k in reads:
            s = self.st.setdefault(k, [None, []])
            s[1].append(tag)
            if len(s[1]) > 48:
                best = {}
                for sk, v in s[1]:
                    if v > best.get(sk, 0):
                        best[sk] = v
                s[1] = list(best.items())
        for k in writes:
            self.st[k] = [tag, []]
        self.n_ins += 1
        return tag

    def finish(self, e="sp"):
        deps = []
        for k, s in self.st.items():
            deps.append(s[0])
            deps.extend(s[1])
        self._wait(e, deps)


def make_ident(nc, T, dtype, name="ident"):
    ident = nc.alloc_sbuf_tensor(name, [128, 128], dtype)
    T.op("pool", lambda e: e.memset(ident[:], 1.0), writes=[name])
    T.op("pool", lambda e: e.affine_select(out=ident[:], in_=ident[:], pattern=[[-1, 128]],
                                           compare_op=ALU.is_equal, fill=0.0, base=0,
                                           channel_multiplier=1), reads=[name], writes=[name])
    return ident


def build_l1(stop=None, TPC=TPC):
    nc = bass.Bass("TRN2", target_bir_lowering=False)
    x = nc.dram_tensor("x", [TPC, D], F32, kind="ExternalInput").ap()
    cT = nc.dram_tensor("cT", [128, 16], F32, kind="ExternalInput").ap()
    wada = nc.dram_tensor("wada", [D, 8], F32, kind="ExternalInput").ap()
    badaT = nc.dram_tensor("badaT", [128, 32], F32, kind="ExternalInput").ap()
    w_in = nc.dram_tensor("w_in", [D, IN_COLS], F32, kind="ExternalInput").ap()
    proj = nc.dram_tensor("proj", [TPC, IN_COLS], F32, kind="ExternalOutput").ap()
    projb = nc.dram_tensor("projb", [TPC, 3072], BF16, kind="ExternalOutput").ap()
    T = Trk(nc)
    NT = TPC // 128
    obf = [nc.alloc_sbuf_tensor("obf%d" % i, [128, 512], BF16) for i in range(2)]

    wst = [nc.alloc_sbuf_tensor("wst%d" % i, [128, 8, 512], F32) for i in range(2)]
    wbf = [nc.alloc_sbuf_tensor("wbf%d" % i, [128, 16, 512], BF16) for i in range(2)]
    uT = nc.alloc_sbuf_tensor("uT", [128, 16, TPC], BF16)
    xt = [nc.alloc_sbuf_tensor("xt%d" % i, [128, D], F32) for i in range(2)]
    xn = nc.alloc_sbuf_tensor("xn", [128, D], BF16)
    junk = nc.alloc_sbuf_tensor("junk", [128, D], BF16)
    ost = [nc.alloc_sbuf_tensor("ost%d" % i, [128, 512], F32) for i in range(4)]
    cs = nc.alloc_sbuf_tensor("cs", [128, 16], F32)
    ca = nc.alloc_sbuf_tensor("ca", [128, 16], F32)
    bad = nc.alloc_sbuf_tensor("bad", [128, 32], F32)
    modT = nc.alloc_sbuf_tensor("modT", [128, 32], F32)
    st = nc.alloc_sbuf_tensor("st", [128, 16], F32)
    tp = [nc.alloc_psum_tensor("tp%d" % i, [128, 8, 128], BF16) for i in range(2)]
    acc = [nc.alloc_psum_tensor("acc%d" % i, [128, 512], F32) for i in range(4)]
    modps = nc.alloc_psum_tensor("modps", [128, 512], F32)
    ident = make_ident(nc, T, BF16)

    T.op("sp", lambda e: e.dma_start(out=modT[:], in_=badaT[:, :]), writes=["modT"], dma=True)
    T.op("dve", lambda e: e.tensor_scalar(out=modT[:, 16:32], in0=modT[:, 16:32], scalar1=1.0, scalar2=None, op0=ALU.add),
         reads=["modT"], writes=["modT"])
    ld = 0

    if stop == "A":
        T.op("sp", lambda e: e.dma_start(out=proj[0:128, 0:32], in_=modT[:]), reads=["modT"], writes=["proj"], dma=True)
        T.finish("sp")
        return nc
    def ld_x(i_):
        b_ = i_ % 2
        T.op("sp", lambda e: e.dma_start(out=xt[b_][:], in_=x[i_ * 128:(i_ + 1) * 128, :]), writes=["xt%d" % b_], dma=True)
    ld_x(0)
    for i in range(NT):
        b = i % 2
        if i + 1 < NT:
            ld_x(i + 1)
        T.op("dve", lambda e: e.reduce_sum(out=st[:, 0:1], in_=xt[b][:], axis=AX.X), reads=["xt%d" % b], writes=["st0"])
        T.op("act", lambda e: e.activation(out=junk[:], in_=xt[b][:], func=AF.Square, accum_out=st[:, 1:2]),
             reads=["xt%d" % b], writes=["st1", "junk"])
        T.op("dve", lambda e: e.tensor_scalar(out=st[:, 2:3], in0=st[:, 0:1], scalar1=1.0 / D, scalar2=None, op0=ALU.mult),
             reads=["st0"], writes=["st2"])
        T.op("dve", lambda e: e.tensor_tensor(out=st[:, 3:4], in0=st[:, 2:3], in1=st[:, 2:3], op=ALU.mult),
             reads=["st2"], writes=["st3"])
        T.op("dve", lambda e: e.scalar_tensor_tensor(out=st[:, 4:5], in0=st[:, 1:2], scalar=1.0 / D, in1=st[:, 3:4],
                                                     op0=ALU.mult, op1=ALU.subtract), reads=["st1", "st3"], writes=["st4"])
        T.op("dve", lambda e: e.tensor_scalar(out=st[:, 6:7], in0=st[:, 4:5], scalar1=LN_EPS, scalar2=None, op0=ALU.add),
             reads=["st4"], writes=["st6"])
        T.op("act", lambda e: e.activation(out=st[:, 7:8], in_=st[:, 6:7], func=AF.Sqrt), reads=["st6"], writes=["st7"])
        T.op("dve", lambda e: e.reciprocal(out=st[:, 5:6], in_=st[:, 7:8]), reads=["st7"], writes=["st5"])
        T.op("dve", lambda e: e.tensor_scalar(out=xn[:], in0=xt[b][:], scalar1=st[:, 2:3], scalar2=st[:, 5:6],
                                              op0=ALU.subtract, op1=ALU.mult), reads=["xt%d" % b, "st2", "st5"], writes=["xn"])
        for kq in range(2):
            pb = kq % 2
            for j in range(8):
                k = kq * 8 + j
                T.op("pe", lambda e: e.transpose(out=tp[pb][:, j, :], in_=xn[:, k * 128:(k + 1) * 128], identity=ident[:]),
                     reads=["xn", "ident"], writes=["tp%d" % pb])
            for j in range(8):
                k = kq * 8 + j
                T.op("act", lambda e: e.activation(out=uT[:, k, i * 128:(i + 1) * 128], in_=tp[pb][:, j, :], func=AF.Identity,
                                                   scale=modT[:, 16 + k:17 + k], bias=modT[:, k:k + 1]),
                     reads=["modT"], writes=["uT_%d" % i, "tp%d" % pb])

    if stop in ("B", "B1", "B2"):
        T.op("sp", lambda e: e.dma_start(out=proj[0:128, 0:32], in_=modT[:]), reads=["modT"], writes=["proj"], dma=True)
        T.finish("sp")
        return nc
    w_v = w_in.rearrange("(k p) n -> p k n", p=128)
    ngrp = (IN_COLS + 511) // 512
    ev = 0
    for g in range(ngrp):
        c0 = g * 512
        cw = min(512, IN_COLS - c0)
        wb = g % 2
        for kh in range(2):
            b = ld % 2
            ld += 1
            T.op("sp", lambda e: e.dma_start(out=wst[b][:, :, 0:cw], in_=w_v[:, kh * 8:(kh + 1) * 8, c0:c0 + cw]),
                 writes=["wst%d" % b], dma=True)
            T.op("pool", lambda e: e.tensor_copy(out=wbf[wb][:, kh * 8:(kh + 1) * 8, 0:cw], in_=wst[b][:, :, 0:cw]),
                 reads=["wst%d" % b], writes=["wbf%d_%d" % (wb, kh)])
        for i in range(NT):
            a = ev % 4
            for k in range(16):
                T.op("pe", lambda e: e.matmul(acc[a][:, 0:cw], lhsT=uT[:, k, i * 128:(i + 1) * 128], rhs=wbf[wb][:, k, 0:cw],
                                              start=(k == 0), stop=(k == 15)),
                     reads=["uT_%d" % i, "wbf%d_%d" % (wb, k // 8)], writes=["acc%d" % a])
            if ev % 2 == 0:
                T.op("act", lambda e: e.copy(out=ost[a][:, 0:cw], in_=acc[a][:, 0:cw]), writes=["ost%d" % a, "acc%d" % a])
            else:
                T.op("dve", lambda e: e.tensor_copy(out=ost[a][:, 0:cw], in_=acc[a][:, 0:cw]), writes=["ost%d" % a, "acc%d" % a])
            T.op("pool", lambda e: e.dma_start(out=proj[i * 128:(i + 1) * 128, c0:c0 + cw], in_=ost[a][:, 0:cw]),
                 reads=["ost%d" % a], writes=["proj"], dma=True)
            if g < 6:
                ob = obf[ev % 2]
                if ev % 2 == 0:
                    T.op("dve", lambda e: e.tensor_copy(out=ob[:], in_=acc[a][:]), writes=["obf%d" % (ev % 2), "acc%d" % a])
                else:
                    T.op("act", lambda e: e.copy(out=ob[:], in_=acc[a][:]), writes=["obf%d" % (ev % 2), "acc%d" % a])
                T.op("sp", lambda e: e.dma_start(out=projb[i * 128:(i + 1) * 128, c0:c0 + 512], in_=ob[:]),
                     reads=["obf%d" % (ev % 2)], writes=["projb"], dma=True)
            ev += 1
    T.finish("sp")
    T.finish("pool")
    print("L1 n_ins", T.n_ins, "n_wait", T.n_wait)
    return nc


def run_l1(x, modrow, w_in, stop=None, TPC=TPC, NCORES=NCORES):
    nc = build_l1(stop, TPC)
    x2 = np.ascontiguousarray(x.reshape(S, D))
    mod1 = np.ascontiguousarray(np.concatenate([modrow[0:D].reshape(16, 128).T, modrow[D:2 * D].reshape(16, 128).T], axis=1))
    w = np.ascontiguousarray(w_in[0])
    dummy = np.zeros((128, 16), np.float32)
    in_maps = [{"x": x2[i * TPC:(i + 1) * TPC], "cT": dummy, "wada": np.zeros((D, 8), np.float32), "badaT": mod1, "w_in": w} for i in range(NCORES)]
    res = _launch("l1", nc, in_maps, NCORES)
    return np.concatenate([r["proj"] for r in res.results], axis=0), np.concatenate([r["projb"] for r in res.results], axis=0)


def build_masks(nc, T, name="cmask"):
    mk = nc.alloc_sbuf_tensor(name, [128, 4, 512], BF16)
    T.op("pool", lambda e: e.memset(mk[:], 0.0), writes=[name])
    for j in range(4):
        T.op("pool", lambda e: e.affine_select(out=mk[:, j, :], in_=mk[:, j, :], pattern=[[1, 512]],
                                               compare_op=ALU.is_ge, fill=-30000.0, base=-128 * j,
                                               channel_multiplier=-1), reads=[name], writes=[name])
    return mk


def build_fox(Sx=S):
    nc = bass.Bass("TRN2", target_bir_lowering=False)
    NKB = Sx // 128
    NQC = Sx // 512
    qT = nc.dram_tensor("qT", [64, Sx], BF16, kind="ExternalInput").ap()
    kT = nc.dram_tensor("kT", [64, Sx], BF16, kind="ExternalInput").ap()
    v = nc.dram_tensor("v", [Sx, 64], BF16, kind="ExternalInput").ap()
    f2 = nc.dram_tensor("f2", [128, NKB], F32, kind="ExternalInput").ap()
    bfg = nc.dram_tensor("bfg", [128, 1], F32, kind="ExternalInput").ap()
    o = nc.dram_tensor("o", [Sx, 64], F32, kind="ExternalOutput").ap()
    scr = nc.dram_tensor("scr", [2, Sx], BF16).ap()
    T = Trk(nc)

    KTa = nc.alloc_sbuf_tensor("KTa", [128, Sx], BF16)
    QTa = nc.alloc_sbuf_tensor("QTa", [128, Sx], BF16)
    Vp = nc.alloc_sbuf_tensor("Vp", [128, NKB, 65], BF16)
    PT = [nc.alloc_sbuf_tensor("PT%d" % i, [128, 512], BF16) for i in range(4)]
    ost = [nc.alloc_sbuf_tensor("ost%d" % i, [128, 4, 64], F32) for i in range(2)]
    rc = nc.alloc_sbuf_tensor("rc", [128, 4], F32)
    fz = nc.alloc_sbuf_tensor("fz", [128, NKB], F32)
    lf = nc.alloc_sbuf_tensor("lf", [128, NKB], F32)
    nb = nc.alloc_sbuf_tensor("nb", [128, 1], F32)
    Usb = nc.alloc_sbuf_tensor("Usb", [128, 128], F32)
    SUsb = nc.alloc_sbuf_tensor("SUsb", [128, 128], F32)
    ones = nc.alloc_sbuf_tensor("ones", [128, 128], F32)
    totT = nc.alloc_sbuf_tensor("totT", [128, 128], F32)
    Fsb = nc.alloc_sbuf_tensor("Fsb", [128, NKB], F32)
    Fofs = nc.alloc_sbuf_tensor("Fofs", [128, NKB], F32)
    Gq = nc.alloc_sbuf_tensor("Gq", [128, NKB], F32)
    Ghi = nc.alloc_sbuf_tensor("Ghi", [128, 128], BF16)
    Ghf = nc.alloc_sbuf_tensor("Ghf", [128, NKB], F32)
    Glo = nc.alloc_sbuf_tensor("Glo", [128, 128], BF16)
    GT = nc.alloc_sbuf_tensor("GT", [128, 2, 128], BF16)
    bm = [nc.alloc_sbuf_tensor("bm%d" % i, [128, NKB], F32) for i in range(2)]
    Sps = [nc.alloc_psum_tensor("Sps%d" % i, [128, 512], F32) for i in range(3)]
    Ops = [nc.alloc_psum_tensor("Ops%d" % i, [128, 4, 128], F32) for i in range(2)]
    Fps = nc.alloc_psum_tensor("Fps", [128, 512], F32)
    Tps = nc.alloc_psum_tensor("Tps", [128, 8, 128], BF16)
    identb = make_ident(nc, T, BF16, "identb")
    mk = build_masks(nc, T)

    T.op("sp", lambda e: e.dma_start(out=fz[:], in_=f2[:, :]), writes=["fz"], dma=True)
    T.op("sp", lambda e: e.dma_start(out=nb[:], in_=bfg[:, :]), writes=["nb"], dma=True)
    for h in range(0, Sx, 2048):
        w = min(2048, Sx - h)
        T.op("sp", lambda e: e.dma_start(out=KTa[0:64, h:h + w], in_=kT[:, h:h + w]), writes=["KTa"], dma=True)
        T.op("act", lambda e: e.dma_start(out=QTa[0:64, h:h + w], in_=qT[:, h:h + w]), writes=["QTa"], dma=True)
    vv = v.rearrange("(kb p) d -> p kb d", p=128)
    for h in range(0, NKB, 16):
        w = min(16, NKB - h)
        T.op("sp", lambda e: e.dma_start(out=Vp[:, h:h + w, 0:64], in_=vv[:, h:h + w, :]), writes=["Vp"], dma=True)
    T.op("dve", lambda e: e.memset(Vp[:, :, 64:65], 1.0), writes=["Vp1"])
    T.op("dve", lambda e: e.memset(KTa[64:66, :], 8.0), writes=["KTa8"])

    T.op("pool", lambda e: e.memset(ones[:], 1.0), writes=["ones"])
    T.op("pool", lambda e: e.memset(Usb[:], 1.0), writes=["Usb"])
    T.op("pool", lambda e: e.affine_select(out=Usb[:], in_=Usb[:], pattern=[[1, 128]], compare_op=ALU.is_ge, fill=0.0,
                                           base=0, channel_multiplier=-1), reads=["Usb"], writes=["Usb"])
    T.op("pool", lambda e: e.memset(SUsb[:], 1.0), writes=["SUsb"])
    T.op("pool", lambda e: e.affine_select(out=SUsb[:], in_=SUsb[:], pattern=[[1, 128]], compare_op=ALU.is_ge, fill=0.0,
                                           base=-1, channel_multiplier=-1), reads=["SUsb"], writes=["SUsb"])

    T.op("dve", lambda e: e.tensor_scalar(out=nb[:], in0=nb[:], scalar1=-1.0, scalar2=None, op0=ALU.mult), reads=["nb"], writes=["nb"])
    T.op("act", lambda e: e.activation(out=lf[:], in_=fz[:], func=AF.Exp, scale=-1.0, bias=nb[:, 0:1]), reads=["fz", "nb"], writes=["lf"])
    T.op("act", lambda e: e.activation(out=lf[:], in_=lf[:], func=AF.Ln, bias=1.0), reads=["lf"], writes=["lf"])
    T.op("dve", lambda e: e.tensor_scalar(out=lf[:], in0=lf[:], scalar1=-1.0, scalar2=None, op0=ALU.mult), reads=["lf"], writes=["lf"])
    T.op("pe", lambda e: e.matmul(Fps[0:NKB, 0:128], lhsT=lf[:, 0:NKB], rhs=ones[:, :], start=True, stop=True),
         reads=["lf", "ones"], writes=["Fps"])
    T.op("dve", lambda e: e.memset(totT[:], 0.0), writes=["totT"])
    T.op("dve", lambda e: e.tensor_copy(out=totT[0:NKB, :], in_=Fps[0:NKB, 0:128]), writes=["totT", "Fps"])
    T.op("pe", lambda e: e.matmul(Fps[:, 128:128 + NKB], lhsT=totT[:, :], rhs=SUsb[:, 0:NKB], start=True, stop=True),
         reads=["totT", "SUsb"], writes=["Fps"])
    T.op("dve", lambda e: e.tensor_copy(out=Fofs[:], in_=Fps[:, 128:128 + NKB]), writes=["Fofs", "Fps"])
    T.op("pe", lambda e: e.matmul(Fps[:, 256:256 + NKB], lhsT=Usb[:, :], rhs=lf[:, 0:NKB], start=True, stop=True),
         reads=["Usb", "lf"], writes=["Fps"])
    T.op("dve", lambda e: e.tensor_tensor(out=Fsb[:], in0=Fps[:, 256:256 + NKB], in1=Fofs[:], op=ALU.add),
         reads=["Fofs"], writes=["Fsb", "Fps"])
    Gv = Gq[:].rearrange("j (c f) -> j c f", f=4)
    Fv = Fsb[:].rearrange("j (c f) -> j c f", f=4)
    Ov = Fofs[:].rearrange("j (c f) -> j c f", f=4)
    for f in range(4):
        T.op("dve", lambda e: e.tensor_tensor(out=Gv[:, :, f], in0=Fv[:, :, f], in1=Ov[:, :, 0], op=ALU.subtract),
             reads=["Fsb", "Fofs"], writes=["Gq"])
    T.op("dve", lambda e: e.memset(Ghi[:], 0.0), writes=["Ghi"])
    T.op("dve", lambda e: e.memset(Glo[:], 0.0), writes=["Glo"])
    T.op("dve", lambda e: e.tensor_copy(out=Ghi[:, 0:NKB], in_=Gq[:]), reads=["Gq"], writes=["Ghi"])
    T.op("dve", lambda e: e.tensor_copy(out=Ghf[:], in_=Ghi[:, 0:NKB]), reads=["Ghi"], writes=["Ghf"])
    T.op("dve", lambda e: e.tensor_tensor(out=Glo[:, 0:NKB], in0=Gq[:], in1=Ghf[:], op=ALU.subtract), reads=["Gq", "Ghf"], writes=["Glo"])
    T.op("pe", lambda e: e.transpose(out=Tps[:, 0, :], in_=Ghi[:], identity=identb[:]), reads=["Ghi", "identb"], writes=["Tps"])
    T.op("pe", lambda e: e.transpose(out=Tps[:, 1, :], in_=Glo[:], identity=identb[:]), reads=["Glo", "identb"], writes=["Tps"])
    T.op("dve", lambda e: e.tensor_copy(out=GT[:], in_=Tps[:, 0:2, :]), writes=["GT", "Tps"])
    for r in range(2):
        T.op("sp", lambda e: e.dma_start(out=scr[r:r + 1, :].rearrange("o (p j) -> (o p) j", j=128), in_=GT[0:NKB, r, :]),
             reads=["GT"], writes=["scr"], dma=True)
    T.op("sp", lambda e: e.dma_start(out=QTa[64:66, :], in_=scr[:, :]), reads=["scr"], writes=["QTaG"], dma=True)

    it = 0
    for qc in range(NQC):
        ob = qc % 2
        nk = 4 * qc + 4
        bmq = bm[qc % 2]
        T.op("dve", lambda e: e.tensor_scalar(out=bmq[:, 0:nk], in0=Fsb[:, 0:nk], scalar1=-1.0, scalar2=Fofs[:, 4 * qc:4 * qc + 1],
                                              op0=ALU.mult, op1=ALU.add), reads=["Fsb", "Fofs"], writes=["bm%d" % (qc % 2)])
        T.op("dve", lambda e: e.memset(Ops[ob][:], 0.0), writes=["Ops%d" % ob])

        def qk(kb, it):
            sb = it % 3
            j = kb - 4 * qc
            T.op("pe", lambda e: e.matmul(Sps[sb][:], lhsT=KTa[0:66, kb * 128:(kb + 1) * 128], rhs=QTa[0:66, qc * 512:(qc + 1) * 512],
                                          start=True, stop=(j < 0)),
                 reads=["KTa", "KTa8", "QTa", "QTaG"], writes=["Sps%d" % sb])
            if j >= 0:
                T.op("pe", lambda e: e.matmul(Sps[sb][:], lhsT=identb[:], rhs=mk[:, j, :], start=False, stop=True),
                     reads=["identb", "cmask"], writes=["Sps%d" % sb])

        def ex_pv(kb, it):
            sb = it % 3
            pb = it % 4
            j = kb - 4 * qc
            T.op("act", lambda e: e.activation(out=PT[pb][:], in_=Sps[sb][:], func=AF.Exp, scale=0.125, bias=bmq[:, kb:kb + 1]),
                 reads=["bm%d" % (qc % 2)], writes=["PT%d" % pb, "Sps%d" % sb])
            for jj in range(max(j, 0), 4):
                T.op("pe", lambda e: e.matmul(Ops[ob][:, jj, 0:65], lhsT=PT[pb][:, jj * 128:(jj + 1) * 128], rhs=Vp[:, kb, :],
                                              start=False, stop=False, skip_group_check=True),
                     reads=["PT%d" % pb, "Vp", "Vp1"], writes=["Ops%d" % ob])

        qk(0, it)
        if nk > 1:
            qk(1, it + 1)
        for kb in range(nk):
            if kb + 2 < nk:
                qk(kb + 2, it + 2)
            ex_pv(kb, it)
            it += 1
        osb = ost[qc % 2]
        T.op("dve", lambda e: e.reciprocal(out=rc[:], in_=Ops[ob][:, :, 64]), writes=["rc", "Ops%d" % ob])
        for jj in range(4):
            T.op("dve", lambda e: e.tensor_scalar(out=osb[:, jj, :], in0=Ops[ob][:, jj, 0:64], scalar1=rc[:, jj:jj + 1], scalar2=None,
                                                  op0=ALU.mult), reads=["rc"], writes=["ost%d" % (qc % 2), "Ops%d" % ob])
        T.op("sp", lambda e: e.dma_start(out=o[qc * 512:(qc + 1) * 512, :].rearrange("(jj p) d -> p jj d", p=128), in_=osb[:]),
             reads=["ost%d" % (qc % 2)], writes=["o"], dma=True)
    T.finish("sp")
    print("FOX n_ins", T.n_ins, "n_wait", T.n_wait)
    return nc


def fox_inputs(proj, projb, b_fgt, Sx=S):
    OFF_FOX = 512 + 768 + 24
    OFF_FGT = OFF_FOX + 1536
    maps = []
    for h in range(8):
        q = projb[:Sx, OFF_FOX + h * 64:OFF_FOX + (h + 1) * 64]
        k = projb[:Sx, OFF_FOX + 512 + h * 64:OFF_FOX + 512 + (h + 1) * 64]
        v = projb[:Sx, OFF_FOX + 1024 + h * 64:OFF_FOX + 1024 + (h + 1) * 64]
        f = proj[:Sx, OFF_FGT + h]
        maps.append({"qT": np.ascontiguousarray(q.T), "kT": np.ascontiguousarray(k.T), "v": np.ascontiguousarray(v),
                     "f2": np.ascontiguousarray(f.reshape(Sx // 128, 128).T),
                     "bfg": np.full((128, 1), b_fgt[0, h], np.float32)})
    return maps


def run_fox(proj, projb, b_fgt, Sx=S, cores=NCORES):
    nc = build_fox(Sx)
    maps = fox_inputs(proj, projb, b_fgt, Sx)[:cores]
    res = _launch("fox", nc, maps, cores)
    return np.concatenate([r["o"] for r in res.results], axis=1)


BIGNEG = -30000.0


def build_nsa(Sx=S):
    nc = bass.Bass("TRN2", target_bir_lowering=False)
    NKB = Sx // 128
    NB = Sx // 512
    NCT = max(1, Sx // 2048)
    NCMP = Sx // 16
    NJ = Sx // 64
    dI = lambda name, shape: nc.dram_tensor(name, shape, F32, kind="ExternalInput").ap()
    A = lambda name, shape, dt: nc.alloc_sbuf_tensor("sb_" + name, shape, dt)
    dB = lambda name, shape: nc.dram_tensor(name, shape, BF16, kind="ExternalInput").ap()
    qT4 = dB("qT4", [64, NB, 512])
    kc_s = dB("kc_s", [64, Sx]); vc_s = dB("vc_s", [64, Sx])
    kslcT = dB("kslcT", [64, Sx]); kwinT = dB("kwinT", [64, Sx])
    vslc1 = dB("vslc1", [Sx, 65]); vwin1 = dB("vwin1", [Sx, 65])
    gates = dI("gates", [128, NB * 12])
    w1d = dI("w1", [64, 2 * 32 * 256]); b1T = dI("b1T", [128, 4]); w2d = dI("w2", [128, 4 * 64]); peT = dI("peT", [64, 64])
    D0d = dI("D0", [128, 512]); D1d = dI("D1", [128, 512]); CBd = dI("CB", [128, 18 * 512]); Cfd = dI("Cfull", [128, 512])
    crow = dI("crow", [1, 512])
    wseld = dI("wsel", [128, NCT * NJ]); cvald = dI("cvalid", [128, NCT]); f0d = dI("force0", [128, NJ])
    o = nc.dram_tensor("o", [NB * 128, 256], F32, kind="ExternalOutput").ap()
    T = Trk(nc)
    bufA = A("bufA", [128, Sx], BF16)
    bufB = A("bufB", [128, Sx], BF16)
    bufC = A("bufC", [128, max(2 * NKB * 65, 16384)], BF16)
    w1v = bufC[0:64, 0:16384].rearrange("d (z l h) -> d z l h", z=2, l=32)
    Vs = bufC[:, 0:NKB * 65].rearrange("p (k c) -> p k c", c=65)
    Vw = bufC[:, NKB * 65:2 * NKB * 65].rearrange("p (k c) -> p k c", c=65)
    E = A("E", [128, 64, 128], BF16)
    CB = A("CB", [128, 18, 512], BF16)
    D0 = A("D0", [128, 512], BF16); D1 = A("D1", [128, 512], BF16); W4 = A("W4", [128, 512], BF16)
    hidT = A("hidT", [128, 2, 2, NCMP], BF16)
    KcT = A("KcT", [128, NCMP], BF16)
    Vc = A("Vc", [128, NCT, 65], BF16)
    wsel = A("wsel", [128, NCT, NJ], BF16)
    cval = A("cval", [128, NCT], F32)
    f0 = A("f0", [128, NJ], F32)
    w2 = A("w2", [128, 2, 2, 64], BF16)
    b1 = A("b1", [128, 4], F32); cb = A("cb", [128, 4], F32)
    pe = A("pe", [64, 2, 32], BF16)
    gts = A("gts", [128, NB, 4, 3], F32)
    QT = [A("QT%d" % i, [128, 512], BF16) for i in range(2)]
    PT = [A("PT%d" % i, [128, 512], BF16) for i in range(4)]
    xs = A("xs", [128, 512], F32); x2 = A("x2", [128, 512], F32); sg = A("sg", [128, 512], F32)
    imp = A("imp", [128, NJ], F32); work = A("work", [128, NJ], F32); selm = A("selm", [128, NJ], F32)
    mx8 = A("mx8", [128, 8], F32)
    selbT = A("selbT", [128, 2, 4, 128], BF16)
    rs = A("rs", [128, 3, 4], F32)
    oacc = [A("oacc%d" % i, [128, 4, 64], F32) for i in range(2)]
    vtmp = A("vtmp", [128, 64], F32)
    ident8 = A("ident8", [128, 128], BF16)
    P = nc.alloc_psum_tensor
    Sps = [P("Sps%d" % i, [128, 512], F32) for i in range(3)]
    Ops = [P("Ops%d" % i, [128, 4, 128], F32) for i in range(3)]
    Ips = [P("Ips%d" % i, [128, 2, 256], F32) for i in range(2)]
    identb = make_ident(nc, T, BF16, "identb")
    identf = make_ident(nc, T, F32, "identf")
    T.op("pool", lambda e: e.tensor_scalar(out=ident8[:], in0=identb[:], scalar1=8.0, scalar2=None, op0=ALU.mult),
         reads=["identb"], writes=["ident8"])

    def cast_load(dst, src, key, eng="pool"):
        T.op(eng, lambda e: e.dma_start(out=dst, in_=src), writes=[key], dma=True)

    cast_load(CB[:].rearrange("p r c -> p (r c)"), CBd[:, :], "CB")
    cast_load(D0[:], D0d[:, :], "D0"); cast_load(D1[:], D1d[:, :], "D1"); cast_load(W4[:], Cfd[:, :], "W4")
    cast_load(wsel[:].rearrange("p a b -> p (a b)"), wseld[:, :], "wsel")
    cast_load(w2[:].rearrange("p z c d -> p (z c d)"), w2d[:, :], "w2")
    cast_load(pe[:].rearrange("d z l -> d (z l)"), peT[:, :], "pe")
    cast_load(w1v.rearrange("d z l h -> d (z l h)"), w1d[:, :], "bufC")
    T.op("sp", lambda e: e.dma_start(out=cval[:], in_=cvald[:, :]), writes=["cval"], dma=True)
    T.op("sp", lambda e: e.dma_start(out=f0[:], in_=f0d[:, :]), writes=["f0"], dma=True)
    T.op("sp", lambda e: e.dma_start(out=b1[:], in_=b1T[:, :]), writes=["b1"], dma=True)
    T.op("sp", lambda e: e.dma_start(out=gts[:].rearrange("p a h b -> p (a h b)"), in_=gates[:, :]), writes=["gts"], dma=True)
    T.op("act", lambda e: e.activation(out=gts[:].rearrange("p a h b -> p (a h b)"), in_=gts[:].rearrange("p a h b -> p (a h b)"), func=AF.Sigmoid),
         reads=["gts"], writes=["gts"])
    for i in range(2):
        cast_load(QT[i][64:65, :], crow[:, :], "QTc%d" % i)
    v4 = lambda t: t[:].rearrange("p (h t) -> p h t", h=4)
    T.op("pool", lambda e: e.affine_select(out=v4(D0), in_=v4(D0), pattern=[[0, 4], [1, 128]], compare_op=ALU.is_ge, fill=BIGNEG,
                                           base=0, channel_multiplier=-1), reads=["D0"], writes=["D0"])
    T.op("pool", lambda e: e.affine_select(out=v4(W4), in_=v4(W4), pattern=[[0, 4], [-1, 128]], compare_op=ALU.is_ge, fill=BIGNEG,
                                           base=-1, channel_multiplier=1), reads=["W4"], writes=["W4"])
    for R in range(18):
        cbv = CB[:, R, :].rearrange("p (h t) -> p h t", h=4)
        T.op("pool", lambda e: e.affine_select(out=cbv, in_=cbv, pattern=[[0, 4], [1, 128]], compare_op=ALU.is_ge, fill=BIGNEG,
                                               base=128 * R - 31, channel_multiplier=-16), reads=["CB"], writes=["CB"])
    T.op("pool", lambda e: e.memset(E[:], 1.0), writes=["E"])
    for hs in range(2):
        ev = E[:, :, hs * 64:(hs + 1) * 64]
        T.op("pool", lambda e: e.affine_select(out=ev, in_=ev, pattern=[[-2, 64], [0, 64]], compare_op=ALU.is_equal, fill=0.0,
                                               base=-hs, channel_multiplier=1), reads=["E"], writes=["E"])

    for h in range(0, Sx, 2048):
        w = min(2048, Sx - h)
        cast_load(bufA[0:64, h:h + w], kc_s[:, h:h + w], "bufA", "sp")
        cast_load(bufB[0:64, h:h + w], vc_s[:, h:h + w], "bufB", "act")
    T.op("dve", lambda e: e.memset(hidT[:], 0.0), writes=["hidT"])
    T.op("dve", lambda e: e.memset(KcT[:], 0.0), writes=["KcT"])
    T.op("dve", lambda e: e.memset(KcT[64:65, :], 8.0), reads=[], writes=["KcT"])
    for z in range(2):
        for c2 in range(2):
            col = z * 2 + c2
            for l in range(32):
                T.op("pe", lambda e: e.matmul(Sps[0][:, col:col + 1], lhsT=w1v[:, z, l, c2 * 128:(c2 + 1) * 128], rhs=pe[:, z, l:l + 1],
                                              start=(l == 0), stop=(l == 31)), reads=["bufC", "pe"], writes=["Sps0"])
    T.op("dve", lambda e: e.tensor_tensor(out=cb[:], in0=Sps[0][:, 0:4], in1=b1[:], op=ALU.add), reads=["b1"], writes=["cb", "Sps0"])
    nvalid = NCMP - 1
    src = [bufA, bufB]
    it = 0
    for z in range(2):
        for c2 in range(2):
            for n0 in range(0, nvalid, 512):
                nw = min(512, nvalid - n0)
                sb = it % 2
                it += 1
                for l in range(32):
                    T.op("pe", lambda e: e.matmul(Sps[sb][:, 0:nw], lhsT=w1v[:, z, l, c2 * 128:(c2 + 1) * 128],
                                                  rhs=src[z][0:64, 16 * n0 + l:16 * n0 + l + 16 * (nw - 1) + 1:16],
                                                  start=(l == 0), stop=(l == 31)),
                         reads=["bufC", "bufA" if z == 0 else "bufB"], writes=["Sps%d" % sb])
                col = z * 2 + c2
                T.op("act", lambda e: e.activation(out=xs[:, 0:nw], in_=Sps[sb][:, 0:nw], func=AF.Identity, bias=cb[:, col:col + 1]),
                     reads=["cb"], writes=["xs", "Sps%d" % sb])
                T.op("dve", lambda e: e.tensor_tensor(out=x2[:, 0:nw], in0=xs[:, 0:nw], in1=xs[:, 0:nw], op=ALU.mult), reads=["xs"], writes=["x2"])
                T.op("dve", lambda e: e.tensor_scalar(out=x2[:, 0:nw], in0=x2[:, 0:nw], scalar1=0.044715, scalar2=1.0, op0=ALU.mult, op1=ALU.add),
                     reads=["x2"], writes=["x2"])
                T.op("dve", lambda e: e.tensor_tensor(out=x2[:, 0:nw], in0=x2[:, 0:nw], in1=xs[:, 0:nw], op=ALU.mult), reads=["x2", "xs"], writes=["x2"])
                T.op("act", lambda e: e.activation(out=sg[:, 0:nw], in_=x2[:, 0:nw], func=AF.Sigmoid, scale=1.5957691216057308),
                     reads=["x2"], writes=["sg"])
                T.op("dve", lambda e: e.tensor_tensor(out=hidT[:, z, c2, n0:n0 + nw], in0=xs[:, 0:nw], in1=sg[:, 0:nw], op=ALU.mult),
                     reads=["xs", "sg"], writes=["hidT"])
    for n0 in range(0, NCMP, 512):
        nw = min(512, NCMP - n0)
        for c2 in range(2):
            T.op("pe", lambda e: e.matmul(Sps[0][0:64, 0:nw], lhsT=w2[:, 0, c2, :], rhs=hidT[:, 0, c2, n0:n0 + nw], start=(c2 == 0), stop=(c2 == 1)),
                 reads=["w2", "hidT"], writes=["Sps0"])
        T.op("act", lambda e: e.copy(out=KcT[0:64, n0:n0 + nw], in_=Sps[0][0:64, 0:nw]), writes=["KcT", "Sps0"])
    for nt in range(NCT):
        for c2 in range(2):
            T.op("pe", lambda e: e.matmul(Sps[1][:, 0:64], lhsT=hidT[:, 1, c2, nt * 128:(nt + 1) * 128], rhs=w2[:, 1, c2, :], start=(c2 == 0), stop=(c2 == 1)),
                 reads=["w2", "hidT"], writes=["Sps1"])
        T.op("dve", lambda e: e.tensor_scalar(out=Vc[:, nt, 0:64], in0=Sps[1][:, 0:64], scalar1=cval[:, nt:nt + 1], scalar2=None, op0=ALU.mult),
             reads=["cval"], writes=["Vc", "Sps1"])
        T.op("dve", lambda e: e.tensor_copy(out=Vc[:, nt, 64:65], in_=cval[:, nt:nt + 1]), reads=["cval"], writes=["Vc"])

    for h in range(0, Sx, 2048):
        w = min(2048, Sx - h)
        cast_load(bufA[0:64, h:h + w], kslcT[:, h:h + w], "bufA", "sp")
        cast_load(bufB[0:64, h:h + w], kwinT[:, h:h + w], "bufB", "act")
    T.op("dve", lambda e: e.memset(bufA[64:65, :], 8.0), writes=["bufA8"])
    T.op("dve", lambda e: e.memset(bufB[64:65, :], 8.0), writes=["bufB8"])
    vsv = vslc1.rearrange("(kb p) d -> p kb d", p=128)
    vwv = vwin1.rearrange("(kb p) d -> p kb d", p=128)
    for h in range(0, NKB, 16):
        w = min(16, NKB - h)
        cast_load(Vs[:, h:h + w, :], vsv[:, h:h + w, :], "bufC", "sp")
        cast_load(Vw[:, h:h + w, :], vwv[:, h:h + w, :], "bufC", "act")

    sidx = [0]
    pidx = [0]

    pendq = []

    def flush():
        while pendq:
            pendq.pop(0)()

    def tile_step(Kbuf, kkey, col0, Vap, vkey, ob, qt, qkey, far, bias_tile, bias_key, sel_chunk=None, sel_m=None, imp_nt=None):
        sb = sidx[0] % 3
        sidx[0] += 1
        pb = pidx[0] % 4
        pidx[0] += 1
        kk = 65 if far else 64
        last_plain = (bias_tile is None and sel_chunk is None)
        T.op("pe", lambda e: e.matmul(Sps[sb][:], lhsT=Kbuf[0:kk, col0:col0 + 128], rhs=qt[0:kk, :], start=True, stop=last_plain),
             reads=[kkey, kkey + "8", qkey, qkey.replace("QT", "QTc")], writes=["Sps%d" % sb])
        if bias_tile is not None:
            T.op("pe", lambda e: e.matmul(Sps[sb][:], lhsT=ident8[:], rhs=bias_tile, start=False, stop=(sel_chunk is None)),
                 reads=["ident8", bias_key], writes=["Sps%d" % sb])
        if sel_chunk is not None:
            T.op("pe", lambda e: e.matmul(Sps[sb][:], lhsT=E[:, sel_m, :], rhs=selbT[:, sel_chunk, :, :].rearrange("p h t -> p (h t)"),
                                          start=False, stop=True), reads=["E", "selbT"], writes=["Sps%d" % sb])

        def rest():
            T.op("act", lambda e: e.activation(out=PT[pb][:], in_=Sps[sb][:], func=AF.Exp, scale=0.125), writes=["PT%d" % pb, "Sps%d" % sb])
            for h in range(4):
                T.op("pe", lambda e: e.matmul(Ops[ob][:, h, 0:65], lhsT=PT[pb][:, h * 128:(h + 1) * 128], rhs=Vap,
                                              start=False, stop=False, skip_group_check=True),
                     reads=["PT%d" % pb, vkey], writes=["Ops%d" % ob])
            if imp_nt is not None:
                for h in range(4):
                    T.op("pe", lambda e: e.matmul(Ips[h // 2][:, h % 2, 0:NJ], lhsT=PT[pb][:, h * 128:(h + 1) * 128], rhs=wsel[:, imp_nt, :],
                                                  start=False, stop=False, skip_group_check=True),
                         reads=["PT%d" % pb, "wsel"], writes=["Ips%d" % (h // 2)])
        pendq.append(rest)
        if len(pendq) > 2:
            pendq.pop(0)()

    for i in range(NB):
        md = 4 * i + 3
        qt = QT[i % 2]
        qkey = "QT%d" % (i % 2)
        cast_load(qt[0:64, :], qT4[:, i, :], qkey, "sp")
        for b in range(3):
            T.op("dve", lambda e: e.memset(Ops[b][:], 0.0), writes=["Ops%d" % b])
        for b in range(2):
            T.op("dve", lambda e: e.memset(Ips[b][:], 0.0), writes=["Ips%d" % b])
        for nt in range(NCT):
            R = md - 16 * nt
            if R < 0:
                continue
            far = R >= 18
            tile_step(KcT, "KcT", nt * 128, Vc[:, nt, :], "Vc", 0, qt, qkey, far,
                      None if far else CB[:, R, :], "CB", imp_nt=nt)
        flush()
        T.op("dve", lambda e: e.tensor_scalar(out=rs[:, 0, :], in0=Ops[0][:, :, 64], scalar1=1e-30, scalar2=None, op0=ALU.max),
             writes=["rs0", "Ops0"])
        T.op("dve", lambda e: e.reciprocal(out=rs[:, 0, :], in_=rs[:, 0, :]), reads=["rs0"], writes=["rs0"])
        for h in range(4):
            if h == 0:
                T.op("dve", lambda e: e.tensor_scalar(out=imp[:], in0=Ips[0][:, 0, 0:NJ], scalar1=rs[:, 0, 0:1], scalar2=None, op0=ALU.mult),
                     reads=["rs0"], writes=["imp", "Ips0"])
            else:
                T.op("dve", lambda e: e.scalar_tensor_tensor(out=imp[:], in0=Ips[h // 2][:, h % 2, 0:NJ], scalar=rs[:, 0, h:h + 1], in1=imp[:],
                                                             op0=ALU.mult, op1=ALU.add), reads=["rs0", "imp"], writes=["imp", "Ips%d" % (h // 2)])
        c1 = 2 * md + 1
        if c1 + 1 < NJ:
            T.op("dve", lambda e: e.memset(imp[:, c1 + 1:NJ], -1.0), reads=["imp"], writes=["imp"])
        T.op("dve", lambda e: e.memset(imp[0:64, c1:c1 + 1], -1.0), reads=["imp"], writes=["imp"])
        T.op("dve", lambda e: e.memset(imp[64:128, c1:c1 + 1], 20000.0), reads=["imp"], writes=["imp"])
        T.op("dve", lambda e: e.memset(imp[0:64, c1 - 1:c1], 20000.0), reads=["imp"], writes=["imp"])
        T.op("dve", lambda e: e.memset(imp[64:128, c1 - 1:c1], 10000.0), reads=["imp"], writes=["imp"])
        T.op("dve", lambda e: e.memset(imp[0:64, c1 - 2:c1 - 1], 10000.0), reads=["imp"], writes=["imp"])
        T.op("dve", lambda e: e.tensor_tensor(out=imp[:], in0=imp[:], in1=f0[:], op=ALU.max), reads=["imp", "f0"], writes=["imp"])
        T.op("dve", lambda e: e.tensor_copy(out=work[:], in_=imp[:]), reads=["imp"], writes=["work"])
        for rnd in range(2):
            T.op("dve", lambda e: e.max(out=mx8[:], in_=work[:]), reads=["work"], writes=["mx8"])
            T.op("dve", lambda e: e.match_replace(out=work[:], in_to_replace=mx8[:], in_values=work[:], imm_value=-1e9),
                 reads=["mx8", "work"], writes=["work"])
        T.op("dve", lambda e: e.tensor_scalar(out=selm[:], in0=work[:], scalar1=-1e8, scalar2=None, op0=ALU.is_le), reads=["work"], writes=["selm"])
        nch = (NJ + 127) // 128
        for ch in range(nch):
            cwid = min(128, NJ - ch * 128)
            T.op("pe", lambda e: e.transpose(out=Ips[1][0:cwid, 0, ch * 128:(ch + 1) * 128], in_=selm[:, ch * 128:ch * 128 + cwid], identity=identf[:]),
                 reads=["selm", "identf"], writes=["Ips1"])
        for ch in range(nch):
            cwid = min(128, NJ - ch * 128)
            for h in range(4):
                T.op("dve", lambda e: e.tensor_scalar(out=selbT[0:cwid, ch, h, :], in0=Ips[1][0:cwid, 0, ch * 128:(ch + 1) * 128], scalar1=-1.0, scalar2=-BIGNEG,
                                                      op0=ALU.add, op1=ALU.mult), writes=["selbT", "Ips1"])
        for m in range(max(0, md - 4), md + 1):
            rel = md - m
            bt, bk = {0: (D0[:], "D0"), 1: (D1[:], "D1"), 4: (W4[:], "W4")}.get(rel, (None, None))
            tile_step(bufB, "bufB", m * 128, Vw[:, m, :], "bufC", 2, qt, qkey, bt is None, bt, bk)
        for m in range(0, md + 1):
            rel = md - m
            bt, bk = {0: (D0[:], "D0"), 1: (D1[:], "D1")}.get(rel, (None, None))
            tile_step(bufA, "bufA", m * 128, Vs[:, m, :], "bufC", 1, qt, qkey, bt is None, bt, bk, sel_chunk=m // 64, sel_m=m % 64)
        flush()
        oa = oacc[i % 2]
        for b in range(3):
            if b > 0:
                T.op("dve", lambda e: e.tensor_scalar(out=rs[:, b, :], in0=Ops[b][:, :, 64], scalar1=1e-30, scalar2=None, op0=ALU.max),
                     writes=["rs%d" % b, "Ops%d" % b])
                T.op("dve", lambda e: e.reciprocal(out=rs[:, b, :], in_=rs[:, b, :]), reads=["rs%d" % b], writes=["rs%d" % b])
            T.op("dve", lambda e: e.tensor_tensor(out=rs[:, b, :], in0=rs[:, b, :], in1=gts[:, i, :, b], op=ALU.mult),
                 reads=["rs%d" % b, "gts"], writes=["rs%d" % b])
            for h in range(4):
                if b == 0:
                    T.op("dve", lambda e: e.tensor_scalar(out=oa[:, h, :], in0=Ops[b][:, h, 0:64], scalar1=rs[:, b, h:h + 1], scalar2=None, op0=ALU.mult),
                         reads=["rs%d" % b], writes=["oacc%d" % (i % 2), "Ops%d" % b])
                else:
                    T.op("dve", lambda e: e.scalar_tensor_tensor(out=oa[:, h, :], in0=Ops[b][:, h, 0:64], scalar=rs[:, b, h:h + 1], in1=oa[:, h, :],
                                                                 op0=ALU.mult, op1=ALU.add), reads=["rs%d" % b], writes=["oacc%d" % (i % 2), "Ops%d" % b])
        T.op("sp", lambda e: e.dma_start(out=o[i * 128:(i + 1) * 128, :], in_=oa[:].rearrange("p h d -> p (h d)")),
             reads=["oacc%d" % (i % 2)], writes=["o"], dma=True)
    T.finish("sp")
    print("NSA n_ins", T.n_ins, "n_wait", T.n_wait)
    return nc


def t5_bucket_np(dist):
    n = np.maximum(dist, 0)
    ratio = np.log(np.maximum(n, 16).astype(np.float32) / np.float32(16))
    big = 16 + (ratio / np.float32(np.log(128 / 16)) * np.float32(16)).astype(np.int32)
    return np.where(n < 16, n, np.minimum(big, 31))


def nsa_inputs(proj, projb, t5_table, cmp_pe, cmp_w1, cmp_b1, cmp_w2, Sx=S):
    OFF_KV = 512
    OFF_GATE = 512 + 768
    NB = Sx // 512
    NCT = max(1, Sx // 2048)
    NJ = Sx // 64
    NCMP = Sx // 16
    maps = []
    si = np.arange(128)[:, None]
    ti = np.arange(128)[None, :]
    w1 = np.ascontiguousarray(cmp_w1[0].reshape(2, 32, 64, 256).transpose(2, 0, 1, 3).reshape(64, -1))
    b1T = np.ascontiguousarray(cmp_b1[0].reshape(2, 2, 128).transpose(2, 0, 1).reshape(128, 4))
    w2 = np.ascontiguousarray(cmp_w2[0].reshape(2, 2, 128, 64).transpose(2, 0, 1, 3).reshape(128, -1))
    peT = np.ascontiguousarray(cmp_pe[0].transpose(2, 0, 1).reshape(64, 64))
    for c in range(8):
        g, r = c // 4, c % 4
        sh = 3 - r
        p = proj[:Sx]
        pb = projb[:Sx]
        tb = t5_table.T[4 * g:4 * g + 4]
        kvc = lambda z: pb[:, OFF_KV + (z * 2 + g) * 64:OFF_KV + (z * 2 + g + 1) * 64]

        def shiftT(a):
            out = np.zeros((64, Sx), pb.dtype)
            if sh * 128 < Sx:
                out[:, sh * 128:] = a[:Sx - sh * 128].T
            return out

        def shiftV1(a):
            out = np.zeros((Sx, 65), pb.dtype)
            if sh * 128 < Sx:
                out[sh * 128:, 0:64] = a[:Sx - sh * 128]
                out[sh * 128:, 64] = 1.0
            return out
        q = pb[:, g * 256:(g + 1) * 256].reshape(Sx // 128, 128, 4, 64)[r::4]
        qT4 = np.ascontiguousarray(q.transpose(3, 0, 2, 1).reshape(64, NB, 512))
        gt = p[:, OFF_GATE + g * 12:OFF_GATE + (g + 1) * 12].reshape(Sx // 128, 128, 12)[r::4]
        gates = np.ascontiguousarray(gt.transpose(1, 0, 2).reshape(128, NB * 12))
        D0 = np.stack([tb[h][t5_bucket_np(ti - si)] for h in range(4)], axis=1).reshape(128, 512)
        D1 = np.stack([tb[h][t5_bucket_np(128 + ti - si)] for h in range(4)], axis=1).reshape(128, 512)
        CB = np.stack([np.stack([tb[h][t5_bucket_np(128 * R + ti - 16 * si - 31)] for h in range(4)], axis=1).reshape(128, 512)
                       for R in range(18)], axis=1).reshape(128, 18 * 512)
        Cfull = np.ascontiguousarray(np.broadcast_to(np.repeat(tb[:, 31], 128)[None, :], (128, 512)))
        crow = np.ascontiguousarray(np.repeat(tb[:, 31], 128)[None, :])
        npad = 8 * sh
        nn = np.arange(NCT * 128)[:, None]
        jj = np.arange(NJ)[None, :]
        dlt = nn - 4 * jj
        wsel = np.where((dlt >= 0) & (dlt <= 2), 1.0, np.where((dlt == -1) | (dlt == 3), 0.5, 0.0)).astype(np.float32)
        wsel[:npad] = 0.0
        wsel[NCMP - 1 - 0:] = 0.0 if True else 0.0
        wsel = np.ascontiguousarray(wsel.reshape(NCT, 128, NJ).transpose(1, 0, 2).reshape(128, NCT * NJ))
        cvalid = (np.arange(NCT * 128) >= npad).astype(np.float32)
        cvalid[NCMP - 1:] = 0.0
        cvalid = np.ascontiguousarray(cvalid.reshape(NCT, 128).T)
        force0 = np.zeros((128, NJ), np.float32)
        force0[:, 2 * sh] = 30000.0
        maps.append({"qT4": qT4, "kc_s": shiftT(kvc(0)), "vc_s": shiftT(kvc(1)), "kslcT": shiftT(kvc(2)), "kwinT": shiftT(kvc(4)),
                     "vslc1": shiftV1(kvc(3)), "vwin1": shiftV1(kvc(5)), "gates": gates, "w1": w1, "b1T": b1T, "w2": w2, "peT": peT,
                     "D0": np.ascontiguousarray(D0), "D1": np.ascontiguousarray(D1), "CB": np.ascontiguousarray(CB), "Cfull": Cfull, "crow": crow,
                     "wsel": wsel, "cvalid": cvalid, "force0": force0})
    return maps


def run_nsa(proj, projb, t5_table, cmp_pe, cmp_w1, cmp_b1, cmp_w2, Sx=S, cores=NCORES):
    nc = build_nsa(Sx)
    maps = nsa_inputs(proj, projb, t5_table, cmp_pe, cmp_w1, cmp_b1, cmp_w2, Sx)[:cores]
    res = _launch("nsa", nc, maps, cores)
    out = np.zeros((Sx, 512), np.float32)
    for c in range(cores):
        g, r = c // 4, c % 4
        oc = res.results[c]["o"].reshape(Sx // 512, 128, 256)
        out.reshape(Sx // 128, 128, 512)[r::4, :, g * 256:(g + 1) * 256] = oc
    return out


ALPHA = 2.0 ** 0.25


def build_l3a(TPC=TPC):
    nc = bass.Bass("TRN2", target_bir_lowering=False)
    NT = TPC // 128
    NG = TPC // 512
    dI = lambda name, shape: nc.dram_tensor(name, shape, F32, kind="ExternalInput").ap()
    A = lambda name, shape, dt: nc.alloc_sbuf_tensor("sb_" + name, shape, dt)
    onT = dI("onT", [512, TPC]); ofT = dI("ofT", [512, TPC]); mgT = dI("mgT", [4096, TPC]); x = dI("x", [TPC, D])
    wbn = dI("wbn", [512, D]); wbf = dI("wbf", [512, D]); wo = dI("wo", [D, D])
    g1b = dI("g1b", [128, D]); ln1g = dI("ln1g", [128, D]); ln1b = dI("ln1b", [128, D])
    mod2 = dI("mod2", [128, 32]); wr = dI("wr", [D, 72]); brb = dI("brb", [128, 72])
    x1o = nc.dram_tensor("x1", [TPC, D], F32, kind="ExternalOutput").ap()
    xn2o = nc.dram_tensor("xn2", [TPC, D], BF16, kind="ExternalOutput").ap()
    rwo = nc.dram_tensor("rw", [TPC, 64], F32, kind="ExternalOutput").ap()
    oho = nc.dram_tensor("oh", [TPC, 8], F32, kind="ExternalOutput").ap()
    mTd = nc.dram_tensor("mTd", [16, 128, TPC], BF16).ap()
    T = Trk(nc)
    big = A("big", [128, 32768], BF16)
    wbn_s = big[:, 0:8192].rearrange("p (k n) -> p k n", k=4)
    wbf_s = big[:, 8192:16384].rearrange("p (k n) -> p k n", k=4)
    onT_s = big[:, 16384:16384 + 4 * TPC].rearrange("p (k n) -> p k n", k=4)
    ofT_s = big[:, 24576:24576 + 4 * TPC].rearrange("p (k n) -> p k n", k=4)
    wo_s = big[:, :].rearrange("p (k n) -> p k n", k=16)
    mgs = [A("mgs%d" % i, [128, 2, 512], F32) for i in range(2)]
    t1 = A("t1", [128, 512], F32); t2 = A("t2", [128, 512], F32)
    mo = [A("mo%d" % i, [128, 512], BF16) for i in range(2)]
    P = nc.alloc_psum_tensor
    acc = [P("acc%d" % i, [128, 512], F32) for i in range(4)]
    tpf = [P("tpf%d" % i, [128, 4, 128], F32) for i in range(2)]
    rps = P("rps", [128, 512], F32)

    def cast_load(dst, src, key):
        T.op("pool", lambda e: e.dma_start(out=dst, in_=src), writes=[key], dma=True)
    cast_load(wbn_s, wbn.rearrange("(k p) n -> p k n", p=128), "big")
    cast_load(wbf_s, wbf.rearrange("(k p) n -> p k n", p=128), "big")
    cast_load(onT_s, onT.rearrange("(k p) n -> p k n", p=128), "big")
    cast_load(ofT_s, ofT.rearrange("(k p) n -> p k n", p=128), "big")
    it = 0
    sched = [(dc_, tg_) for dc_ in range(16) for tg_ in range(NG)]

    def ld_mg(j):
        dc_, tg_ = sched[j]
        mb_ = mgs[j % 2]
        T.op("sp", lambda e: e.dma_start(out=mb_[:, 0, :], in_=mgT[dc_ * 128:(dc_ + 1) * 128, tg_ * 512:(tg_ + 1) * 512]), writes=["mgs%d" % (j % 2)], dma=True)
        T.op("sp", lambda e: e.dma_start(out=mb_[:, 1, :], in_=mgT[2048 + dc_ * 128:2048 + (dc_ + 1) * 128, tg_ * 512:(tg_ + 1) * 512]), writes=["mgs%d" % (j % 2)], dma=True)
    ld_mg(0)
    for dc in range(16):
        for tg in range(NG):
            mb = mgs[it % 2]
            if it + 1 < len(sched):
                ld_mg(it + 1)
            T.op("act", lambda e: e.activation(out=mb[:].rearrange("p a n -> p (a n)"), in_=mb[:].rearrange("p a n -> p (a n)"), func=AF.Sigmoid),
                 reads=["mgs%d" % (it % 2)], writes=["mgs%d" % (it % 2)])
            for br, (ws, os_) in enumerate([(wbn_s, onT_s), (wbf_s, ofT_s)]):
                a = (it % 2) * 2 + br
                for k in range(4):
                    T.op("pe", lambda e: e.matmul(acc[a][:], lhsT=ws[:, k, dc * 128:(dc + 1) * 128], rhs=os_[:, k, tg * 512:(tg + 1) * 512],
                                                  start=(k == 0), stop=(k == 3)), reads=["big"], writes=["acc%d" % a])
            a0 = (it % 2) * 2
            T.op("dve", lambda e: e.tensor_tensor(out=t1[:], in0=acc[a0][:], in1=mb[:, 0, :], op=ALU.mult), reads=["mgs%d" % (it % 2)], writes=["t1", "acc%d" % a0])
            T.op("dve", lambda e: e.tensor_tensor(out=t2[:], in0=acc[a0 + 1][:], in1=mb[:, 1, :], op=ALU.mult), reads=["mgs%d" % (it % 2)], writes=["t2", "acc%d" % (a0 + 1)])
            T.op("dve", lambda e: e.tensor_tensor(out=mo[it % 2][:], in0=t1[:], in1=t2[:], op=ALU.add), reads=["t1", "t2"], writes=["mo%d" % (it % 2)])
            T.op("sp", lambda e: e.dma_start(out=mTd[dc, :, tg * 512:(tg + 1) * 512], in_=mo[it % 2][:]), reads=["mo%d" % (it % 2)], writes=["mTd"], dma=True)
            it += 1
    for h in range(4):
        cast_load(wo_s[:, 4 * h:4 * h + 4, :], wo.rearrange("(k p) n -> p k n", p=128)[:, 4 * h:4 * h + 4, :], "big")
    g1p = A("g1p", [128, D], F32); lg_ = A("lng", [128, D], F32); lb_ = A("lnb", [128, D], F32)
    T.op("sp", lambda e: e.dma_start(out=g1p[:], in_=g1b[:, :]), writes=["g1p"], dma=True)
    T.op("sp", lambda e: e.dma_start(out=lg_[:], in_=ln1g[:, :]), writes=["lng"], dma=True)
    T.op("sp", lambda e: e.dma_start(out=lb_[:], in_=ln1b[:, :]), writes=["lnb"], dma=True)
    T.op("dve", lambda e: e.tensor_scalar(out=g1p[:], in0=g1p[:], scalar1=1.0, scalar2=None, op0=ALU.add), reads=["g1p"], writes=["g1p"])
    m2 = A("m2", [128, 32], F32); wrs = A("wrs", [128, 16, 72], F32); brs = A("brs", [128, 72], F32)
    T.op("sp", lambda e: e.dma_start(out=m2[:], in_=mod2[:, :]), writes=["m2"], dma=True)
    T.op("dve", lambda e: e.tensor_scalar(out=m2[:, 16:32], in0=m2[:, 16:32], scalar1=1.0, scalar2=None, op0=ALU.add), reads=["m2"], writes=["m2"])
    T.op("sp", lambda e: e.dma_start(out=wrs[:], in_=wr.rearrange("(k p) n -> p k n", p=128)), writes=["wrs"], dma=True)
    T.op("sp", lambda e: e.dma_start(out=brs[:], in_=brb[:, :]), writes=["brs"], dma=True)
    identf = make_ident(nc, T, F32, "identf")
    mt = [A("mt%d" % i, [128, 16, 128], BF16) for i in range(2)]
    xt = [A("xt%d" % i, [128, D], F32) for i in range(2)]
    v = A("v", [128, D], F32); xn = A("xn", [128, D], F32); junk = A("junk", [128, D], BF16)
    xnb = A("xnb", [128, D], BF16)
    u2T = A("u2T", [128, 16, 128], F32)
    st = A("st", [128, 16], F32)
    lgt = A("lgt", [128, 72], F32); ml = A("ml", [128, 64], F32); mx8 = A("mx8", [128, 8], F32)
    r1 = A("r1", [128, 16], F32); ra = A("ra", [128, 64], F32); rb = A("rb", [128, 64], F32); ohs = A("ohs", [128, 8], F32)
    ge = A("ge", [128, 8], F32)

    def ln_stats(src, key):
        T.op("dve", lambda e: e.reduce_sum(out=st[:, 0:1], in_=src, axis=AX.X), reads=[key], writes=["st0"])
        T.op("act", lambda e: e.activation(out=junk[:], in_=src, func=AF.Square, accum_out=st[:, 1:2]), reads=[key], writes=["st1", "junk"])
        T.op("dve", lambda e: e.tensor_scalar(out=st[:, 2:3], in0=st[:, 0:1], scalar1=1.0 / D, scalar2=None, op0=ALU.mult), reads=["st0"], writes=["st2"])
        T.op("dve", lambda e: e.tensor_tensor(out=st[:, 3:4], in0=st[:, 2:3], in1=st[:, 2:3], op=ALU.mult), reads=["st2"], writes=["st3"])
        T.op("dve", lambda e: e.scalar_tensor_tensor(out=st[:, 4:5], in0=st[:, 1:2], scalar=1.0 / D, in1=st[:, 3:4], op0=ALU.mult, op1=ALU.subtract),
             reads=["st1", "st3"], writes=["st4"])
        T.op("dve", lambda e: e.tensor_scalar(out=st[:, 6:7], in0=st[:, 4:5], scalar1=LN_EPS, scalar2=None, op0=ALU.add), reads=["st4"], writes=["st6"])
        T.op("act", lambda e: e.activation(out=st[:, 7:8], in_=st[:, 6:7], func=AF.Sqrt), reads=["st6"], writes=["st7"])
        T.op("dve", lambda e: e.reciprocal(out=st[:, 5:6], in_=st[:, 7:8]), reads=["st7"], writes=["st5"])

    def ld_t(i_):
        b_ = i_ % 2
        T.op("sp", lambda e: e.dma_start(out=mt[b_][:], in_=mTd[:, :, i_ * 128:(i_ + 1) * 128].rearrange("k p t -> p k t")), reads=["mTd"], writes=["mt%d" % b_], dma=True)
        T.op("sp", lambda e: e.dma_start(out=xt[b_][:], in_=x[i_ * 128:(i_ + 1) * 128, :]), writes=["xt%d" % b_], dma=True)
    ld_t(0)
    for i in range(NT):
        b = i % 2
        if i + 1 < NT:
            ld_t(i + 1)
        for cg in range(4):
            for k in range(16):
                T.op("pe", lambda e: e.matmul(acc[cg][:], lhsT=mt[b][:, k, :], rhs=wo_s[:, k, cg * 512:(cg + 1) * 512], start=(k == 0), stop=(k == 15)),
                     reads=["mt%d" % b, "big"], writes=["acc%d" % cg])
            T.op("dve", lambda e: e.tensor_tensor(out=v[:, cg * 512:(cg + 1) * 512], in0=acc[cg][:], in1=g1p[:, cg * 512:(cg + 1) * 512], op=ALU.mult),
                 reads=["g1p"], writes=["v", "acc%d" % cg])
        T.op("dve", lambda e: e.scalar_tensor_tensor(out=v[:], in0=xt[b][:], scalar=ALPHA, in1=v[:], op0=ALU.mult, op1=ALU.add),
             reads=["xt%d" % b, "v"], writes=["v"])
        ln_stats(v[:], "v")
        T.op("dve", lambda e: e.tensor_scalar(out=xn[:], in0=v[:], scalar1=st[:, 2:3], scalar2=st[:, 5:6], op0=ALU.subtract, op1=ALU.mult),
             reads=["v", "st2", "st5"], writes=["xn"])
        T.op("dve", lambda e: e.tensor_tensor(out=xn[:], in0=xn[:], in1=lg_[:], op=ALU.mult), reads=["xn", "lng"], writes=["xn"])
        T.op("dve", lambda e: e.tensor_tensor(out=xn[:], in0=xn[:], in1=lb_[:], op=ALU.add), reads=["xn", "lnb"], writes=["xn"])
        T.op("sp", lambda e: e.dma_start(out=x1o[i * 128:(i + 1) * 128, :], in_=xn[:]), reads=["xn"], writes=["x1o"], dma=True)
        ln_stats(xn[:], "xn")
        T.op("dve", lambda e: e.tensor_scalar(out=v[:], in0=xn[:], scalar1=st[:, 2:3], scalar2=st[:, 5:6], op0=ALU.subtract, op1=ALU.mult),
             reads=["xn", "st2", "st5"], writes=["v"])
        T.op("act", lambda e: e.copy(out=xnb[:], in_=v[:]), reads=["v"], writes=["xnb"])
        T.op("sp", lambda e: e.dma_start(out=xn2o[i * 128:(i + 1) * 128, :], in_=xnb[:]), reads=["xnb"], writes=["xn2o"], dma=True)
        for kq in range(4):
            pb = kq % 2
            for j in range(4):
                k = kq * 4 + j
                T.op("pe", lambda e: e.transpose(out=tpf[pb][:, j, :], in_=v[:, k * 128:(k + 1) * 128], identity=identf[:]),
                     reads=["v", "identf"], writes=["tpf%d" % pb])
            for j in range(4):
                k = kq * 4 + j
                T.op("act", lambda e: e.activation(out=u2T[:, k, :], in_=tpf[pb][:, j, :], func=AF.Identity, scale=m2[:, 16 + k:17 + k], bias=m2[:, k:k + 1]),
                     reads=["m2"], writes=["u2T", "tpf%d" % pb])
        for k in range(16):
            T.op("pe", lambda e: e.matmul(rps[:, 0:72], lhsT=u2T[:, k, :], rhs=wrs[:, k, :], start=(k == 0), stop=(k == 15)),
                 reads=["u2T", "wrs"], writes=["rps"])
        T.op("dve", lambda e: e.tensor_tensor(out=lgt[:], in0=rps[:, 0:72], in1=brs[:], op=ALU.add), reads=["brs"], writes=["lgt", "rps"])
        T.op("dve", lambda e: e.reduce_max(out=r1[:, 0:1], in_=lgt[:, 0:8], axis=AX.X), reads=["lgt"], writes=["r1a"])
        T.op("dve", lambda e: e.tensor_scalar(out=r1[:, 1:2], in0=r1[:, 0:1], scalar1=-1.0, scalar2=None, op0=ALU.mult), reads=["r1a"], writes=["r1b"])
        T.op("act", lambda e: e.activation(out=ge[:], in_=lgt[:, 0:8], func=AF.Exp, bias=r1[:, 1:2], accum_out=r1[:, 2:3]), reads=["lgt", "r1b"], writes=["ge", "r1c"])
        T.op("dve", lambda e: e.reciprocal(out=r1[:, 3:4], in_=r1[:, 2:3]), reads=["r1c"], writes=["r1d"])
        T.op("dve", lambda e: e.tensor_scalar(out=ohs[:], in0=lgt[:, 0:8], scalar1=r1[:, 0:1], scalar2=None, op0=ALU.is_equal), reads=["lgt", "r1a"], writes=["ohs"])
        T.op("sp", lambda e: e.dma_start(out=oho[i * 128:(i + 1) * 128, :], in_=ohs[:]), reads=["ohs"], writes=["oho"], dma=True)
        T.op("dve", lambda e: e.tensor_scalar(out=ge[:], in0=ohs[:], scalar1=-1.0, scalar2=1e9, op0=ALU.add, op1=ALU.mult), reads=["ohs", "ge"], writes=["ge"])
        for g in range(8):
            T.op("dve", lambda e: e.tensor_scalar(out=ml[:, g * 8:(g + 1) * 8], in0=lgt[:, 8 + g * 8:16 + g * 8], scalar1=ge[:, g:g + 1], scalar2=None, op0=ALU.add),
                 reads=["lgt", "ge"], writes=["ml"])
        T.op("dve", lambda e: e.max(out=mx8[:], in_=ml[:]), reads=["ml"], writes=["mx8"])
        T.op("dve", lambda e: e.tensor_tensor(out=r1[:, 4:5], in0=mx8[:, 1:2], in1=mx8[:, 0:1], op=ALU.subtract), reads=["mx8"], writes=["r1e"])
        T.op("act", lambda e: e.activation(out=r1[:, 5:6], in_=r1[:, 4:5], func=AF.Exp), reads=["r1e"], writes=["r1f"])
        T.op("dve", lambda e: e.tensor_scalar(out=r1[:, 6:7], in0=r1[:, 5:6], scalar1=1.0, scalar2=None, op0=ALU.add), reads=["r1f"], writes=["r1g"])
        T.op("dve", lambda e: e.reciprocal(out=r1[:, 7:8], in_=r1[:, 6:7]), reads=["r1g"], writes=["r1h"])
        T.op("dve", lambda e: e.tensor_tensor(out=r1[:, 8:9], in0=r1[:, 7:8], in1=r1[:, 3:4], op=ALU.mult), reads=["r1h", "r1d"], writes=["r1i"])
        T.op("dve", lambda e: e.tensor_tensor(out=r1[:, 9:10], in0=r1[:, 8:9], in1=r1[:, 5:6], op=ALU.mult), reads=["r1i", "r1f"], writes=["r1j"])
        T.op("dve", lambda e: e.tensor_scalar(out=ra[:], in0=ml[:], scalar1=mx8[:, 0:1], scalar2=r1[:, 8:9], op0=ALU.is_equal, op1=ALU.mult),
             reads=["ml", "mx8", "r1i"], writes=["ra"])
        T.op("dve", lambda e: e.tensor_scalar(out=rb[:], in0=ml[:], scalar1=mx8[:, 1:2], scalar2=r1[:, 9:10], op0=ALU.is_equal, op1=ALU.mult),
             reads=["ml", "mx8", "r1j"], writes=["rb"])
        T.op("dve", lambda e: e.tensor_tensor(out=ra[:], in0=ra[:], in1=rb[:], op=ALU.add), reads=["ra", "rb"], writes=["ra"])
        T.op("sp", lambda e: e.dma_start(out=rwo[i * 128:(i + 1) * 128, :], in_=ra[:]), reads=["ra"], writes=["rwo"], dma=True)
    T.finish("sp")
    print("L3a n_ins", T.n_ins, "n_wait", T.n_wait)
    return nc


def run_l3a(o_nsa, o_fox, proj, x, modrow, w_br_nsa, w_br_fox, w_o, ln1_g, ln1_b, w_rg, b_rg, w_re, b_re, TPC=TPC, cores=NCORES):
    nc = build_l3a(TPC)
    rep = lambda a: np.ascontiguousarray(np.broadcast_to(a.reshape(1, -1), (128, a.size))).astype(np.float32)
    wr = np.ascontiguousarray(np.concatenate([w_rg[0], w_re[0].reshape(D, 64)], axis=1))
    brb = rep(np.concatenate([b_rg[0], b_re[0].reshape(64)]))
    mod2 = np.ascontiguousarray(np.concatenate([modrow[3 * D:4 * D].reshape(16, 128).T, modrow[4 * D:5 * D].reshape(16, 128).T], axis=1))
    OFF_MERGE = 512 + 768 + 24 + 1536 + 8
    x2 = x.reshape(S, D)
    maps = []
    for c in range(cores):
        sl = slice(c * TPC, (c + 1) * TPC)
        maps.append({"onT": np.ascontiguousarray(o_nsa[sl].T), "ofT": np.ascontiguousarray(o_fox[sl].T),
                     "mgT": np.ascontiguousarray(proj[sl, OFF_MERGE:OFF_MERGE + 4096].T), "x": np.ascontiguousarray(x2[sl]),
                     "wbn": w_br_nsa[0], "wbf": w_br_fox[0], "wo": w_o[0], "g1b": rep(modrow[2 * D:3 * D]), "ln1g": rep(ln1_g[0]), "ln1b": rep(ln1_b[0]),
                     "mod2": mod2, "wr": wr, "brb": brb})
    res = _launch("l3a", nc, maps, cores)
    cat = lambda k: np.concatenate([r[k] for r in res.results], axis=0)
    return cat("x1"), cat("xn2"), cat("rw"), cat("oh")


CAP = 512


def _rank_setup(nc, T, A, P_, oh, TPC):
    NT = TPC // 128
    ohs = A("ohs", [128, NT, 8], F32)
    ohb = A("ohb", [128, NT, 8], BF16)
    rank = A("rank", [128, NT, 8], F32)
    onesb = A("onesb", [128, 128], BF16)
    sub = A("sub", [128, 128], BF16)
    rkps = P_("rkps", [128, 512], F32)
    T.op("sp", lambda e: e.dma_start(out=ohs[:], in_=oh.rearrange("(c p) g -> p c g", p=128)), writes=["ohs"], dma=True)
    T.op("dve", lambda e: e.tensor_copy(out=ohb[:], in_=ohs[:]), reads=["ohs"], writes=["ohb"])
    T.op("pool", lambda e: e.memset(onesb[:], 1.0), writes=["onesb"])
    T.op("pool", lambda e: e.memset(sub[:], 1.0), writes=["sub"])
    T.op("pool", lambda e: e.affine_select(out=sub[:], in_=sub[:], pattern=[[1, 128]], compare_op=ALU.is_ge, fill=0.0, base=-1,
                                           channel_multiplier=-1), reads=["sub"], writes=["sub"])
    for c in range(NT):
        for c2 in range(c):
            T.op("pe", lambda e: e.matmul(rkps[:, c * 8:(c + 1) * 8], lhsT=onesb[:], rhs=ohb[:, c2, :], start=(c2 == 0), stop=False),
                 reads=["onesb", "ohb"], writes=["rkps"])
        T.op("pe", lambda e: e.matmul(rkps[:, c * 8:(c + 1) * 8], lhsT=sub[:], rhs=ohb[:, c, :], start=(c == 0), stop=True),
             reads=["sub", "ohb"], writes=["rkps"])
    T.op("dve", lambda e: e.tensor_copy(out=rank[:].rearrange("p c g -> p (c g)"), in_=rkps[:, 0:NT * 8]), writes=["rank", "rkps"])
    return ohs, rank


def build_dispatch(TPC=TPC):
    nc = bass.Bass("TRN2", target_bir_lowering=False)
    NT = TPC // 128
    A = lambda name, shape, dt: nc.alloc_sbuf_tensor("sb_" + name, shape, dt)
    P_ = nc.alloc_psum_tensor
    xn2 = nc.dram_tensor("xn2", [TPC, D], BF16, kind="ExternalInput").ap()
    oh = nc.dram_tensor("oh", [TPC, 8], F32, kind="ExternalInput").ap()
    rw = nc.dram_tensor("rw", [TPC, 64], F32, kind="ExternalInput").ap()
    mod2 = nc.dram_tensor("mod2", [128, 32], F32, kind="ExternalInput").ap()
    iotad = nc.dram_tensor("iota", [128, CAP], F32, kind="ExternalInput").ap()
    xso = nc.dram_tensor("xs", [8, D, CAP], BF16, kind="ExternalOutput").ap()
    rwso = nc.dram_tensor("rws", [8, CAP, 8], F32, kind="ExternalOutput").ap()
    T = Trk(nc)
    ohs, rank = _rank_setup(nc, T, A, P_, oh, TPC)
    xs_ = A("xn2s", [128, NT, D], BF16)
    for h in range(0, NT, 4):
        hw = min(4, NT - h)
        T.op("sp", lambda e: e.dma_start(out=xs_[:, h:h + hw, :], in_=xn2.rearrange("(c p) d -> p c d", p=128)[:, h:h + hw, :]), writes=["xn2s"], dma=True)
    rws_ = A("rwf", [128, NT, 64], F32)
    rwh = A("rwh", [128, NT, 64], BF16); rwhf = A("rwhf", [128, NT, 64], F32); rwl = A("rwl", [128, NT, 64], BF16)
    T.op("sp", lambda e: e.dma_start(out=rws_[:], in_=rw.rearrange("(c p) g -> p c g", p=128)), writes=["rwf"], dma=True)
    T.op("dve", lambda e: e.tensor_copy(out=rwh[:], in_=rws_[:]), reads=["rwf"], writes=["rwh"])
    T.op("dve", lambda e: e.tensor_copy(out=rwhf[:], in_=rwh[:]), reads=["rwh"], writes=["rwhf"])
    T.op("dve", lambda e: e.tensor_tensor(out=rwl[:], in0=rws_[:], in1=rwhf[:], op=ALU.subtract), reads=["rwf", "rwhf"], writes=["rwl"])
    m2 = A("m2", [128, 32], F32); iota = A("iota", [128, CAP], F32)
    T.op("sp", lambda e: e.dma_start(out=m2[:], in_=mod2[:, :]), writes=["m2"], dma=True)
    T.op("sp", lambda e: e.dma_start(out=iota[:], in_=iotad[:, :]), writes=["iota"], dma=True)
    T.op("dve", lambda e: e.tensor_scalar(out=m2[:, 16:32], in0=m2[:, 16:32], scalar1=1.0, scalar2=None, op0=ALU.add), reads=["m2"], writes=["m2"])
    Pm = [A("Pm%d" % i, [128, NT, CAP], BF16) for i in range(2)]
    xo = [A("xo%d" % i, [128, 4, CAP], BF16) for i in range(2)]
    ro = A("ro", [128, 4, 8], F32)
    acc = [P_("acc%d" % i, [128, 512], F32) for i in range(4)]
    rps = P_("rps", [128, 512], F32)
    ev = 0
    import os
    DBG = int(os.environ.get("DBG", "9"))
    for g in range(8 if DBG > 0 else 0):
        Pg = Pm[g % 2]
        pk = "Pm%d" % (g % 2)
        for c in range(NT):
            T.op("dve", lambda e: e.tensor_scalar(out=Pg[:, c, :], in0=iota[:], scalar1=rank[:, c, g:g + 1], scalar2=ohs[:, c, g:g + 1],
                                                  op0=ALU.is_equal, op1=ALU.mult), reads=["iota", "rank", "ohs"], writes=[pk])
        for dg in range(4 if DBG > 1 else 0):
            for c in range(NT):
                for dd in range(4):
                    k = dg * 4 + dd
                    T.op("pe", lambda e: e.matmul(acc[dd][:], lhsT=xs_[:, c, k * 128:(k + 1) * 128], rhs=Pg[:, c, :], start=(c == 0), stop=(c == NT - 1)),
                         reads=["xn2s", pk], writes=["acc%d" % dd])
            xb = xo[ev % 2]
            for dd in range(4):
                k = dg * 4 + dd
                T.op("act", lambda e: e.activation(out=xb[:, dd, :], in_=acc[dd][:], func=AF.Identity, scale=m2[:, 16 + k:17 + k], bias=m2[:, k:k + 1]),
                     reads=["m2"], writes=["xo%d" % (ev % 2), "acc%d" % dd])
            T.op("sp", lambda e: e.dma_start(out=xso[g, dg * 512:(dg + 1) * 512, :].rearrange("(dd p) s -> p dd s", p=128), in_=xb[:]),
                 reads=["xo%d" % (ev % 2)], writes=["xso"], dma=True)
            ev += 1
        for st in range(4 if DBG > 2 else 0):
            n = 0
            for c in range(NT):
                for hl in (rwh, rwl):
                    T.op("pe", lambda e: e.matmul(rps[:, st * 8:(st + 1) * 8], lhsT=Pg[:, c, st * 128:(st + 1) * 128], rhs=hl[:, c, g * 8:(g + 1) * 8],
                                                  start=(n == 0), stop=(n == 2 * NT - 1)), reads=[pk, "rwh", "rwl"], writes=["rps"])
                    n += 1
        if DBG > 3:
            T.op("dve", lambda e: e.tensor_copy(out=ro[:].rearrange("p a b -> p (a b)"), in_=rps[:, 0:32]), writes=["ro", "rps"])
        if DBG > 4:
            T.op("sp", lambda e: e.dma_start(out=rwso[g].rearrange("(st p) e -> p st e", p=128), in_=ro[:]), reads=["ro"], writes=["rwso"], dma=True)
    T.finish("sp")
    print("DISP n_ins", T.n_ins, "n_wait", T.n_wait)
    return nc


def build_experts(NSL=8 * CAP, NE=8):
    nc = bass.Bass("TRN2", target_bir_lowering=False)
    NSC = NSL // 512
    A = lambda name, shape, dt: nc.alloc_sbuf_tensor("sb_" + name, shape, dt)
    P_ = nc.alloc_psum_tensor
    xs = nc.dram_tensor("xs", [D, NSL], BF16, kind="ExternalInput").ap()
    rws = nc.dram_tensor("rws", [NSL, 8], F32, kind="ExternalInput").ap()
    wg = nc.dram_tensor("wg", [NE, D, 512], F32, kind="ExternalInput").ap()
    wu = nc.dram_tensor("wu", [NE, D, 512], F32, kind="ExternalInput").ap()
    wd = nc.dram_tensor("wd", [NE, 512, D], F32, kind="ExternalInput").ap()
    ys = nc.dram_tensor("ys", [NSL, D], F32, kind="ExternalOutput").ap()
    T = Trk(nc)
    W = [[A("w%d_%d" % (b, j), [128, 2048], BF16) for j in range(12)] for b in range(2)]
    stg = [A("stg%d" % i, [128, 2048], F32) for i in range(3)]
    xc = [A("xc%d" % i, [128, 16, 512], BF16) for i in range(2)]
    gs = A("gs", [128, 4, 512], BF16); hT = A("hT", [128, 4, 512], BF16)
    yt = [A("yt%d" % i, [128, D], F32) for i in range(3)]
    rs = A("rs", [128, NSL // 128, 8], F32)
    T.op("act", lambda e: e.dma_start(out=rs[:], in_=rws.rearrange("(st p) e -> p st e", p=128)), writes=["rs"], dma=True)
    acc = [P_("acc%d" % i, [128, 512], F32) for i in range(4)]
    acc2 = [P_("accd%d" % i, [128, 512], F32) for i in range(2)]
    ld = [0]

    def load_weights(e):
        b = e % 2
        for j in range(12):
            sb = ld[0] % 3
            ld[0] += 1
            if j < 8:
                src = (wg if j < 4 else wu)[e, (j % 4) * 512:(j % 4 + 1) * 512, :].rearrange("(kk p) n -> p kk n", p=128)
                dst = stg[sb][:].rearrange("p (kk n) -> p kk n", kk=4)
            else:
                src = wd[e, (j - 8) * 128:(j - 7) * 128, :]
                dst = stg[sb][:]
            T.op("sp", lambda e_: e_.dma_start(out=dst, in_=src), writes=["stg%d" % sb], dma=True)
            eng = "pool" if j % 2 == 0 else "act"
            if eng == "pool":
                T.op("pool", lambda e_: e_.tensor_copy(out=W[b][j][:], in_=stg[sb][:]), reads=["stg%d" % sb], writes=["w%d_%d" % (b, j)])
            else:
                T.op("act", lambda e_: e_.copy(out=W[b][j][:], in_=stg[sb][:]), reads=["stg%d" % sb], writes=["w%d_%d" % (b, j)])

    load_weights(0)
    dn = 0
    chunks = [(e, sc) for e in range(NE) for sc in range(NSC)]
    tiles = [(e, sc, st) for e in range(NE) for sc in range(NSC) for st in range(4)]

    def load_chunk(ci):
        e, sc = chunks[ci]
        xb = xc[ci % 2]
        T.op("act", lambda e_: e_.dma_start(out=xb[:], in_=xs[:, sc * 512:(sc + 1) * 512].rearrange("(k p) s -> p k s", p=128)),
             writes=["xc%d" % (ci % 2)], dma=True)

    def load_ytile(ti):
        e, sc, st = tiles[ti]
        if e == 0:
            return
        sl = sc * 4 + st
        ytb = yt[ti % 3]
        T.op("act", lambda e_: e_.dma_start(out=ytb[:], in_=ys[sl * 128:(sl + 1) * 128, :]), reads=["ys%d" % sl], writes=["yt%d" % (ti % 3)], dma=True)

    load_chunk(0)
    for ci, (e, sc) in enumerate(chunks):
        b = e % 2
        if sc == 0 and e + 1 < NE:
            load_weights(e + 1)
        if ci + 1 < len(chunks):
            load_chunk(ci + 1)
        xb = xc[ci % 2]
        xk = "xc%d" % (ci % 2)
        for ph in range(2):
            for fc in range(4):
                for k in range(16):
                    wt = W[b][ph * 4 + k // 4]
                    T.op("pe", lambda e_: e_.matmul(acc[fc][:], lhsT=wt[:, (k % 4) * 512 + fc * 128:(k % 4) * 512 + (fc + 1) * 128], rhs=xb[:, k, :],
                                                    start=(k == 0), stop=(k == 15)), reads=["w%d_%d" % (b, ph * 4 + k // 4), xk], writes=["acc%d" % fc])
                if ph == 0:
                    T.op("act", lambda e_: e_.activation(out=gs[:, fc, :], in_=acc[fc][:], func=AF.Silu), writes=["gs%d" % fc, "acc%d" % fc])
                else:
                    T.op("dve", lambda e_: e_.tensor_tensor(out=hT[:, fc, :], in0=acc[fc][:], in1=gs[:, fc, :], op=ALU.mult),
                         reads=["gs%d" % fc], writes=["hT%d" % fc, "acc%d" % fc])
        for st in range(4):
            ti = ci * 4 + st
            sl = sc * 4 + st
            ytb = yt[ti % 3]
            yk = "yt%d" % (ti % 3)
            if ti + 1 < len(tiles):
                load_ytile(ti + 1)
            for dmc in range(4):
                a2 = dn % 2
                dn += 1
                for fc in range(4):
                    T.op("pe", lambda e_: e_.matmul(acc2[a2][:], lhsT=hT[:, fc, st * 128:(st + 1) * 128], rhs=W[b][8 + fc][:, dmc * 512:(dmc + 1) * 512],
                                                    start=(fc == 0), stop=(fc == 3)), reads=["hT%d" % fc, "w%d_%d" % (b, 8 + fc)], writes=["accd%d" % a2])
                if e == 0:
                    T.op("dve", lambda e_: e_.tensor_scalar(out=ytb[:, dmc * 512:(dmc + 1) * 512], in0=acc2[a2][:], scalar1=rs[:, sl, e:e + 1], scalar2=None,
                                                            op0=ALU.mult), reads=["rs"], writes=[yk, "accd%d" % a2])
                else:
                    T.op("dve", lambda e_: e_.scalar_tensor_tensor(out=ytb[:, dmc * 512:(dmc + 1) * 512], in0=acc2[a2][:], scalar=rs[:, sl, e:e + 1],
                                                                   in1=ytb[:, dmc * 512:(dmc + 1) * 512], op0=ALU.mult, op1=ALU.add),
                         reads=["rs"], writes=[yk, "accd%d" % a2])
            T.op("pool", lambda e_: e_.dma_start(out=ys[sl * 128:(sl + 1) * 128, :], in_=ytb[:]), reads=[yk], writes=["ys%d" % sl], dma=True)
    T.finish("sp")
    T.finish("pool")
    print("EXP n_ins", T.n_ins, "n_wait", T.n_wait)
    return nc


def build_combine(TPC=TPC):
    nc = bass.Bass("TRN2", target_bir_lowering=False)
    NT = TPC // 128
    A = lambda name, shape, dt: nc.alloc_sbuf_tensor("sb_" + name, shape, dt)
    P_ = nc.alloc_psum_tensor
    dI = lambda name, shape: nc.dram_tensor(name, shape, F32, kind="ExternalInput").ap()
    ysl = dI("ysl", [8, CAP, D]); oh = dI("oh", [TPC, 8]); x1 = dI("x1", [TPC, D]); iotad = dI("iota", [128, CAP])
    g2b = dI("g2b", [128, D]); ln2g = dI("ln2g", [128, D]); ln2b = dI("ln2b", [128, D])
    out = nc.dram_tensor("out", [TPC, D], F32, kind="ExternalOutput").ap()
    y2d = nc.dram_tensor("y2d", [TPC, D], F32).ap()
    T = Trk(nc)
    ohs, rank = _rank_setup(nc, T, A, P_, oh, TPC)
    iota = A("iota", [128, CAP], F32)
    T.op("sp", lambda e: e.dma_start(out=iota[:], in_=iotad[:, :]), writes=["iota"], dma=True)
    identb = make_ident(nc, T, BF16, "identb")
    yh = A("yh", [128, 32, 1024], BF16)
    Pt = A("Pt", [128, 8, CAP], BF16)
    PT = [A("PT%d" % i, [128, 32, 128], BF16) for i in range(2)]
    yo = [A("yo%d" % i, [128, 1024], F32) for i in range(2)]
    tps = [P_("tps%d" % i, [128, 8, 128], BF16) for i in range(2)]
    acc = [P_("acc%d" % i, [128, 512], F32) for i in range(2)]
    ysv = ysl.rearrange("g (st p) d -> p g st d", p=128)
    tn = 0
    an = 0
    for h in range(2):
        for g in range(8):
            T.op("pool", lambda e: e.dma_start(out=yh[:, g * 4:(g + 1) * 4, :], in_=ysv[:, g, :, h * 1024:(h + 1) * 1024]), writes=["yh"], dma=True)
        for c in range(NT):
            ptb = PT[c % 2]
            pk = "PT%d" % (c % 2)
            for g in range(8):
                T.op("dve", lambda e: e.tensor_scalar(out=Pt[:, g, :], in0=iota[:], scalar1=rank[:, c, g:g + 1], scalar2=ohs[:, c, g:g + 1],
                                                      op0=ALU.is_equal, op1=ALU.mult), reads=["iota", "rank", "ohs"], writes=["Pt"])
            for q in range(4):
                tb = tn % 2
                tn += 1
                for j in range(8):
                    gi = q * 8 + j
                    T.op("pe", lambda e: e.transpose(out=tps[tb][:, j, :], in_=Pt[:, gi // 4, (gi % 4) * 128:(gi % 4 + 1) * 128], identity=identb[:]),
                         reads=["Pt", "identb"], writes=["tps%d" % tb])
                T.op("act", lambda e: e.copy(out=ptb[:, q * 8:(q + 1) * 8, :], in_=tps[tb][:]), writes=[pk, "tps%d" % tb])
            yb = yo[c % 2]
            for dq in range(2):
                a = an % 2
                an += 1
                for gi in range(32):
                    T.op("pe", lambda e: e.matmul(acc[a][:], lhsT=ptb[:, gi, :], rhs=yh[:, gi, dq * 512:(dq + 1) * 512], start=(gi == 0), stop=(gi == 31)),
                         reads=[pk, "yh"], writes=["acc%d" % a])
                T.op("dve", lambda e: e.tensor_copy(out=yb[:, dq * 512:(dq + 1) * 512], in_=acc[a][:]), writes=["yo%d" % (c % 2), "acc%d" % a])
            T.op("sp", lambda e: e.dma_start(out=y2d[c * 128:(c + 1) * 128, h * 1024:(h + 1) * 1024], in_=yb[:]), reads=["yo%d" % (c % 2)], writes=["y2d%d" % c], dma=True)
    g2p = A("g2p", [128, D], F32); lg_ = A("lng", [128, D], F32); lb_ = A("lnb", [128, D], F32)
    T.op("sp", lambda e: e.dma_start(out=g2p[:], in_=g2b[:, :]), writes=["g2p"], dma=True)
    T.op("sp", lambda e: e.dma_start(out=lg_[:], in_=ln2g[:, :]), writes=["lng"], dma=True)
    T.op("sp", lambda e: e.dma_start(out=lb_[:], in_=ln2b[:, :]), writes=["lnb"], dma=True)
    T.op("dve", lambda e: e.tensor_scalar(out=g2p[:], in0=g2p[:], scalar1=1.0, scalar2=None, op0=ALU.add), reads=["g2p"], writes=["g2p"])
    xt = [A("xt%d" % i, [128, D], F32) for i in range(2)]
    y2 = [A("y2%d" % i, [128, D], F32) for i in range(2)]
    junk = A("junk", [128, D], BF16)
    st = A("st", [128, 16], F32)
    def ld_f(c_):
        b_ = c_ % 2
        T.op("act", lambda e: e.dma_start(out=xt[b_][:], in_=x1[c_ * 128:(c_ + 1) * 128, :]), writes=["xt%d" % b_], dma=True)
        T.op("act", lambda e: e.dma_start(out=y2[b_][:], in_=y2d[c_ * 128:(c_ + 1) * 128, :]), reads=["y2d%d" % c_], writes=["y2%d" % b_], dma=True)
    ld_f(0)
    for c in range(NT):
        b = c % 2
        if c + 1 < NT:
            ld_f(c + 1)
        v = y2[b]
        vk = "y2%d" % b
        T.op("dve", lambda e: e.tensor_tensor(out=v[:], in0=v[:], in1=g2p[:], op=ALU.mult), reads=[vk, "g2p"], writes=[vk])
        T.op("dve", lambda e: e.scalar_tensor_tensor(out=v[:], in0=xt[b][:], scalar=ALPHA, in1=v[:], op0=ALU.mult, op1=ALU.add), reads=["xt%d" % b, vk], writes=[vk])
        T.op("dve", lambda e: e.reduce_sum(out=st[:, 0:1], in_=v[:], axis=AX.X), reads=[vk], writes=["st0"])
        T.op("act", lambda e: e.activation(out=junk[:], in_=v[:], func=AF.Square, accum_out=st[:, 1:2]), reads=[vk], writes=["st1", "junk"])
        T.op("dve", lambda e: e.tensor_scalar(out=st[:, 2:3], in0=st[:, 0:1], scalar1=1.0 / D, scalar2=None, op0=ALU.mult), reads=["st0"], writes=["st2"])
        T.op("dve", lambda e: e.tensor_tensor(out=st[:, 3:4], in0=st[:, 2:3], in1=st[:, 2:3], op=ALU.mult), reads=["st2"], writes=["st3"])
        T.op("dve", lambda e: e.scalar_tensor_tensor(out=st[:, 4:5], in0=st[:, 1:2], scalar=1.0 / D, in1=st[:, 3:4], op0=ALU.mult, op1=ALU.subtract),
             reads=["st1", "st3"], writes=["st4"])
        T.op("dve", lambda e: e.tensor_scalar(out=st[:, 6:7], in0=st[:, 4:5], scalar1=LN_EPS, scalar2=None, op0=ALU.add), reads=["st4"], writes=["st6"])
        T.op("act", lambda e: e.activation(out=st[:, 7:8], in_=st[:, 6:7], func=AF.Sqrt), reads=["st6"], writes=["st7"])
        T.op("dve", lambda e: e.reciprocal(out=st[:, 5:6], in_=st[:, 7:8]), reads=["st7"], writes=["st5"])
        T.op("dve", lambda e: e.tensor_scalar(out=v[:], in0=v[:], scalar1=st[:, 2:3], scalar2=st[:, 5:6], op0=ALU.subtract, op1=ALU.mult),
             reads=[vk, "st2", "st5"], writes=[vk])
        T.op("dve", lambda e: e.tensor_tensor(out=v[:], in0=v[:], in1=lg_[:], op=ALU.mult), reads=[vk, "lng"], writes=[vk])
        T.op("dve", lambda e: e.tensor_tensor(out=v[:], in0=v[:], in1=lb_[:], op=ALU.add), reads=[vk, "lnb"], writes=[vk])
        T.op("sp", lambda e: e.dma_start(out=out[c * 128:(c + 1) * 128, :], in_=v[:]), reads=[vk], writes=["out"], dma=True)
    T.finish("sp")
    print("COMB n_ins", T.n_ins, "n_wait", T.n_wait)
    return nc


def run_moe(xn2, rw, oh, x1, modrow, w_gate, w_up, w_down, ln2_g, ln2_b, TPC=TPC, cores=NCORES, ne=8):
    rep = lambda a: np.ascontiguousarray(np.broadcast_to(a.reshape(1, -1), (128, a.size))).astype(np.float32)
    iota = rep(np.arange(CAP, dtype=np.float32))
    mod2 = np.ascontiguousarray(np.concatenate([modrow[3 * D:4 * D].reshape(16, 128).T, modrow[4 * D:5 * D].reshape(16, 128).T], axis=1))
    nc1 = build_dispatch(TPC)
    maps = [{"xn2": np.ascontiguousarray(xn2[c * TPC:(c + 1) * TPC]), "oh": np.ascontiguousarray(oh[c * TPC:(c + 1) * TPC]),
             "rw": np.ascontiguousarray(rw[c * TPC:(c + 1) * TPC]), "mod2": mod2, "iota": iota} for c in range(cores)]
    r1 = _launch("dispatch", nc1, maps, cores).results
    nc2 = build_experts(cores * CAP, ne)
    maps2 = []
    for g in range(8):
        maps2.append({"xs": np.ascontiguousarray(np.concatenate([r1[c]["xs"][g] for c in range(cores)], axis=1)),
                      "rws": np.ascontiguousarray(np.concatenate([r1[c]["rws"][g] for c in range(cores)], axis=0)),
                      "wg": np.ascontiguousarray(w_gate[0][g * 8:g * 8 + ne]), "wu": np.ascontiguousarray(w_up[0][g * 8:g * 8 + ne]),
                      "wd": np.ascontiguousarray(w_down[0][g * 8:g * 8 + ne])})
    r2 = _launch("experts", nc2, maps2, 8).results
    nc3 = build_combine(TPC)
    maps3 = []
    for c in range(cores):
        maps3.append({"ysl": np.ascontiguousarray(np.stack([r2[g]["ys"][c * CAP:(c + 1) * CAP] for g in range(8)], axis=0)),
                      "oh": np.ascontiguousarray(oh[c * TPC:(c + 1) * TPC]), "x1": np.ascontiguousarray(x1[c * TPC:(c + 1) * TPC]), "iota": iota,
                      "g2b": rep(modrow[5 * D:6 * D]), "ln2g": rep(ln2_g[0]), "ln2b": rep(ln2_b[0])})
    r3 = _launch("combine", nc3, maps3, cores).results
    return np.concatenate([r["out"] for r in r3], axis=0)


def build_mod():
    nc = bass.Bass("TRN2", target_bir_lowering=False)
    cT = nc.dram_tensor("cT", [128, 16], F32, kind="ExternalInput").ap()
    wada = nc.dram_tensor("wada", [D, 1536], F32, kind="ExternalInput").ap()
    badaT = nc.dram_tensor("badaT", [128, 12], F32, kind="ExternalInput").ap()
    modT = nc.dram_tensor("modT", [128, 12], F32, kind="ExternalOutput").ap()
    T = Trk(nc)
    A = lambda name, shape, dt: nc.alloc_sbuf_tensor("sb_" + name, shape, dt)
    wst = [A("wst%d" % i, [128, 16, 256], F32) for i in range(2)]
    cs = A("cs", [128, 16], F32); ca = A("ca", [128, 16], F32); bad = A("bad", [128, 12], F32); mo = A("mo", [128, 12], F32)
    modps = nc.alloc_psum_tensor("modps", [128, 512], F32)
    T.op("sp", lambda e: e.dma_start(out=cs[:], in_=cT[:, :]), writes=["cs"], dma=True)
    T.op("sp", lambda e: e.dma_start(out=bad[:], in_=badaT[:, :]), writes=["bad"], dma=True)
    T.op("act", lambda e: e.activation(out=ca[:], in_=cs[:], func=AF.Silu), reads=["cs"], writes=["ca"])
    wv = wada.rearrange("(k p) n -> p k n", p=128)
    for g in range(6):
        b = g % 2
        T.op("sp", lambda e: e.dma_start(out=wst[b][:], in_=wv[:, :, g * 256:(g + 1) * 256]), writes=["wst%d" % b], dma=True)
        for j in range(2):
            n = g * 2 + j
            for k in range(16):
                T.op("pe", lambda e: e.matmul(modps[:, n:n + 1], lhsT=wst[b][:, k, j * 128:(j + 1) * 128], rhs=ca[:, k:k + 1], start=(k == 0), stop=(k == 15)),
                     reads=["wst%d" % b, "ca"], writes=["modps"])
    T.op("dve", lambda e: e.tensor_tensor(out=mo[:], in0=modps[:, 0:12], in1=bad[:], op=ALU.add), reads=["bad"], writes=["mo", "modps"])
    T.op("sp", lambda e: e.dma_start(out=modT[:, :], in_=mo[:]), reads=["mo"], writes=["modT"], dma=True)
    T.finish("sp")
    return nc


def run_mod(c, w_ada, b_ada):
    nc = build_mod()
    cT = np.ascontiguousarray(c.reshape(16, 128).T)
    maps = [{"cT": cT, "wada": np.ascontiguousarray(w_ada[0][:, i * 1536:(i + 1) * 1536]),
             "badaT": np.ascontiguousarray(b_ada[0][i * 1536:(i + 1) * 1536].reshape(12, 128).T)} for i in range(NCORES)]
    res = _launch("mod", nc, maps, NCORES)
    return np.concatenate([r["modT"].T.reshape(-1) for r in res.results])


def kernel(x, c, w_ada, b_ada, w_in, b_fgt, t5_table, cmp_pe, cmp_w1, cmp_b1, cmp_w2, w_br_nsa, w_br_fox, w_o, ln1_g, ln1_b,
           w_rg, b_rg, w_re, b_re, w_gate, w_up, w_down, ln2_g, ln2_b):
    f = lambda a: np.asarray(a, dtype=np.float32)
    x, c, w_ada, b_ada, w_in = f(x), f(c), f(w_ada), f(b_ada), f(w_in)
    modrow = run_mod(c, w_ada, b_ada)
    proj, projb = run_l1(x, modrow, w_in)
    o_fox = run_fox(proj, projb, f(b_fgt))
    o_nsa = run_nsa(proj, projb, f(t5_table), f(cmp_pe), f(cmp_w1), f(cmp_b1), f(cmp_w2))
    x1, xn2, rw, oh = run_l3a(o_nsa, o_fox, proj, x, modrow, f(w_br_nsa), f(w_br_fox), f(w_o), f(ln1_g), f(ln1_b), f(w_rg), f(b_rg), f(w_re), f(b_re))
    out = run_moe(xn2, rw, oh, x1, modrow, f(w_gate), f(w_up), f(w_down), f(ln2_g), f(ln2_b))
    return out.reshape(1, S, D).astype(np.float32)
```
